# Optimizing a Trainium2 kernel written in Bass

```python
import math
import jax, jax.numpy as jnp
from jax import lax
import numpy as np

D_MODEL = 1024
BATCH = 8
SEQ = 2048
DEPTH = 1
DEC_BATCH = 128
DEC_SEQ = 4
PAST_LEN = 8192
PAGE_SIZE = 128

ATTN_HEADS = 8
ATTN_KV_HEADS = 2
HEAD_DIM = 64
ATTN_GROUP = ATTN_HEADS // ATTN_KV_HEADS
WINDOW = 128
ATTN_BLOCK = 128
ROT_DIM = HEAD_DIM // 4
ROPE_THETA = 500000.0
GDN_HEADS = 4
GDN_DK = 128
GDN_DV = 128
CONV_W = 4
GDN_CHUNK = 64
QK_COLS = GDN_HEADS * GDN_DK
CONV_DIM = 2 * QK_COLS + GDN_HEADS * GDN_DV
Z_COLS = GDN_HEADS * GDN_DV
Q_COLS = ATTN_HEADS * HEAD_DIM
KV_COLS = ATTN_KV_HEADS * HEAD_DIM
IN_COLS = Q_COLS + 2 * KV_COLS + CONV_DIM + Z_COLS + 2 * GDN_HEADS
MIX_WIDTH = Q_COLS + GDN_HEADS * GDN_DV
N_GROUPS = 4
EXPERTS_PER_GROUP = 8
N_EXPERTS = N_GROUPS * EXPERTS_PER_GROUP
TOP_K = 2
EXPERT_FF = 256
NORM_EPS = 1e-5
L2_EPS = 1e-6
DEEPNORM_ALPHA = (2 * DEPTH) ** 0.25
DEEPNORM_BETA = (8 * DEPTH) ** -0.25

kernel_name = "hymba_swa_sink_gdn_hier_moe_step"


def layer_norm(x, g, b):
    xf = x.astype(jnp.float32)
    mu = jnp.mean(xf, -1, keepdims=True)
    var = jnp.mean(jnp.square(xf - mu), -1, keepdims=True)
    return ((xf - mu) * lax.rsqrt(var + NORM_EPS) * g.astype(jnp.float32) + b.astype(jnp.float32)).astype(x.dtype)


def partial_rope(x, pos):
    half = ROT_DIM // 2
    inv_freq = ROPE_THETA ** (-jnp.arange(half, dtype=jnp.float32) * 2.0 / ROT_DIM)
    ang = pos.astype(jnp.float32)[:, None] * inv_freq[None, :]
    cos = jnp.cos(ang)[None, :, None, :]
    sin = jnp.sin(ang)[None, :, None, :]
    xr = x[..., :ROT_DIM].astype(jnp.float32)
    x1, x2 = xr[..., :half], xr[..., half:]
    rot = jnp.concatenate([x1 * cos - x2 * sin, x2 * cos + x1 * sin], axis=-1).astype(x.dtype)
    return jnp.concatenate([rot, x[..., ROT_DIM:]], axis=-1)


def sink_attention(q, k, v, q_pos, k_pos, sinks):
    s = jnp.einsum('bnqhgd,bnkhd->bnhgqk', q, k).astype(jnp.float32) * (HEAD_DIM ** -0.5)
    dpos = q_pos[:, :, None] - k_pos[:, None, :]
    visible = (dpos >= 0) & (dpos < WINDOW) & (k_pos[:, None, :] >= 0)
    s = jnp.where(visible[None, :, None, None], s, -jnp.inf)
    sink = sinks.astype(jnp.float32).reshape(ATTN_KV_HEADS, ATTN_GROUP)[None, None, :, :, None, None]
    m = jnp.maximum(jnp.max(s, -1, keepdims=True), sink)
    p = jnp.exp(s - m)
    p = p / (jnp.sum(p, -1, keepdims=True) + jnp.exp(sink - m))
    return jnp.einsum('bnhgqk,bnkhd->bnqhgd', p.astype(v.dtype), v)


def swa_prompt(q, k, v, sinks):
    B, S = q.shape[:2]
    nb = S // ATTN_BLOCK
    qb = q.reshape(B, nb, ATTN_BLOCK, ATTN_KV_HEADS, ATTN_GROUP, HEAD_DIM)

    def band(t):
        tb = t.reshape(B, nb, ATTN_BLOCK, ATTN_KV_HEADS, HEAD_DIM)
        prev = jnp.pad(tb[:, :-1], ((0, 0), (1, 0), (0, 0), (0, 0), (0, 0)))
        return jnp.concatenate([prev, tb], axis=2)

    pos = jnp.arange(S, dtype=jnp.int32).reshape(nb, ATTN_BLOCK)
    k_pos = jnp.concatenate([pos - ATTN_BLOCK, pos], axis=1)
    o = sink_attention(qb, band(k), band(v), pos, k_pos, sinks)
    return o.reshape(B, S, Q_COLS)


def swa_sample(q, k_new, v_new, win_k, win_v, sinks):
    Bd, T = q.shape[:2]
    k_all = jnp.concatenate([win_k.astype(k_new.dtype), k_new], axis=1)
    v_all = jnp.concatenate([win_v.astype(v_new.dtype), v_new], axis=1)
    q_pos = PAST_LEN + jnp.arange(T, dtype=jnp.int32)
    k_pos = jnp.concatenate([PAST_LEN - WINDOW + jnp.arange(WINDOW, dtype=jnp.int32), q_pos])
    qb = q.reshape(Bd, 1, T, ATTN_KV_HEADS, ATTN_GROUP, HEAD_DIM)
    o = sink_attention(qb, k_all[:, None], v_all[:, None], q_pos[None], k_pos[None], sinks)
    return o.reshape(Bd, T, Q_COLS), k_all[:, -WINDOW:], v_all[:, -WINDOW:]


def causal_conv(u, buf, w):
    T = u.shape[1]
    ext = jnp.concatenate([buf.astype(u.dtype), u], axis=1)
    out = ext[:, 0:T] * w[0]
    for i in range(1, CONV_W):
        out = out + ext[:, i:i + T] * w[i]
    return jax.nn.silu(out), ext[:, -(CONV_W - 1):]


def l2_normalize(t):
    tf = t.astype(jnp.float32)
    return tf * lax.rsqrt(jnp.sum(tf * tf, -1, keepdims=True) + L2_EPS)


def gated_delta_rule(q, k, v, g, beta, s0, chunk):
    B, T, H, DK = k.shape
    nc = T // chunk
    f32 = jnp.float32

    def blocks(t):
        t = t.astype(f32).reshape((B, nc, chunk) + t.shape[2:])
        return jnp.moveaxis(t, 3, 1)

    q, k, v, g, beta = blocks(q), blocks(k), blocks(v), blocks(g), blocks(beta)
    q = q * (DK ** -0.5)
    gc = jnp.cumsum(g, axis=-1)
    idx = jnp.arange(chunk)
    causal = idx[:, None] >= idx[None, :]
    strict = idx[:, None] > idx[None, :]
    decay = jnp.exp(jnp.where(causal, gc[..., :, None] - gc[..., None, :], -jnp.inf))
    kb = k * beta[..., None]
    vb = v * beta[..., None]
    m = jnp.where(strict, jnp.einsum('bhnid,bhnjd->bhnij', kb, k) * decay, 0.0)
    eye = jnp.eye(chunk, dtype=f32)
    tmat = lax.linalg.triangular_solve(eye + m, jnp.broadcast_to(eye, m.shape), left_side=True, lower=True)
    u = tmat @ vb
    w = tmat @ (kb * jnp.exp(gc)[..., None])
    attn = jnp.einsum('bhnid,bhnjd->bhnij', q, k) * decay
    q_dec = q * jnp.exp(gc)[..., None]
    g_last = gc[..., -1]
    k_dec = k * jnp.exp(g_last[..., None] - gc)[..., None]

    def step(s, xs):
        u_c, w_c, attn_c, qd_c, kd_c, gl_c = xs
        v_new = u_c - w_c @ s
        o_c = qd_c @ s + attn_c @ v_new
        s = s * jnp.exp(gl_c)[..., None, None] + jnp.swapaxes(kd_c, -1, -2) @ v_new
        return s, o_c

    xs = tuple(jnp.moveaxis(t, 2, 0) for t in (u, w, attn, q_dec, k_dec, g_last))
    s_final, o = lax.scan(step, s0.astype(f32), xs)
    o = jnp.moveaxis(jnp.moveaxis(o, 0, 2), 1, 3).reshape(B, T, H, v.shape[-1])
    return o, s_final


def gated_rms_norm(o, z, w):
    of = o.astype(jnp.float32)
    of = of * lax.rsqrt(jnp.mean(of * of, -1, keepdims=True) + NORM_EPS) * w.astype(jnp.float32)
    return of * jax.nn.silu(z.astype(jnp.float32))


def gdn_mixer(qkv_raw, z, a, b, conv_buf, s0, chunk, conv_w, a_log, dt_bias, norm_w):
    B, T, _ = qkv_raw.shape
    qkv, new_buf = causal_conv(qkv_raw, conv_buf, conv_w)
    qg = l2_normalize(qkv[..., :QK_COLS].reshape(B, T, GDN_HEADS, GDN_DK))
    kg = l2_normalize(qkv[..., QK_COLS:2 * QK_COLS].reshape(B, T, GDN_HEADS, GDN_DK))
    vg = qkv[..., 2 * QK_COLS:].reshape(B, T, GDN_HEADS, GDN_DV).astype(jnp.float32)
    g = -jnp.exp(a_log.astype(jnp.float32)) * jax.nn.softplus(a.astype(jnp.float32) + dt_bias.astype(jnp.float32))
    beta = jax.nn.sigmoid(b.astype(jnp.float32))
    o, s_new = gated_delta_rule(qg, kg, vg, g, beta, s0, chunk)
    o = gated_rms_norm(o, z.reshape(B, T, GDN_HEADS, GDN_DV), norm_w)
    return o.reshape(B, T, Z_COLS).astype(qkv_raw.dtype), s_new.astype(qkv_raw.dtype), new_buf


def hier_moe(x, w_router_group, w_router_expert, w_gate, w_up, w_down):
    B, T, D = x.shape
    xt = x.reshape(B * T, D)
    n = xt.shape[0]
    g_prob = jax.nn.softmax((xt @ w_router_group).astype(jnp.float32), axis=-1)
    g_top_p, g_top = lax.top_k(g_prob, 1)
    e_logits = (xt @ w_router_expert).astype(jnp.float32).reshape(n, N_GROUPS, EXPERTS_PER_GROUP)
    e_sel = e_logits[jnp.arange(n), g_top[:, 0]]
    e_top_p, e_top = lax.top_k(jax.nn.softmax(e_sel, axis=-1), TOP_K)
    e_top_p = e_top_p / jnp.sum(e_top_p, -1, keepdims=True)
    expert_id = g_top * EXPERTS_PER_GROUP + e_top
    gates = g_top_p * e_top_p
    combine = jnp.sum(jax.nn.one_hot(expert_id, N_EXPERTS, dtype=jnp.float32) * gates[..., None], axis=1)
    h = jax.nn.silu(jnp.einsum('nd,edf->nef', xt, w_gate)) * jnp.einsum('nd,edf->nef', xt, w_up)
    h = h * combine.astype(h.dtype)[..., None]
    y = jnp.einsum('nef,efd->nd', h, w_down)
    return y.reshape(B, T, D)


def hybrid_layer(x, pos, win_k, win_v, conv_buf, s0, chunk,
                 w_in, w_out, attn_sinks, conv_w, a_log, dt_bias, gdn_norm_w, ln1_g, ln1_b,
                 w_router_group, w_router_expert, w_gate, w_up, w_down, ln2_g, ln2_b):
    B, T, _ = x.shape
    proj = x @ w_in
    o1 = Q_COLS
    o2 = o1 + KV_COLS
    o3 = o2 + KV_COLS
    o4 = o3 + CONV_DIM
    o5 = o4 + Z_COLS
    o6 = o5 + GDN_HEADS
    q, k, v, qkv_g, z, a, b = jnp.split(proj, [o1, o2, o3, o4, o5, o6], axis=-1)
    q = partial_rope(q.reshape(B, T, ATTN_HEADS, HEAD_DIM), pos)
    k = partial_rope(k.reshape(B, T, ATTN_KV_HEADS, HEAD_DIM), pos)
    v = v.reshape(B, T, ATTN_KV_HEADS, HEAD_DIM)
    if win_k is None:
        o_attn = swa_prompt(q, k, v, attn_sinks)
        new_k, new_v = k[:, -WINDOW:], v[:, -WINDOW:]
        conv_buf = jnp.zeros((B, CONV_W - 1, CONV_DIM), x.dtype)
        s0 = jnp.zeros((B, GDN_HEADS, GDN_DK, GDN_DV), jnp.float32)
    else:
        o_attn, new_k, new_v = swa_sample(q, k, v, win_k, win_v, attn_sinks)
    o_gdn, new_s, new_conv = gdn_mixer(qkv_g, z, a, b, conv_buf, s0, chunk, conv_w, a_log, dt_bias, gdn_norm_w)
    mix = jnp.concatenate([o_attn, o_gdn], axis=-1) @ w_out
    x = layer_norm(DEEPNORM_ALPHA * x + mix, ln1_g, ln1_b)
    x = layer_norm(DEEPNORM_ALPHA * x + hier_moe(x, w_router_group, w_router_expert, w_gate, w_up, w_down), ln2_g, ln2_b)
    return x, new_k, new_v, new_s, new_conv


def setup_inputs(seed: int = 0) -> dict:
    key = jax.random.key(seed)
    ks = jax.random.split(key, 24)
    f32 = jnp.float32

    def nrm(k, shape, scale):
        return jax.random.normal(k, shape, f32) * scale

    dt = jnp.exp(jax.random.uniform(ks[11], (DEPTH, GDN_HEADS), f32, math.log(1e-3), math.log(1e-1)))
    return {
        'x_prompt': nrm(ks[0], (BATCH, SEQ, D_MODEL), 1.0),
        'x_sample': nrm(ks[1], (DEC_BATCH, DEC_SEQ, D_MODEL), 1.0),
        'cache_attn_k': nrm(ks[2], (DEPTH, DEC_BATCH, WINDOW, ATTN_KV_HEADS, HEAD_DIM), 1.0),
        'cache_attn_v': nrm(ks[3], (DEPTH, DEC_BATCH, WINDOW, ATTN_KV_HEADS, HEAD_DIM), 1.0),
        'state_gdn': nrm(ks[4], (DEPTH, DEC_BATCH, GDN_HEADS, GDN_DK, GDN_DV), 0.1),
        'state_conv': nrm(ks[5], (DEPTH, DEC_BATCH, CONV_W - 1, CONV_DIM), 1.0),
        'w_in': nrm(ks[6], (DEPTH, D_MODEL, IN_COLS), D_MODEL ** -0.5),
        'w_out': nrm(ks[7], (DEPTH, MIX_WIDTH, D_MODEL), MIX_WIDTH ** -0.5 * DEEPNORM_BETA),
        'attn_sinks': nrm(ks[8], (DEPTH, ATTN_HEADS), 1.0),
        'conv_w': nrm(ks[9], (DEPTH, CONV_W, CONV_DIM), CONV_W ** -0.5),
        'a_log': jnp.log(jax.random.uniform(ks[10], (DEPTH, GDN_HEADS), f32, 1.0, 16.0)),
        'dt_bias': dt + jnp.log(-jnp.expm1(-dt)),
        'gdn_norm_w': 1.0 + nrm(ks[12], (DEPTH, GDN_DV), 0.02),
        'ln1_g': 1.0 + nrm(ks[13], (DEPTH, D_MODEL), 0.02),
        'ln1_b': nrm(ks[14], (DEPTH, D_MODEL), 0.02),
        'w_router_group': nrm(ks[15], (DEPTH, D_MODEL, N_GROUPS), D_MODEL ** -0.5),
        'w_router_expert': nrm(ks[16], (DEPTH, D_MODEL, N_EXPERTS), D_MODEL ** -0.5),
        'w_gate': nrm(ks[17], (DEPTH, N_EXPERTS, D_MODEL, EXPERT_FF), D_MODEL ** -0.5),
        'w_up': nrm(ks[18], (DEPTH, N_EXPERTS, D_MODEL, EXPERT_FF), D_MODEL ** -0.5),
        'w_down': nrm(ks[19], (DEPTH, N_EXPERTS, EXPERT_FF, D_MODEL), EXPERT_FF ** -0.5 * DEEPNORM_BETA),
        'ln2_g': 1.0 + nrm(ks[20], (DEPTH, D_MODEL), 0.02),
        'ln2_b': nrm(ks[21], (DEPTH, D_MODEL), 0.02),
    }


def reference(x_prompt, x_sample, cache_attn_k, cache_attn_v, state_gdn, state_conv,
              w_in, w_out, attn_sinks, conv_w, a_log, dt_bias, gdn_norm_w, ln1_g, ln1_b,
              w_router_group, w_router_expert, w_gate, w_up, w_down, ln2_g, ln2_b):
    pos_p = jnp.arange(x_prompt.shape[1], dtype=jnp.int32)
    pos_s = PAST_LEN + jnp.arange(x_sample.shape[1], dtype=jnp.int32)
    yp, ys = x_prompt, x_sample
    kp, vp, sp, cp, ksm, vsm, ssm, csm = [], [], [], [], [], [], [], []
    for l in range(DEPTH):
        wl = (w_in[l], w_out[l], attn_sinks[l], conv_w[l], a_log[l], dt_bias[l], gdn_norm_w[l], ln1_g[l], ln1_b[l],
              w_router_group[l], w_router_expert[l], w_gate[l], w_up[l], w_down[l], ln2_g[l], ln2_b[l])
        yp, k_, v_, s_, c_ = hybrid_layer(yp, pos_p, None, None, None, None, GDN_CHUNK, *wl)
        kp.append(k_); vp.append(v_); sp.append(s_); cp.append(c_)
        ys, k_, v_, s_, c_ = hybrid_layer(ys, pos_s, cache_attn_k[l], cache_attn_v[l], state_conv[l], state_gdn[l],
                                          x_sample.shape[1], *wl)
        ksm.append(k_); vsm.append(v_); ssm.append(s_); csm.append(c_)
    return (yp, ys, jnp.stack(kp), jnp.stack(vp), jnp.stack(sp), jnp.stack(cp),
            jnp.stack(ksm), jnp.stack(vsm), jnp.stack(ssm), jnp.stack(csm))
```

```python
import math
from contextlib import ExitStack

import numpy as np
import concourse.bass as bass
import concourse.mybir as mybir
from concourse.bass_utils import run_bass_kernel_spmd

F32 = mybir.dt.float32
BF16 = mybir.dt.bfloat16
I32 = mybir.dt.int32
AF = mybir.ActivationFunctionType
ALU = mybir.AluOpType
AX = mybir.AxisListType

NCORES = 8
D = 1024
SEQ = 2048
NT = 16
NS = 16
TS = 4
NTOK = SEQ + NS * TS
PAST = 8192
INC = 2824
ALPHA = 2.0 ** 0.25
NORM_EPS = 1e-5
L2_EPS = 1e-6
THETA = 500000.0
NE = 32
MAGIC = 12582912.0


class _Stop(Exception):
    pass


import os
STOP = os.environ.get("K_STOP", "")


_SCHED = []


def stop(tag):
    if STOP == tag:
        _SCHED[-1].dead = True


class Sched:
    def __init__(self, nc, es):
        self.nc = nc
        self.es = es
        self.E = {"pe": nc.tensor, "act": nc.scalar, "dve": nc.vector, "pool": nc.gpsimd, "sp": nc.sync}
        self.semh = {}
        for k in self.E:
            self.semh["e_" + k] = es.enter_context(nc.semaphore("sem_" + k))
        self.cnt = {k: 0 for k in self.E}
        self.seen = {k: {} for k in self.E}
        self.lastw = {}
        self.readers = {}
        self.pend = {k: [] for k in self.E}
        self.pend_r = {k: set() for k in self.E}
        self.pend_w = {k: set() for k in self.E}
        self.slots = {}
        self.psf_list = []
        self.psb_list = []
        self.psf_i = 0
        self.psb_i = 0
        self.dead = False
        _SCHED.append(self)

    def _waits(self, e, r, w, is_dma):
        need = {}

        def add(ev):
            semk, val, eng = ev
            if need.get(semk, 0) < val:
                need[semk] = val

        for k in list(r) + list(w):
            for e2 in self.E:
                if e2 != e or is_dma:
                    assert k not in self.pend_w[e2], f"key {k} pending write on {e2}"
        for k in w:
            for e2 in self.E:
                if e2 != e or is_dma:
                    assert k not in self.pend_r[e2], f"key {k} pending read on {e2}"
        for k in r:
            ev = self.lastw.get(k)
            if ev is not None:
                if ev[2] == e and e == "pe" and not is_dma:
                    continue
                add(ev)
            if k.startswith("ps"):
                for ev in self.readers.get(k, ()):
                    if ev[2] != e:
                        add(ev)
        for k in w:
            ev = self.lastw.get(k)
            if ev is not None and (is_dma or ev[2] != e or e != "pe"):
                add(ev)
            for ev in self.readers.get(k, ()):
                if is_dma or ev[2] != e or e != "pe":
                    add(ev)
        for semk, val in need.items():
            if self.seen[e].get(semk, 0) < val:
                self.E[e].wait_ge(self.semh[semk], val)
                self.seen[e][semk] = val

    def _register(self, r, w, ev):
        for k in r:
            self.readers.setdefault(k, []).append(ev)
        for k in w:
            self.lastw[k] = ev
            self.readers[k] = []

    def op(self, e, fn, r=(), w=(), inc=True):
        if self.dead:
            return None
        self._waits(e, r, w, False)
        ins = fn(self.E[e])
        if not inc:
            self.pend[e].append((tuple(r), tuple(w)))
            self.pend_r[e].update(r)
            self.pend_w[e].update(w)
            return ins
        self.cnt[e] += 1
        ins.then_inc(self.semh["e_" + e], 1)
        ev = ("e_" + e, self.cnt[e], e)
        for (pr, pw) in self.pend[e]:
            self._register(pr, pw, ev)
        self.pend[e] = []
        self.pend_r[e] = set()
        self.pend_w[e] = set()
        self._register(r, w, ev)
        return ins

    def dma(self, q, out, in_, r=(), w=(), slot=None):
        if slot is None:
            slot = w[0] if w else r[0]
        if self.dead:
            return None
        sk = "d_" + slot
        if sk not in self.slots:
            self.semh[sk] = self.es.enter_context(self.nc.semaphore(sk))
            self.slots[sk] = 0
        self._waits(q, r, w, True)
        ins = self.E[q].dma_start(out=out, in_=in_)
        self.slots[sk] += 16
        ins.then_inc(self.semh[sk], 16)
        ev = (sk, self.slots[sk], "dma")
        self._register(r, w, ev)
        return ins

    def barrier(self):
        if self.dead:
            return
        for e in self.E:
            assert not self.pend[e]
        for e in self.E:
            for e2 in self.E:
                if self.cnt[e2] > self.seen[e].get("e_" + e2, 0):
                    self.E[e].wait_ge(self.semh["e_" + e2], self.cnt[e2])
                    self.seen[e]["e_" + e2] = self.cnt[e2]
            for sk, v in self.slots.items():
                if v > self.seen[e].get(sk, 0):
                    self.E[e].wait_ge(self.semh[sk], v)
                    self.seen[e][sk] = v
        self.lastw = {}
        self.readers = {}

    def final_wait(self):
        self.dead = False
        for sk, v in self.slots.items():
            if v > self.seen["sp"].get(sk, 0):
                self.E["sp"].wait_ge(self.semh[sk], v)
                self.seen["sp"][sk] = v
        for e2 in self.E:
            if e2 != "sp" and self.cnt[e2] > self.seen["sp"].get("e_" + e2, 0):
                self.E["sp"].wait_ge(self.semh["e_" + e2], self.cnt[e2])

    def psf(self):
        i = self.psf_i % len(self.psf_list)
        self.psf_i += 1
        return self.psf_list[i], f"psf{i}"

    def psb(self):
        i = self.psb_i % len(self.psb_list)
        self.psb_i += 1
        return self.psb_list[i], f"psb{i}"

    def mm(self, out, lhsT, rhs, start, stop, r, w, inc=None):
        if inc is None:
            inc = stop
        return self.op("pe", lambda E: E.matmul(out, lhsT=lhsT, rhs=rhs, start=start, stop=stop), r, w, inc)

    def tr(self, out, in_, ident, r, w, inc=True):
        return self.op("pe", lambda E: E.transpose(out=out, in_=in_, identity=ident), r, w, inc)

    def act(self, out, in_, func, r, w, bias=0.0, scale=1.0, accum_out=None):
        if accum_out is not None:
            return self.op("act", lambda E: E.activation(out=out, in_=in_, func=func, bias=bias, scale=scale,
                                                         accum_out=accum_out), r, w)
        return self.op("act", lambda E: E.activation(out=out, in_=in_, func=func, bias=bias, scale=scale), r, w)

    def tt(self, e, out, in0, in1, op, r, w):
        return self.op(e, lambda E: E.tensor_tensor(out=out, in0=in0, in1=in1, op=op), r, w)

    def ts(self, e, out, in0, s1, s2, op0, op1, r, w):
        if s2 is None:
            return self.op(e, lambda E: E.tensor_scalar(out=out, in0=in0, scalar1=s1, scalar2=None, op0=op0), r, w)
        return self.op(e, lambda E: E.tensor_scalar(out=out, in0=in0, scalar1=s1, scalar2=s2, op0=op0, op1=op1), r, w)

    def stt(self, e, out, in0, scalar, in1, op0, op1, r, w):
        return self.op(e, lambda E: E.scalar_tensor_tensor(out=out, in0=in0, scalar=scalar, in1=in1, op0=op0, op1=op1),
                       r, w)

    def cp(self, e, out, in_, r, w):
        if e == "act":
            return self.op("act", lambda E: E.copy(out=out, in_=in_), r, w)
        return self.op(e, lambda E: E.tensor_copy(out=out, in_=in_), r, w)


def build(dbg=False):
    nc = bass.Bass("TRN2", target_bir_lowering=False)

    def din(name, shape, dt=F32):
        return nc.dram_tensor(name, shape, dt, kind="ExternalInput").ap()

    def dout(name, shape):
        return nc.dram_tensor(name, shape, F32, kind="ExternalOutput").ap()

    x_p = din("x_p", [SEQ, D])
    x_s = din("x_s", [NS * TS, D])
    ck_d = din("ck", [NS, 128, 128])
    cv_d = din("cv", [NS, 128, 128])
    sg_d = din("sg", [NS, 4, 128, 128])
    sc_d = din("sc", [NS * 3, 1536])
    w_in = din("w_in", [D, INC])
    w_out = din("w_out", [D, D])
    sinks_d = din("sinks", [8])
    convw_d = din("conv_w", [4, 1536])
    alog_d = din("a_log", [4])
    dtb_d = din("dt_bias", [4])
    gnw_d = din("gnw", [128])
    ln1g_d = din("ln1_g", [D])
    ln1b_d = din("ln1_b", [D])
    wr_d = din("w_r", [D, 36])
    wg_d = din("w_gate", [NE, D, 256])
    wu_d = din("w_up", [NE, D, 256])
    wd_d = din("w_down", [NE, 256, D])
    ln2g_d = din("ln2_g", [D])
    ln2b_d = din("ln2_b", [D])

    y_p = dout("y_p", [SEQ, D])
    y_s = dout("y_s", [NS * TS, D])
    nk_p = dout("nk_p", [128, 128])
    nv_p = dout("nv_p", [128, 128])
    ng_p = dout("ng_p", [4, 128, 128])
    nc_p = dout("nc_p", [3, 1536])
    nk_s = dout("nk_s", [NS, 128, 128])
    nv_s = dout("nv_s", [NS, 128, 128])
    ng_s = dout("ng_s", [NS, 4, 128, 128])
    nc_s = dout("nc_s", [NS * 3, 1536])
    dbg_outs = {}

    with ExitStack() as es0, nc.allow_non_contiguous_dma(reason="small transposed param loads"):
        S = Sched(nc, es0)
        try:

            def sb(es, name, shape, dt=F32):
                return es.enter_context(nc.sbuf_tensor(name, shape, dt))

            for i in range(6):
                S.psf_list.append(es0.enter_context(nc.psum_tensor(f"psf{i}", [128, 512], F32)))
            for i in range(2):
                S.psb_list.append(es0.enter_context(nc.psum_tensor(f"psb{i}", [128, 1024], BF16)))

            def dump(name, ap_src, shape, keys):
                if not dbg:
                    return
                o = dout("dbg_" + name, shape)
                dbg_outs[name] = o
                S.dma("pool" if ap_src.dtype != F32 else "sp", o, ap_src, r=keys, w=[], slot="dbg_" + name)

            onesf = sb(es0, "onesf", [128, 128])
            ones_bf = sb(es0, "ones_bf", [128, 128], BF16)
            idf = sb(es0, "idf", [128, 128])
            idb = sb(es0, "idb", [128, 128], BF16)
            m_strict = sb(es0, "m_strict", [128, 128])
            m_up = sb(es0, "m_up", [128, 128])
            mb_prev = sb(es0, "mb_prev", [128, 128], BF16)
            mb_cur = sb(es0, "mb_cur", [128, 128], BF16)
            S.op("pool", lambda E: E.memset(onesf[:], 1.0), w=["onesf"])
            S.op("pool", lambda E: E.memset(ones_bf[:], 1.0), w=["ones_bf"])
            S.op("pool", lambda E: E.affine_select(out=idf[:], in_=onesf[:], pattern=[[-1, 128]], compare_op=ALU.is_equal,
                                                   fill=0.0, base=0, channel_multiplier=1), r=["onesf"], w=["idf"])
            S.op("pool", lambda E: E.affine_select(out=m_strict[:], in_=onesf[:], pattern=[[-1, 128]], compare_op=ALU.is_gt,
                                                   fill=0.0, base=0, channel_multiplier=1), r=["onesf"], w=["m_strict"])
            S.op("pool", lambda E: E.affine_select(out=m_up[:], in_=onesf[:], pattern=[[1, 128]], compare_op=ALU.is_ge,
                                                   fill=0.0, base=0, channel_multiplier=-1), r=["onesf"], w=["m_up"])
            S.cp("dve", idb[:], idf[:], r=["idf"], w=["idb"])
            S.cp("dve", mb_prev[:], m_strict[:], r=["m_strict"], w=["mb_prev"])
            S.cp("dve", mb_cur[:], m_up[:], r=["m_up"], w=["mb_cur"])

            esink = sb(es0, "esink", [128, 8])
            nexpA = sb(es0, "nexpA", [128, 4])
            dtb = sb(es0, "dtb", [128, 4])
            gnw = sb(es0, "gnw_bc", [128, 128])
            cw = sb(es0, "cw", [128, 4, 12])
            S.dma("sp", esink[:], sinks_d.partition_broadcast(128), w=["esink"])
            S.dma("sp", nexpA[:], alog_d.partition_broadcast(128), w=["nexpA"])
            S.dma("sp", dtb[:], dtb_d.partition_broadcast(128), w=["dtb"])
            S.dma("sp", gnw[:], gnw_d.partition_broadcast(128), w=["gnw"])
            for k in range(4):
                S.dma("sp", cw[:, k, :], convw_d[k].rearrange("(c p) -> p c", p=128), w=["cw"])
            S.act(esink[:], esink[:], AF.Exp, r=["esink"], w=["esink"])
            S.act(nexpA[:], nexpA[:], AF.Exp, r=["nexpA"], w=["nexpA"])
            S.op("act", lambda E: E.mul(out=nexpA[:], in_=nexpA[:], mul=-1.0), r=["nexpA"], w=["nexpA"])

            cosT = sb(es0, "cosT", [128, NT + 1, 8])
            sinT = sb(es0, "sinT", [128, NT + 1, 8])
            with ExitStack() as esr:
                posi = sb(esr, "posi", [128, NT + 1], I32)
                pos1 = sb(esr, "pos1", [128, 1], I32)
                posf = sb(esr, "posf", [128, NT + 1])
                ang = sb(esr, "ang", [128, NT + 1, 8])
                kk = sb(esr, "kk", [128, NT + 1, 8])
                anl = sb(esr, "anl", [128, NT + 1, 8])
                S.op("pool", lambda E: E.iota(posi[:], pattern=[[128, NT + 1]], base=0, channel_multiplier=1), w=["posi"])
                S.op("pool", lambda E: E.iota(pos1[:], pattern=[[0, 1]], base=0, channel_multiplier=1), w=["pos1"])
                S.op("dve", lambda E: E.tensor_single_scalar(out=pos1[:], in_=pos1[:], scalar=3, op=ALU.bitwise_and),
                     r=["pos1"], w=["pos1"])
                S.cp("dve", posf[:], posi[:], r=["posi"], w=["posf"])
                S.cp("dve", posf[:, NT:NT + 1], pos1[:], r=["pos1", "posf"], w=["posf"])
                S.ts("dve", posf[:, NT:NT + 1], posf[:, NT:NT + 1], float(PAST), None, ALU.add, None, r=["posf"], w=["posf"])
                for j in range(8):
                    f = THETA ** (-(2.0 * j) / 16.0)
                    m_, e_ = math.frexp(f)
                    f_hi = math.ldexp(round(m_ * 1024.0) / 1024.0, e_)
                    f_lo = f - f_hi
                    S.ts("dve", ang[:, :, j], posf[:], float(f_hi), None, ALU.mult, None, r=["posf"], w=["ang"])
                    S.ts("dve", anl[:, :, j], posf[:], float(f_lo), None, ALU.mult, None, r=["posf"], w=["anl"])
                S.tt("dve", kk[:], ang[:], anl[:], ALU.add, r=["ang", "anl"], w=["kk"])
                S.ts("dve", kk[:], kk[:], float(1.0 / (2 * math.pi)), MAGIC, ALU.mult, ALU.add, r=["kk"], w=["kk"])
                S.ts("dve", kk[:], kk[:], MAGIC, None, ALU.subtract, None, r=["kk"], w=["kk"])
                C1 = 6.28125
                C2 = 0.00193548202514648
                C3 = 2 * math.pi - C1 - C2
                for cc in (C1, C2, C3):
                    S.stt("dve", ang[:], kk[:], float(-cc), ang[:], ALU.mult, ALU.add, r=["kk", "ang"], w=["ang"])
                S.tt("dve", ang[:], ang[:], anl[:], ALU.add, r=["ang", "anl"], w=["ang"])
                PI_S = 3.1415925
                S.ts("dve", ang[:], ang[:], -PI_S, PI_S, ALU.max, ALU.min, r=["ang"], w=["ang"])
                S.act(sinT[:], ang[:], AF.Sin, r=["ang"], w=["sinT"])
                S.stt("dve", ang[:], ang[:], -1.0, ang[:], ALU.mult, ALU.max, r=["ang"], w=["ang"])
                S.ts("dve", ang[:], ang[:], -1.0, float(math.pi / 2), ALU.mult, ALU.add, r=["ang"], w=["ang"])
                S.act(cosT[:], ang[:], AF.Sin, r=["ang"], w=["cosT"])
                S.barrier()
            stop("p0")

            mix_all = sb(es0, "mix_all", [128, NT + 1, D], BF16)

            with ExitStack() as es1:
                w_in_bf = sb(es1, "w_in_bf", [128, 8, INC], BF16)
                for kc in range(8):
                    S.dma("pool", w_in_bf[:, kc, :], w_in[kc * 128:(kc + 1) * 128, :], w=[f"w_in{kc}"])
                WIN = [f"w_in{kc}" for kc in range(8)]
                S.op("pool", lambda E: E.memset(mix_all[:, NT, :], 0.0), w=["mix16"])

                with ExitStack() as es:
                    n = NS * TS
                    xTs = sb(es, "xTs", [128, 8, n], BF16)
                    S_b = sb(es, "S_b", [128, NS * 4, 128], BF16)
                    qkTs = sb(es, "qkTs", [128, 8, n], BF16)
                    vTs = sb(es, "vTs", [128, 4, n], BF16)
                    knvs = sb(es, "knvs", [n, 8, 128], BF16)
                    tqkv = sb(es, "tqkv", [n, 768])
                    tz = sb(es, "tz", [n, 512])
                    tab = sb(es, "tab", [n, 8])
                    blk = sb(es, "blk", [n, n])
                    rowm = sb(es, "rowm", [n, NS])
                    U_s = sb(es, "U_s", [n, n])
                    L_s = sb(es, "L_s", [n, n])
                    U_sb = sb(es, "U_sb", [n, n], BF16)
                    smb = sb(es, "smb", [128, NS, n], BF16)
                    nsmb = sb(es, "nsmb", [128, NS, n], BF16)
                    S_f = sb(es, "S_f", [128, 32, 128])
                    esA = ExitStack()
                    xbs = sb(esA, "xbs", [n, D], BF16)
                    cK = sb(esA, "cK", [128, NS, 128], BF16)
                    cVa = sb(esA, "cVa", [128, NS, 2, 65], BF16)
                    cKT = sb(esA, "cKT", [128, NS, 128], BF16)
                    scs = sb(esA, "scs", [NS * 3, 1536])
                    ext = sb(esA, "ext", [128, 12, NS, 7])
                    cprod = sb(esA, "cprod", [128, 12, NS, 4])
                    cacs = sb(esA, "cacs", [128, 12, NS, 4])
                    ncsf = sb(esA, "ncsf", [128, 12, NS * 3])
                    sTs = sb(esA, "sTs", [128, 8, n])
                    sq8 = sb(esA, "sq8", [128, 8, n], BF16)
                    ln8 = sb(esA, "ln8", [128, 8, n])
                    rots = sb(esA, "rots", [n, 10, 16])
                    rtm = sb(esA, "rtm", [n, 4, 10, 8])
                    qbs = sb(esA, "qbs", [n, 4, 2, 64], BF16)
                    kbs = sb(esA, "kbs", [n, 2, 64], BF16)
                    krs = sb(esA, "krs", [n, 2, 64])
                    vna = sb(esA, "vna", [n, 2, 65], BF16)
                    qTs = sb(esA, "qTs", [128, 4, n], BF16)
                    qTsr = sb(esA, "qTsr", [128, NS, 16], BF16)
                    kTn = sb(esA, "kTn", [128, n], BF16)
                    PTs = [sb(esA, f"PTs{i}", [128, 512], BF16) for i in range(2)]
                    mk16 = sb(esA, "mk16", [128, NS, n], BF16)
                    PTn = sb(esA, "PTn", [n, 512], BF16)
                    mc4 = sb(esA, "mc4", [128, 4], BF16)
                    oc = sb(esA, "oc", [65, 512])
                    ot = sb(esA, "ot", [65, 512])
                    dn = sb(esA, "dn", [65, 512])
                    oTb = sb(esA, "oTb", [64, 8, n], BF16)
                    ii = sb(esA, "ii", [n, 64], I32)
                    ip = sb(esA, "ip", [n, 1], I32)
                    colid = sb(esA, "colid", [n, 64])
                    rowid = sb(esA, "rowid", [n, 1])
                    sidx = sb(esA, "sidx", [n, NS])
                    smf = sb(esA, "smf", [128, NS, n])
                    S.dma("pool", xbs[:], x_s, w=["xbs"])
                    S.dma("pool", S_b[:], sg_d.rearrange("s h k v -> k (s h) v"), w=["S_b"])
                    S.dma("pool", cK[:], ck_d.rearrange("s r c -> r s c"), w=["cK"])
                    S.op("pool", lambda E: E.memset(cVa[:], 1.0), w=["cVa"])
                    for jk in range(2):
                        S.dma("pool", cVa[:, :, jk, 0:64], cv_d[:, :, jk * 64:(jk + 1) * 64].rearrange("s r d -> r s d"),
                              w=["cVa"], slot=f"cVa{jk}")
                    S.dma("sp", scs[:], sc_d, w=["scs"])
                    S.op("pool", lambda E: E.memset(vna[:], 1.0), w=["vna"])
                    S.dma("sp", nk_s[:, 0:124, :], ck_d[:, 4:128, :], slot="nk_s0")
                    S.dma("sp", nv_s[:, 0:124, :], cv_d[:, 4:128, :], slot="nv_s0")
                    S.op("pool", lambda E: E.iota(ii[:], pattern=[[1, 64]], base=0, channel_multiplier=0), w=["ii"])
                    S.op("pool", lambda E: E.iota(ip[:], pattern=[[0, 1]], base=0, channel_multiplier=1), w=["ip"])
                    S.op("dve", lambda E: E.tensor_single_scalar(out=ii[:], in_=ii[:], scalar=2, op=ALU.arith_shift_right),
                         r=["ii"], w=["ii"])
                    S.op("dve", lambda E: E.tensor_single_scalar(out=ip[:], in_=ip[:], scalar=2, op=ALU.arith_shift_right),
                         r=["ip"], w=["ip"])
                    S.cp("dve", colid[:], ii[:], r=["ii"], w=["colid"])
                    S.cp("dve", rowid[:], ip[:], r=["ip"], w=["rowid"])
                    S.ts("dve", blk[:], colid[:], rowid[:, 0:1], None, ALU.is_equal, None, r=["colid", "rowid"], w=["blk"])
                    S.op("pool", lambda E: E.iota(ii[:, 0:NS], pattern=[[1, NS]], base=0, channel_multiplier=0), r=["colid"], w=["ii"])
                    S.cp("dve", sidx[:], ii[:, 0:NS], r=["ii"], w=["sidx"])
                    S.ts("dve", rowm[:], sidx[:], rowid[:, 0:1], None, ALU.is_equal, None, r=["sidx", "rowid"], w=["rowm"])
                    S.tt("dve", U_s[:], m_up[0:n, 0:n], blk[:], ALU.mult, r=["blk"], w=["U_s"])
                    S.tt("dve", L_s[:], m_strict[0:n, 0:n], blk[:], ALU.mult, r=["blk"], w=["L_s"])
                    S.cp("dve", U_sb[:], U_s[:], r=["U_s"], w=["U_sb"])
                    S.op("pool", lambda E: E.memset(smf[:], 1.0), w=["smf"])
                    S.op("pool", lambda E: E.affine_select(out=smf[:], in_=smf[:], pattern=[[-4, NS], [1, n]],
                                                           compare_op=ALU.is_ge, fill=0.0, base=0, channel_multiplier=0),
                         r=["smf"], w=["smf"])
                    S.op("pool", lambda E: E.affine_select(out=smf[:], in_=smf[:], pattern=[[4, NS], [-1, n]],
                                                           compare_op=ALU.is_ge, fill=0.0, base=3, channel_multiplier=0),
                         r=["smf"], w=["smf"])
                    S.cp("dve", smb[:], smf[:], r=["smf"], w=["smb"])
                    S.ts("dve", nsmb[:], smf[:], -1.0, None, ALU.mult, None, r=["smf"], w=["nsmb"])
                    S.op("pool", lambda E: E.affine_select(out=mc4[:], in_=ones_bf[:, 0:4], pattern=[[-1, 4]],
                                                           compare_op=ALU.is_gt, fill=0.0, base=0, channel_multiplier=1),
                         w=["mc4"])
                    stop("sA")
                    pb, pbk = S.psb()
                    for kc in range(8):
                        S.tr(pb[:, kc * 128:kc * 128 + n], xbs[:, kc * 128:(kc + 1) * 128], idb[0:n, 0:n], r=["xbs"], w=[pbk],
                             inc=(kc == 7))
                    S.cp("act", xTs[:], pb[:].rearrange("p (k t) -> p k t", k=8)[:, :, 0:n], r=[pbk], w=["xTs"])
                    for grp in range(2):
                        pf, pfk = S.psf()
                        ncg = 8 if grp == 0 else 4
                        for cc in range(ncg):
                            c = grp * 8 + cc
                            S.tr(pf[:, cc * 48:(cc + 1) * 48], scs[:, c * 128:(c + 1) * 128], idf[0:48, 0:48], r=["scs"],
                                 w=[pfk], inc=(cc == ncg - 1))
                        S.cp("act", ext[:, grp * 8:grp * 8 + ncg, :, 0:3],
                             pf[:, 0:ncg * 48].rearrange("p (c s r) -> p c s r", c=ncg, s=NS), r=[pfk], w=["ext"])
                    for c in range(12):
                        pf, pfk = S.psf()
                        for kc in range(8):
                            S.mm(pf[:, 0:n], w_in_bf[:, kc, 768 + c * 128:768 + (c + 1) * 128], xTs[:, kc, :], kc == 0, kc == 7,
                                 r=[WIN[kc], "xTs"], w=[pfk])
                        S.cp("act" if c % 2 else "dve", ext[:, c, :, 3:7], pf[:, 0:n].rearrange("p (s t) -> p s t", s=NS),
                             r=[pfk], w=["ext"])
                    S.cp("pool", ncsf[:].rearrange("p c (s r) -> p c s r", s=NS), ext[:, :, :, 4:7], r=["ext"], w=["ncsf"])
                    for grp in range(3):
                        pf, pfk = S.psf()
                        for cc in range(4):
                            c = grp * 4 + cc
                            S.tr(pf[0:48, cc * 128:(cc + 1) * 128], ncsf[:, c, :], idf[:], r=["ncsf"], w=[pfk], inc=(cc == 3))
                        S.cp("act", scs[:, grp * 512:(grp + 1) * 512], pf[0:48, :], r=[pfk], w=["scs"])
                    S.dma("sp", nc_s, scs[:], r=["scs"], slot="nc_s")
                    stop("sB")
                    for k in range(4):
                        cwb = cw[:, k, :].unsqueeze(2).unsqueeze(3).to_broadcast([128, 12, NS, 4])
                        if k == 0:
                            S.tt("dve", cacs[:], ext[:, :, :, 0:4], cwb, ALU.mult, r=["ext", "cw"], w=["cacs"])
                        else:
                            S.tt("pool", cprod[:], ext[:, :, :, k:k + 4], cwb, ALU.mult, r=["ext", "cw"], w=["cprod"])
                            S.tt("dve", cacs[:], cacs[:], cprod[:], ALU.add, r=["cacs", "cprod"], w=["cacs"])
                    S.act(sTs[:].rearrange("p c (s t) -> p c s t", s=NS), cacs[:, 0:8], AF.Silu, r=["cacs"], w=["sTs"])
                    S.act(vTs[:].rearrange("p c (s t) -> p c s t", s=NS), cacs[:, 8:12], AF.Silu, r=["cacs"], w=["vTs"])
                    S.act(sq8[:], sTs[:], AF.Square, r=["sTs"], w=["sq8"])
                    pf, pfk = S.psf()
                    S.mm(pf[:, 0:8 * n], ones_bf[:], sq8[:].rearrange("p c t -> p (c t)"), True, True, r=["sq8"], w=[pfk])
                    S.act(ln8[:].rearrange("p c t -> p (c t)"), pf[:, 0:8 * n], AF.Ln, r=[pfk], w=["ln8"], bias=L2_EPS)
                    S.act(ln8[:, 0:4, :], ln8[:, 0:4, :], AF.Exp, r=["ln8"], w=["ln8"], bias=-0.5 * math.log(128.0), scale=-0.5)
                    S.act(ln8[:, 4:8, :], ln8[:, 4:8, :], AF.Exp, r=["ln8"], w=["ln8"], scale=-0.5)
                    S.tt("dve", qkTs[:], sTs[:], ln8[:], ALU.mult, r=["sTs", "ln8"], w=["qkTs"])
                    pb, pbk = S.psb()
                    for h in range(4):
                        S.tr(pb[0:n, h * 128:(h + 1) * 128], qkTs[:, 4 + h, :], idb[:], r=["qkTs"], w=[pbk], inc=False)
                    for h in range(4):
                        S.tr(pb[0:n, (4 + h) * 128:(5 + h) * 128], vTs[:, h, :], idb[:], r=["vTs"], w=[pbk], inc=(h == 3))
                    S.cp("act", knvs[:], pb[0:n, :].rearrange("p (k t) -> p k t", k=8), r=[pbk], w=["knvs"])
                    for (c0, c1, dst, dk_) in ((0, 512, tqkv[:, 0:512], "tq_s"), (512, 768, tqkv[:, 512:768], "tkv_s"),
                                               (2304, 2816, tz[:], "tz_s"), (2816, 2824, tab[:], "tab_s")):
                        pf, pfk = S.psf()
                        for kc in range(8):
                            S.mm(pf[0:n, 0:c1 - c0], xTs[:, kc, :], w_in_bf[:, kc, c0:c1], kc == 0, kc == 7,
                                 r=[WIN[kc], "xTs"], w=[pfk])
                        S.cp("act" if dk_ in ("tq_s", "tz_s") else "dve", dst, pf[0:n, 0:c1 - c0], r=[pfk], w=[dk_])
                    qk3 = tqkv[:, 0:640].rearrange("p (h d) -> p h d", d=64)
                    cb = cosT[0:n, NT, :].unsqueeze(1).to_broadcast([n, 10, 8])
                    sbb = sinT[0:n, NT, :].unsqueeze(1).to_broadcast([n, 10, 8])
                    RK = ["tq_s", "tkv_s"]
                    S.tt("dve", rtm[:, 0], qk3[:, :, 0:8], cb, ALU.mult, r=RK, w=["rtm0"])
                    S.tt("dve", rtm[:, 1], qk3[:, :, 8:16], sbb, ALU.mult, r=RK, w=["rtm1"])
                    S.tt("dve", rtm[:, 2], qk3[:, :, 8:16], cb, ALU.mult, r=RK, w=["rtm2"])
                    S.tt("dve", rtm[:, 3], qk3[:, :, 0:8], sbb, ALU.mult, r=RK, w=["rtm3"])
                    S.tt("dve", rots[:, :, 0:8], rtm[:, 0], rtm[:, 1], ALU.subtract, r=["rtm0", "rtm1"], w=["rots"])
                    S.tt("dve", rots[:, :, 8:16], rtm[:, 2], rtm[:, 3], ALU.add, r=["rtm2", "rtm3"], w=["rots"])
                    q4 = tqkv[:, 0:512].rearrange("p (j c d) -> p c j d", j=2, c=4)
                    S.cp("dve", qbs[:, :, :, 0:16], rots[:, 0:8, :].rearrange("p (j c) d -> p c j d", j=2), r=["rots"], w=["qbs"])
                    S.cp("dve", qbs[:, :, :, 16:64], q4[:, :, :, 16:64], r=["tq_s"], w=["qbs"])
                    k3 = tqkv[:, 512:640].rearrange("p (j d) -> p j d", j=2)
                    S.cp("dve", kbs[:, :, 0:16], rots[:, 8:10, :], r=["rots"], w=["kbs"])
                    S.cp("dve", kbs[:, :, 16:64], k3[:, :, 16:64], r=["tkv_s"], w=["kbs"])
                    S.cp("dve", krs[:, :, 0:16], rots[:, 8:10, :], r=["rots"], w=["krs"])
                    S.cp("dve", krs[:, :, 16:64], k3[:, :, 16:64], r=["tkv_s"], w=["krs"])
                    S.cp("dve", vna[:, :, 0:64], tqkv[:, 640:768].rearrange("p (j d) -> p j d", j=2), r=["tkv_s", "vna"], w=["vna"])
                    S.dma("sp", nk_s[:, 124:128, :].rearrange("s t c -> (s t) c") if False else nk_s[:, 124:128, :],
                          krs[:].rearrange("(s t) j d -> s t (j d)", t=TS) if False else krs[:].rearrange("p j d -> p (j d)"),
                          r=["krs"], slot="nk_s1")
                    S.dma("sp", nv_s[:, 124:128, :], tqkv[:, 640:768], r=["tkv_s"], slot="nv_s1")
                    stop("sC")
                    pb, pbk = S.psb()
                    for c in range(4):
                        S.tr(pb[:, c * 128:c * 128 + n], qbs[:, c].rearrange("p j d -> p (j d)"), idb[0:n, 0:n], r=["qbs"],
                             w=[pbk], inc=False)
                    S.tr(pb[:, 512:512 + n], kbs[:].rearrange("p j d -> p (j d)"), idb[0:n, 0:n], r=["kbs"], w=[pbk])
                    S.cp("act", qTs[:], pb[:, 0:512].rearrange("p (c t) -> p c t", c=4)[:, :, 0:n], r=[pbk], w=["qTs"])
                    S.cp("act", kTn[:], pb[:, 512:512 + n], r=[pbk], w=["kTn"])
                    S.cp("dve", qTsr[:].rearrange("p s (c t) -> p s c t", c=4),
                         qTs[:].rearrange("p c (s t) -> p s c t", s=NS), r=["qTs"], w=["qTsr"])
                    for grp in range(2):
                        pb, pbk = S.psb()
                        for ss in range(8):
                            s_ = grp * 8 + ss
                            S.tr(pb[:, ss * 128:(ss + 1) * 128], cK[:, s_, :], idb[:], r=["cK"], w=[pbk], inc=(ss == 7))
                        S.cp("act" if grp else "dve", cKT[:, grp * 8:(grp + 1) * 8, :], pb[:].rearrange("p (s t) -> p s t", s=8),
                             r=[pbk], w=["cKT"])
                    stop("sC2")
                    S.tt("dve", mk16[:].rearrange("p s (a t) -> p (s a) t", t=4), smb[:].rearrange("p s (a t) -> p (s a) t", t=4),
                         mc4[:].unsqueeze(1).to_broadcast([128, NS * NS, 4]), ALU.mult, r=["smb", "mc4"], w=["mk16"])
                    stop("sD0")
                    base = S.psf_i % 6
                    S.psf_i += 6
                    bk = [(base + d) % 6 for d in range(6)]
                    po_ = [(S.psf_list[bk[j]], f"psf{bk[j]}") for j in range(2)]
                    ps_ = [(S.psf_list[bk[2 + j]], f"psf{bk[2 + j]}") for j in range(4)]
                    for s_ in range(NS):
                        pt_, ptk_ = PTs[s_ % 2], f"PTs{s_ % 2}"
                        for jk in range(2):
                            psc, psck = ps_[(s_ % 2) * 2 + jk]
                            S.mm(psc[:, 0:256], cKT[jk * 64:(jk + 1) * 64, s_, :],
                                 qTs[jk * 64:(jk + 1) * 64, :, :].rearrange("p c t -> p (c t)"), True, True,
                                 r=["cKT", "qTs"], w=[psck], inc=True)
                            S.act(pt_[:, jk * 256:(jk + 1) * 256], psc[:, 0:256], AF.Exp, r=[psck], w=[ptk_], scale=0.125)
                        S.tt("dve", pt_[:].rearrange("p (a t) -> p a t", a=8), pt_[:].rearrange("p (a t) -> p a t", a=8),
                             mk16[:, s_, :].unsqueeze(1).to_broadcast([128, 8, n]), ALU.mult, r=[ptk_, "mk16"], w=[ptk_])
                        for jk in range(2):
                            S.mm(po_[jk][0][0:65, 0:256], cVa[:, s_, jk, :], pt_[:, jk * 256:(jk + 1) * 256], s_ == 0, False,
                                 r=["cVa", ptk_], w=[po_[jk][1]], inc=(jk == 1))
                    stop("sD1")
                    for jk in range(2):
                        psn, psnk = ps_[jk]
                        S.mm(psn[0:n, 0:256], kTn[jk * 64:(jk + 1) * 64, :],
                             qTs[jk * 64:(jk + 1) * 64, :, :].rearrange("p c t -> p (c t)"), True, True, r=["kTn", "qTs"],
                             w=[psnk], inc=True)
                        S.act(PTn[:, jk * 256:(jk + 1) * 256], psn[0:n, 0:256], AF.Exp, r=[psnk], w=["PTn"], scale=0.125)
                    S.tt("pool", PTn[:].rearrange("p (a t) -> p a t", a=8), PTn[:].rearrange("p (a t) -> p a t", a=8),
                         U_sb[:].unsqueeze(1).to_broadcast([n, 8, n]), ALU.mult, r=["PTn", "U_sb"], w=["PTn"])
                    for jk in range(2):
                        S.mm(po_[jk][0][0:65, 0:256], vna[:, jk, :], PTn[:, jk * 256:(jk + 1) * 256], False, True,
                             r=["vna", "PTn"], w=[po_[jk][1]], inc=True)
                    stop("sD3")
                    for jk in range(2):
                        S.cp("act" if jk else "dve", ot[:, jk * 256:(jk + 1) * 256], po_[jk][0][0:65, 0:256], r=[po_[jk][1]],
                             w=["ot"])
                    S.tt("dve", dn[64:65, :].rearrange("p (h t) -> p h t", h=8), ot[64:65, :].rearrange("p (h t) -> p h t", h=8),
                         esink[64:65, :].unsqueeze(2).to_broadcast([1, 8, n]), ALU.add, r=["ot", "esink"], w=["dn"])
                    S.op("dve", lambda E: E.reciprocal(out=dn[64:65, :], in_=dn[64:65, :]), r=["dn"], w=["dn"])
                    stop("sD4")
                    pbc, pbck = S.psf()
                    S.mm(pbc[0:64, :], onesf[64:65, 0:64], dn[64:65, :], True, True, r=["dn"], w=[pbck])
                    S.tt("dve", oTb[:].rearrange("p h t -> p (h t)"), pbc[0:64, :], ot[0:64, :], ALU.mult, r=[pbck, "ot"],
                         w=["oTb"])
                    stop("sD5")
                    pb, pbk = S.psb()
                    for h in range(8):
                        S.tr(pb[0:n, h * 64:(h + 1) * 64], oTb[:, h, :], idb[0:64, 0:64], r=["oTb"], w=[pbk], inc=(h == 7))
                    S.cp("act", mix_all[0:n, NT, 0:512], pb[0:n, 0:512], r=[pbk], w=["mix16"])
                    S.barrier()
                    esA.close()
                    gabs = sb(es, "gabs", [n, 8, 4])
                    gmk = sb(es, "gmk", [n, NS, 4])
                    abc = sb(es, "abc", [128, NS * 4])
                    gUs = sb(es, "gUs", [n, 4, n])
                    eDs = sb(es, "eDs", [n, 4, n])
                    eDTs = sb(es, "eDTs", [n, 4, n])
                    ATs = sb(es, "ATs", [n, 4, n], BF16)
                    P0s = sb(es, "P0s", [n, 4, n])
                    P0Ts = sb(es, "P0Ts", [n, 4, n])
                    P1s = sb(es, "P1s", [n, 4, n])
                    Q0s = sb(es, "Q0s", [n, 4, n])
                    Qbs = sb(es, "Qbs", [n, 4, n], BF16)
                    kbes = sb(es, "kbes", [n, 4, 128], BF16)
                    kdcs = sb(es, "kdcs", [n, 4, 128], BF16)
                    vbs = sb(es, "vbs", [n, 4, 128], BF16)
                    vns = sb(es, "vns", [n, 4, 128], BF16)
                    o1ss = sb(es, "o1ss", [n, 4, 128])
                    ogs = sb(es, "ogs", [n, 4, 128])
                    osqs = sb(es, "osqs", [n, 4, 128])
                    ssqs = sb(es, "ssqs", [n, 4])
                    zss = sb(es, "zss", [n, 512])
                    Stm = [sb(es, f"Stm{i}", [128, 4, 128]) for i in range(2)]
                    nwTh = [sb(es, f"nwTh{i}", [128, NS, n], BF16) for i in range(2)]
                    qTh = [sb(es, f"qTh{i}", [128, NS, n], BF16) for i in range(2)]
                    vnh = [sb(es, f"vnh{i}", [n, NS, 128], BF16) for i in range(2)]

                    stop("sD")
                    g_ = gabs[:, 0, :]
                    beta_ = gabs[:, 1, :]
                    gc_ = gabs[:, 2, :]
                    egc_ = gabs[:, 3, :]
                    kds_ = gabs[:, 4, :]
                    tmp_ = gabs[:, 6, :]
                    nbeta_ = gabs[:, 7, :]
                    S.tt("dve", tmp_, tab[:, 0:4], dtb[0:n, :], ALU.add, r=["tab_s"], w=["s_tmp"])
                    S.act(tmp_, tmp_, AF.Exp, r=["s_tmp"], w=["s_tmp"])
                    S.act(tmp_, tmp_, AF.Ln, r=["s_tmp"], w=["s_tmp"], bias=1.0)
                    S.tt("dve", g_, tmp_, nexpA[0:n, :], ALU.mult, r=["s_tmp"], w=["s_g"])
                    S.act(beta_, tab[:, 4:8], AF.Exp, r=["tab_s"], w=["s_beta"], scale=-1.0)
                    S.ts("dve", beta_, beta_, 1.0, None, ALU.add, None, r=["s_beta"], w=["s_beta"])
                    S.op("dve", lambda E: E.reciprocal(out=beta_, in_=beta_), r=["s_beta"], w=["s_beta"])
                    S.ts("dve", nbeta_, beta_, -1.0, None, ALU.mult, None, r=["s_beta"], w=["s_nbeta"])
                    S.tt("dve", gmk[:], g_.unsqueeze(1).to_broadcast([n, NS, 4]), rowm[:].unsqueeze(2).to_broadcast([n, NS, 4]),
                         ALU.mult, r=["s_g", "rowm"], w=["gmk"])
                    pf, pfk = S.psf()
                    S.mm(pf[0:n, 0:4], U_s[:], g_, True, True, r=["U_s", "s_g"], w=[pfk])
                    S.mm(pf[0:n, 4:8], blk[:], g_, True, True, r=["blk", "s_g"], w=[pfk])
                    S.cp("dve", gc_, pf[0:n, 0:4], r=[pfk], w=["s_gc"])
                    S.act(egc_, pf[0:n, 0:4], AF.Exp, r=[pfk], w=["s_egc"])
                    S.tt("dve", kds_, pf[0:n, 4:8], gc_, ALU.subtract, r=[pfk, "s_gc"], w=["s_kds"])
                    S.act(kds_, kds_, AF.Exp, r=["s_kds"], w=["s_kds"])
                    pa, pak = S.psf()
                    S.mm(pa[:, 0:NS * 4], onesf[0:n, :], gmk[:].rearrange("p s h -> p (s h)"), True, True, r=["gmk"], w=[pak])
                    S.act(abc[:], pa[:, 0:NS * 4], AF.Exp, r=[pak], w=["abc"])
                    pD, pDk = S.psf()
                    pDT, pDTk = S.psf()
                    pG, pGk = S.psf()
                    pA, pAk = S.psf()
                    v3 = lambda p: p[0:n, 0:4 * n].rearrange("p (h t) -> p h t", h=4)
                    pD3, pDT3, pG3, pA3 = v3(pD), v3(pDT), v3(pG), v3(pA)
                    for h in range(4):
                        S.ts("dve", gUs[:, h, :], U_s[:], gabs[:, 0, h:h + 1], None, ALU.mult, None, r=["U_s", "s_g"], w=["gUs"])
                    for h in range(4):
                        S.mm(pD3[:, h, :], gUs[:, h, :], L_s[:], True, True, r=["gUs", "L_s"], w=[pDk], inc=(h == 3))
                    for h in range(4):
                        S.mm(pDT3[:, h, :], L_s[:], gUs[:, h, :], True, True, r=["gUs", "L_s"], w=[pDTk], inc=(h == 3))
                    for h in range(4):
                        S.mm(pG3[:, h, :], qkTs[:, 4 + h, :], qkTs[:, 4 + h, :], True, True, r=["qkTs"], w=[pGk], inc=(h == 3))
                    for h in range(4):
                        S.mm(pA3[:, h, :], qkTs[:, 4 + h, :], qkTs[:, h, :], True, True, r=["qkTs"], w=[pAk], inc=(h == 3))
                    S.act(eDs[:], pD3, AF.Exp, r=[pDk], w=["eDs"])
                    S.tt("pool", eDs[:], eDs[:], L_s[:].unsqueeze(1).to_broadcast([n, 4, n]), ALU.mult, r=["eDs", "L_s"], w=["eDs"])
                    S.act(eDTs[:], pDT3, AF.Exp, r=[pDTk], w=["eDTs"])
                    S.tt("pool", eDTs[:], eDTs[:], U_s[:].unsqueeze(1).to_broadcast([n, 4, n]), ALU.mult, r=["eDTs", "U_s"],
                         w=["eDTs"])
                    for h in range(4):
                        S.stt("dve", P0s[:, h, :], pG3[:, h, :], gabs[:, 7, h:h + 1], eDs[:, h, :], ALU.mult, ALU.mult,
                              r=[pGk, "s_nbeta", "eDs"], w=["P0s"])
                    S.tt("dve", ATs[:], pA3, eDTs[:], ALU.mult, r=[pAk, "eDTs"], w=["ATs"])
                    pt, ptk = S.psf()
                    pt3 = v3(pt)
                    for h in range(4):
                        S.tr(pt3[:, h, :], P0s[:, h, :], idf[0:n, 0:n], r=["P0s"], w=[ptk], inc=(h == 3))
                    S.cp("act", P0Ts[:], pt3, r=[ptk], w=["P0Ts"])
                    S.tt("dve", Q0s[:], pt3, idf[0:n, 0:n].unsqueeze(1).to_broadcast([n, 4, n]), ALU.add, r=[ptk], w=["Q0s"])
                    pP, pPk = S.psf()
                    pP3 = v3(pP)
                    for h in range(4):
                        S.mm(pP3[:, h, :], P0Ts[:, h, :], P0s[:, h, :], True, True, r=["P0Ts", "P0s"], w=[pPk], inc=(h == 3))
                    S.cp("act", P1s[:], pP3, r=[pPk], w=["P1s"])
                    pQ, pQk = S.psf()
                    pQ3 = v3(pQ)
                    for h in range(4):
                        S.mm(pQ3[:, h, :], P1s[:, h, :], Q0s[:, h, :], True, True, r=["P1s", "Q0s"], w=[pQk], inc=(h == 3))
                    S.tt("dve", Qbs[:], pQ3, Q0s[:], ALU.add, r=[pQk, "Q0s"], w=["Qbs"])
                    stop("sE")
                    kn3 = knvs[:, 0:4, :]
                    vv3 = knvs[:, 4:8, :]
                    S.tt("dve", tmp_, beta_, egc_, ALU.mult, r=["s_beta", "s_egc", "s_tmp"], w=["s_tmp"])
                    S.tt("pool", kbes[:], kn3, tmp_.unsqueeze(2).to_broadcast([n, 4, 128]), ALU.mult, r=["knvs", "s_tmp"], w=["kbes"])
                    S.tt("pool", kdcs[:], kn3, kds_.unsqueeze(2).to_broadcast([n, 4, 128]), ALU.mult, r=["knvs", "s_kds"], w=["kdcs"])
                    S.tt("dve", vbs[:], vv3, beta_.unsqueeze(2).to_broadcast([n, 4, 128]), ALU.mult, r=["knvs", "s_beta"], w=["vbs"])
                    pw, pwk = S.psf()
                    pw3 = pw[:, 0:4 * n].rearrange("p (h t) -> p h t", h=4)
                    for h in range(4):
                        S.mm(pw3[:, h, :], kbes[:, h, :], Qbs[:, h, :], True, True, r=["kbes", "Qbs"], w=[pwk], inc=(h == 3))
                    pv, pvk = S.psf()
                    pv3 = pv[0:n, :].rearrange("p (h t) -> p h t", h=4)
                    for h in range(4):
                        S.tt("dve", nwTh[h % 2][:], pw3[:, h, :].unsqueeze(1).to_broadcast([128, NS, n]), nsmb[:], ALU.mult,
                             r=[pwk, "nsmb"], w=[f"nwTh{h % 2}"])
                        S.mm(pv3[:, h, :], Qbs[:, h, :], vbs[:, h, :], True, False, r=["Qbs", "vbs"], w=[pvk], inc=False)
                        for s_ in range(NS):
                            S.mm(pv3[:, h, :], nwTh[h % 2][:, s_, :], S_b[:, s_ * 4 + h, :], False, s_ == NS - 1,
                                 r=[f"nwTh{h % 2}", "S_b"], w=[pvk], inc=(s_ == NS - 1))
                    S.cp("act", vns[:], pv3, r=[pvk], w=["vns"])
                    po1, po1k = S.psf()
                    po13 = po1[0:n, :].rearrange("p (h t) -> p h t", h=4)
                    for h in range(4):
                        S.tt("pool", qTh[h % 2][:], qkTs[:, h, :].unsqueeze(1).to_broadcast([128, NS, n]), smb[:], ALU.mult,
                             r=["qkTs", "smb"], w=[f"qTh{h % 2}"])
                        for s_ in range(NS):
                            S.mm(po13[:, h, :], qTh[h % 2][:, s_, :], S_b[:, s_ * 4 + h, :], s_ == 0, s_ == NS - 1,
                                 r=[f"qTh{h % 2}", "S_b"], w=[po1k], inc=(s_ == NS - 1))
                    po2, po2k = S.psf()
                    po23 = po2[0:n, :].rearrange("p (h t) -> p h t", h=4)
                    for h in range(4):
                        S.mm(po23[:, h, :], ATs[:, h, :], vns[:, h, :], True, True, r=["ATs", "vns"], w=[po2k], inc=(h == 3))
                    S.tt("dve", o1ss[:], po13, egc_.unsqueeze(2).to_broadcast([n, 4, 128]), ALU.mult, r=[po1k, "s_egc"], w=["o1ss"])
                    S.tt("dve", ogs[:], po23, o1ss[:], ALU.add, r=[po2k, "o1ss"], w=["ogs"])
                    stop("sF")
                    S_f4 = S_f[:].rearrange("p (s h) v -> p s h v", h=4)
                    abc3 = abc[:].rearrange("p (s h) -> p s h", h=4)
                    for half in range(2):
                        S.dma("sp", S_f[:], sg_d[half * 8:(half + 1) * 8].rearrange("s h k v -> k (s h) v"), w=["S_f"])
                        for h in range(4):
                            vh, vhk = vnh[h % 2], f"vnh{h % 2}"
                            S.tt("dve", vh[:], vns[:, h, :].unsqueeze(1).to_broadcast([n, NS, 128]),
                                 rowm[:].unsqueeze(2).to_broadcast([n, NS, 128]), ALU.mult, r=["vns", "rowm"], w=[vhk])
                            for sg_ in range(2):
                                pS, pSk = S.psf()
                                pS3 = pS[:].rearrange("p (s t) -> p s t", s=4)
                                for sl in range(4):
                                    s_ = half * 8 + sg_ * 4 + sl
                                    S.mm(pS3[:, sl, :], kdcs[:, h, :], vh[:, s_, :], True, True, r=["kdcs", vhk], w=[pSk],
                                         inc=(sl == 3))
                                stt_, sttk = Stm[sg_ % 2], f"Stm{sg_ % 2}"
                                sl0 = sg_ * 4
                                s0 = half * 8 + sg_ * 4
                                S.tt("pool", stt_[:], S_f4[:, sl0:sl0 + 4, h, :],
                                     abc3[:, s0:s0 + 4, h].unsqueeze(2).to_broadcast([128, 4, 128]), ALU.mult,
                                     r=["S_f", "abc"], w=[sttk])
                                S.tt("dve", S_f4[:, sl0:sl0 + 4, h, :], pS3, stt_[:], ALU.add, r=[pSk, sttk], w=["S_f"])
                        S.dma("sp", ng_s[half * 8:(half + 1) * 8].rearrange("s h k v -> k (s h) v"), S_f[:], r=["S_f"], slot="ng_s")
                    S.tt("pool", osqs[:], ogs[:], ogs[:], ALU.mult, r=["ogs"], w=["osqs"])
                    S.op("dve", lambda E: E.tensor_reduce(out=ssqs[:], in_=osqs[:], axis=AX.X, op=ALU.add), r=["osqs"], w=["ssqs"])
                    S.act(ssqs[:], ssqs[:], AF.Ln, r=["ssqs"], w=["ssqs"], bias=NORM_EPS, scale=1.0 / 128.0)
                    S.act(ssqs[:], ssqs[:], AF.Exp, r=["ssqs"], w=["ssqs"], scale=-0.5)
                    S.act(zss[:], tz[:], AF.Silu, r=["tz_s"], w=["zss"])
                    S.tt("pool", zss[:].rearrange("p (h d) -> p h d", h=4), zss[:].rearrange("p (h d) -> p h d", h=4),
                         gnw[0:n, :].unsqueeze(1).to_broadcast([n, 4, 128]), ALU.mult, r=["zss"], w=["zss"])
                    S.tt("dve", ogs[:], ogs[:], ssqs[:].unsqueeze(2).to_broadcast([n, 4, 128]), ALU.mult, r=["ogs", "ssqs"], w=["ogs"])
                    S.tt("dve", mix_all[0:n, NT, 512:1024].rearrange("p (h d) -> p h d", h=4), ogs[:],
                         zss[:].rearrange("p (h d) -> p h d", h=4), ALU.mult, r=["ogs", "zss"], w=["mix16"])
                    S.barrier()

                with ExitStack() as es:
                    ST = 256
                    xb = [sb(es, f"xb{i}", [128, D], BF16) for i in range(2)]
                    xT = [sb(es, f"xT{i}", [128, 8, ST], BF16) for i in range(2)]
                    gT = sb(es, "gT", [128, 12, ST + 3])
                    cacc = [sb(es, f"cacc{i}", [128, ST]) for i in range(2)]
                    sT = sb(es, "sT", [128, 8, ST])
                    qkT2 = [sb(es, f"qkT_{i}", [128, 8, ST], BF16) for i in range(2)]
                    vT2 = [sb(es, f"vT_{i}", [128, 4, ST], BF16) for i in range(2)]
                    sq4 = sb(es, "sq4", [128, 4, ST], BF16)
                    ln4 = sb(es, "ln4", [128, 4, ST])
                    knv = sb(es, "knv", [128, 8, 128], BF16)
                    tm_qkv = sb(es, "tm_qkv", [128, 768])
                    zs4 = [sb(es, f"zs2_{i}", [128, 512]) for i in range(4)]
                    tm_ab = sb(es, "tm_ab", [128, 8])
                    rot = sb(es, "rot", [128, 10, 16])
                    rtmp = sb(es, "rtmp", [128, 4, 10, 8])
                    q_bf = sb(es, "q_bf", [128, 4, 2, 64], BF16)
                    k_bf = sb(es, "k_bf", [128, 2, 64], BF16)
                    k_rot = sb(es, "k_rot", [128, 2, 64])
                    v_aug = [sb(es, f"v_aug{i}", [128, 2, 65], BF16) for i in range(2)]
                    kTb = [sb(es, f"kTb{i}", [128, 128], BF16) for i in range(2)]
                    qT = sb(es, "qT", [128, 4, 128], BF16)
                    PTe = [sb(es, f"PTe{i}", [128, 4, 128], BF16) for i in range(2)]
                    den = sb(es, "den", [128, 2, 4])
                    gab = sb(es, "gab", [128, 8, 4])
                    gU = sb(es, "gU", [128, 4, 128])
                    eD = sb(es, "eD", [128, 4, 128])
                    eDT = sb(es, "eDT", [128, 4, 128])
                    ATb = sb(es, "ATb", [128, 4, 128], BF16)
                    Pm = [sb(es, f"Pm{i}", [128, 4, 128]) for i in range(2)]
                    PmT = [sb(es, f"PmT{i}", [128, 4, 128]) for i in range(2)]
                    Qm = [sb(es, f"Qm{i}", [128, 4, 128]) for i in range(2)]
                    Qb = sb(es, "Qb", [128, 4, 128], BF16)
                    kbe = sb(es, "kbe", [128, 4, 128], BF16)
                    kdc = sb(es, "kdc", [128, 4, 128], BF16)
                    vb = sb(es, "vb", [128, 4, 128], BF16)
                    nwT = sb(es, "nwT", [128, 4, 128], BF16)
                    vnew = sb(es, "vnew", [128, 4, 128], BF16)
                    Sf = sb(es, "Sf", [128, 4, 128])
                    Sb = sb(es, "Sb", [128, 4, 128], BF16)
                    Stmp = sb(es, "Stmp", [128, 4, 128])
                    o1s = sb(es, "o1s", [128, 4, 128])
                    og = sb(es, "og", [128, 4, 128])
                    osq = sb(es, "osq", [128, 4, 128])
                    ssq = sb(es, "ssq", [128, 4])

                    S.op("pool", lambda E: E.memset(gT[:, :, 0:3], 0.0), w=[f"gT{c}" for c in range(12)])
                    S.op("pool", lambda E: E.memset(Sf[:], 0.0), w=["Sf"])
                    S.op("pool", lambda E: E.memset(Sb[:], 0.0), w=["Sb"])
                    for i in range(2):
                        S.op("pool", lambda E, i=i: E.memset(v_aug[i][:], 1.0), w=[f"v_aug{i}"])

                    GT = [f"gT{c}" for c in range(12)]
                    def fm(st):
                        qkT, vT, zs2 = qkT2[st % 2], vT2[st % 2], zs4[(st % 2) * 2:(st % 2) * 2 + 2]
                        PAR = st % 2
                        xTs = xT[st % 2]
                        xTk = f"xT{st % 2}"
                        for j in range(ST // 128):
                            i = st * (ST // 128) + j
                            xbt = xb[i % 2]
                            xbk = f"xb{i % 2}"
                            S.dma("pool", xbt[:], x_p[i * 128:(i + 1) * 128, :], w=[xbk])
                            pb, pbk = S.psb()
                            for kc in range(8):
                                S.tr(pb[:, kc * 128:(kc + 1) * 128], xbt[:, kc * 128:(kc + 1) * 128], idb[:],
                                     r=[xbk, "idb"], w=[pbk], inc=(kc == 7))
                            S.cp("act", xTs[:, :, j * 128:(j + 1) * 128], pb[:].rearrange("p (k t) -> p k t", k=8),
                                 r=[pbk], w=[xTk])
                        for c in range(12):
                            pf, pfk = S.psf()
                            for kc in range(8):
                                S.mm(pf[:, 0:ST], w_in_bf[:, kc, 768 + c * 128:768 + (c + 1) * 128], xTs[:, kc, :],
                                     kc == 0, kc == 7, r=[WIN[kc], xTk], w=[pfk])
                            S.cp("act" if c % 2 else "dve", gT[:, c, 3:3 + ST], pf[:, 0:ST], r=[pfk], w=[GT[c]])
                            acc = cacc[c % 2]
                            ak = f"cacc{c % 2}"
                            ce = "dve"
                            S.ts(ce, acc[:], gT[:, c, 0:ST], cw[:, 0, c:c + 1], None, ALU.mult, None, r=[GT[c], "cw"], w=[ak])
                            for k in range(1, 4):
                                S.stt(ce, acc[:], gT[:, c, k:k + ST], cw[:, k, c:c + 1], acc[:], ALU.mult, ALU.add,
                                      r=[GT[c], "cw", ak], w=[ak])
                            if c < 8:
                                S.act(sT[:, c, :], acc[:], AF.Silu, r=[ak], w=[f"sT{c}"])
                            else:
                                S.act(vT[:, c - 8, :], acc[:], AF.Silu, r=[ak], w=[f"vT_{PAR}"])
                        for j in range(ST // 128):
                            pf, pfk = S.psf()
                            for kc in range(8):
                                S.mm(pf[:, :], xTs[:, kc, j * 128:(j + 1) * 128], w_in_bf[:, kc, 2304:2816], kc == 0, kc == 7,
                                     r=[WIN[kc], xTk], w=[pfk])
                            S.act(zs2[j][:], pf[:, :], AF.Silu, r=[pfk], w=[f"zs2_{PAR}_{j}"])
                            S.tt("pool", zs2[j][:].rearrange("p (h d) -> p h d", h=4), zs2[j][:].rearrange("p (h d) -> p h d", h=4),
                                 gnw[:].unsqueeze(1).to_broadcast([128, 4, 128]), ALU.mult, r=[f"zs2_{PAR}_{j}", "gnw"], w=[f"zs2_{PAR}_{j}"])
                        if st == SEQ // ST - 1:
                            for t in range(3):
                                S.dma("sp", nc_p[t].rearrange("(c p) -> p c", p=128), gT[:, :, ST + t], r=GT, w=[],
                                      slot="nc_p")
                        else:
                            S.cp("pool", gT[:, :, 0:3], gT[:, :, ST:ST + 3], r=GT, w=GT)
                        for half in range(2):
                            for hh in range(4):
                                c = half * 4 + hh
                                S.act(sq4[:, hh, :], sT[:, c, :], AF.Square, r=[f"sT{c}"], w=[f"sq{hh}"])
                            pfs = []
                            for hh in range(4):
                                pf, pfk = S.psf()
                                S.mm(pf[:, 0:ST], ones_bf[:], sq4[:, hh, :], True, True, r=["ones_bf", f"sq{hh}"], w=[pfk])
                                pfs.append((pf, pfk))
                            for hh in range(4):
                                pf, pfk = pfs[hh]
                                S.act(ln4[:, hh, :], pf[:, 0:ST], AF.Ln, r=[pfk], w=[f"ln{hh}"], bias=L2_EPS)
                            for hh in range(4):
                                bq = -0.5 * math.log(128.0) if half == 0 else 0.0
                                S.act(ln4[:, hh, :], ln4[:, hh, :], AF.Exp, r=[f"ln{hh}"], w=[f"ln{hh}"], bias=bq, scale=-0.5)
                            for hh in range(4):
                                c = half * 4 + hh
                                S.tt("pool" if hh % 2 else "dve", qkT[:, c, :], sT[:, c, :], ln4[:, hh, :], ALU.mult,
                                     r=[f"sT{c}", f"ln{hh}"], w=[f"qkT{c}_{PAR}"])
                        if st == 0:
                            stop("st0a")

                    def tile(st, j):
                        qkT, vT, zs2 = qkT2[st % 2], vT2[st % 2], zs4[(st % 2) * 2:(st % 2) * 2 + 2]
                        PAR = st % 2
                        xTs = xT[st % 2]
                        xTk = f"xT{st % 2}"
                        i = st * (ST // 128) + j
                        tok = slice(j * 128, (j + 1) * 128)
                        pb, pbk = S.psb()
                        for h in range(4):
                            S.tr(pb[:, h * 128:(h + 1) * 128], qkT[:, 4 + h, tok], idb[:], r=[f"qkT{4 + h}_{PAR}", "idb"],
                                 w=[pbk], inc=False)
                        for h in range(4):
                            S.tr(pb[:, (4 + h) * 128:(5 + h) * 128], vT[:, h, tok], idb[:], r=[f"vT_{PAR}", "idb"], w=[pbk],
                                 inc=(h == 3))
                        S.cp("act", knv[:], pb[:].rearrange("p (k t) -> p k t", k=8), r=[pbk], w=["knv"])
                        if i == 0:
                            stop("t0a")
                        for (c0, c1, dst, dk_) in ((0, 512, tm_qkv[:, 0:512], "tm_q"), (512, 768, tm_qkv[:, 512:768], "tm_kv"),
                                                   (2816, 2824, tm_ab[:], "tm_ab")):
                            pf, pfk = S.psf()
                            for kc in range(8):
                                S.mm(pf[:, 0:c1 - c0], xTs[:, kc, tok], w_in_bf[:, kc, c0:c1], kc == 0, kc == 7,
                                     r=[WIN[kc], xTk], w=[pfk])
                            S.cp("act" if dk_ in ("tm_q", "tm_z") else "dve", dst, pf[:, 0:c1 - c0], r=[pfk], w=[dk_])
                        if i == 0:
                            stop("t0b")
                        qk3 = tm_qkv[:, 0:640].rearrange("p (h d) -> p h d", d=64)
                        cb = cosT[:, i, :].unsqueeze(1).to_broadcast([128, 10, 8])
                        sbb = sinT[:, i, :].unsqueeze(1).to_broadcast([128, 10, 8])
                        RK = ["tm_q", "tm_kv", "cosT", "sinT"]
                        S.tt("dve", rtmp[:, 0], qk3[:, :, 0:8], cb, ALU.mult, r=RK, w=["rt0"])
                        S.tt("dve", rtmp[:, 1], qk3[:, :, 8:16], sbb, ALU.mult, r=RK, w=["rt1"])
                        S.tt("dve", rtmp[:, 2], qk3[:, :, 8:16], cb, ALU.mult, r=RK, w=["rt2"])
                        S.tt("dve", rtmp[:, 3], qk3[:, :, 0:8], sbb, ALU.mult, r=RK, w=["rt3"])
                        S.tt("dve", rot[:, :, 0:8], rtmp[:, 0], rtmp[:, 1], ALU.subtract, r=["rt0", "rt1"], w=["rot"])
                        S.tt("dve", rot[:, :, 8:16], rtmp[:, 2], rtmp[:, 3], ALU.add, r=["rt2", "rt3"], w=["rot"])
                        q4 = tm_qkv[:, 0:512].rearrange("p (j c d) -> p c j d", j=2, c=4)
                        S.cp("dve", q_bf[:, :, :, 0:16], rot[:, 0:8, :].rearrange("p (j c) d -> p c j d", j=2),
                             r=["rot"], w=["q_bf"])
                        S.cp("act", q_bf[:, :, :, 16:64], q4[:, :, :, 16:64], r=["tm_q"], w=["q_bf"])
                        k3 = tm_qkv[:, 512:640].rearrange("p (j d) -> p j d", j=2)
                        S.cp("dve", k_bf[:, :, 0:16], rot[:, 8:10, :], r=["rot"], w=["k_bf"])
                        S.cp("dve", k_bf[:, :, 16:64], k3[:, :, 16:64], r=["tm_kv"], w=["k_bf"])
                        va = v_aug[i % 2]
                        vak = f"v_aug{i % 2}"
                        S.cp("pool", va[:, :, 0:64], tm_qkv[:, 640:768].rearrange("p (j d) -> p j d", j=2), r=["tm_kv"],
                             w=[vak])
                        if i == 0:
                            stop("t0c")
                        if i == NT - 1:
                            S.cp("dve", k_rot[:, :, 0:16], rot[:, 8:10, :], r=["rot"], w=["k_rot"])
                            S.cp("dve", k_rot[:, :, 16:64], k3[:, :, 16:64], r=["tm_kv"], w=["k_rot"])
                            S.dma("sp", nk_p, k_rot[:].rearrange("p j d -> p (j d)"), r=["k_rot"], slot="nk_p")
                            S.dma("sp", nv_p, tm_qkv[:, 640:768], r=["tm_kv"], slot="nv_p")
                        pb, pbk = S.psb()
                        for c in range(4):
                            S.tr(pb[:, c * 128:(c + 1) * 128], q_bf[:, c].rearrange("p j d -> p (j d)"), idb[:],
                                 r=["q_bf", "idb"], w=[pbk], inc=False)
                        S.tr(pb[:, 512:640], k_bf[:].rearrange("p j d -> p (j d)"), idb[:], r=["k_bf", "idb"], w=[pbk])
                        kTc = kTb[i % 2]
                        kTk = f"kTb{i % 2}"
                        S.cp("act", qT[:], pb[:, 0:512].rearrange("p (c t) -> p c t", c=4), r=[pbk], w=["qT"])
                        S.cp("dve", kTc[:], pb[:, 512:640], r=[pbk], w=[kTk])
                        if i == 0:
                            stop("t0d")
                        for jk in range(2):
                            blocks = ([] if i == 0 else [(kTb[(i - 1) % 2], f"kTb{(i - 1) % 2}", mb_prev, "mb_prev",
                                                          v_aug[(i - 1) % 2], f"v_aug{(i - 1) % 2}")])
                            blocks.append((kTc, kTk, mb_cur, "mb_cur", va, vak))
                            for bi, (kt, ktk, mk, mkk, _, _) in enumerate(blocks):
                                pf, pfk = S.psf()
                                S.mm(pf[:], kt[jk * 64:(jk + 1) * 64, :],
                                     qT[jk * 64:(jk + 1) * 64, :, :].rearrange("p c t -> p (c t)"), True, True,
                                     r=[ktk, "qT"], w=[pfk])
                                S.act(PTe[bi][:].rearrange("p c t -> p (c t)"), pf[:], AF.Exp, r=[pfk], w=[f"PTe{bi}"],
                                      scale=0.125)
                                S.tt("dve", PTe[bi][:], PTe[bi][:], mk[:].unsqueeze(1).to_broadcast([128, 4, 128]),
                                     ALU.mult, r=[f"PTe{bi}", mkk], w=[f"PTe{bi}"])
                                if i == 0:
                                    stop("t0e")
                            po, pok = S.psf()
                            po3 = po[:, 0:260].rearrange("p (c d) -> p c d", c=4)
                            for c in range(4):
                                for bi, (_, _, _, _, vv, vvk) in enumerate(blocks):
                                    S.mm(po3[:, c, :], PTe[bi][:, c, :], vv[:, jk, :], bi == 0, bi == len(blocks) - 1,
                                         r=[f"PTe{bi}", vvk], w=[pok], inc=(c == 3 and bi == len(blocks) - 1))
                            S.tt("dve", den[:, jk, :], po3[:, :, 64], esink[:, jk * 4:(jk + 1) * 4], ALU.add,
                                 r=[pok, "esink"], w=["den"])
                            S.op("dve", lambda E, jk=jk: E.reciprocal(out=den[:, jk, :], in_=den[:, jk, :]), r=["den"],
                                 w=["den"])
                            S.tt("dve", mix_all[:, i, jk * 256:(jk + 1) * 256].rearrange("p (c d) -> p c d", c=4),
                                 po3[:, :, 0:64], den[:, jk, :].unsqueeze(2).to_broadcast([128, 4, 64]), ALU.mult,
                                 r=[pok, "den"], w=[f"mix{i}"])
                        if i == 0:
                            stop("t0attn")
                        if i == 1:
                            stop("t1attn")
                        g_ = gab[:, 0, :]
                        beta_ = gab[:, 1, :]
                        gc_ = gab[:, 2, :]
                        egc_ = gab[:, 3, :]
                        kds_ = gab[:, 4, :]
                        atot_ = gab[:, 5, :]
                        tmp_ = gab[:, 6, :]
                        nbeta_ = gab[:, 7, :]
                        S.tt("dve", tmp_, tm_ab[:, 0:4], dtb[:], ALU.add, r=["tm_ab", "dtb"], w=["g_tmp"])
                        S.act(tmp_, tmp_, AF.Exp, r=["g_tmp"], w=["g_tmp"])
                        S.act(tmp_, tmp_, AF.Ln, r=["g_tmp"], w=["g_tmp"], bias=1.0)
                        S.tt("dve", g_, tmp_, nexpA[:], ALU.mult, r=["g_tmp", "nexpA"], w=["g_g"])
                        S.act(beta_, tm_ab[:, 4:8], AF.Exp, r=["tm_ab"], w=["g_beta"], scale=-1.0)
                        S.ts("dve", beta_, beta_, 1.0, None, ALU.add, None, r=["g_beta"], w=["g_beta"])
                        S.op("dve", lambda E: E.reciprocal(out=beta_, in_=beta_), r=["g_beta"], w=["g_beta"])
                        S.ts("dve", nbeta_, beta_, -1.0, None, ALU.mult, None, r=["g_beta"], w=["g_nbeta"])
                        pf, pfk = S.psf()
                        S.mm(pf[:, 0:4], m_up[:], g_, True, True, r=["m_up", "g_g"], w=[pfk])
                        S.mm(pf[:, 4:8], onesf[:], g_, True, True, r=["onesf", "g_g"], w=[pfk])
                        S.cp("dve", gc_, pf[:, 0:4], r=[pfk], w=["g_gc"])
                        S.act(egc_, pf[:, 0:4], AF.Exp, r=[pfk], w=["g_egc"])
                        S.act(atot_, pf[:, 4:8], AF.Exp, r=[pfk], w=["g_atot"])
                        S.tt("dve", kds_, pf[:, 4:8], gc_, ALU.subtract, r=[pfk, "g_gc"], w=["g_kds"])
                        S.act(kds_, kds_, AF.Exp, r=["g_kds"], w=["g_kds"])
                        pD, pDk = S.psf()
                        pDT, pDTk = S.psf()
                        pG, pGk = S.psf()
                        pA, pAk = S.psf()
                        pD3 = pD[:].rearrange("p (h t) -> p h t", h=4)
                        pDT3 = pDT[:].rearrange("p (h t) -> p h t", h=4)
                        pG3 = pG[:].rearrange("p (h t) -> p h t", h=4)
                        pA3 = pA[:].rearrange("p (h t) -> p h t", h=4)
                        for h in range(4):
                            if h % 2:
                                S.act(gU[:, h, :], m_up[:], AF.Copy, r=["m_up", "g_g"], w=[f"gU{h}"], scale=gab[:, 0, h:h + 1])
                            else:
                                S.ts("dve", gU[:, h, :], m_up[:], gab[:, 0, h:h + 1], None, ALU.mult, None,
                                     r=["m_up", "g_g"], w=[f"gU{h}"])
                        for h in range(4):
                            S.mm(pD3[:, h, :], gU[:, h, :], m_strict[:], True, True, r=[f"gU{h}", "m_strict"], w=[pDk],
                                 inc=(h == 3))
                        for h in range(4):
                            S.mm(pDT3[:, h, :], m_strict[:], gU[:, h, :], True, True, r=[f"gU{h}", "m_strict"], w=[pDTk],
                                 inc=(h == 3))
                        for h in range(4):
                            S.mm(pG3[:, h, :], qkT[:, 4 + h, tok], qkT[:, 4 + h, tok], True, True, r=[f"qkT{4 + h}_{PAR}"],
                                 w=[pGk], inc=(h == 3))
                        for h in range(4):
                            S.mm(pA3[:, h, :], qkT[:, 4 + h, tok], qkT[:, h, tok], True, True,
                                 r=[f"qkT{4 + h}_{PAR}", f"qkT{h}_{PAR}"], w=[pAk], inc=(h == 3))
                        S.act(eD[:], pD3, AF.Exp, r=[pDk], w=["eD"])
                        S.tt("dve", eD[:], eD[:], m_strict[:].unsqueeze(1).to_broadcast([128, 4, 128]), ALU.mult,
                             r=["eD", "m_strict"], w=["eD"])
                        S.act(eDT[:], pDT3, AF.Exp, r=[pDTk], w=["eDT"])
                        S.tt("pool", eDT[:], eDT[:], m_up[:].unsqueeze(1).to_broadcast([128, 4, 128]), ALU.mult,
                             r=["eDT", "m_up"], w=["eDT"])
                        P0 = Pm[0]
                        for h in range(4):
                            S.stt("dve", P0[:, h, :], pG3[:, h, :], gab[:, 7, h:h + 1], eD[:, h, :], ALU.mult, ALU.mult,
                                  r=[pGk, "g_nbeta", "eD"], w=[f"Pm0g{h // 2}"])
                        S.tt("dve", ATb[:], pA3, eDT[:], ALU.mult, r=[pAk, "eDT"], w=["ATb"])
                        pt, ptk = S.psf()
                        pt3 = pt[:].rearrange("p (h t) -> p h t", h=4)
                        for h in range(4):
                            S.tr(pt3[:, h, :], P0[:, h, :], idf[:], r=[f"Pm0g{h // 2}", "idf"], w=[ptk], inc=(h == 3))
                        S.cp("act", PmT[0][:], pt3, r=[ptk], w=["PmT0g0", "PmT0g1"])
                        S.tt("dve", Qm[0][:], pt3, idf[:].unsqueeze(1).to_broadcast([128, 4, 128]), ALU.add,
                             r=[ptk, "idf"], w=["Qm0g0", "Qm0g1"])
                        NIT = 6
                        GR = ((0, 2), (2, 4))
                        for k in range(NIT):
                            a, b = k % 2, (k + 1) % 2
                            last = (k == NIT - 1)
                            pPs, pPTs = [], []
                            for gi, (h0, h1) in enumerate(GR):
                                pP, pPk = S.psf()
                                pP3 = pP[:, 0:256].rearrange("p (h t) -> p h t", h=2)
                                for h in range(h0, h1):
                                    S.mm(pP3[:, h - h0, :], PmT[a][:, h, :], Pm[a][:, h, :], True, True,
                                         r=[f"PmT{a}g{gi}", f"Pm{a}g{gi}"], w=[pPk], inc=(h == h1 - 1))
                                pPs.append((pP3, pPk))
                                if not last:
                                    pPT, pPTk = S.psf()
                                    pPT3 = pPT[:, 0:256].rearrange("p (h t) -> p h t", h=2)
                                    for h in range(h0, h1):
                                        S.mm(pPT3[:, h - h0, :], Pm[a][:, h, :], PmT[a][:, h, :], True, True,
                                             r=[f"PmT{a}g{gi}", f"Pm{a}g{gi}"], w=[pPTk], inc=(h == h1 - 1))
                                    pPTs.append((pPT3, pPTk))
                            for gi, (h0, h1) in enumerate(GR):
                                S.cp("act", Pm[b][:, h0:h1, :], pPs[gi][0], r=[pPs[gi][1]], w=[f"Pm{b}g{gi}"])
                                if not last:
                                    S.cp("dve", PmT[b][:, h0:h1, :], pPTs[gi][0], r=[pPTs[gi][1]], w=[f"PmT{b}g{gi}"])
                            pQs = []
                            for gi, (h0, h1) in enumerate(GR):
                                pQ, pQk = S.psf()
                                pQ3 = pQ[:, 0:256].rearrange("p (h t) -> p h t", h=2)
                                for h in range(h0, h1):
                                    S.mm(pQ3[:, h - h0, :], Pm[b][:, h, :], Qm[a][:, h, :], True, True,
                                         r=[f"Pm{b}g{gi}", f"Qm{a}g{gi}"], w=[pQk], inc=(h == h1 - 1))
                                pQs.append((pQ3, pQk))
                            for gi, (h0, h1) in enumerate(GR):
                                if not last:
                                    S.tt("dve", Qm[b][:, h0:h1, :], pQs[gi][0], Qm[a][:, h0:h1, :], ALU.add,
                                         r=[pQs[gi][1], f"Qm{a}g{gi}"], w=[f"Qm{b}g{gi}"])
                                else:
                                    S.tt("dve", Qb[:, h0:h1, :], pQs[gi][0], Qm[a][:, h0:h1, :], ALU.add,
                                         r=[pQs[gi][1], f"Qm{a}g{gi}"], w=["Qb"])
                        kn3 = knv[:, 0:4, :]
                        v3 = knv[:, 4:8, :]
                        S.tt("dve", tmp_, beta_, egc_, ALU.mult, r=["g_beta", "g_egc", "g_tmp"], w=["g_tmp"])
                        S.tt("pool", kbe[:], kn3, tmp_.unsqueeze(2).to_broadcast([128, 4, 128]), ALU.mult,
                             r=["knv", "g_tmp"], w=["kbe"])
                        S.tt("pool", kdc[:], kn3, kds_.unsqueeze(2).to_broadcast([128, 4, 128]), ALU.mult,
                             r=["knv", "g_kds"], w=["kdc"])
                        S.tt("dve", vb[:], v3, beta_.unsqueeze(2).to_broadcast([128, 4, 128]), ALU.mult,
                             r=["knv", "g_beta"], w=["vb"])
                        pw, pwk = S.psf()
                        pw3 = pw[:].rearrange("p (h t) -> p h t", h=4)
                        for h in range(4):
                            S.mm(pw3[:, h, :], kbe[:, h, :], Qb[:, h, :], True, True, r=["kbe", "Qb"], w=[pwk],
                                 inc=(h == 3))
                        S.op("act", lambda E: E.mul(out=nwT[:], in_=pw3, mul=-1.0), r=[pwk], w=["nwT"])
                        pv, pvk = S.psf()
                        pv3 = pv[:].rearrange("p (h t) -> p h t", h=4)
                        for h in range(4):
                            S.mm(pv3[:, h, :], Qb[:, h, :], vb[:, h, :], True, False, r=["Qb", "vb"], w=[pvk], inc=False)
                            S.mm(pv3[:, h, :], nwT[:, h, :], Sb[:, h, :], False, True, r=["nwT", "Sb"], w=[pvk],
                                 inc=(h == 3))
                        S.cp("act", vnew[:], pv3, r=[pvk], w=["vnew"])
                        po1, po1k = S.psf()
                        po13 = po1[:].rearrange("p (h t) -> p h t", h=4)
                        for h in range(4):
                            S.mm(po13[:, h, :], qkT[:, h, tok], Sb[:, h, :], True, True, r=[f"qkT{h}_{PAR}", "Sb"], w=[po1k],
                                 inc=(h == 3))
                        po2, po2k = S.psf()
                        po23 = po2[:].rearrange("p (h t) -> p h t", h=4)
                        for h in range(4):
                            S.mm(po23[:, h, :], ATb[:, h, :], vnew[:, h, :], True, True, r=["ATb", "vnew"], w=[po2k],
                                 inc=(h == 3))
                        pS, pSk = S.psf()
                        pS3 = pS[:].rearrange("p (h t) -> p h t", h=4)
                        for h in range(4):
                            S.mm(pS3[:, h, :], kdc[:, h, :], vnew[:, h, :], True, True, r=["kdc", "vnew"], w=[pSk],
                                 inc=(h == 3))
                        S.tt("dve", o1s[:], po13, egc_.unsqueeze(2).to_broadcast([128, 4, 128]), ALU.mult,
                             r=[po1k, "g_egc"], w=["o1s"])
                        S.tt("dve", og[:], po23, o1s[:], ALU.add, r=[po2k, "o1s"], w=["og"])
                        S.tt("pool", Stmp[:], Sf[:], atot_.unsqueeze(2).to_broadcast([128, 4, 128]), ALU.mult,
                             r=["Sf", "g_atot"], w=["Stmp"])
                        S.tt("dve", Sf[:], pS3, Stmp[:], ALU.add, r=[pSk, "Stmp"], w=["Sf"])
                        S.cp("act", Sb[:], Sf[:], r=["Sf"], w=["Sb"])
                        S.tt("dve", osq[:], og[:], og[:], ALU.mult, r=["og"], w=["osq"])
                        S.op("dve", lambda E: E.tensor_reduce(out=ssq[:], in_=osq[:], axis=AX.X, op=ALU.add), r=["osq"],
                             w=["ssq"])
                        S.act(ssq[:], ssq[:], AF.Ln, r=["ssq"], w=["ssq"], bias=NORM_EPS, scale=1.0 / 128.0)
                        S.act(ssq[:], ssq[:], AF.Exp, r=["ssq"], w=["ssq"], scale=-0.5)
                        S.tt("dve", og[:], og[:], ssq[:].unsqueeze(2).to_broadcast([128, 4, 128]), ALU.mult,
                             r=["og", "ssq"], w=["og"])
                        S.tt("dve", mix_all[:, i, 512:1024].rearrange("p (h d) -> p h d", h=4), og[:],
                             zs2[j][:].rearrange("p (h d) -> p h d", h=4), ALU.mult, r=["og", f"zs2_{PAR}_{j}"], w=[f"mix{i}"])
                        if i == 0:
                            stop("t0")
                        if i == 1:
                            stop("t1")
                        if i == 3:
                            stop("t3")

                    NST = SEQ // ST
                    fm(0)
                    for st in range(NST):
                        tile(st, 0)
                        if st + 1 < NST:
                            fm(st + 1)
                        tile(st, 1)
                    S.dma("sp", ng_p.rearrange("h k v -> k h v"), Sf[:], r=["Sf"], slot="ng_p")
                    if dbg:
                        dump("mix", mix_all[:, 0:NT, :], [128, NT, D], [f"mix{i}" for i in range(NT)])
                    S.barrier()


            y_acc = sb(es0, "y_acc", [128, NT + 1, D])
            with ExitStack() as es:
                w_out_bf = sb(es, "w_out_bf", [128, 8, D], BF16)
                g1 = sb(es, "g1", [128, D])
                b1 = sb(es, "b1", [128, D])
                mixT = [sb(es, f"mixT{i}", [128, 8, 128], BF16) for i in range(2)]
                xf = [sb(es, f"xf{i}", [128, D]) for i in range(2)]
                tb = [sb(es, f"tb{i}", [128, D]) for i in range(3)]
                stats = [sb(es, f"stats{i}", [128, 2, 6]) for i in range(3)]
                mv = [sb(es, f"mv{i}", [128, 2]) for i in range(3)]
                S.dma("pool", w_out_bf[:], w_out.rearrange("(c p) n -> p c n", p=128), w=["w_out"])
                S.dma("sp", g1[:], ln1g_d.partition_broadcast(128), w=["g1"])
                S.dma("sp", b1[:], ln1b_d.partition_broadcast(128), w=["b1"])
                def ln1_A(i):
                    n = 128 if i < NT else NS * TS
                    xsrc = x_p[i * 128:(i + 1) * 128, :] if i < NT else x_s
                    xft, xfk = xf[i % 2], f"xf{i % 2}"
                    S.dma("sp", xft[:n, :], xsrc, w=[xfk])
                    mt, mtk = mixT[i % 2], f"mixT{i % 2}"
                    pb, pbk = S.psb()
                    for c in range(8):
                        S.tr(pb[:, c * 128:c * 128 + n], mix_all[:n, i, c * 128:(c + 1) * 128], idb[:n, :n],
                             r=["idb"], w=[pbk], inc=(c == 7))
                    S.cp("act", mt[:, :, 0:n], pb[:].rearrange("p (c t) -> p c t", c=8)[:, :, 0:n], r=[pbk], w=[mtk])
                    tbt, tbk = tb[i % 3], f"tb{i % 3}"
                    st_, mv_, mvk = stats[i % 3], mv[i % 3], f"mv{i % 3}"
                    for half in range(2):
                        pf, pfk = S.psf()
                        for c in range(8):
                            S.mm(pf[:n, :], mt[:, c, 0:n], w_out_bf[:, c, half * 512:(half + 1) * 512], c == 0, c == 7,
                                 r=[mtk, "w_out"], w=[pfk])
                        S.stt("dve", tbt[:n, half * 512:(half + 1) * 512], xft[:n, half * 512:(half + 1) * 512], ALPHA,
                              pf[:n, :], ALU.mult, ALU.add, r=[xfk, pfk], w=[tbk])
                        S.op("dve", lambda E, half=half: E.bn_stats(out=st_[:n, half, :],
                                                                    in_=tbt[:n, half * 512:(half + 1) * 512]),
                             r=[tbk], w=[mvk])
                    S.op("dve", lambda E: E.bn_aggr(out=mv_[:n, :], in_=st_[:n].rearrange("p a b -> p (a b)")),
                         r=[mvk], w=[mvk])
                    S.act(mv_[:n, 1:2], mv_[:n, 1:2], AF.Ln, r=[mvk], w=[mvk], bias=NORM_EPS)
                    S.act(mv_[:n, 1:2], mv_[:n, 1:2], AF.Exp, r=[mvk], w=[mvk], scale=-0.5)

                def ln1_B(i):
                    n = 128 if i < NT else NS * TS
                    tbt, tbk = tb[i % 3], f"tb{i % 3}"
                    mv_, mvk = mv[i % 3], f"mv{i % 3}"
                    S.stt("dve", mv_[:n, 0:1], mv_[:n, 0:1], -1.0, mv_[:n, 1:2], ALU.mult, ALU.mult, r=[mvk], w=[mvk])
                    S.act(tbt[:n, :], tbt[:n, :], AF.Identity, r=[tbk, mvk], w=[tbk], bias=mv_[:n, 0:1], scale=mv_[:n, 1:2])
                    S.tt("dve", tbt[:n, :], tbt[:n, :], g1[:n, :], ALU.mult, r=[tbk, "g1"], w=[tbk])
                    S.tt("pool", y_acc[:n, i, :], tbt[:n, :], b1[:n, :], ALU.add, r=[tbk, "b1"], w=[f"y{i}"])

                for i in range(NT + 1):
                    ln1_A(i)
                    if i >= 1:
                        ln1_B(i - 1)
                ln1_B(NT)
                if dbg:
                    dump("x1", y_acc[:], [128, NT + 1, D], [f"y{i}" for i in range(NT + 1)])
                S.barrier()

            with ExitStack() as es:
                x1T = mix_all[:].rearrange("p a b -> p (a b)")[:, 0:8 * NTOK].rearrange("p (k t) -> p k t", k=8)
                comb = sb(es, "comb", [128, NT + 1, NE])
                wr_sb = sb(es, "wr_sb", [128, 8, 36])
                x1Tf = sb(es, "x1Tf", [128, 8, 128])
                T_ = NT + 1
                rl_all = sb(es, "rl_all", [128, T_, 36])
                r_oh = sb(es, "r_oh", [128, T_, 4])
                r_t4 = sb(es, "r_t4", [128, T_, 4])
                r_pr = sb(es, "r_pr", [128, T_, 4, 8])
                r_es = sb(es, "r_es", [128, T_, 8])
                r_m1 = sb(es, "r_m1", [128, T_, 8])
                r_e2 = sb(es, "r_e2", [128, T_, 8])
                r_m2 = sb(es, "r_m2", [128, T_, 8])
                r_ew = sb(es, "r_ew", [128, T_, 8])
                r_s = sb(es, "r_s", [128, 8, T_])
                S.op("pool", lambda E: E.memset(rl_all[:], 0.0), w=["rl_all"])
                S.dma("sp", wr_sb[:], wr_d.rearrange("(c p) n -> p c n", p=128), w=["wr"])
                YK = [f"y{i}" for i in range(NT + 1)]
                for i in range(NT + 1):
                    n = 128 if i < NT else NS * TS
                    t0 = i * 128
                    pfa, pfak = S.psf()
                    pfb, pfbk = S.psf()
                    for kc in range(8):
                        pf, pfk = (pfa, pfak) if kc < 4 else (pfb, pfbk)
                        S.tr(pf[:, (kc % 4) * 128:(kc % 4) * 128 + n], y_acc[:n, i, kc * 128:(kc + 1) * 128], idf[:n, :n],
                             r=[YK[i], "idf"], w=[pfk], inc=(kc % 4 == 3))
                    for hf, (pf, pfk) in enumerate(((pfa, pfak), (pfb, pfbk))):
                        src = pf[:].rearrange("p (k t) -> p k t", k=4)[:, :, 0:n]
                        S.cp("act", x1Tf[:, hf * 4:(hf + 1) * 4, 0:n], src, r=[pfk], w=["x1Tf"])
                        S.cp("dve", x1T[:, hf * 4:(hf + 1) * 4, t0:t0 + n], src, r=[pfk], w=["x1T"])
                    pr, prk = S.psf()
                    for kc in range(8):
                        S.mm(pr[:n, 0:36], x1Tf[:, kc, 0:n], wr_sb[:, kc, :], kc == 0, kc == 7, r=["x1Tf", "wr"], w=[prk])
                    S.cp("dve", rl_all[:n, i, :], pr[:n, 0:36], r=[prk], w=["rl_all"])
                    S.op("act", lambda E, n=n, i=i: E.mul(out=y_acc[:n, i, :], in_=y_acc[:n, i, :], mul=ALPHA),
                         r=[YK[i], "x1T", "x1Tf"], w=[YK[i]])
                R = ["rl_all", "rr"]
                gl = rl_all[:, :, 0:4]
                el = rl_all[:, :, 4:36].rearrange("p t (g e) -> p t g e", g=4)
                bc3 = lambda a, k: a.unsqueeze(2).to_broadcast([128, T_, k])
                gmax, gtp, m1, m2, ex, w1, w2 = (r_s[:, j, :] for j in range(7))
                S.op("dve", lambda E: E.tensor_reduce(out=gmax, in_=gl, axis=AX.X, op=ALU.max), r=R, w=R)
                S.tt("dve", r_oh[:], gl, bc3(gmax, 4), ALU.is_equal, r=R, w=R)
                S.tt("dve", r_t4[:], gl, bc3(gmax, 4), ALU.subtract, r=R, w=R)
                S.act(r_t4[:], r_t4[:], AF.Exp, r=R, w=R)
                S.op("dve", lambda E: E.tensor_reduce(out=gtp, in_=r_t4[:], axis=AX.X, op=ALU.add), r=R, w=R)
                S.op("dve", lambda E: E.reciprocal(out=gtp, in_=gtp), r=R, w=R)
                S.tt("dve", r_pr[:], el, r_oh[:].unsqueeze(3).to_broadcast([128, T_, 4, 8]), ALU.mult, r=R, w=R)
                S.op("dve", lambda E: E.tensor_reduce(out=r_es[:], in_=r_pr[:].rearrange("p t g e -> p t e g"), axis=AX.X,
                                                      op=ALU.add), r=R, w=R)
                S.op("dve", lambda E: E.tensor_reduce(out=m1, in_=r_es[:], axis=AX.X, op=ALU.max), r=R, w=R)
                S.tt("dve", r_m1[:], r_es[:], bc3(m1, 8), ALU.is_equal, r=R, w=R)
                S.stt("dve", r_e2[:], r_m1[:], -1e30, r_es[:], ALU.mult, ALU.add, r=R, w=R)
                S.op("dve", lambda E: E.tensor_reduce(out=m2, in_=r_e2[:], axis=AX.X, op=ALU.max), r=R, w=R)
                S.tt("dve", r_m2[:], r_e2[:], bc3(m2, 8), ALU.is_equal, r=R, w=R)
                S.tt("dve", ex, m2, m1, ALU.subtract, r=R, w=R)
                S.act(ex, ex, AF.Exp, r=R, w=R)
                S.ts("dve", w1, ex, 1.0, None, ALU.add, None, r=R, w=R)
                S.op("dve", lambda E: E.reciprocal(out=w1, in_=w1), r=R, w=R)
                S.tt("dve", w2, ex, w1, ALU.mult, r=R, w=R)
                S.tt("dve", w1, w1, gtp, ALU.mult, r=R, w=R)
                S.tt("dve", w2, w2, gtp, ALU.mult, r=R, w=R)
                S.tt("dve", r_ew[:], r_m1[:], bc3(w1, 8), ALU.mult, r=R, w=R)
                S.tt("dve", r_m2[:], r_m2[:], bc3(w2, 8), ALU.mult, r=R, w=R)
                S.tt("dve", r_ew[:], r_ew[:], r_m2[:], ALU.add, r=R, w=R)
                S.tt("dve", comb[:].rearrange("p t (g e) -> p t g e", g=4), r_oh[:].unsqueeze(3).to_broadcast([128, T_, 4, 8]),
                     r_ew[:].unsqueeze(2).to_broadcast([128, T_, 4, 8]), ALU.mult, r=R, w=["comb"])
                if dbg:
                    dump("comb", comb[:], [128, NT + 1, NE], ["comb"])

                wg = [sb(es, f"wg{i}", [128, 8, 256], BF16) for i in range(2)]
                wu = [sb(es, f"wu{i}", [128, 8, 256], BF16) for i in range(2)]
                wd = [sb(es, f"wd{i}", [128, 2, D], BF16) for i in range(2)]
                hT = [sb(es, f"hT{i}", [128, 2, NTOK], BF16) for i in range(2)]
                sgt = [sb(es, f"sgt{i}", [128, 512]) for i in range(2)]
                spans = [(t, min(512, NTOK - t)) for t in range(0, NTOK, 512)]
                for pbt in S.psb_list:
                    S.psf_list.append(pbt[:].bitcast(F32))
                acct = [sb(es, f"acct{i}", [128, 512]) for i in range(4)]
                g2 = sb(es, "g2", [128, D])
                b2 = sb(es, "b2", [128, D])
                ob = [sb(es, f"ob{i}", [128, D]) for i in range(3)]
                stats2 = [sb(es, f"stats2_{i}", [128, 2, 6]) for i in range(3)]
                mv2 = [sb(es, f"mv2_{i}", [128, 2]) for i in range(3)]
                S.dma("sp", g2[:], ln2g_d.partition_broadcast(128), w=["g2"])
                S.dma("sp", b2[:], ln2b_d.partition_broadcast(128), w=["b2"])
                acc_i = 0

                def ln2_A(i):
                    n = 128 if i < NT else NS * TS
                    st2, mvt, mk_ = stats2[i % 3], mv2[i % 3], f"mv2_{i % 3}"
                    for half in range(2):
                        S.op("dve", lambda E, half=half: E.bn_stats(out=st2[:n, half, :],
                                                                    in_=y_acc[:n, i, half * 512:(half + 1) * 512]),
                             r=[YK[i]], w=[mk_])
                    S.op("dve", lambda E: E.bn_aggr(out=mvt[:n, :], in_=st2[:n].rearrange("p a b -> p (a b)")),
                         r=[mk_], w=[mk_])
                    S.act(mvt[:n, 1:2], mvt[:n, 1:2], AF.Ln, r=[mk_], w=[mk_], bias=NORM_EPS)
                    S.act(mvt[:n, 1:2], mvt[:n, 1:2], AF.Exp, r=[mk_], w=[mk_], scale=-0.5)

                def ln2_B(i):
                    n = 128 if i < NT else NS * TS
                    mvt, mk_ = mv2[i % 3], f"mv2_{i % 3}"
                    obt, obk = ob[i % 3], f"ob{i % 3}"
                    S.stt("dve", mvt[:n, 0:1], mvt[:n, 0:1], -1.0, mvt[:n, 1:2], ALU.mult, ALU.mult, r=[mk_], w=[mk_])
                    S.act(obt[:n, :], y_acc[:n, i, :], AF.Identity, r=[YK[i], mk_], w=[obk], bias=mvt[:n, 0:1],
                          scale=mvt[:n, 1:2])
                    S.tt("dve", obt[:n, :], obt[:n, :], g2[:n, :], ALU.mult, r=[obk, "g2"], w=[obk])
                    S.tt("pool", obt[:n, :], obt[:n, :], b2[:n, :], ALU.add, r=[obk, "b2"], w=[obk])
                    dst = y_p[i * 128:(i + 1) * 128, :] if i < NT else y_s
                    S.dma("sp", dst, obt[:n, :], r=[obk], slot=obk + "o")

                def load_expert(e):
                    b = e % 2
                    S.dma("pool", wg[b][:], wg_d[e].rearrange("(c p) f -> p c f", p=128), w=[f"wg{b}"])
                    S.dma("pool", wu[b][:], wu_d[e].rearrange("(c p) f -> p c f", p=128), w=[f"wu{b}"])
                    S.dma("pool", wd[b][:], wd_d[e].rearrange("(c p) n -> p c n", p=128), w=[f"wd{b}"])

                load_expert(0)
                for e in range(NE):
                    b = e % 2
                    if e + 1 < NE:
                        load_expert(e + 1)
                    hk = f"hT{b}"
                    si = 0
                    for fc in range(2):
                        for (t0, tn) in spans:
                            pg, pgk = S.psf()
                            pu, puk = S.psf()
                            for kc in range(8):
                                S.mm(pg[:, 0:tn], wg[b][:, kc, fc * 128:(fc + 1) * 128], x1T[:, kc, t0:t0 + tn], kc == 0,
                                     kc == 7, r=[f"wg{b}", "x1T"], w=[pgk])
                            for kc in range(8):
                                S.mm(pu[:, 0:tn], wu[b][:, kc, fc * 128:(fc + 1) * 128], x1T[:, kc, t0:t0 + tn], kc == 0,
                                     kc == 7, r=[f"wu{b}", "x1T"], w=[puk])
                            sg_, sgk = sgt[si % 2], f"sgt{si % 2}"
                            si += 1
                            S.act(sg_[:, 0:tn], pg[:, 0:tn], AF.Silu, r=[pgk], w=[sgk])
                            S.tt("dve", hT[b][:, fc, t0:t0 + tn], pu[:, 0:tn], sg_[:, 0:tn], ALU.mult, r=[puk, sgk], w=[hk])
                    for i in range(NT + 1):
                        n = 128 if i < NT else NS * TS
                        t0 = i * 128
                        for half in range(2):
                            py, pyk = S.psf()
                            for fc in range(2):
                                S.mm(py[:n, :], hT[b][:, fc, t0:t0 + n], wd[b][:, fc, half * 512:(half + 1) * 512], fc == 0,
                                     fc == 1, r=[hk, f"wd{b}"], w=[pyk])
                            ysl = y_acc[:n, i, half * 512:(half + 1) * 512]
                            if (i * 2 + half) % 7 in (1, 3, 5, 6) and e < NE - 1:
                                at, atk = acct[acc_i % 4], f"acct{acc_i % 4}"
                                acc_i += 1
                                S.act(at[:n, :], py[:n, :], AF.Copy, r=[pyk, "comb"], w=[atk], scale=comb[:n, i, e:e + 1])
                                S.tt("pool", ysl, ysl, at[:n, :], ALU.add, r=[atk, YK[i]], w=[YK[i]])
                            else:
                                S.stt("dve", ysl, py[:n, :], comb[:n, i, e:e + 1], ysl, ALU.mult, ALU.add,
                                      r=[pyk, "comb", YK[i]], w=[YK[i]])
                        if e == NE - 1:
                            if i >= 1:
                                ln2_A(i - 1)
                            if i >= 2:
                                ln2_B(i - 2)
                ln2_A(NT)
                ln2_B(NT - 1)
                ln2_B(NT)
        except _Stop:
            pass
        S.final_wait()
    return nc, dbg_outs


_CACHE = {}


def _get_nc(dbg=False):
    if dbg not in _CACHE:
        _CACHE[dbg] = build(dbg)
    return _CACHE[dbg]


def make_in_maps(inputs):
    f = lambda a: np.ascontiguousarray(np.asarray(a, dtype=np.float32))
    g = {k: f(v) for k, v in inputs.items()}
    wr = np.ascontiguousarray(np.concatenate([g["w_router_group"][0], g["w_router_expert"][0]], axis=1))
    maps = []
    for c in range(NCORES):
        s0, s1 = c * NS, (c + 1) * NS
        maps.append({
            "x_p": g["x_prompt"][c],
            "x_s": np.ascontiguousarray(g["x_sample"][s0:s1].reshape(NS * TS, D)),
            "ck": np.ascontiguousarray(g["cache_attn_k"][0, s0:s1].reshape(NS, 128, 128)),
            "cv": np.ascontiguousarray(g["cache_attn_v"][0, s0:s1].reshape(NS, 128, 128)),
            "sg": np.ascontiguousarray(g["state_gdn"][0, s0:s1]),
            "sc": np.ascontiguousarray(g["state_conv"][0, s0:s1].reshape(NS * 3, 1536)),
            "w_in": g["w_in"][0], "w_out": g["w_out"][0], "sinks": g["attn_sinks"][0], "conv_w": g["conv_w"][0],
            "a_log": g["a_log"][0], "dt_bias": g["dt_bias"][0], "gnw": g["gdn_norm_w"][0],
            "ln1_g": g["ln1_g"][0], "ln1_b": g["ln1_b"][0], "w_r": wr,
            "w_gate": g["w_gate"][0], "w_up": g["w_up"][0], "w_down": g["w_down"][0],
            "ln2_g": g["ln2_g"][0], "ln2_b": g["ln2_b"][0],
        })
    return maps


def assemble(results):
    cat = lambda k: np.stack([np.asarray(r[k]) for r in results])
    y_p = cat("y_p")
    y_s = cat("y_s").reshape(128, TS, D)
    nk_p = cat("nk_p").reshape(1, 8, 128, 2, 64)
    nv_p = cat("nv_p").reshape(1, 8, 128, 2, 64)
    ng_p = cat("ng_p").reshape(1, 8, 4, 128, 128)
    nc_p = cat("nc_p").reshape(1, 8, 3, 1536)
    nk_s = cat("nk_s").reshape(1, 128, 128, 2, 64)
    nv_s = cat("nv_s").reshape(1, 128, 128, 2, 64)
    ng_s = cat("ng_s").reshape(1, 128, 4, 128, 128)
    nc_s = cat("nc_s").reshape(1, 128, 3, 1536)
    return tuple(np.ascontiguousarray(a.astype(np.float32)) for a in
                 (y_p, y_s, nk_p, nv_p, ng_p, nc_p, nk_s, nv_s, ng_s, nc_s))


def kernel(**inputs):
    nc, _ = _get_nc(False)
    maps = make_in_maps(inputs)
    res = run_bass_kernel_spmd(nc, maps, core_ids=list(range(NCORES)))
    return assemble(res.results)
```

```python
import math
from contextlib import ExitStack

import numpy as np
import concourse.bass as bass
import concourse.mybir as mybir
from concourse.bass_utils import run_bass_kernel_spmd

F32 = mybir.dt.float32
BF16 = mybir.dt.bfloat16
I32 = mybir.dt.int32
AF = mybir.ActivationFunctionType
ALU = mybir.AluOpType
AX = mybir.AxisListType

NCORES = 8
D = 1024
SEQ = 2048
NT = 16
NS = 16
TS = 4
NTOK = SEQ + NS * TS
PAST = 8192
INC = 2824
ALPHA = 2.0 ** 0.25
NORM_EPS = 1e-5
L2_EPS = 1e-6
THETA = 500000.0
NE = 32
MAGIC = 12582912.0


class _Stop(Exception):
    pass


import os
STOP = os.environ.get("K_STOP", "")


_SCHED = []


def stop(tag):
    if STOP == tag:
        _SCHED[-1].dead = True


class Sched:
    def __init__(self, nc, es):
        self.nc = nc
        self.es = es
        self.E = {"pe": nc.tensor, "act": nc.scalar, "dve": nc.vector, "pool": nc.gpsimd, "sp": nc.sync}
        self.semh = {}
        for k in self.E:
            self.semh["e_" + k] = es.enter_context(nc.semaphore("sem_" + k))
        self.cnt = {k: 0 for k in self.E}
        self.seen = {k: {} for k in self.E}
        self.lastw = {}
        self.readers = {}
        self.pend = {k: [] for k in self.E}
        self.pend_r = {k: set() for k in self.E}
        self.pend_w = {k: set() for k in self.E}
        self.slots = {}
        self.psf_list = []
        self.psb_list = []
        self.psf_i = 0
        self.psb_i = 0
        self.dead = False
        _SCHED.append(self)

    def _waits(self, e, r, w, is_dma):
        need = {}

        def add(ev):
            semk, val, eng = ev
            if need.get(semk, 0) < val:
                need[semk] = val

        for k in list(r) + list(w):
            for e2 in self.E:
                if e2 != e or is_dma:
                    assert k not in self.pend_w[e2], f"key {k} pending write on {e2}"
        for k in w:
            for e2 in self.E:
                if e2 != e or is_dma:
                    assert k not in self.pend_r[e2], f"key {k} pending read on {e2}"
        for k in r:
            ev = self.lastw.get(k)
            if ev is not None:
                if ev[2] == e and e == "pe" and not is_dma:
                    continue
                add(ev)
            if k.startswith("ps"):
                for ev in self.readers.get(k, ()):
                    if ev[2] != e:
                        add(ev)
        for k in w:
            ev = self.lastw.get(k)
            if ev is not None and (is_dma or ev[2] != e or e != "pe"):
                add(ev)
            for ev in self.readers.get(k, ()):
                if is_dma or ev[2] != e or e != "pe":
                    add(ev)
        for semk, val in need.items():
            if self.seen[e].get(semk, 0) < val:
                self.E[e].wait_ge(self.semh[semk], val)
                self.seen[e][semk] = val

    def _register(self, r, w, ev):
        for k in r:
            self.readers.setdefault(k, []).append(ev)
        for k in w:
            self.lastw[k] = ev
            self.readers[k] = []

    def op(self, e, fn, r=(), w=(), inc=True):
        if self.dead:
            return None
        self._waits(e, r, w, False)
        ins = fn(self.E[e])
        if not inc:
            self.pend[e].append((tuple(r), tuple(w)))
            self.pend_r[e].update(r)
            self.pend_w[e].update(w)
            return ins
        self.cnt[e] += 1
        ins.then_inc(self.semh["e_" + e], 1)
        ev = ("e_" + e, self.cnt[e], e)
        for (pr, pw) in self.pend[e]:
            self._register(pr, pw, ev)
        self.pend[e] = []
        self.pend_r[e] = set()
        self.pend_w[e] = set()
        self._register(r, w, ev)
        return ins

    def dma(self, q, out, in_, r=(), w=(), slot=None):
        if slot is None:
            slot = w[0] if w else r[0]
        if self.dead:
            return None
        sk = "d_" + slot
        if sk not in self.slots:
            self.semh[sk] = self.es.enter_context(self.nc.semaphore(sk))
            self.slots[sk] = 0
        self._waits(q, r, w, True)
        ins = self.E[q].dma_start(out=out, in_=in_)
        self.slots[sk] += 16
        ins.then_inc(self.semh[sk], 16)
        ev = (sk, self.slots[sk], "dma")
        self._register(r, w, ev)
        return ins

    def barrier(self):
        if self.dead:
            return
        for e in self.E:
            assert not self.pend[e]
        for e in self.E:
            for e2 in self.E:
                if self.cnt[e2] > self.seen[e].get("e_" + e2, 0):
                    self.E[e].wait_ge(self.semh["e_" + e2], self.cnt[e2])
                    self.seen[e]["e_" + e2] = self.cnt[e2]
            for sk, v in self.slots.items():
                if v > self.seen[e].get(sk, 0):
                    self.E[e].wait_ge(self.semh[sk], v)
                    self.seen[e][sk] = v
        self.lastw = {}
        self.readers = {}

    def final_wait(self):
        self.dead = False
        for sk, v in self.slots.items():
            if v > self.seen["sp"].get(sk, 0):
                self.E["sp"].wait_ge(self.semh[sk], v)
                self.seen["sp"][sk] = v
        for e2 in self.E:
            if e2 != "sp" and self.cnt[e2] > self.seen["sp"].get("e_" + e2, 0):
                self.E["sp"].wait_ge(self.semh["e_" + e2], self.cnt[e2])

    def psf(self):
        i = self.psf_i % len(self.psf_list)
        self.psf_i += 1
        return self.psf_list[i], f"psf{i}"

    def psb(self):
        i = self.psb_i % len(self.psb_list)
        self.psb_i += 1
        return self.psb_list[i], f"psb{i}"

    def mm(self, out, lhsT, rhs, start, stop, r, w, inc=None):
        if inc is None:
            inc = stop
        return self.op("pe", lambda E: E.matmul(out, lhsT=lhsT, rhs=rhs, start=start, stop=stop), r, w, inc)

    def tr(self, out, in_, ident, r, w, inc=True):
        return self.op("pe", lambda E: E.transpose(out=out, in_=in_, identity=ident), r, w, inc)

    def act(self, out, in_, func, r, w, bias=0.0, scale=1.0, accum_out=None):
        if accum_out is not None:
            return self.op("act", lambda E: E.activation(out=out, in_=in_, func=func, bias=bias, scale=scale,
                                                         accum_out=accum_out), r, w)
        return self.op("act", lambda E: E.activation(out=out, in_=in_, func=func, bias=bias, scale=scale), r, w)

    def tt(self, e, out, in0, in1, op, r, w):
        return self.op(e, lambda E: E.tensor_tensor(out=out, in0=in0, in1=in1, op=op), r, w)

    def ts(self, e, out, in0, s1, s2, op0, op1, r, w):
        if s2 is None:
            return self.op(e, lambda E: E.tensor_scalar(out=out, in0=in0, scalar1=s1, scalar2=None, op0=op0), r, w)
        return self.op(e, lambda E: E.tensor_scalar(out=out, in0=in0, scalar1=s1, scalar2=s2, op0=op0, op1=op1), r, w)

    def stt(self, e, out, in0, scalar, in1, op0, op1, r, w):
        return self.op(e, lambda E: E.scalar_tensor_tensor(out=out, in0=in0, scalar=scalar, in1=in1, op0=op0, op1=op1),
                       r, w)

    def cp(self, e, out, in_, r, w):
        if e == "act":
            return self.op("act", lambda E: E.copy(out=out, in_=in_), r, w)
        return self.op(e, lambda E: E.tensor_copy(out=out, in_=in_), r, w)


def build(dbg=False):
    nc = bass.Bass("TRN2", target_bir_lowering=False)

    def din(name, shape, dt=F32):
        return nc.dram_tensor(name, shape, dt, kind="ExternalInput").ap()

    def dout(name, shape):
        return nc.dram_tensor(name, shape, F32, kind="ExternalOutput").ap()

    x_p = din("x_p", [SEQ, D])
    x_s = din("x_s", [NS * TS, D])
    ck_d = din("ck", [NS, 128, 128])
    cv_d = din("cv", [NS, 128, 128])
    sg_d = din("sg", [NS, 4, 128, 128])
    sc_d = din("sc", [NS * 3, 1536])
    w_in = din("w_in", [D, INC])
    w_out = din("w_out", [D, D])
    sinks_d = din("sinks", [8])
    convw_d = din("conv_w", [4, 1536])
    alog_d = din("a_log", [4])
    dtb_d = din("dt_bias", [4])
    gnw_d = din("gnw", [128])
    ln1g_d = din("ln1_g", [D])
    ln1b_d = din("ln1_b", [D])
    wr_d = din("w_r", [D, 36])
    wg_d = din("w_gate", [NE, D, 256])
    wu_d = din("w_up", [NE, D, 256])
    wd_d = din("w_down", [NE, 256, D])
    ln2g_d = din("ln2_g", [D])
    ln2b_d = din("ln2_b", [D])

    y_p = dout("y_p", [SEQ, D])
    y_s = dout("y_s", [NS * TS, D])
    nk_p = dout("nk_p", [128, 128])
    nv_p = dout("nv_p", [128, 128])
    ng_p = dout("ng_p", [4, 128, 128])
    nc_p = dout("nc_p", [3, 1536])
    nk_s = dout("nk_s", [NS, 128, 128])
    nv_s = dout("nv_s", [NS, 128, 128])
    ng_s = dout("ng_s", [NS, 4, 128, 128])
    nc_s = dout("nc_s", [NS * 3, 1536])
    dbg_outs = {}

    with ExitStack() as es0, nc.allow_non_contiguous_dma(reason="small transposed param loads"):
        S = Sched(nc, es0)
        try:

            def sb(es, name, shape, dt=F32):
                return es.enter_context(nc.sbuf_tensor(name, shape, dt))

            for i in range(6):
                S.psf_list.append(es0.enter_context(nc.psum_tensor(f"psf{i}", [128, 512], F32)))
            for i in range(2):
                S.psb_list.append(es0.enter_context(nc.psum_tensor(f"psb{i}", [128, 1024], BF16)))

            def dump(name, ap_src, shape, keys):
                if not dbg:
                    return
                o = dout("dbg_" + name, shape)
                dbg_outs[name] = o
                S.dma("pool" if ap_src.dtype != F32 else "sp", o, ap_src, r=keys, w=[], slot="dbg_" + name)

            onesf = sb(es0, "onesf", [128, 128])
            ones_bf = sb(es0, "ones_bf", [128, 128], BF16)
            idf = sb(es0, "idf", [128, 128])
            idb = sb(es0, "idb", [128, 128], BF16)
            m_strict = sb(es0, "m_strict", [128, 128])
            m_up = sb(es0, "m_up", [128, 128])
            mb_prev = sb(es0, "mb_prev", [128, 128], BF16)
            mb_cur = sb(es0, "mb_cur", [128, 128], BF16)
            S.op("pool", lambda E: E.memset(onesf[:], 1.0), w=["onesf"])
            S.op("pool", lambda E: E.memset(ones_bf[:], 1.0), w=["ones_bf"])
            S.op("pool", lambda E: E.affine_select(out=idf[:], in_=onesf[:], pattern=[[-1, 128]], compare_op=ALU.is_equal,
                                                   fill=0.0, base=0, channel_multiplier=1), r=["onesf"], w=["idf"])
            S.op("pool", lambda E: E.affine_select(out=m_strict[:], in_=onesf[:], pattern=[[-1, 128]], compare_op=ALU.is_gt,
                                                   fill=0.0, base=0, channel_multiplier=1), r=["onesf"], w=["m_strict"])
            S.op("pool", lambda E: E.affine_select(out=m_up[:], in_=onesf[:], pattern=[[1, 128]], compare_op=ALU.is_ge,
                                                   fill=0.0, base=0, channel_multiplier=-1), r=["onesf"], w=["m_up"])
            S.cp("dve", idb[:], idf[:], r=["idf"], w=["idb"])
            S.cp("dve", mb_prev[:], m_strict[:], r=["m_strict"], w=["mb_prev"])
            S.cp("dve", mb_cur[:], m_up[:], r=["m_up"], w=["mb_cur"])

            esink = sb(es0, "esink", [128, 8])
            nexpA = sb(es0, "nexpA", [128, 4])
            dtb = sb(es0, "dtb", [128, 4])
            gnw = sb(es0, "gnw_bc", [128, 128])
            cw = sb(es0, "cw", [128, 4, 12])
            S.dma("sp", esink[:], sinks_d.partition_broadcast(128), w=["esink"])
            S.dma("sp", nexpA[:], alog_d.partition_broadcast(128), w=["nexpA"])
            S.dma("sp", dtb[:], dtb_d.partition_broadcast(128), w=["dtb"])
            S.dma("sp", gnw[:], gnw_d.partition_broadcast(128), w=["gnw"])
            for k in range(4):
                S.dma("sp", cw[:, k, :], convw_d[k].rearrange("(c p) -> p c", p=128), w=["cw"])
            S.act(esink[:], esink[:], AF.Exp, r=["esink"], w=["esink"])
            S.act(nexpA[:], nexpA[:], AF.Exp, r=["nexpA"], w=["nexpA"])
            S.op("act", lambda E: E.mul(out=nexpA[:], in_=nexpA[:], mul=-1.0), r=["nexpA"], w=["nexpA"])

            cosT = sb(es0, "cosT", [128, NT + 1, 8])
            sinT = sb(es0, "sinT", [128, NT + 1, 8])
            with ExitStack() as esr:
                posi = sb(esr, "posi", [128, NT + 1], I32)
                pos1 = sb(esr, "pos1", [128, 1], I32)
                posf = sb(esr, "posf", [128, NT + 1])
                ang = sb(esr, "ang", [128, NT + 1, 8])
                kk = sb(esr, "kk", [128, NT + 1, 8])
                anl = sb(esr, "anl", [128, NT + 1, 8])
                S.op("pool", lambda E: E.iota(posi[:], pattern=[[128, NT + 1]], base=0, channel_multiplier=1), w=["posi"])
                S.op("pool", lambda E: E.iota(pos1[:], pattern=[[0, 1]], base=0, channel_multiplier=1), w=["pos1"])
                S.op("dve", lambda E: E.tensor_single_scalar(out=pos1[:], in_=pos1[:], scalar=3, op=ALU.bitwise_and),
                     r=["pos1"], w=["pos1"])
                S.cp("dve", posf[:], posi[:], r=["posi"], w=["posf"])
                S.cp("dve", posf[:, NT:NT + 1], pos1[:], r=["pos1", "posf"], w=["posf"])
                S.ts("dve", posf[:, NT:NT + 1], posf[:, NT:NT + 1], float(PAST), None, ALU.add, None, r=["posf"], w=["posf"])
                for j in range(8):
                    f = THETA ** (-(2.0 * j) / 16.0)
                    m_, e_ = math.frexp(f)
                    f_hi = math.ldexp(round(m_ * 1024.0) / 1024.0, e_)
                    f_lo = f - f_hi
                    S.ts("dve", ang[:, :, j], posf[:], float(f_hi), None, ALU.mult, None, r=["posf"], w=["ang"])
                    S.ts("dve", anl[:, :, j], posf[:], float(f_lo), None, ALU.mult, None, r=["posf"], w=["anl"])
                S.tt("dve", kk[:], ang[:], anl[:], ALU.add, r=["ang", "anl"], w=["kk"])
                S.ts("dve", kk[:], kk[:], float(1.0 / (2 * math.pi)), MAGIC, ALU.mult, ALU.add, r=["kk"], w=["kk"])
                S.ts("dve", kk[:], kk[:], MAGIC, None, ALU.subtract, None, r=["kk"], w=["kk"])
                C1 = 6.28125
                C2 = 0.00193548202514648
                C3 = 2 * math.pi - C1 - C2
                for cc in (C1, C2, C3):
                    S.stt("dve", ang[:], kk[:], float(-cc), ang[:], ALU.mult, ALU.add, r=["kk", "ang"], w=["ang"])
                S.tt("dve", ang[:], ang[:], anl[:], ALU.add, r=["ang", "anl"], w=["ang"])
                PI_S = 3.1415925
                S.ts("dve", ang[:], ang[:], -PI_S, PI_S, ALU.max, ALU.min, r=["ang"], w=["ang"])
                S.act(sinT[:], ang[:], AF.Sin, r=["ang"], w=["sinT"])
                S.stt("dve", ang[:], ang[:], -1.0, ang[:], ALU.mult, ALU.max, r=["ang"], w=["ang"])
                S.ts("dve", ang[:], ang[:], -1.0, float(math.pi / 2), ALU.mult, ALU.add, r=["ang"], w=["ang"])
                S.act(cosT[:], ang[:], AF.Sin, r=["ang"], w=["cosT"])
                S.barrier()
            stop("p0")

            mix_all = sb(es0, "mix_all", [128, NT + 1, D], BF16)

            with ExitStack() as es1:
                w_in_bf = sb(es1, "w_in_bf", [128, 8, INC], BF16)
                for kc in range(8):
                    S.dma("pool", w_in_bf[:, kc, :], w_in[kc * 128:(kc + 1) * 128, :], w=[f"w_in{kc}"])
                WIN = [f"w_in{kc}" for kc in range(8)]
                S.op("pool", lambda E: E.memset(mix_all[:, NT, :], 0.0), w=["mix16"])

                with ExitStack() as es:
                    n = NS * TS
                    xTs = sb(es, "xTs", [128, 8, n], BF16)
                    S_b = sb(es, "S_b", [128, NS * 4, 128], BF16)
                    qkTs = sb(es, "qkTs", [128, 8, n], BF16)
                    vTs = sb(es, "vTs", [128, 4, n], BF16)
                    knvs = sb(es, "knvs", [n, 8, 128], BF16)
                    tqkv = sb(es, "tqkv", [n, 768])
                    tz = sb(es, "tz", [n, 512])
                    tab = sb(es, "tab", [n, 8])
                    blk = sb(es, "blk", [n, n])
                    rowm = sb(es, "rowm", [n, NS])
                    U_s = sb(es, "U_s", [n, n])
                    L_s = sb(es, "L_s", [n, n])
                    U_sb = sb(es, "U_sb", [n, n], BF16)
                    smb = sb(es, "smb", [128, NS, n], BF16)
                    nsmb = sb(es, "nsmb", [128, NS, n], BF16)
                    S_f = sb(es, "S_f", [128, 32, 128])
                    esA = ExitStack()
                    xbs = sb(esA, "xbs", [n, D], BF16)
                    cK = sb(esA, "cK", [128, NS, 128], BF16)
                    cVa = sb(esA, "cVa", [128, NS, 2, 65], BF16)
                    cKT = sb(esA, "cKT", [128, NS, 128], BF16)
                    scs = sb(esA, "scs", [NS * 3, 1536])
                    ext = sb(esA, "ext", [128, 12, NS, 7])
                    cprod = sb(esA, "cprod", [128, 12, NS, 4])
                    cacs = sb(esA, "cacs", [128, 12, NS, 4])
                    ncsf = sb(esA, "ncsf", [128, 12, NS * 3])
                    sTs = sb(esA, "sTs", [128, 8, n])
                    sq8 = sb(esA, "sq8", [128, 8, n], BF16)
                    ln8 = sb(esA, "ln8", [128, 8, n])
                    rots = sb(esA, "rots", [n, 10, 16])
                    rtm = sb(esA, "rtm", [n, 4, 10, 8])
                    qbs = sb(esA, "qbs", [n, 4, 2, 64], BF16)
                    kbs = sb(esA, "kbs", [n, 2, 64], BF16)
                    krs = sb(esA, "krs", [n, 2, 64])
                    vna = sb(esA, "vna", [n, 2, 65], BF16)
                    qTs = sb(esA, "qTs", [128, 4, n], BF16)
                    qTsr = sb(esA, "qTsr", [128, NS, 16], BF16)
                    kTn = sb(esA, "kTn", [128, n], BF16)
                    PTs = [sb(esA, f"PTs{i}", [128, 512], BF16) for i in range(2)]
                    mk16 = sb(esA, "mk16", [128, NS, n], BF16)
                    PTn = sb(esA, "PTn", [n, 512], BF16)
                    mc4 = sb(esA, "mc4", [128, 4], BF16)
                    oc = sb(esA, "oc", [65, 512])
                    ot = sb(esA, "ot", [65, 512])
                    dn = sb(esA, "dn", [65, 512])
                    oTb = sb(esA, "oTb", [64, 8, n], BF16)
                    ii = sb(esA, "ii", [n, 64], I32)
                    ip = sb(esA, "ip", [n, 1], I32)
                    colid = sb(esA, "colid", [n, 64])
                    rowid = sb(esA, "rowid", [n, 1])
                    sidx = sb(esA, "sidx", [n, NS])
                    smf = sb(esA, "smf", [128, NS, n])
                    S.dma("pool", xbs[:], x_s, w=["xbs"])
                    S.dma("pool", S_b[:], sg_d.rearrange("s h k v -> k (s h) v"), w=["S_b"])
                    S.dma("pool", cK[:], ck_d.rearrange("s r c -> r s c"), w=["cK"])
                    S.op("pool", lambda E: E.memset(cVa[:], 1.0), w=["cVa"])
                    for jk in range(2):
                        S.dma("pool", cVa[:, :, jk, 0:64], cv_d[:, :, jk * 64:(jk + 1) * 64].rearrange("s r d -> r s d"),
                              w=["cVa"], slot=f"cVa{jk}")
                    S.dma("sp", scs[:], sc_d, w=["scs"])
                    S.op("pool", lambda E: E.memset(vna[:], 1.0), w=["vna"])
                    S.dma("sp", nk_s[:, 0:124, :], ck_d[:, 4:128, :], slot="nk_s0")
                    S.dma("sp", nv_s[:, 0:124, :], cv_d[:, 4:128, :], slot="nv_s0")
                    S.op("pool", lambda E: E.iota(ii[:], pattern=[[1, 64]], base=0, channel_multiplier=0), w=["ii"])
                    S.op("pool", lambda E: E.iota(ip[:], pattern=[[0, 1]], base=0, channel_multiplier=1), w=["ip"])
                    S.op("dve", lambda E: E.tensor_single_scalar(out=ii[:], in_=ii[:], scalar=2, op=ALU.arith_shift_right),
                         r=["ii"], w=["ii"])
                    S.op("dve", lambda E: E.tensor_single_scalar(out=ip[:], in_=ip[:], scalar=2, op=ALU.arith_shift_right),
                         r=["ip"], w=["ip"])
                    S.cp("dve", colid[:], ii[:], r=["ii"], w=["colid"])
                    S.cp("dve", rowid[:], ip[:], r=["ip"], w=["rowid"])
                    S.ts("dve", blk[:], colid[:], rowid[:, 0:1], None, ALU.is_equal, None, r=["colid", "rowid"], w=["blk"])
                    S.op("pool", lambda E: E.iota(ii[:, 0:NS], pattern=[[1, NS]], base=0, channel_multiplier=0), r=["colid"], w=["ii"])
                    S.cp("dve", sidx[:], ii[:, 0:NS], r=["ii"], w=["sidx"])
                    S.ts("dve", rowm[:], sidx[:], rowid[:, 0:1], None, ALU.is_equal, None, r=["sidx", "rowid"], w=["rowm"])
                    S.tt("dve", U_s[:], m_up[0:n, 0:n], blk[:], ALU.mult, r=["blk"], w=["U_s"])
                    S.tt("dve", L_s[:], m_strict[0:n, 0:n], blk[:], ALU.mult, r=["blk"], w=["L_s"])
                    S.cp("dve", U_sb[:], U_s[:], r=["U_s"], w=["U_sb"])
                    S.op("pool", lambda E: E.memset(smf[:], 1.0), w=["smf"])
                    S.op("pool", lambda E: E.affine_select(out=smf[:], in_=smf[:], pattern=[[-4, NS], [1, n]],
                                                           compare_op=ALU.is_ge, fill=0.0, base=0, channel_multiplier=0),
                         r=["smf"], w=["smf"])
                    S.op("pool", lambda E: E.affine_select(out=smf[:], in_=smf[:], pattern=[[4, NS], [-1, n]],
                                                           compare_op=ALU.is_ge, fill=0.0, base=3, channel_multiplier=0),
                         r=["smf"], w=["smf"])
                    S.cp("dve", smb[:], smf[:], r=["smf"], w=["smb"])
                    S.ts("dve", nsmb[:], smf[:], -1.0, None, ALU.mult, None, r=["smf"], w=["nsmb"])
                    S.op("pool", lambda E: E.affine_select(out=mc4[:], in_=ones_bf[:, 0:4], pattern=[[-1, 4]],
                                                           compare_op=ALU.is_gt, fill=0.0, base=0, channel_multiplier=1),
                         w=["mc4"])
                    stop("sA")
                    pb, pbk = S.psb()
                    for kc in range(8):
                        S.tr(pb[:, kc * 128:kc * 128 + n], xbs[:, kc * 128:(kc + 1) * 128], idb[0:n, 0:n], r=["xbs"], w=[pbk],
                             inc=(kc == 7))
                    S.cp("act", xTs[:], pb[:].rearrange("p (k t) -> p k t", k=8)[:, :, 0:n], r=[pbk], w=["xTs"])
                    for grp in range(2):
                        pf, pfk = S.psf()
                        ncg = 8 if grp == 0 else 4
                        for cc in range(ncg):
                            c = grp * 8 + cc
                            S.tr(pf[:, cc * 48:(cc + 1) * 48], scs[:, c * 128:(c + 1) * 128], idf[0:48, 0:48], r=["scs"],
                                 w=[pfk], inc=(cc == ncg - 1))
                        S.cp("act", ext[:, grp * 8:grp * 8 + ncg, :, 0:3],
                             pf[:, 0:ncg * 48].rearrange("p (c s r) -> p c s r", c=ncg, s=NS), r=[pfk], w=["ext"])
                    for c in range(12):
                        pf, pfk = S.psf()
                        for kc in range(8):
                            S.mm(pf[:, 0:n], w_in_bf[:, kc, 768 + c * 128:768 + (c + 1) * 128], xTs[:, kc, :], kc == 0, kc == 7,
                                 r=[WIN[kc], "xTs"], w=[pfk])
                        S.cp("act" if c % 2 else "dve", ext[:, c, :, 3:7], pf[:, 0:n].rearrange("p (s t) -> p s t", s=NS),
                             r=[pfk], w=["ext"])
                    S.cp("pool", ncsf[:].rearrange("p c (s r) -> p c s r", s=NS), ext[:, :, :, 4:7], r=["ext"], w=["ncsf"])
                    for grp in range(3):
                        pf, pfk = S.psf()
                        for cc in range(4):
                            c = grp * 4 + cc
                            S.tr(pf[0:48, cc * 128:(cc + 1) * 128], ncsf[:, c, :], idf[:], r=["ncsf"], w=[pfk], inc=(cc == 3))
                        S.cp("act", scs[:, grp * 512:(grp + 1) * 512], pf[0:48, :], r=[pfk], w=["scs"])
                    S.dma("sp", nc_s, scs[:], r=["scs"], slot="nc_s")
                    stop("sB")
                    for k in range(4):
                        cwb = cw[:, k, :].unsqueeze(2).unsqueeze(3).to_broadcast([128, 12, NS, 4])
                        if k == 0:
                            S.tt("dve", cacs[:], ext[:, :, :, 0:4], cwb, ALU.mult, r=["ext", "cw"], w=["cacs"])
                        else:
                            S.tt("pool", cprod[:], ext[:, :, :, k:k + 4], cwb, ALU.mult, r=["ext", "cw"], w=["cprod"])
                            S.tt("dve", cacs[:], cacs[:], cprod[:], ALU.add, r=["cacs", "cprod"], w=["cacs"])
                    S.act(sTs[:].rearrange("p c (s t) -> p c s t", s=NS), cacs[:, 0:8], AF.Silu, r=["cacs"], w=["sTs"])
                    S.act(vTs[:].rearrange("p c (s t) -> p c s t", s=NS), cacs[:, 8:12], AF.Silu, r=["cacs"], w=["vTs"])
                    S.act(sq8[:], sTs[:], AF.Square, r=["sTs"], w=["sq8"])
                    pf, pfk = S.psf()
                    S.mm(pf[:, 0:8 * n], ones_bf[:], sq8[:].rearrange("p c t -> p (c t)"), True, True, r=["sq8"], w=[pfk])
                    S.act(ln8[:].rearrange("p c t -> p (c t)"), pf[:, 0:8 * n], AF.Ln, r=[pfk], w=["ln8"], bias=L2_EPS)
                    S.act(ln8[:, 0:4, :], ln8[:, 0:4, :], AF.Exp, r=["ln8"], w=["ln8"], bias=-0.5 * math.log(128.0), scale=-0.5)
                    S.act(ln8[:, 4:8, :], ln8[:, 4:8, :], AF.Exp, r=["ln8"], w=["ln8"], scale=-0.5)
                    S.tt("dve", qkTs[:], sTs[:], ln8[:], ALU.mult, r=["sTs", "ln8"], w=["qkTs"])
                    pb, pbk = S.psb()
                    for h in range(4):
                        S.tr(pb[0:n, h * 128:(h + 1) * 128], qkTs[:, 4 + h, :], idb[:], r=["qkTs"], w=[pbk], inc=False)
                    for h in range(4):
                        S.tr(pb[0:n, (4 + h) * 128:(5 + h) * 128], vTs[:, h, :], idb[:], r=["vTs"], w=[pbk], inc=(h == 3))
                    S.cp("act", knvs[:], pb[0:n, :].rearrange("p (k t) -> p k t", k=8), r=[pbk], w=["knvs"])
                    for (c0, c1, dst, dk_) in ((0, 512, tqkv[:, 0:512], "tq_s"), (512, 768, tqkv[:, 512:768], "tkv_s"),
                                               (2304, 2816, tz[:], "tz_s"), (2816, 2824, tab[:], "tab_s")):
                        pf, pfk = S.psf()
                        for kc in range(8):
                            S.mm(pf[0:n, 0:c1 - c0], xTs[:, kc, :], w_in_bf[:, kc, c0:c1], kc == 0, kc == 7,
                                 r=[WIN[kc], "xTs"], w=[pfk])
                        S.cp("act" if dk_ in ("tq_s", "tz_s") else "dve", dst, pf[0:n, 0:c1 - c0], r=[pfk], w=[dk_])
                    qk3 = tqkv[:, 0:640].rearrange("p (h d) -> p h d", d=64)
                    cb = cosT[0:n, NT, :].unsqueeze(1).to_broadcast([n, 10, 8])
                    sbb = sinT[0:n, NT, :].unsqueeze(1).to_broadcast([n, 10, 8])
                    RK = ["tq_s", "tkv_s"]
                    S.tt("dve", rtm[:, 0], qk3[:, :, 0:8], cb, ALU.mult, r=RK, w=["rtm0"])
                    S.tt("dve", rtm[:, 1], qk3[:, :, 8:16], sbb, ALU.mult, r=RK, w=["rtm1"])
                    S.tt("dve", rtm[:, 2], qk3[:, :, 8:16], cb, ALU.mult, r=RK, w=["rtm2"])
                    S.tt("dve", rtm[:, 3], qk3[:, :, 0:8], sbb, ALU.mult, r=RK, w=["rtm3"])
                    S.tt("dve", rots[:, :, 0:8], rtm[:, 0], rtm[:, 1], ALU.subtract, r=["rtm0", "rtm1"], w=["rots"])
                    S.tt("dve", rots[:, :, 8:16], rtm[:, 2], rtm[:, 3], ALU.add, r=["rtm2", "rtm3"], w=["rots"])
                    q4 = tqkv[:, 0:512].rearrange("p (j c d) -> p c j d", j=2, c=4)
                    S.cp("dve", qbs[:, :, :, 0:16], rots[:, 0:8, :].rearrange("p (j c) d -> p c j d", j=2), r=["rots"], w=["qbs"])
                    S.cp("dve", qbs[:, :, :, 16:64], q4[:, :, :, 16:64], r=["tq_s"], w=["qbs"])
                    k3 = tqkv[:, 512:640].rearrange("p (j d) -> p j d", j=2)
                    S.cp("dve", kbs[:, :, 0:16], rots[:, 8:10, :], r=["rots"], w=["kbs"])
                    S.cp("dve", kbs[:, :, 16:64], k3[:, :, 16:64], r=["tkv_s"], w=["kbs"])
                    S.cp("dve", krs[:, :, 0:16], rots[:, 8:10, :], r=["rots"], w=["krs"])
                    S.cp("dve", krs[:, :, 16:64], k3[:, :, 16:64], r=["tkv_s"], w=["krs"])
                    S.cp("dve", vna[:, :, 0:64], tqkv[:, 640:768].rearrange("p (j d) -> p j d", j=2), r=["tkv_s", "vna"], w=["vna"])
                    S.dma("sp", nk_s[:, 124:128, :].rearrange("s t c -> (s t) c") if False else nk_s[:, 124:128, :],
                          krs[:].rearrange("(s t) j d -> s t (j d)", t=TS) if False else krs[:].rearrange("p j d -> p (j d)"),
                          r=["krs"], slot="nk_s1")
                    S.dma("sp", nv_s[:, 124:128, :], tqkv[:, 640:768], r=["tkv_s"], slot="nv_s1")
                    stop("sC")
                    pb, pbk = S.psb()
                    for c in range(4):
                        S.tr(pb[:, c * 128:c * 128 + n], qbs[:, c].rearrange("p j d -> p (j d)"), idb[0:n, 0:n], r=["qbs"],
                             w=[pbk], inc=False)
                    S.tr(pb[:, 512:512 + n], kbs[:].rearrange("p j d -> p (j d)"), idb[0:n, 0:n], r=["kbs"], w=[pbk])
                    S.cp("act", qTs[:], pb[:, 0:512].rearrange("p (c t) -> p c t", c=4)[:, :, 0:n], r=[pbk], w=["qTs"])
                    S.cp("act", kTn[:], pb[:, 512:512 + n], r=[pbk], w=["kTn"])
                    S.cp("dve", qTsr[:].rearrange("p s (c t) -> p s c t", c=4),
                         qTs[:].rearrange("p c (s t) -> p s c t", s=NS), r=["qTs"], w=["qTsr"])
                    for grp in range(2):
                        pb, pbk = S.psb()
                        for ss in range(8):
                            s_ = grp * 8 + ss
                            S.tr(pb[:, ss * 128:(ss + 1) * 128], cK[:, s_, :], idb[:], r=["cK"], w=[pbk], inc=(ss == 7))
                        S.cp("act" if grp else "dve", cKT[:, grp * 8:(grp + 1) * 8, :], pb[:].rearrange("p (s t) -> p s t", s=8),
                             r=[pbk], w=["cKT"])
                    stop("sC2")
                    S.tt("dve", mk16[:].rearrange("p s (a t) -> p (s a) t", t=4), smb[:].rearrange("p s (a t) -> p (s a) t", t=4),
                         mc4[:].unsqueeze(1).to_broadcast([128, NS * NS, 4]), ALU.mult, r=["smb", "mc4"], w=["mk16"])
                    stop("sD0")
                    base = S.psf_i % 6
                    S.psf_i += 6
                    bk = [(base + d) % 6 for d in range(6)]
                    po_ = [(S.psf_list[bk[j]], f"psf{bk[j]}") for j in range(2)]
                    ps_ = [(S.psf_list[bk[2 + j]], f"psf{bk[2 + j]}") for j in range(4)]
                    for s_ in range(NS):
                        pt_, ptk_ = PTs[s_ % 2], f"PTs{s_ % 2}"
                        for jk in range(2):
                            psc, psck = ps_[(s_ % 2) * 2 + jk]
                            S.mm(psc[:, 0:256], cKT[jk * 64:(jk + 1) * 64, s_, :],
                                 qTs[jk * 64:(jk + 1) * 64, :, :].rearrange("p c t -> p (c t)"), True, True,
                                 r=["cKT", "qTs"], w=[psck], inc=True)
                            S.act(pt_[:, jk * 256:(jk + 1) * 256], psc[:, 0:256], AF.Exp, r=[psck], w=[ptk_], scale=0.125)
                        S.tt("dve", pt_[:].rearrange("p (a t) -> p a t", a=8), pt_[:].rearrange("p (a t) -> p a t", a=8),
                             mk16[:, s_, :].unsqueeze(1).to_broadcast([128, 8, n]), ALU.mult, r=[ptk_, "mk16"], w=[ptk_])
                        for jk in range(2):
                            S.mm(po_[jk][0][0:65, 0:256], cVa[:, s_, jk, :], pt_[:, jk * 256:(jk + 1) * 256], s_ == 0, False,
                                 r=["cVa", ptk_], w=[po_[jk][1]], inc=(jk == 1))
                    stop("sD1")
                    for jk in range(2):
                        psn, psnk = ps_[jk]
                        S.mm(psn[0:n, 0:256], kTn[jk * 64:(jk + 1) * 64, :],
                             qTs[jk * 64:(jk + 1) * 64, :, :].rearrange("p c t -> p (c t)"), True, True, r=["kTn", "qTs"],
                             w=[psnk], inc=True)
                        S.act(PTn[:, jk * 256:(jk + 1) * 256], psn[0:n, 0:256], AF.Exp, r=[psnk], w=["PTn"], scale=0.125)
                    S.tt("pool", PTn[:].rearrange("p (a t) -> p a t", a=8), PTn[:].rearrange("p (a t) -> p a t", a=8),
                         U_sb[:].unsqueeze(1).to_broadcast([n, 8, n]), ALU.mult, r=["PTn", "U_sb"], w=["PTn"])
                    for jk in range(2):
                        S.mm(po_[jk][0][0:65, 0:256], vna[:, jk, :], PTn[:, jk * 256:(jk + 1) * 256], False, True,
                             r=["vna", "PTn"], w=[po_[jk][1]], inc=True)
                    stop("sD3")
                    for jk in range(2):
                        S.cp("act" if jk else "dve", ot[:, jk * 256:(jk + 1) * 256], po_[jk][0][0:65, 0:256], r=[po_[jk][1]],
                             w=["ot"])
                    S.tt("dve", dn[64:65, :].rearrange("p (h t) -> p h t", h=8), ot[64:65, :].rearrange("p (h t) -> p h t", h=8),
                         esink[64:65, :].unsqueeze(2).to_broadcast([1, 8, n]), ALU.add, r=["ot", "esink"], w=["dn"])
                    S.op("dve", lambda E: E.reciprocal(out=dn[64:65, :], in_=dn[64:65, :]), r=["dn"], w=["dn"])
                    stop("sD4")
                    pbc, pbck = S.psf()
                    S.mm(pbc[0:64, :], onesf[64:65, 0:64], dn[64:65, :], True, True, r=["dn"], w=[pbck])
                    S.tt("dve", oTb[:].rearrange("p h t -> p (h t)"), pbc[0:64, :], ot[0:64, :], ALU.mult, r=[pbck, "ot"],
                         w=["oTb"])
                    stop("sD5")
                    pb, pbk = S.psb()
                    for h in range(8):
                        S.tr(pb[0:n, h * 64:(h + 1) * 64], oTb[:, h, :], idb[0:64, 0:64], r=["oTb"], w=[pbk], inc=(h == 7))
                    S.cp("act", mix_all[0:n, NT, 0:512], pb[0:n, 0:512], r=[pbk], w=["mix16"])
                    S.barrier()
                    esA.close()
                    gabs = sb(es, "gabs", [n, 8, 4])
                    gmk = sb(es, "gmk", [n, NS, 4])
                    abc = sb(es, "abc", [128, NS * 4])
                    gUs = sb(es, "gUs", [n, 4, n])
                    eDs = sb(es, "eDs", [n, 4, n])
                    eDTs = sb(es, "eDTs", [n, 4, n])
                    ATs = sb(es, "ATs", [n, 4, n], BF16)
                    P0s = sb(es, "P0s", [n, 4, n])
                    P0Ts = sb(es, "P0Ts", [n, 4, n])
                    P1s = sb(es, "P1s", [n, 4, n])
                    Q0s = sb(es, "Q0s", [n, 4, n])
                    Qbs = sb(es, "Qbs", [n, 4, n], BF16)
                    kbes = sb(es, "kbes", [n, 4, 128], BF16)
                    kdcs = sb(es, "kdcs", [n, 4, 128], BF16)
                    vbs = sb(es, "vbs", [n, 4, 128], BF16)
                    vns = sb(es, "vns", [n, 4, 128], BF16)
                    o1ss = sb(es, "o1ss", [n, 4, 128])
                    ogs = sb(es, "ogs", [n, 4, 128])
                    osqs = sb(es, "osqs", [n, 4, 128])
                    ssqs = sb(es, "ssqs", [n, 4])
                    zss = sb(es, "zss", [n, 512])
                    Stm = [sb(es, f"Stm{i}", [128, 4, 128]) for i in range(2)]
                    nwTh = [sb(es, f"nwTh{i}", [128, NS, n], BF16) for i in range(2)]
                    qTh = [sb(es, f"qTh{i}", [128, NS, n], BF16) for i in range(2)]
                    vnh = [sb(es, f"vnh{i}", [n, NS, 128], BF16) for i in range(2)]

                    stop("sD")
                    g_ = gabs[:, 0, :]
                    beta_ = gabs[:, 1, :]
                    gc_ = gabs[:, 2, :]
                    egc_ = gabs[:, 3, :]
                    kds_ = gabs[:, 4, :]
                    tmp_ = gabs[:, 6, :]
                    nbeta_ = gabs[:, 7, :]
                    S.tt("dve", tmp_, tab[:, 0:4], dtb[0:n, :], ALU.add, r=["tab_s"], w=["s_tmp"])
                    S.act(tmp_, tmp_, AF.Exp, r=["s_tmp"], w=["s_tmp"])
                    S.act(tmp_, tmp_, AF.Ln, r=["s_tmp"], w=["s_tmp"], bias=1.0)
                    S.tt("dve", g_, tmp_, nexpA[0:n, :], ALU.mult, r=["s_tmp"], w=["s_g"])
                    S.act(beta_, tab[:, 4:8], AF.Exp, r=["tab_s"], w=["s_beta"], scale=-1.0)
                    S.ts("dve", beta_, beta_, 1.0, None, ALU.add, None, r=["s_beta"], w=["s_beta"])
                    S.op("dve", lambda E: E.reciprocal(out=beta_, in_=beta_), r=["s_beta"], w=["s_beta"])
                    S.ts("dve", nbeta_, beta_, -1.0, None, ALU.mult, None, r=["s_beta"], w=["s_nbeta"])
                    S.tt("dve", gmk[:], g_.unsqueeze(1).to_broadcast([n, NS, 4]), rowm[:].unsqueeze(2).to_broadcast([n, NS, 4]),
                         ALU.mult, r=["s_g", "rowm"], w=["gmk"])
                    pf, pfk = S.psf()
                    S.mm(pf[0:n, 0:4], U_s[:], g_, True, True, r=["U_s", "s_g"], w=[pfk])
                    S.mm(pf[0:n, 4:8], blk[:], g_, True, True, r=["blk", "s_g"], w=[pfk])
                    S.cp("dve", gc_, pf[0:n, 0:4], r=[pfk], w=["s_gc"])
                    S.act(egc_, pf[0:n, 0:4], AF.Exp, r=[pfk], w=["s_egc"])
                    S.tt("dve", kds_, pf[0:n, 4:8], gc_, ALU.subtract, r=[pfk, "s_gc"], w=["s_kds"])
                    S.act(kds_, kds_, AF.Exp, r=["s_kds"], w=["s_kds"])
                    pa, pak = S.psf()
                    S.mm(pa[:, 0:NS * 4], onesf[0:n, :], gmk[:].rearrange("p s h -> p (s h)"), True, True, r=["gmk"], w=[pak])
                    S.act(abc[:], pa[:, 0:NS * 4], AF.Exp, r=[pak], w=["abc"])
                    pD, pDk = S.psf()
                    pDT, pDTk = S.psf()
                    pG, pGk = S.psf()
                    pA, pAk = S.psf()
                    v3 = lambda p: p[0:n, 0:4 * n].rearrange("p (h t) -> p h t", h=4)
                    pD3, pDT3, pG3, pA3 = v3(pD), v3(pDT), v3(pG), v3(pA)
                    for h in range(4):
                        S.ts("dve", gUs[:, h, :], U_s[:], gabs[:, 0, h:h + 1], None, ALU.mult, None, r=["U_s", "s_g"], w=["gUs"])
                    for h in range(4):
                        S.mm(pD3[:, h, :], gUs[:, h, :], L_s[:], True, True, r=["gUs", "L_s"], w=[pDk], inc=(h == 3))
                    for h in range(4):
                        S.mm(pDT3[:, h, :], L_s[:], gUs[:, h, :], True, True, r=["gUs", "L_s"], w=[pDTk], inc=(h == 3))
                    for h in range(4):
                        S.mm(pG3[:, h, :], qkTs[:, 4 + h, :], qkTs[:, 4 + h, :], True, True, r=["qkTs"], w=[pGk], inc=(h == 3))
                    for h in range(4):
                        S.mm(pA3[:, h, :], qkTs[:, 4 + h, :], qkTs[:, h, :], True, True, r=["qkTs"], w=[pAk], inc=(h == 3))
                    S.act(eDs[:], pD3, AF.Exp, r=[pDk], w=["eDs"])
                    S.tt("pool", eDs[:], eDs[:], L_s[:].unsqueeze(1).to_broadcast([n, 4, n]), ALU.mult, r=["eDs", "L_s"], w=["eDs"])
                    S.act(eDTs[:], pDT3, AF.Exp, r=[pDTk], w=["eDTs"])
                    S.tt("pool", eDTs[:], eDTs[:], U_s[:].unsqueeze(1).to_broadcast([n, 4, n]), ALU.mult, r=["eDTs", "U_s"],
                         w=["eDTs"])
                    for h in range(4):
                        S.stt("dve", P0s[:, h, :], pG3[:, h, :], gabs[:, 7, h:h + 1], eDs[:, h, :], ALU.mult, ALU.mult,
                              r=[pGk, "s_nbeta", "eDs"], w=["P0s"])
                    S.tt("dve", ATs[:], pA3, eDTs[:], ALU.mult, r=[pAk, "eDTs"], w=["ATs"])
                    pt, ptk = S.psf()
                    pt3 = v3(pt)
                    for h in range(4):
                        S.tr(pt3[:, h, :], P0s[:, h, :], idf[0:n, 0:n], r=["P0s"], w=[ptk], inc=(h == 3))
                    S.cp("act", P0Ts[:], pt3, r=[ptk], w=["P0Ts"])
                    S.tt("dve", Q0s[:], pt3, idf[0:n, 0:n].unsqueeze(1).to_broadcast([n, 4, n]), ALU.add, r=[ptk], w=["Q0s"])
                    pP, pPk = S.psf()
                    pP3 = v3(pP)
                    for h in range(4):
                        S.mm(pP3[:, h, :], P0Ts[:, h, :], P0s[:, h, :], True, True, r=["P0Ts", "P0s"], w=[pPk], inc=(h == 3))
                    S.cp("act", P1s[:], pP3, r=[pPk], w=["P1s"])
                    pQ, pQk = S.psf()
                    pQ3 = v3(pQ)
                    for h in range(4):
                        S.mm(pQ3[:, h, :], P1s[:, h, :], Q0s[:, h, :], True, True, r=["P1s", "Q0s"], w=[pQk], inc=(h == 3))
                    S.tt("dve", Qbs[:], pQ3, Q0s[:], ALU.add, r=[pQk, "Q0s"], w=["Qbs"])
                    stop("sE")
                    kn3 = knvs[:, 0:4, :]
                    vv3 = knvs[:, 4:8, :]
                    S.tt("dve", tmp_, beta_, egc_, ALU.mult, r=["s_beta", "s_egc", "s_tmp"], w=["s_tmp"])
                    S.tt("pool", kbes[:], kn3, tmp_.unsqueeze(2).to_broadcast([n, 4, 128]), ALU.mult, r=["knvs", "s_tmp"], w=["kbes"])
                    S.tt("pool", kdcs[:], kn3, kds_.unsqueeze(2).to_broadcast([n, 4, 128]), ALU.mult, r=["knvs", "s_kds"], w=["kdcs"])
                    S.tt("dve", vbs[:], vv3, beta_.unsqueeze(2).to_broadcast([n, 4, 128]), ALU.mult, r=["knvs", "s_beta"], w=["vbs"])
                    pw, pwk = S.psf()
                    pw3 = pw[:, 0:4 * n].rearrange("p (h t) -> p h t", h=4)
                    for h in range(4):
                        S.mm(pw3[:, h, :], kbes[:, h, :], Qbs[:, h, :], True, True, r=["kbes", "Qbs"], w=[pwk], inc=(h == 3))
                    pv, pvk = S.psf()
                    pv3 = pv[0:n, :].rearrange("p (h t) -> p h t", h=4)
                    for h in range(4):
                        S.tt("dve", nwTh[h % 2][:], pw3[:, h, :].unsqueeze(1).to_broadcast([128, NS, n]), nsmb[:], ALU.mult,
                             r=[pwk, "nsmb"], w=[f"nwTh{h % 2}"])
                        S.mm(pv3[:, h, :], Qbs[:, h, :], vbs[:, h, :], True, False, r=["Qbs", "vbs"], w=[pvk], inc=False)
                        for s_ in range(NS):
                            S.mm(pv3[:, h, :], nwTh[h % 2][:, s_, :], S_b[:, s_ * 4 + h, :], False, s_ == NS - 1,
                                 r=[f"nwTh{h % 2}", "S_b"], w=[pvk], inc=(s_ == NS - 1))
                    S.cp("act", vns[:], pv3, r=[pvk], w=["vns"])
                    po1, po1k = S.psf()
                    po13 = po1[0:n, :].rearrange("p (h t) -> p h t", h=4)
                    for h in range(4):
                        S.tt("pool", qTh[h % 2][:], qkTs[:, h, :].unsqueeze(1).to_broadcast([128, NS, n]), smb[:], ALU.mult,
                             r=["qkTs", "smb"], w=[f"qTh{h % 2}"])
                        for s_ in range(NS):
                            S.mm(po13[:, h, :], qTh[h % 2][:, s_, :], S_b[:, s_ * 4 + h, :], s_ == 0, s_ == NS - 1,
                                 r=[f"qTh{h % 2}", "S_b"], w=[po1k], inc=(s_ == NS - 1))
                    po2, po2k = S.psf()
                    po23 = po2[0:n, :].rearrange("p (h t) -> p h t", h=4)
                    for h in range(4):
                        S.mm(po23[:, h, :], ATs[:, h, :], vns[:, h, :], True, True, r=["ATs", "vns"], w=[po2k], inc=(h == 3))
                    S.tt("dve", o1ss[:], po13, egc_.unsqueeze(2).to_broadcast([n, 4, 128]), ALU.mult, r=[po1k, "s_egc"], w=["o1ss"])
                    S.tt("dve", ogs[:], po23, o1ss[:], ALU.add, r=[po2k, "o1ss"], w=["ogs"])
                    stop("sF")
                    S_f4 = S_f[:].rearrange("p (s h) v -> p s h v", h=4)
                    abc3 = abc[:].rearrange("p (s h) -> p s h", h=4)
                    for half in range(2):
                        S.dma("sp", S_f[:], sg_d[half * 8:(half + 1) * 8].rearrange("s h k v -> k (s h) v"), w=["S_f"])
                        for h in range(4):
                            vh, vhk = vnh[h % 2], f"vnh{h % 2}"
                            S.tt("dve", vh[:], vns[:, h, :].unsqueeze(1).to_broadcast([n, NS, 128]),
                                 rowm[:].unsqueeze(2).to_broadcast([n, NS, 128]), ALU.mult, r=["vns", "rowm"], w=[vhk])
                            for sg_ in range(2):
                                pS, pSk = S.psf()
                                pS3 = pS[:].rearrange("p (s t) -> p s t", s=4)
                                for sl in range(4):
                                    s_ = half * 8 + sg_ * 4 + sl
                                    S.mm(pS3[:, sl, :], kdcs[:, h, :], vh[:, s_, :], True, True, r=["kdcs", vhk], w=[pSk],
                                         inc=(sl == 3))
                                stt_, sttk = Stm[sg_ % 2], f"Stm{sg_ % 2}"
                                sl0 = sg_ * 4
                                s0 = half * 8 + sg_ * 4
                                S.tt("pool", stt_[:], S_f4[:, sl0:sl0 + 4, h, :],
                                     abc3[:, s0:s0 + 4, h].unsqueeze(2).to_broadcast([128, 4, 128]), ALU.mult,
                                     r=["S_f", "abc"], w=[sttk])
                                S.tt("dve", S_f4[:, sl0:sl0 + 4, h, :], pS3, stt_[:], ALU.add, r=[pSk, sttk], w=["S_f"])
                        S.dma("sp", ng_s[half * 8:(half + 1) * 8].rearrange("s h k v -> k (s h) v"), S_f[:], r=["S_f"], slot="ng_s")
                    S.tt("pool", osqs[:], ogs[:], ogs[:], ALU.mult, r=["ogs"], w=["osqs"])
                    S.op("dve", lambda E: E.tensor_reduce(out=ssqs[:], in_=osqs[:], axis=AX.X, op=ALU.add), r=["osqs"], w=["ssqs"])
                    S.act(ssqs[:], ssqs[:], AF.Ln, r=["ssqs"], w=["ssqs"], bias=NORM_EPS, scale=1.0 / 128.0)
                    S.act(ssqs[:], ssqs[:], AF.Exp, r=["ssqs"], w=["ssqs"], scale=-0.5)
                    S.act(zss[:], tz[:], AF.Silu, r=["tz_s"], w=["zss"])
                    S.tt("pool", zss[:].rearrange("p (h d) -> p h d", h=4), zss[:].rearrange("p (h d) -> p h d", h=4),
                         gnw[0:n, :].unsqueeze(1).to_broadcast([n, 4, 128]), ALU.mult, r=["zss"], w=["zss"])
                    S.tt("dve", ogs[:], ogs[:], ssqs[:].unsqueeze(2).to_broadcast([n, 4, 128]), ALU.mult, r=["ogs", "ssqs"], w=["ogs"])
                    S.tt("dve", mix_all[0:n, NT, 512:1024].rearrange("p (h d) -> p h d", h=4), ogs[:],
                         zss[:].rearrange("p (h d) -> p h d", h=4), ALU.mult, r=["ogs", "zss"], w=["mix16"])
                    S.barrier()

                with ExitStack() as es:
                    ST = 256
                    xb = [sb(es, f"xb{i}", [128, D], BF16) for i in range(2)]
                    xT = [sb(es, f"xT{i}", [128, 8, ST], BF16) for i in range(2)]
                    gT = sb(es, "gT", [128, 12, ST + 3])
                    cacc = [sb(es, f"cacc{i}", [128, ST]) for i in range(2)]
                    sT = sb(es, "sT", [128, 8, ST])
                    qkT2 = [sb(es, f"qkT_{i}", [128, 8, ST], BF16) for i in range(2)]
                    vT2 = [sb(es, f"vT_{i}", [128, 4, ST], BF16) for i in range(2)]
                    sq4 = sb(es, "sq4", [128, 4, ST], BF16)
                    ln4 = sb(es, "ln4", [128, 4, ST])
                    knv = sb(es, "knv", [128, 8, 128], BF16)
                    tm_qkv = sb(es, "tm_qkv", [128, 768])
                    zs4 = [sb(es, f"zs2_{i}", [128, 512]) for i in range(4)]
                    tm_ab = sb(es, "tm_ab", [128, 8])
                    rot = sb(es, "rot", [128, 10, 16])
                    rtmp = sb(es, "rtmp", [128, 4, 10, 8])
                    q_bf = sb(es, "q_bf", [128, 4, 2, 64], BF16)
                    k_bf = sb(es, "k_bf", [128, 2, 64], BF16)
                    k_rot = sb(es, "k_rot", [128, 2, 64])
                    v_aug = [sb(es, f"v_aug{i}", [128, 2, 65], BF16) for i in range(2)]
                    kTb = [sb(es, f"kTb{i}", [128, 128], BF16) for i in range(2)]
                    qT = sb(es, "qT", [128, 4, 128], BF16)
                    PTe = [sb(es, f"PTe{i}", [128, 4, 128], BF16) for i in range(2)]
                    den = sb(es, "den", [128, 2, 4])
                    gab = sb(es, "gab", [128, 8, 4])
                    gU = sb(es, "gU", [128, 4, 128])
                    eD = sb(es, "eD", [128, 4, 128])
                    eDT = sb(es, "eDT", [128, 4, 128])
                    ATb = sb(es, "ATb", [128, 4, 128], BF16)
                    Pm = [sb(es, f"Pm{i}", [128, 4, 128]) for i in range(2)]
                    PmT = [sb(es, f"PmT{i}", [128, 4, 128]) for i in range(2)]
                    Qm = [sb(es, f"Qm{i}", [128, 4, 128]) for i in range(2)]
                    Qb = sb(es, "Qb", [128, 4, 128], BF16)
                    kbe = sb(es, "kbe", [128, 4, 128], BF16)
                    kdc = sb(es, "kdc", [128, 4, 128], BF16)
                    vb = sb(es, "vb", [128, 4, 128], BF16)
                    nwT = sb(es, "nwT", [128, 4, 128], BF16)
                    vnew = sb(es, "vnew", [128, 4, 128], BF16)
                    Sf = sb(es, "Sf", [128, 4, 128])
                    Sb = sb(es, "Sb", [128, 4, 128], BF16)
                    Stmp = sb(es, "Stmp", [128, 4, 128])
                    o1s = sb(es, "o1s", [128, 4, 128])
                    og = sb(es, "og", [128, 4, 128])
                    osq = sb(es, "osq", [128, 4, 128])
                    ssq = sb(es, "ssq", [128, 4])

                    S.op("pool", lambda E: E.memset(gT[:, :, 0:3], 0.0), w=[f"gT{c}" for c in range(12)])
                    S.op("pool", lambda E: E.memset(Sf[:], 0.0), w=["Sf"])
                    S.op("pool", lambda E: E.memset(Sb[:], 0.0), w=["Sb"])
                    for i in range(2):
                        S.op("pool", lambda E, i=i: E.memset(v_aug[i][:], 1.0), w=[f"v_aug{i}"])

                    GT = [f"gT{c}" for c in range(12)]
                    def fm(st):
                        qkT, vT, zs2 = qkT2[st % 2], vT2[st % 2], zs4[(st % 2) * 2:(st % 2) * 2 + 2]
                        PAR = st % 2
                        xTs = xT[st % 2]
                        xTk = f"xT{st % 2}"
                        for j in range(ST // 128):
                            i = st * (ST // 128) + j
                            xbt = xb[i % 2]
                            xbk = f"xb{i % 2}"
                            S.dma("pool", xbt[:], x_p[i * 128:(i + 1) * 128, :], w=[xbk])
                            pb, pbk = S.psb()
                            for kc in range(8):
                                S.tr(pb[:, kc * 128:(kc + 1) * 128], xbt[:, kc * 128:(kc + 1) * 128], idb[:],
                                     r=[xbk, "idb"], w=[pbk], inc=(kc == 7))
                            S.cp("act", xTs[:, :, j * 128:(j + 1) * 128], pb[:].rearrange("p (k t) -> p k t", k=8),
                                 r=[pbk], w=[xTk])
                        for c in range(12):
                            pf, pfk = S.psf()
                            for kc in range(8):
                                S.mm(pf[:, 0:ST], w_in_bf[:, kc, 768 + c * 128:768 + (c + 1) * 128], xTs[:, kc, :],
                                     kc == 0, kc == 7, r=[WIN[kc], xTk], w=[pfk])
                            S.cp("act" if c % 2 else "dve", gT[:, c, 3:3 + ST], pf[:, 0:ST], r=[pfk], w=[GT[c]])
                            acc = cacc[c % 2]
                            ak = f"cacc{c % 2}"
                            ce = "dve"
                            S.ts(ce, acc[:], gT[:, c, 0:ST], cw[:, 0, c:c + 1], None, ALU.mult, None, r=[GT[c], "cw"], w=[ak])
                            for k in range(1, 4):
                                S.stt(ce, acc[:], gT[:, c, k:k + ST], cw[:, k, c:c + 1], acc[:], ALU.mult, ALU.add,
                                      r=[GT[c], "cw", ak], w=[ak])
                            if c < 8:
                                S.act(sT[:, c, :], acc[:], AF.Silu, r=[ak], w=[f"sT{c}"])
                            else:
                                S.act(vT[:, c - 8, :], acc[:], AF.Silu, r=[ak], w=[f"vT_{PAR}"])
                        for j in range(ST // 128):
                            pf, pfk = S.psf()
                            for kc in range(8):
                                S.mm(pf[:, :], xTs[:, kc, j * 128:(j + 1) * 128], w_in_bf[:, kc, 2304:2816], kc == 0, kc == 7,
                                     r=[WIN[kc], xTk], w=[pfk])
                            S.act(zs2[j][:], pf[:, :], AF.Silu, r=[pfk], w=[f"zs2_{PAR}_{j}"])
                            S.tt("pool", zs2[j][:].rearrange("p (h d) -> p h d", h=4), zs2[j][:].rearrange("p (h d) -> p h d", h=4),
                                 gnw[:].unsqueeze(1).to_broadcast([128, 4, 128]), ALU.mult, r=[f"zs2_{PAR}_{j}", "gnw"], w=[f"zs2_{PAR}_{j}"])
                        if st == SEQ // ST - 1:
                            for t in range(3):
                                S.dma("sp", nc_p[t].rearrange("(c p) -> p c", p=128), gT[:, :, ST + t], r=GT, w=[],
                                      slot="nc_p")
                        else:
                            S.cp("pool", gT[:, :, 0:3], gT[:, :, ST:ST + 3], r=GT, w=GT)
                        for half in range(2):
                            for hh in range(4):
                                c = half * 4 + hh
                                S.act(sq4[:, hh, :], sT[:, c, :], AF.Square, r=[f"sT{c}"], w=[f"sq{hh}"])
                            pfs = []
                            for hh in range(4):
                                pf, pfk = S.psf()
                                S.mm(pf[:, 0:ST], ones_bf[:], sq4[:, hh, :], True, True, r=["ones_bf", f"sq{hh}"], w=[pfk])
                                pfs.append((pf, pfk))
                            for hh in range(4):
                                pf, pfk = pfs[hh]
                                S.act(ln4[:, hh, :], pf[:, 0:ST], AF.Ln, r=[pfk], w=[f"ln{hh}"], bias=L2_EPS)
                            for hh in range(4):
                                bq = -0.5 * math.log(128.0) if half == 0 else 0.0
                                S.act(ln4[:, hh, :], ln4[:, hh, :], AF.Exp, r=[f"ln{hh}"], w=[f"ln{hh}"], bias=bq, scale=-0.5)
                            for hh in range(4):
                                c = half * 4 + hh
                                S.tt("pool" if hh % 2 else "dve", qkT[:, c, :], sT[:, c, :], ln4[:, hh, :], ALU.mult,
                                     r=[f"sT{c}", f"ln{hh}"], w=[f"qkT{c}_{PAR}"])
                        if st == 0:
                            stop("st0a")

                    def tile(st, j):
                        qkT, vT, zs2 = qkT2[st % 2], vT2[st % 2], zs4[(st % 2) * 2:(st % 2) * 2 + 2]
                        PAR = st % 2
                        xTs = xT[st % 2]
                        xTk = f"xT{st % 2}"
                        i = st * (ST // 128) + j
                        tok = slice(j * 128, (j + 1) * 128)
                        pb, pbk = S.psb()
                        for h in range(4):
                            S.tr(pb[:, h * 128:(h + 1) * 128], qkT[:, 4 + h, tok], idb[:], r=[f"qkT{4 + h}_{PAR}", "idb"],
                                 w=[pbk], inc=False)
                        for h in range(4):
                            S.tr(pb[:, (4 + h) * 128:(5 + h) * 128], vT[:, h, tok], idb[:], r=[f"vT_{PAR}", "idb"], w=[pbk],
                                 inc=(h == 3))
                        S.cp("act", knv[:], pb[:].rearrange("p (k t) -> p k t", k=8), r=[pbk], w=["knv"])
                        if i == 0:
                            stop("t0a")
                        for (c0, c1, dst, dk_) in ((0, 512, tm_qkv[:, 0:512], "tm_q"), (512, 768, tm_qkv[:, 512:768], "tm_kv"),
                                                   (2816, 2824, tm_ab[:], "tm_ab")):
                            pf, pfk = S.psf()
                            for kc in range(8):
                                S.mm(pf[:, 0:c1 - c0], xTs[:, kc, tok], w_in_bf[:, kc, c0:c1], kc == 0, kc == 7,
                                     r=[WIN[kc], xTk], w=[pfk])
                            S.cp("act" if dk_ in ("tm_q", "tm_z") else "dve", dst, pf[:, 0:c1 - c0], r=[pfk], w=[dk_])
                        if i == 0:
                            stop("t0b")
                        qk3 = tm_qkv[:, 0:640].rearrange("p (h d) -> p h d", d=64)
                        cb = cosT[:, i, :].unsqueeze(1).to_broadcast([128, 10, 8])
                        sbb = sinT[:, i, :].unsqueeze(1).to_broadcast([128, 10, 8])
                        RK = ["tm_q", "tm_kv", "cosT", "sinT"]
                        S.tt("dve", rtmp[:, 0], qk3[:, :, 0:8], cb, ALU.mult, r=RK, w=["rt0"])
                        S.tt("dve", rtmp[:, 1], qk3[:, :, 8:16], sbb, ALU.mult, r=RK, w=["rt1"])
                        S.tt("dve", rtmp[:, 2], qk3[:, :, 8:16], cb, ALU.mult, r=RK, w=["rt2"])
                        S.tt("dve", rtmp[:, 3], qk3[:, :, 0:8], sbb, ALU.mult, r=RK, w=["rt3"])
                        S.tt("dve", rot[:, :, 0:8], rtmp[:, 0], rtmp[:, 1], ALU.subtract, r=["rt0", "rt1"], w=["rot"])
                        S.tt("dve", rot[:, :, 8:16], rtmp[:, 2], rtmp[:, 3], ALU.add, r=["rt2", "rt3"], w=["rot"])
                        q4 = tm_qkv[:, 0:512].rearrange("p (j c d) -> p c j d", j=2, c=4)
                        S.cp("dve", q_bf[:, :, :, 0:16], rot[:, 0:8, :].rearrange("p (j c) d -> p c j d", j=2),
                             r=["rot"], w=["q_bf"])
                        S.cp("act", q_bf[:, :, :, 16:64], q4[:, :, :, 16:64], r=["tm_q"], w=["q_bf"])
                        k3 = tm_qkv[:, 512:640].rearrange("p (j d) -> p j d", j=2)
                        S.cp("dve", k_bf[:, :, 0:16], rot[:, 8:10, :], r=["rot"], w=["k_bf"])
                        S.cp("dve", k_bf[:, :, 16:64], k3[:, :, 16:64], r=["tm_kv"], w=["k_bf"])
                        va = v_aug[i % 2]
                        vak = f"v_aug{i % 2}"
                        S.cp("pool", va[:, :, 0:64], tm_qkv[:, 640:768].rearrange("p (j d) -> p j d", j=2), r=["tm_kv"],
                             w=[vak])
                        if i == 0:
                            stop("t0c")
                        if i == NT - 1:
                            S.cp("dve", k_rot[:, :, 0:16], rot[:, 8:10, :], r=["rot"], w=["k_rot"])
                            S.cp("dve", k_rot[:, :, 16:64], k3[:, :, 16:64], r=["tm_kv"], w=["k_rot"])
                            S.dma("sp", nk_p, k_rot[:].rearrange("p j d -> p (j d)"), r=["k_rot"], slot="nk_p")
                            S.dma("sp", nv_p, tm_qkv[:, 640:768], r=["tm_kv"], slot="nv_p")
                        pb, pbk = S.psb()
                        for c in range(4):
                            S.tr(pb[:, c * 128:(c + 1) * 128], q_bf[:, c].rearrange("p j d -> p (j d)"), idb[:],
                                 r=["q_bf", "idb"], w=[pbk], inc=False)
                        S.tr(pb[:, 512:640], k_bf[:].rearrange("p j d -> p (j d)"), idb[:], r=["k_bf", "idb"], w=[pbk])
                        kTc = kTb[i % 2]
                        kTk = f"kTb{i % 2}"
                        S.cp("act", qT[:], pb[:, 0:512].rearrange("p (c t) -> p c t", c=4), r=[pbk], w=["qT"])
                        S.cp("dve", kTc[:], pb[:, 512:640], r=[pbk], w=[kTk])
                        if i == 0:
                            stop("t0d")
                        for jk in range(2):
                            blocks = ([] if i == 0 else [(kTb[(i - 1) % 2], f"kTb{(i - 1) % 2}", mb_prev, "mb_prev",
                                                          v_aug[(i - 1) % 2], f"v_aug{(i - 1) % 2}")])
                            blocks.append((kTc, kTk, mb_cur, "mb_cur", va, vak))
                            for bi, (kt, ktk, mk, mkk, _, _) in enumerate(blocks):
                                pf, pfk = S.psf()
                                S.mm(pf[:], kt[jk * 64:(jk + 1) * 64, :],
                                     qT[jk * 64:(jk + 1) * 64, :, :].rearrange("p c t -> p (c t)"), True, True,
                                     r=[ktk, "qT"], w=[pfk])
                                S.act(PTe[bi][:].rearrange("p c t -> p (c t)"), pf[:], AF.Exp, r=[pfk], w=[f"PTe{bi}"],
                                      scale=0.125)
                                S.tt("dve", PTe[bi][:], PTe[bi][:], mk[:].unsqueeze(1).to_broadcast([128, 4, 128]),
                                     ALU.mult, r=[f"PTe{bi}", mkk], w=[f"PTe{bi}"])
                                if i == 0:
                                    stop("t0e")
                            po, pok = S.psf()
                            po3 = po[:, 0:260].rearrange("p (c d) -> p c d", c=4)
                            for c in range(4):
                                for bi, (_, _, _, _, vv, vvk) in enumerate(blocks):
                                    S.mm(po3[:, c, :], PTe[bi][:, c, :], vv[:, jk, :], bi == 0, bi == len(blocks) - 1,
                                         r=[f"PTe{bi}", vvk], w=[pok], inc=(c == 3 and bi == len(blocks) - 1))
                            S.tt("dve", den[:, jk, :], po3[:, :, 64], esink[:, jk * 4:(jk + 1) * 4], ALU.add,
                                 r=[pok, "esink"], w=["den"])
                            S.op("dve", lambda E, jk=jk: E.reciprocal(out=den[:, jk, :], in_=den[:, jk, :]), r=["den"],
                                 w=["den"])
                            S.tt("dve", mix_all[:, i, jk * 256:(jk + 1) * 256].rearrange("p (c d) -> p c d", c=4),
                                 po3[:, :, 0:64], den[:, jk, :].unsqueeze(2).to_broadcast([128, 4, 64]), ALU.mult,
                                 r=[pok, "den"], w=[f"mix{i}"])
                        if i == 0:
                            stop("t0attn")
                        if i == 1:
                            stop("t1attn")
                        g_ = gab[:, 0, :]
                        beta_ = gab[:, 1, :]
                        gc_ = gab[:, 2, :]
                        egc_ = gab[:, 3, :]
                        kds_ = gab[:, 4, :]
                        atot_ = gab[:, 5, :]
                        tmp_ = gab[:, 6, :]
                        nbeta_ = gab[:, 7, :]
                        S.tt("dve", tmp_, tm_ab[:, 0:4], dtb[:], ALU.add, r=["tm_ab", "dtb"], w=["g_tmp"])
                        S.act(tmp_, tmp_, AF.Exp, r=["g_tmp"], w=["g_tmp"])
                        S.act(tmp_, tmp_, AF.Ln, r=["g_tmp"], w=["g_tmp"], bias=1.0)
                        S.tt("dve", g_, tmp_, nexpA[:], ALU.mult, r=["g_tmp", "nexpA"], w=["g_g"])
                        S.act(beta_, tm_ab[:, 4:8], AF.Exp, r=["tm_ab"], w=["g_beta"], scale=-1.0)
                        S.ts("dve", beta_, beta_, 1.0, None, ALU.add, None, r=["g_beta"], w=["g_beta"])
                        S.op("dve", lambda E: E.reciprocal(out=beta_, in_=beta_), r=["g_beta"], w=["g_beta"])
                        S.ts("dve", nbeta_, beta_, -1.0, None, ALU.mult, None, r=["g_beta"], w=["g_nbeta"])
                        pf, pfk = S.psf()
                        S.mm(pf[:, 0:4], m_up[:], g_, True, True, r=["m_up", "g_g"], w=[pfk])
                        S.mm(pf[:, 4:8], onesf[:], g_, True, True, r=["onesf", "g_g"], w=[pfk])
                        S.cp("dve", gc_, pf[:, 0:4], r=[pfk], w=["g_gc"])
                        S.act(egc_, pf[:, 0:4], AF.Exp, r=[pfk], w=["g_egc"])
                        S.act(atot_, pf[:, 4:8], AF.Exp, r=[pfk], w=["g_atot"])
                        S.tt("dve", kds_, pf[:, 4:8], gc_, ALU.subtract, r=[pfk, "g_gc"], w=["g_kds"])
                        S.act(kds_, kds_, AF.Exp, r=["g_kds"], w=["g_kds"])
                        pD, pDk = S.psf()
                        pDT, pDTk = S.psf()
                        pG, pGk = S.psf()
                        pA, pAk = S.psf()
                        pD3 = pD[:].rearrange("p (h t) -> p h t", h=4)
                        pDT3 = pDT[:].rearrange("p (h t) -> p h t", h=4)
                        pG3 = pG[:].rearrange("p (h t) -> p h t", h=4)
                        pA3 = pA[:].rearrange("p (h t) -> p h t", h=4)
                        for h in range(4):
                            if h % 2:
                                S.act(gU[:, h, :], m_up[:], AF.Copy, r=["m_up", "g_g"], w=[f"gU{h}"], scale=gab[:, 0, h:h + 1])
                            else:
                                S.ts("dve", gU[:, h, :], m_up[:], gab[:, 0, h:h + 1], None, ALU.mult, None,
                                     r=["m_up", "g_g"], w=[f"gU{h}"])
                        for h in range(4):
                            S.mm(pD3[:, h, :], gU[:, h, :], m_strict[:], True, True, r=[f"gU{h}", "m_strict"], w=[pDk],
                                 inc=(h == 3))
                        for h in range(4):
                            S.mm(pDT3[:, h, :], m_strict[:], gU[:, h, :], True, True, r=[f"gU{h}", "m_strict"], w=[pDTk],
                                 inc=(h == 3))
                        for h in range(4):
                            S.mm(pG3[:, h, :], qkT[:, 4 + h, tok], qkT[:, 4 + h, tok], True, True, r=[f"qkT{4 + h}_{PAR}"],
                                 w=[pGk], inc=(h == 3))
                        for h in range(4):
                            S.mm(pA3[:, h, :], qkT[:, 4 + h, tok], qkT[:, h, tok], True, True,
                                 r=[f"qkT{4 + h}_{PAR}", f"qkT{h}_{PAR}"], w=[pAk], inc=(h == 3))
                        S.act(eD[:], pD3, AF.Exp, r=[pDk], w=["eD"])
                        S.tt("dve", eD[:], eD[:], m_strict[:].unsqueeze(1).to_broadcast([128, 4, 128]), ALU.mult,
                             r=["eD", "m_strict"], w=["eD"])
                        S.act(eDT[:], pDT3, AF.Exp, r=[pDTk], w=["eDT"])
                        S.tt("pool", eDT[:], eDT[:], m_up[:].unsqueeze(1).to_broadcast([128, 4, 128]), ALU.mult,
                             r=["eDT", "m_up"], w=["eDT"])
                        P0 = Pm[0]
                        for h in range(4):
                            S.stt("dve", P0[:, h, :], pG3[:, h, :], gab[:, 7, h:h + 1], eD[:, h, :], ALU.mult, ALU.mult,
                                  r=[pGk, "g_nbeta", "eD"], w=[f"Pm0g{h // 2}"])
                        S.tt("dve", ATb[:], pA3, eDT[:], ALU.mult, r=[pAk, "eDT"], w=["ATb"])
                        pt, ptk = S.psf()
                        pt3 = pt[:].rearrange("p (h t) -> p h t", h=4)
                        for h in range(4):
                            S.tr(pt3[:, h, :], P0[:, h, :], idf[:], r=[f"Pm0g{h // 2}", "idf"], w=[ptk], inc=(h == 3))
                        S.cp("act", PmT[0][:], pt3, r=[ptk], w=["PmT0g0", "PmT0g1"])
                        S.tt("dve", Qm[0][:], pt3, idf[:].unsqueeze(1).to_broadcast([128, 4, 128]), ALU.add,
                             r=[ptk, "idf"], w=["Qm0g0", "Qm0g1"])
                        NIT = 6
                        GR = ((0, 2), (2, 4))
                        for k in range(NIT):
                            a, b = k % 2, (k + 1) % 2
                            last = (k == NIT - 1)
                            pPs, pPTs = [], []
                            for gi, (h0, h1) in enumerate(GR):
                                pP, pPk = S.psf()
                                pP3 = pP[:, 0:256].rearrange("p (h t) -> p h t", h=2)
                                for h in range(h0, h1):
                                    S.mm(pP3[:, h - h0, :], PmT[a][:, h, :], Pm[a][:, h, :], True, True,
                                         r=[f"PmT{a}g{gi}", f"Pm{a}g{gi}"], w=[pPk], inc=(h == h1 - 1))
                                pPs.append((pP3, pPk))
                                if not last:
                                    pPT, pPTk = S.psf()
                                    pPT3 = pPT[:, 0:256].rearrange("p (h t) -> p h t", h=2)
                                    for h in range(h0, h1):
                                        S.mm(pPT3[:, h - h0, :], Pm[a][:, h, :], PmT[a][:, h, :], True, True,
                                             r=[f"PmT{a}g{gi}", f"Pm{a}g{gi}"], w=[pPTk], inc=(h == h1 - 1))
                                    pPTs.append((pPT3, pPTk))
                            for gi, (h0, h1) in enumerate(GR):
                                S.cp("act", Pm[b][:, h0:h1, :], pPs[gi][0], r=[pPs[gi][1]], w=[f"Pm{b}g{gi}"])
                                if not last:
                                    S.cp("dve", PmT[b][:, h0:h1, :], pPTs[gi][0], r=[pPTs[gi][1]], w=[f"PmT{b}g{gi}"])
                            pQs = []
                            for gi, (h0, h1) in enumerate(GR):
                                pQ, pQk = S.psf()
                                pQ3 = pQ[:, 0:256].rearrange("p (h t) -> p h t", h=2)
                                for h in range(h0, h1):
                                    S.mm(pQ3[:, h - h0, :], Pm[b][:, h, :], Qm[a][:, h, :], True, True,
                                         r=[f"Pm{b}g{gi}", f"Qm{a}g{gi}"], w=[pQk], inc=(h == h1 - 1))
                                pQs.append((pQ3, pQk))
                            for gi, (h0, h1) in enumerate(GR):
                                if not last:
                                    S.tt("dve", Qm[b][:, h0:h1, :], pQs[gi][0], Qm[a][:, h0:h1, :], ALU.add,
                                         r=[pQs[gi][1], f"Qm{a}g{gi}"], w=[f"Qm{b}g{gi}"])
                                else:
                                    S.tt("dve", Qb[:, h0:h1, :], pQs[gi][0], Qm[a][:, h0:h1, :], ALU.add,
                                         r=[pQs[gi][1], f"Qm{a}g{gi}"], w=["Qb"])
                        kn3 = knv[:, 0:4, :]
                        v3 = knv[:, 4:8, :]
                        S.tt("dve", tmp_, beta_, egc_, ALU.mult, r=["g_beta", "g_egc", "g_tmp"], w=["g_tmp"])
                        S.tt("pool", kbe[:], kn3, tmp_.unsqueeze(2).to_broadcast([128, 4, 128]), ALU.mult,
                             r=["knv", "g_tmp"], w=["kbe"])
                        S.tt("pool", kdc[:], kn3, kds_.unsqueeze(2).to_broadcast([128, 4, 128]), ALU.mult,
                             r=["knv", "g_kds"], w=["kdc"])
                        S.tt("dve", vb[:], v3, beta_.unsqueeze(2).to_broadcast([128, 4, 128]), ALU.mult,
                             r=["knv", "g_beta"], w=["vb"])
                        pw, pwk = S.psf()
                        pw3 = pw[:].rearrange("p (h t) -> p h t", h=4)
                        for h in range(4):
                            S.mm(pw3[:, h, :], kbe[:, h, :], Qb[:, h, :], True, True, r=["kbe", "Qb"], w=[pwk],
                                 inc=(h == 3))
                        S.op("act", lambda E: E.mul(out=nwT[:], in_=pw3, mul=-1.0), r=[pwk], w=["nwT"])
                        pv, pvk = S.psf()
                        pv3 = pv[:].rearrange("p (h t) -> p h t", h=4)
                        for h in range(4):
                            S.mm(pv3[:, h, :], Qb[:, h, :], vb[:, h, :], True, False, r=["Qb", "vb"], w=[pvk], inc=False)
                            S.mm(pv3[:, h, :], nwT[:, h, :], Sb[:, h, :], False, True, r=["nwT", "Sb"], w=[pvk],
                                 inc=(h == 3))
                        S.cp("act", vnew[:], pv3, r=[pvk], w=["vnew"])
                        po1, po1k = S.psf()
                        po13 = po1[:].rearrange("p (h t) -> p h t", h=4)
                        for h in range(4):
                            S.mm(po13[:, h, :], qkT[:, h, tok], Sb[:, h, :], True, True, r=[f"qkT{h}_{PAR}", "Sb"], w=[po1k],
                                 inc=(h == 3))
                        po2, po2k = S.psf()
                        po23 = po2[:].rearrange("p (h t) -> p h t", h=4)
                        for h in range(4):
                            S.mm(po23[:, h, :], ATb[:, h, :], vnew[:, h, :], True, True, r=["ATb", "vnew"], w=[po2k],
                                 inc=(h == 3))
                        pS, pSk = S.psf()
                        pS3 = pS[:].rearrange("p (h t) -> p h t", h=4)
                        for h in range(4):
                            S.mm(pS3[:, h, :], kdc[:, h, :], vnew[:, h, :], True, True, r=["kdc", "vnew"], w=[pSk],
                                 inc=(h == 3))
                        S.tt("dve", o1s[:], po13, egc_.unsqueeze(2).to_broadcast([128, 4, 128]), ALU.mult,
                             r=[po1k, "g_egc"], w=["o1s"])
                        S.tt("dve", og[:], po23, o1s[:], ALU.add, r=[po2k, "o1s"], w=["og"])
                        S.tt("pool", Stmp[:], Sf[:], atot_.unsqueeze(2).to_broadcast([128, 4, 128]), ALU.mult,
                             r=["Sf", "g_atot"], w=["Stmp"])
                        S.tt("dve", Sf[:], pS3, Stmp[:], ALU.add, r=[pSk, "Stmp"], w=["Sf"])
                        S.cp("act", Sb[:], Sf[:], r=["Sf"], w=["Sb"])
                        S.tt("dve", osq[:], og[:], og[:], ALU.mult, r=["og"], w=["osq"])
                        S.op("dve", lambda E: E.tensor_reduce(out=ssq[:], in_=osq[:], axis=AX.X, op=ALU.add), r=["osq"],
                             w=["ssq"])
                        S.act(ssq[:], ssq[:], AF.Ln, r=["ssq"], w=["ssq"], bias=NORM_EPS, scale=1.0 / 128.0)
                        S.act(ssq[:], ssq[:], AF.Exp, r=["ssq"], w=["ssq"], scale=-0.5)
                        S.tt("dve", og[:], og[:], ssq[:].unsqueeze(2).to_broadcast([128, 4, 128]), ALU.mult,
                             r=["og", "ssq"], w=["og"])
                        S.tt("dve", mix_all[:, i, 512:1024].rearrange("p (h d) -> p h d", h=4), og[:],
                             zs2[j][:].rearrange("p (h d) -> p h d", h=4), ALU.mult, r=["og", f"zs2_{PAR}_{j}"], w=[f"mix{i}"])
                        if i == 0:
                            stop("t0")
                        if i == 1:
                            stop("t1")
                        if i == 3:
                            stop("t3")

                    NST = SEQ // ST
                    fm(0)
                    for st in range(NST):
                        tile(st, 0)
                        if st + 1 < NST:
                            fm(st + 1)
                        tile(st, 1)
                    S.dma("sp", ng_p.rearrange("h k v -> k h v"), Sf[:], r=["Sf"], slot="ng_p")
                    if dbg:
                        dump("mix", mix_all[:, 0:NT, :], [128, NT, D], [f"mix{i}" for i in range(NT)])
                    S.barrier()


            y_acc = sb(es0, "y_acc", [128, NT + 1, D])
            with ExitStack() as es:
                w_out_bf = sb(es, "w_out_bf", [128, 8, D], BF16)
                g1 = sb(es, "g1", [128, D])
                b1 = sb(es, "b1", [128, D])
                mixT = [sb(es, f"mixT{i}", [128, 8, 128], BF16) for i in range(2)]
                xf = [sb(es, f"xf{i}", [128, D]) for i in range(2)]
                tb = [sb(es, f"tb{i}", [128, D]) for i in range(3)]
                stats = [sb(es, f"stats{i}", [128, 2, 6]) for i in range(3)]
                mv = [sb(es, f"mv{i}", [128, 2]) for i in range(3)]
                S.dma("pool", w_out_bf[:], w_out.rearrange("(c p) n -> p c n", p=128), w=["w_out"])
                S.dma("sp", g1[:], ln1g_d.partition_broadcast(128), w=["g1"])
                S.dma("sp", b1[:], ln1b_d.partition_broadcast(128), w=["b1"])
                def ln1_A(i):
                    n = 128 if i < NT else NS * TS
                    xsrc = x_p[i * 128:(i + 1) * 128, :] if i < NT else x_s
                    xft, xfk = xf[i % 2], f"xf{i % 2}"
                    S.dma("sp", xft[:n, :], xsrc, w=[xfk])
                    mt, mtk = mixT[i % 2], f"mixT{i % 2}"
                    pb, pbk = S.psb()
                    for c in range(8):
                        S.tr(pb[:, c * 128:c * 128 + n], mix_all[:n, i, c * 128:(c + 1) * 128], idb[:n, :n],
                             r=["idb"], w=[pbk], inc=(c == 7))
                    S.cp("act", mt[:, :, 0:n], pb[:].rearrange("p (c t) -> p c t", c=8)[:, :, 0:n], r=[pbk], w=[mtk])
                    tbt, tbk = tb[i % 3], f"tb{i % 3}"
                    st_, mv_, mvk = stats[i % 3], mv[i % 3], f"mv{i % 3}"
                    for half in range(2):
                        pf, pfk = S.psf()
                        for c in range(8):
                            S.mm(pf[:n, :], mt[:, c, 0:n], w_out_bf[:, c, half * 512:(half + 1) * 512], c == 0, c == 7,
                                 r=[mtk, "w_out"], w=[pfk])
                        S.stt("dve", tbt[:n, half * 512:(half + 1) * 512], xft[:n, half * 512:(half + 1) * 512], ALPHA,
                              pf[:n, :], ALU.mult, ALU.add, r=[xfk, pfk], w=[tbk])
                        S.op("dve", lambda E, half=half: E.bn_stats(out=st_[:n, half, :],
                                                                    in_=tbt[:n, half * 512:(half + 1) * 512]),
                             r=[tbk], w=[mvk])
                    S.op("dve", lambda E: E.bn_aggr(out=mv_[:n, :], in_=st_[:n].rearrange("p a b -> p (a b)")),
                         r=[mvk], w=[mvk])
                    S.act(mv_[:n, 1:2], mv_[:n, 1:2], AF.Ln, r=[mvk], w=[mvk], bias=NORM_EPS)
                    S.act(mv_[:n, 1:2], mv_[:n, 1:2], AF.Exp, r=[mvk], w=[mvk], scale=-0.5)

                def ln1_B(i):
                    n = 128 if i < NT else NS * TS
                    tbt, tbk = tb[i % 3], f"tb{i % 3}"
                    mv_, mvk = mv[i % 3], f"mv{i % 3}"
                    S.stt("dve", mv_[:n, 0:1], mv_[:n, 0:1], -1.0, mv_[:n, 1:2], ALU.mult, ALU.mult, r=[mvk], w=[mvk])
                    S.act(tbt[:n, :], tbt[:n, :], AF.Identity, r=[tbk, mvk], w=[tbk], bias=mv_[:n, 0:1], scale=mv_[:n, 1:2])
                    S.tt("dve", tbt[:n, :], tbt[:n, :], g1[:n, :], ALU.mult, r=[tbk, "g1"], w=[tbk])
                    S.tt("pool", y_acc[:n, i, :], tbt[:n, :], b1[:n, :], ALU.add, r=[tbk, "b1"], w=[f"y{i}"])

                for i in range(NT + 1):
                    ln1_A(i)
                    if i >= 1:
                        ln1_B(i - 1)
                ln1_B(NT)
                if dbg:
                    dump("x1", y_acc[:], [128, NT + 1, D], [f"y{i}" for i in range(NT + 1)])
                S.barrier()

            with ExitStack() as es:
                x1T = mix_all[:].rearrange("p a b -> p (a b)")[:, 0:8 * NTOK].rearrange("p (k t) -> p k t", k=8)
                comb = sb(es, "comb", [128, NT + 1, NE])
                wr_sb = sb(es, "wr_sb", [128, 8, 36])
                x1Tf = sb(es, "x1Tf", [128, 8, 128])
                T_ = NT + 1
                rl_all = sb(es, "rl_all", [128, T_, 36])
                r_oh = sb(es, "r_oh", [128, T_, 4])
                r_t4 = sb(es, "r_t4", [128, T_, 4])
                r_pr = sb(es, "r_pr", [128, T_, 4, 8])
                r_es = sb(es, "r_es", [128, T_, 8])
                r_m1 = sb(es, "r_m1", [128, T_, 8])
                r_e2 = sb(es, "r_e2", [128, T_, 8])
                r_m2 = sb(es, "r_m2", [128, T_, 8])
                r_ew = sb(es, "r_ew", [128, T_, 8])
                r_s = sb(es, "r_s", [128, 8, T_])
                S.op("pool", lambda E: E.memset(rl_all[:], 0.0), w=["rl_all"])
                S.dma("sp", wr_sb[:], wr_d.rearrange("(c p) n -> p c n", p=128), w=["wr"])
                YK = [f"y{i}" for i in range(NT + 1)]
                for i in range(NT + 1):
                    n = 128 if i < NT else NS * TS
                    t0 = i * 128
                    pfa, pfak = S.psf()
                    pfb, pfbk = S.psf()
                    for kc in range(8):
                        pf, pfk = (pfa, pfak) if kc < 4 else (pfb, pfbk)
                        S.tr(pf[:, (kc % 4) * 128:(kc % 4) * 128 + n], y_acc[:n, i, kc * 128:(kc + 1) * 128], idf[:n, :n],
                             r=[YK[i], "idf"], w=[pfk], inc=(kc % 4 == 3))
                    for hf, (pf, pfk) in enumerate(((pfa, pfak), (pfb, pfbk))):
                        src = pf[:].rearrange("p (k t) -> p k t", k=4)[:, :, 0:n]
                        S.cp("act", x1Tf[:, hf * 4:(hf + 1) * 4, 0:n], src, r=[pfk], w=["x1Tf"])
                        S.cp("dve", x1T[:, hf * 4:(hf + 1) * 4, t0:t0 + n], src, r=[pfk], w=["x1T"])
                    pr, prk = S.psf()
                    for kc in range(8):
                        S.mm(pr[:n, 0:36], x1Tf[:, kc, 0:n], wr_sb[:, kc, :], kc == 0, kc == 7, r=["x1Tf", "wr"], w=[prk])
                    S.cp("dve", rl_all[:n, i, :], pr[:n, 0:36], r=[prk], w=["rl_all"])
                    S.op("act", lambda E, n=n, i=i: E.mul(out=y_acc[:n, i, :], in_=y_acc[:n, i, :], mul=ALPHA),
                         r=[YK[i], "x1T", "x1Tf"], w=[YK[i]])
                R = ["rl_all", "rr"]
                gl = rl_all[:, :, 0:4]
                el = rl_all[:, :, 4:36].rearrange("p t (g e) -> p t g e", g=4)
                bc3 = lambda a, k: a.unsqueeze(2).to_broadcast([128, T_, k])
                gmax, gtp, m1, m2, ex, w1, w2 = (r_s[:, j, :] for j in range(7))
                S.op("dve", lambda E: E.tensor_reduce(out=gmax, in_=gl, axis=AX.X, op=ALU.max), r=R, w=R)
                S.tt("dve", r_oh[:], gl, bc3(gmax, 4), ALU.is_equal, r=R, w=R)
                S.tt("dve", r_t4[:], gl, bc3(gmax, 4), ALU.subtract, r=R, w=R)
                S.act(r_t4[:], r_t4[:], AF.Exp, r=R, w=R)
                S.op("dve", lambda E: E.tensor_reduce(out=gtp, in_=r_t4[:], axis=AX.X, op=ALU.add), r=R, w=R)
                S.op("dve", lambda E: E.reciprocal(out=gtp, in_=gtp), r=R, w=R)
                S.tt("dve", r_pr[:], el, r_oh[:].unsqueeze(3).to_broadcast([128, T_, 4, 8]), ALU.mult, r=R, w=R)
                S.op("dve", lambda E: E.tensor_reduce(out=r_es[:], in_=r_pr[:].rearrange("p t g e -> p t e g"), axis=AX.X,
                                                      op=ALU.add), r=R, w=R)
                S.op("dve", lambda E: E.tensor_reduce(out=m1, in_=r_es[:], axis=AX.X, op=ALU.max), r=R, w=R)
                S.tt("dve", r_m1[:], r_es[:], bc3(m1, 8), ALU.is_equal, r=R, w=R)
                S.stt("dve", r_e2[:], r_m1[:], -1e30, r_es[:], ALU.mult, ALU.add, r=R, w=R)
                S.op("dve", lambda E: E.tensor_reduce(out=m2, in_=r_e2[:], axis=AX.X, op=ALU.max), r=R, w=R)
                S.tt("dve", r_m2[:], r_e2[:], bc3(m2, 8), ALU.is_equal, r=R, w=R)
                S.tt("dve", ex, m2, m1, ALU.subtract, r=R, w=R)
                S.act(ex, ex, AF.Exp, r=R, w=R)
                S.ts("dve", w1, ex, 1.0, None, ALU.add, None, r=R, w=R)
                S.op("dve", lambda E: E.reciprocal(out=w1, in_=w1), r=R, w=R)
                S.tt("dve", w2, ex, w1, ALU.mult, r=R, w=R)
                S.tt("dve", w1, w1, gtp, ALU.mult, r=R, w=R)
                S.tt("dve", w2, w2, gtp, ALU.mult, r=R, w=R)
                S.tt("dve", r_ew[:], r_m1[:], bc3(w1, 8), ALU.mult, r=R, w=R)
                S.tt("dve", r_m2[:], r_m2[:], bc3(w2, 8), ALU.mult, r=R, w=R)
                S.tt("dve", r_ew[:], r_ew[:], r_m2[:], ALU.add, r=R, w=R)
                S.tt("dve", comb[:].rearrange("p t (g e) -> p t g e", g=4), r_oh[:].unsqueeze(3).to_broadcast([128, T_, 4, 8]),
                     r_ew[:].unsqueeze(2).to_broadcast([128, T_, 4, 8]), ALU.mult, r=R, w=["comb"])
                if dbg:
                    dump("comb", comb[:], [128, NT + 1, NE], ["comb"])

                wg = [sb(es, f"wg{i}", [128, 8, 256], BF16) for i in range(2)]
                wu = [sb(es, f"wu{i}", [128, 8, 256], BF16) for i in range(2)]
                wd = [sb(es, f"wd{i}", [128, 2, D], BF16) for i in range(2)]
                hT = [sb(es, f"hT{i}", [128, 2, NTOK], BF16) for i in range(2)]
                sgt = [sb(es, f"sgt{i}", [128, 512]) for i in range(2)]
                spans = [(t, min(512, NTOK - t)) for t in range(0, NTOK, 512)]
                for pbt in S.psb_list:
                    S.psf_list.append(pbt[:].bitcast(F32))
                acct = [sb(es, f"acct{i}", [128, 512]) for i in range(6)]
                g2 = sb(es, "g2", [128, D])
                b2 = sb(es, "b2", [128, D])
                ob = [sb(es, f"ob{i}", [128, D]) for i in range(3)]
                stats2 = [sb(es, f"stats2_{i}", [128, 2, 6]) for i in range(3)]
                mv2 = [sb(es, f"mv2_{i}", [128, 2]) for i in range(3)]
                S.dma("sp", g2[:], ln2g_d.partition_broadcast(128), w=["g2"])
                S.dma("sp", b2[:], ln2b_d.partition_broadcast(128), w=["b2"])
                acc_i = 0

                def ln2_A(i):
                    n = 128 if i < NT else NS * TS
                    st2, mvt, mk_ = stats2[i % 3], mv2[i % 3], f"mv2_{i % 3}"
                    for half in range(2):
                        S.op("dve", lambda E, half=half: E.bn_stats(out=st2[:n, half, :],
                                                                    in_=y_acc[:n, i, half * 512:(half + 1) * 512]),
                             r=[YK[i]], w=[mk_])
                    S.op("dve", lambda E: E.bn_aggr(out=mvt[:n, :], in_=st2[:n].rearrange("p a b -> p (a b)")),
                         r=[mk_], w=[mk_])
                    S.act(mvt[:n, 1:2], mvt[:n, 1:2], AF.Ln, r=[mk_], w=[mk_], bias=NORM_EPS)
                    S.act(mvt[:n, 1:2], mvt[:n, 1:2], AF.Exp, r=[mk_], w=[mk_], scale=-0.5)

                def ln2_B(i):
                    n = 128 if i < NT else NS * TS
                    mvt, mk_ = mv2[i % 3], f"mv2_{i % 3}"
                    obt, obk = ob[i % 3], f"ob{i % 3}"
                    S.stt("dve", mvt[:n, 0:1], mvt[:n, 0:1], -1.0, mvt[:n, 1:2], ALU.mult, ALU.mult, r=[mk_], w=[mk_])
                    S.act(obt[:n, :], y_acc[:n, i, :], AF.Identity, r=[YK[i], mk_], w=[obk], bias=mvt[:n, 0:1],
                          scale=mvt[:n, 1:2])
                    S.tt("dve", obt[:n, :], obt[:n, :], g2[:n, :], ALU.mult, r=[obk, "g2"], w=[obk])
                    S.tt("pool", obt[:n, :], obt[:n, :], b2[:n, :], ALU.add, r=[obk, "b2"], w=[obk])
                    dst = y_p[i * 128:(i + 1) * 128, :] if i < NT else y_s
                    S.dma("sp", dst, obt[:n, :], r=[obk], slot=obk + "o")

                def load_expert(e):
                    b = e % 2
                    S.dma("pool", wg[b][:], wg_d[e].rearrange("(c p) f -> p c f", p=128), w=[f"wg{b}"])
                    S.dma("pool", wu[b][:], wu_d[e].rearrange("(c p) f -> p c f", p=128), w=[f"wu{b}"])
                    S.dma("pool", wd[b][:], wd_d[e].rearrange("(c p) n -> p c n", p=128), w=[f"wd{b}"])

                load_expert(0)
                for e in range(NE):
                    b = e % 2
                    if e + 1 < NE:
                        load_expert(e + 1)
                    hk = f"hT{b}"
                    si = 0
                    for fc in range(2):
                        for (t0, tn) in spans:
                            pg, pgk = S.psf()
                            pu, puk = S.psf()
                            for kc in range(8):
                                S.mm(pg[:, 0:tn], wg[b][:, kc, fc * 128:(fc + 1) * 128], x1T[:, kc, t0:t0 + tn], kc == 0,
                                     kc == 7, r=[f"wg{b}", "x1T"], w=[pgk])
                            for kc in range(8):
                                S.mm(pu[:, 0:tn], wu[b][:, kc, fc * 128:(fc + 1) * 128], x1T[:, kc, t0:t0 + tn], kc == 0,
                                     kc == 7, r=[f"wu{b}", "x1T"], w=[puk])
                            sg_, sgk = sgt[si % 2], f"sgt{si % 2}"
                            si += 1
                            S.act(sg_[:, 0:tn], pg[:, 0:tn], AF.Silu, r=[pgk], w=[sgk])
                            S.tt("dve", hT[b][:, fc, t0:t0 + tn], pu[:, 0:tn], sg_[:, 0:tn], ALU.mult, r=[puk, sgk], w=[hk])
                    for i in range(NT + 1):
                        n = 128 if i < NT else NS * TS
                        t0 = i * 128
                        for half in range(2):
                            py, pyk = S.psf()
                            for fc in range(2):
                                S.mm(py[:n, :], hT[b][:, fc, t0:t0 + n], wd[b][:, fc, half * 512:(half + 1) * 512], fc == 0,
                                     fc == 1, r=[hk, f"wd{b}"], w=[pyk])
                            ysl = y_acc[:n, i, half * 512:(half + 1) * 512]
                            if (i * 2 + half) % 2 == 1 and e < NE - 1:
                                at, atk = acct[acc_i % 6], f"acct{acc_i % 6}"
                                acc_i += 1
                                S.act(at[:n, :], py[:n, :], AF.Copy, r=[pyk, "comb"], w=[atk], scale=comb[:n, i, e:e + 1])
                                S.tt("pool", ysl, ysl, at[:n, :], ALU.add, r=[atk, YK[i]], w=[YK[i]])
                            else:
                                S.stt("dve", ysl, py[:n, :], comb[:n, i, e:e + 1], ysl, ALU.mult, ALU.add,
                                      r=[pyk, "comb", YK[i]], w=[YK[i]])
                        if e == NE - 1:
                            if i >= 1:
                                ln2_A(i - 1)
                            if i >= 2:
                                ln2_B(i - 2)
                ln2_A(NT)
                ln2_B(NT - 1)
                ln2_B(NT)
        except _Stop:
            pass
        S.final_wait()
    return nc, dbg_outs


_CACHE = {}


def _get_nc(dbg=False):
    if dbg not in _CACHE:
        _CACHE[dbg] = build(dbg)
    return _CACHE[dbg]


def make_in_maps(inputs):
    f = lambda a: np.ascontiguousarray(np.asarray(a, dtype=np.float32))
    g = {k: f(v) for k, v in inputs.items()}
    wr = np.ascontiguousarray(np.concatenate([g["w_router_group"][0], g["w_router_expert"][0]], axis=1))
    maps = []
    for c in range(NCORES):
        s0, s1 = c * NS, (c + 1) * NS
        maps.append({
            "x_p": g["x_prompt"][c],
            "x_s": np.ascontiguousarray(g["x_sample"][s0:s1].reshape(NS * TS, D)),
            "ck": np.ascontiguousarray(g["cache_attn_k"][0, s0:s1].reshape(NS, 128, 128)),
            "cv": np.ascontiguousarray(g["cache_attn_v"][0, s0:s1].reshape(NS, 128, 128)),
            "sg": np.ascontiguousarray(g["state_gdn"][0, s0:s1]),
            "sc": np.ascontiguousarray(g["state_conv"][0, s0:s1].reshape(NS * 3, 1536)),
            "w_in": g["w_in"][0], "w_out": g["w_out"][0], "sinks": g["attn_sinks"][0], "conv_w": g["conv_w"][0],
            "a_log": g["a_log"][0], "dt_bias": g["dt_bias"][0], "gnw": g["gdn_norm_w"][0],
            "ln1_g": g["ln1_g"][0], "ln1_b": g["ln1_b"][0], "w_r": wr,
            "w_gate": g["w_gate"][0], "w_up": g["w_up"][0], "w_down": g["w_down"][0],
            "ln2_g": g["ln2_g"][0], "ln2_b": g["ln2_b"][0],
        })
    return maps


def assemble(results):
    cat = lambda k: np.stack([np.asarray(r[k]) for r in results])
    y_p = cat("y_p")
    y_s = cat("y_s").reshape(128, TS, D)
    nk_p = cat("nk_p").reshape(1, 8, 128, 2, 64)
    nv_p = cat("nv_p").reshape(1, 8, 128, 2, 64)
    ng_p = cat("ng_p").reshape(1, 8, 4, 128, 128)
    nc_p = cat("nc_p").reshape(1, 8, 3, 1536)
    nk_s = cat("nk_s").reshape(1, 128, 128, 2, 64)
    nv_s = cat("nv_s").reshape(1, 128, 128, 2, 64)
    ng_s = cat("ng_s").reshape(1, 128, 4, 128, 128)
    nc_s = cat("nc_s").reshape(1, 128, 3, 1536)
    return tuple(np.ascontiguousarray(a.astype(np.float32)) for a in
                 (y_p, y_s, nk_p, nv_p, ng_p, nc_p, nk_s, nv_s, ng_s, nc_s))


def kernel(**inputs):
    nc, _ = _get_nc(False)
    maps = make_in_maps(inputs)
    res = run_bass_kernel_spmd(nc, maps, core_ids=list(range(NCORES)))
    return assemble(res.results)
```

```python
import math
from contextlib import ExitStack

import numpy as np
import concourse.bass as bass
import concourse.mybir as mybir
from concourse.bass_utils import run_bass_kernel_spmd

F32 = mybir.dt.float32
BF16 = mybir.dt.bfloat16
I32 = mybir.dt.int32
AF = mybir.ActivationFunctionType
ALU = mybir.AluOpType
AX = mybir.AxisListType

NCORES = 8
D = 1024
SEQ = 2048
NT = 16
NS = 16
TS = 4
NTOK = SEQ + NS * TS
PAST = 8192
INC = 2824
ALPHA = 2.0 ** 0.25
NORM_EPS = 1e-5
L2_EPS = 1e-6
THETA = 500000.0
NE = 32
MAGIC = 12582912.0


class _Stop(Exception):
    pass


import os
STOP = os.environ.get("K_STOP", "")


_SCHED = []


def stop(tag):
    if STOP == tag:
        _SCHED[-1].dead = True


class Sched:
    def __init__(self, nc, es):
        self.nc = nc
        self.es = es
        self.E = {"pe": nc.tensor, "act": nc.scalar, "dve": nc.vector, "pool": nc.gpsimd, "sp": nc.sync}
        self.semh = {}
        for k in self.E:
            self.semh["e_" + k] = es.enter_context(nc.semaphore("sem_" + k))
        self.cnt = {k: 0 for k in self.E}
        self.seen = {k: {} for k in self.E}
        self.lastw = {}
        self.readers = {}
        self.pend = {k: [] for k in self.E}
        self.pend_r = {k: set() for k in self.E}
        self.pend_w = {k: set() for k in self.E}
        self.slots = {}
        self.psf_list = []
        self.psb_list = []
        self.psf_i = 0
        self.psb_i = 0
        self.dead = False
        _SCHED.append(self)

    def _waits(self, e, r, w, is_dma):
        need = {}

        def add(ev):
            semk, val, eng = ev
            if need.get(semk, 0) < val:
                need[semk] = val

        for k in list(r) + list(w):
            for e2 in self.E:
                if e2 != e or is_dma:
                    assert k not in self.pend_w[e2], f"key {k} pending write on {e2}"
        for k in w:
            for e2 in self.E:
                if e2 != e or is_dma:
                    assert k not in self.pend_r[e2], f"key {k} pending read on {e2}"
        for k in r:
            ev = self.lastw.get(k)
            if ev is not None:
                if ev[2] == e and e == "pe" and not is_dma:
                    continue
                add(ev)
            if k.startswith("ps"):
                for ev in self.readers.get(k, ()):
                    if ev[2] != e:
                        add(ev)
        for k in w:
            ev = self.lastw.get(k)
            if ev is not None and (is_dma or ev[2] != e or e != "pe"):
                add(ev)
            for ev in self.readers.get(k, ()):
                if is_dma or ev[2] != e or e != "pe":
                    add(ev)
        for semk, val in need.items():
            if self.seen[e].get(semk, 0) < val:
                self.E[e].wait_ge(self.semh[semk], val)
                self.seen[e][semk] = val

    def _register(self, r, w, ev):
        for k in r:
            self.readers.setdefault(k, []).append(ev)
        for k in w:
            self.lastw[k] = ev
            self.readers[k] = []

    def op(self, e, fn, r=(), w=(), inc=True):
        if self.dead:
            return None
        self._waits(e, r, w, False)
        ins = fn(self.E[e])
        if not inc:
            self.pend[e].append((tuple(r), tuple(w)))
            self.pend_r[e].update(r)
            self.pend_w[e].update(w)
            return ins
        self.cnt[e] += 1
        ins.then_inc(self.semh["e_" + e], 1)
        ev = ("e_" + e, self.cnt[e], e)
        for (pr, pw) in self.pend[e]:
            self._register(pr, pw, ev)
        self.pend[e] = []
        self.pend_r[e] = set()
        self.pend_w[e] = set()
        self._register(r, w, ev)
        return ins

    def dma(self, q, out, in_, r=(), w=(), slot=None):
        if slot is None:
            slot = w[0] if w else r[0]
        if self.dead:
            return None
        sk = "d_" + slot
        if sk not in self.slots:
            self.semh[sk] = self.es.enter_context(self.nc.semaphore(sk))
            self.slots[sk] = 0
        self._waits(q, r, w, True)
        ins = self.E[q].dma_start(out=out, in_=in_)
        self.slots[sk] += 16
        ins.then_inc(self.semh[sk], 16)
        ev = (sk, self.slots[sk], "dma")
        self._register(r, w, ev)
        return ins

    def barrier(self):
        if self.dead:
            return
        for e in self.E:
            assert not self.pend[e]
        for e in self.E:
            for e2 in self.E:
                if self.cnt[e2] > self.seen[e].get("e_" + e2, 0):
                    self.E[e].wait_ge(self.semh["e_" + e2], self.cnt[e2])
                    self.seen[e]["e_" + e2] = self.cnt[e2]
            for sk, v in self.slots.items():
                if v > self.seen[e].get(sk, 0):
                    self.E[e].wait_ge(self.semh[sk], v)
                    self.seen[e][sk] = v
        self.lastw = {}
        self.readers = {}

    def final_wait(self):
        self.dead = False
        for sk, v in self.slots.items():
            if v > self.seen["sp"].get(sk, 0):
                self.E["sp"].wait_ge(self.semh[sk], v)
                self.seen["sp"][sk] = v
        for e2 in self.E:
            if e2 != "sp" and self.cnt[e2] > self.seen["sp"].get("e_" + e2, 0):
                self.E["sp"].wait_ge(self.semh["e_" + e2], self.cnt[e2])

    def psf(self):
        i = self.psf_i % len(self.psf_list)
        self.psf_i += 1
        return self.psf_list[i], f"psf{i}"

    def psb(self):
        i = self.psb_i % len(self.psb_list)
        self.psb_i += 1
        return self.psb_list[i], f"psb{i}"

    def mm(self, out, lhsT, rhs, start, stop, r, w, inc=None):
        if inc is None:
            inc = stop
        return self.op("pe", lambda E: E.matmul(out, lhsT=lhsT, rhs=rhs, start=start, stop=stop), r, w, inc)

    def tr(self, out, in_, ident, r, w, inc=True):
        return self.op("pe", lambda E: E.transpose(out=out, in_=in_, identity=ident), r, w, inc)

    def act(self, out, in_, func, r, w, bias=0.0, scale=1.0, accum_out=None):
        if accum_out is not None:
            return self.op("act", lambda E: E.activation(out=out, in_=in_, func=func, bias=bias, scale=scale,
                                                         accum_out=accum_out), r, w)
        return self.op("act", lambda E: E.activation(out=out, in_=in_, func=func, bias=bias, scale=scale), r, w)

    def tt(self, e, out, in0, in1, op, r, w):
        return self.op(e, lambda E: E.tensor_tensor(out=out, in0=in0, in1=in1, op=op), r, w)

    def ts(self, e, out, in0, s1, s2, op0, op1, r, w):
        if s2 is None:
            return self.op(e, lambda E: E.tensor_scalar(out=out, in0=in0, scalar1=s1, scalar2=None, op0=op0), r, w)
        return self.op(e, lambda E: E.tensor_scalar(out=out, in0=in0, scalar1=s1, scalar2=s2, op0=op0, op1=op1), r, w)

    def stt(self, e, out, in0, scalar, in1, op0, op1, r, w):
        return self.op(e, lambda E: E.scalar_tensor_tensor(out=out, in0=in0, scalar=scalar, in1=in1, op0=op0, op1=op1),
                       r, w)

    def cp(self, e, out, in_, r, w):
        if e == "act":
            return self.op("act", lambda E: E.copy(out=out, in_=in_), r, w)
        return self.op(e, lambda E: E.tensor_copy(out=out, in_=in_), r, w)


def build(dbg=False):
    nc = bass.Bass("TRN2", target_bir_lowering=False)

    def din(name, shape, dt=F32):
        return nc.dram_tensor(name, shape, dt, kind="ExternalInput").ap()

    def dout(name, shape):
        return nc.dram_tensor(name, shape, F32, kind="ExternalOutput").ap()

    x_p = din("x_p", [SEQ, D])
    x_s = din("x_s", [NS * TS, D])
    ck_d = din("ck", [NS, 128, 128])
    cv_d = din("cv", [NS, 128, 128])
    sg_d = din("sg", [NS, 4, 128, 128])
    sc_d = din("sc", [NS * 3, 1536])
    w_in = din("w_in", [D, INC])
    w_out = din("w_out", [D, D])
    sinks_d = din("sinks", [8])
    convw_d = din("conv_w", [4, 1536])
    alog_d = din("a_log", [4])
    dtb_d = din("dt_bias", [4])
    gnw_d = din("gnw", [128])
    ln1g_d = din("ln1_g", [D])
    ln1b_d = din("ln1_b", [D])
    wr_d = din("w_r", [D, 36])
    wg_d = din("w_gate", [NE, D, 256])
    wu_d = din("w_up", [NE, D, 256])
    wd_d = din("w_down", [NE, 256, D])
    ln2g_d = din("ln2_g", [D])
    ln2b_d = din("ln2_b", [D])

    y_p = dout("y_p", [SEQ, D])
    y_s = dout("y_s", [NS * TS, D])
    nk_p = dout("nk_p", [128, 128])
    nv_p = dout("nv_p", [128, 128])
    ng_p = dout("ng_p", [4, 128, 128])
    nc_p = dout("nc_p", [3, 1536])
    nk_s = dout("nk_s", [NS, 128, 128])
    nv_s = dout("nv_s", [NS, 128, 128])
    ng_s = dout("ng_s", [NS, 4, 128, 128])
    nc_s = dout("nc_s", [NS * 3, 1536])
    dbg_outs = {}

    with ExitStack() as es0, nc.allow_non_contiguous_dma(reason="small transposed param loads"):
        S = Sched(nc, es0)
        try:

            def sb(es, name, shape, dt=F32):
                return es.enter_context(nc.sbuf_tensor(name, shape, dt))

            for i in range(6):
                S.psf_list.append(es0.enter_context(nc.psum_tensor(f"psf{i}", [128, 512], F32)))
            for i in range(2):
                S.psb_list.append(es0.enter_context(nc.psum_tensor(f"psb{i}", [128, 1024], BF16)))

            def dump(name, ap_src, shape, keys):
                if not dbg:
                    return
                o = dout("dbg_" + name, shape)
                dbg_outs[name] = o
                S.dma("pool" if ap_src.dtype != F32 else "sp", o, ap_src, r=keys, w=[], slot="dbg_" + name)

            onesf = sb(es0, "onesf", [128, 128])
            ones_bf = sb(es0, "ones_bf", [128, 128], BF16)
            idf = sb(es0, "idf", [128, 128])
            idb = sb(es0, "idb", [128, 128], BF16)
            m_strict = sb(es0, "m_strict", [128, 128])
            m_up = sb(es0, "m_up", [128, 128])
            mb_prev = sb(es0, "mb_prev", [128, 128], BF16)
            mb_cur = sb(es0, "mb_cur", [128, 128], BF16)
            S.op("pool", lambda E: E.memset(onesf[:], 1.0), w=["onesf"])
            S.op("pool", lambda E: E.memset(ones_bf[:], 1.0), w=["ones_bf"])
            S.op("pool", lambda E: E.affine_select(out=idf[:], in_=onesf[:], pattern=[[-1, 128]], compare_op=ALU.is_equal,
                                                   fill=0.0, base=0, channel_multiplier=1), r=["onesf"], w=["idf"])
            S.op("pool", lambda E: E.affine_select(out=m_strict[:], in_=onesf[:], pattern=[[-1, 128]], compare_op=ALU.is_gt,
                                                   fill=0.0, base=0, channel_multiplier=1), r=["onesf"], w=["m_strict"])
            S.op("pool", lambda E: E.affine_select(out=m_up[:], in_=onesf[:], pattern=[[1, 128]], compare_op=ALU.is_ge,
                                                   fill=0.0, base=0, channel_multiplier=-1), r=["onesf"], w=["m_up"])
            S.cp("dve", idb[:], idf[:], r=["idf"], w=["idb"])
            S.cp("dve", mb_prev[:], m_strict[:], r=["m_strict"], w=["mb_prev"])
            S.cp("dve", mb_cur[:], m_up[:], r=["m_up"], w=["mb_cur"])

            esink = sb(es0, "esink", [128, 8])
            nexpA = sb(es0, "nexpA", [128, 4])
            dtb = sb(es0, "dtb", [128, 4])
            gnw = sb(es0, "gnw_bc", [128, 128])
            cw = sb(es0, "cw", [128, 4, 12])
            S.dma("sp", esink[:], sinks_d.partition_broadcast(128), w=["esink"])
            S.dma("sp", nexpA[:], alog_d.partition_broadcast(128), w=["nexpA"])
            S.dma("sp", dtb[:], dtb_d.partition_broadcast(128), w=["dtb"])
            S.dma("sp", gnw[:], gnw_d.partition_broadcast(128), w=["gnw"])
            for k in range(4):
                S.dma("sp", cw[:, k, :], convw_d[k].rearrange("(c p) -> p c", p=128), w=["cw"])
            S.act(esink[:], esink[:], AF.Exp, r=["esink"], w=["esink"])
            S.act(nexpA[:], nexpA[:], AF.Exp, r=["nexpA"], w=["nexpA"])
            S.op("act", lambda E: E.mul(out=nexpA[:], in_=nexpA[:], mul=-1.0), r=["nexpA"], w=["nexpA"])

            cosT = sb(es0, "cosT", [128, NT + 1, 8])
            sinT = sb(es0, "sinT", [128, NT + 1, 8])
            with ExitStack() as esr:
                posi = sb(esr, "posi", [128, NT + 1], I32)
                pos1 = sb(esr, "pos1", [128, 1], I32)
                posf = sb(esr, "posf", [128, NT + 1])
                ang = sb(esr, "ang", [128, NT + 1, 8])
                kk = sb(esr, "kk", [128, NT + 1, 8])
                anl = sb(esr, "anl", [128, NT + 1, 8])
                S.op("pool", lambda E: E.iota(posi[:], pattern=[[128, NT + 1]], base=0, channel_multiplier=1), w=["posi"])
                S.op("pool", lambda E: E.iota(pos1[:], pattern=[[0, 1]], base=0, channel_multiplier=1), w=["pos1"])
                S.op("dve", lambda E: E.tensor_single_scalar(out=pos1[:], in_=pos1[:], scalar=3, op=ALU.bitwise_and),
                     r=["pos1"], w=["pos1"])
                S.cp("dve", posf[:], posi[:], r=["posi"], w=["posf"])
                S.cp("dve", posf[:, NT:NT + 1], pos1[:], r=["pos1", "posf"], w=["posf"])
                S.ts("dve", posf[:, NT:NT + 1], posf[:, NT:NT + 1], float(PAST), None, ALU.add, None, r=["posf"], w=["posf"])
                for j in range(8):
                    f = THETA ** (-(2.0 * j) / 16.0)
                    m_, e_ = math.frexp(f)
                    f_hi = math.ldexp(round(m_ * 1024.0) / 1024.0, e_)
                    f_lo = f - f_hi
                    S.ts("dve", ang[:, :, j], posf[:], float(f_hi), None, ALU.mult, None, r=["posf"], w=["ang"])
                    S.ts("dve", anl[:, :, j], posf[:], float(f_lo), None, ALU.mult, None, r=["posf"], w=["anl"])
                S.tt("dve", kk[:], ang[:], anl[:], ALU.add, r=["ang", "anl"], w=["kk"])
                S.ts("dve", kk[:], kk[:], float(1.0 / (2 * math.pi)), MAGIC, ALU.mult, ALU.add, r=["kk"], w=["kk"])
                S.ts("dve", kk[:], kk[:], MAGIC, None, ALU.subtract, None, r=["kk"], w=["kk"])
                C1 = 6.28125
                C2 = 0.00193548202514648
                C3 = 2 * math.pi - C1 - C2
                for cc in (C1, C2, C3):
                    S.stt("dve", ang[:], kk[:], float(-cc), ang[:], ALU.mult, ALU.add, r=["kk", "ang"], w=["ang"])
                S.tt("dve", ang[:], ang[:], anl[:], ALU.add, r=["ang", "anl"], w=["ang"])
                PI_S = 3.1415925
                S.ts("dve", ang[:], ang[:], -PI_S, PI_S, ALU.max, ALU.min, r=["ang"], w=["ang"])
                S.act(sinT[:], ang[:], AF.Sin, r=["ang"], w=["sinT"])
                S.stt("dve", ang[:], ang[:], -1.0, ang[:], ALU.mult, ALU.max, r=["ang"], w=["ang"])
                S.ts("dve", ang[:], ang[:], -1.0, float(math.pi / 2), ALU.mult, ALU.add, r=["ang"], w=["ang"])
                S.act(cosT[:], ang[:], AF.Sin, r=["ang"], w=["cosT"])
                S.barrier()
            stop("p0")

            mix_all = sb(es0, "mix_all", [128, NT + 1, D], BF16)

            with ExitStack() as es1:
                w_in_bf = sb(es1, "w_in_bf", [128, 8, INC], BF16)
                for kc in range(8):
                    S.dma("pool", w_in_bf[:, kc, :], w_in[kc * 128:(kc + 1) * 128, :], w=[f"w_in{kc}"])
                WIN = [f"w_in{kc}" for kc in range(8)]
                S.op("pool", lambda E: E.memset(mix_all[:, NT, :], 0.0), w=["mix16"])

                with ExitStack() as es:
                    n = NS * TS
                    xTs = sb(es, "xTs", [128, 8, n], BF16)
                    S_b = sb(es, "S_b", [128, NS * 4, 128], BF16)
                    qkTs = sb(es, "qkTs", [128, 8, n], BF16)
                    vTs = sb(es, "vTs", [128, 4, n], BF16)
                    knvs = sb(es, "knvs", [n, 8, 128], BF16)
                    tqkv = sb(es, "tqkv", [n, 768])
                    tz = sb(es, "tz", [n, 512])
                    tab = sb(es, "tab", [n, 8])
                    blk = sb(es, "blk", [n, n])
                    rowm = sb(es, "rowm", [n, NS])
                    U_s = sb(es, "U_s", [n, n])
                    L_s = sb(es, "L_s", [n, n])
                    U_sb = sb(es, "U_sb", [n, n], BF16)
                    smb = sb(es, "smb", [128, NS, n], BF16)
                    nsmb = sb(es, "nsmb", [128, NS, n], BF16)
                    S_f = sb(es, "S_f", [128, 32, 128])
                    esA = ExitStack()
                    xbs = sb(esA, "xbs", [n, D], BF16)
                    cK = sb(esA, "cK", [128, NS, 128], BF16)
                    cVa = sb(esA, "cVa", [128, NS, 2, 65], BF16)
                    cKT = sb(esA, "cKT", [128, NS, 128], BF16)
                    scs = sb(esA, "scs", [NS * 3, 1536])
                    ext = sb(esA, "ext", [128, 12, NS, 7])
                    cprod = sb(esA, "cprod", [128, 12, NS, 4])
                    cacs = sb(esA, "cacs", [128, 12, NS, 4])
                    ncsf = sb(esA, "ncsf", [128, 12, NS * 3])
                    sTs = sb(esA, "sTs", [128, 8, n])
                    sq8 = sb(esA, "sq8", [128, 8, n], BF16)
                    ln8 = sb(esA, "ln8", [128, 8, n])
                    rots = sb(esA, "rots", [n, 10, 16])
                    rtm = sb(esA, "rtm", [n, 4, 10, 8])
                    qbs = sb(esA, "qbs", [n, 4, 2, 64], BF16)
                    kbs = sb(esA, "kbs", [n, 2, 64], BF16)
                    krs = sb(esA, "krs", [n, 2, 64])
                    vna = sb(esA, "vna", [n, 2, 65], BF16)
                    qTs = sb(esA, "qTs", [128, 4, n], BF16)
                    qTsr = sb(esA, "qTsr", [128, NS, 16], BF16)
                    kTn = sb(esA, "kTn", [128, n], BF16)
                    PTs = [sb(esA, f"PTs{i}", [128, 512], BF16) for i in range(2)]
                    mk16 = sb(esA, "mk16", [128, NS, n], BF16)
                    PTn = sb(esA, "PTn", [n, 512], BF16)
                    mc4 = sb(esA, "mc4", [128, 4], BF16)
                    oc = sb(esA, "oc", [65, 512])
                    ot = sb(esA, "ot", [65, 512])
                    dn = sb(esA, "dn", [65, 512])
                    oTb = sb(esA, "oTb", [64, 8, n], BF16)
                    ii = sb(esA, "ii", [n, 64], I32)
                    ip = sb(esA, "ip", [n, 1], I32)
                    colid = sb(esA, "colid", [n, 64])
                    rowid = sb(esA, "rowid", [n, 1])
                    sidx = sb(esA, "sidx", [n, NS])
                    smf = sb(esA, "smf", [128, NS, n])
                    S.dma("pool", xbs[:], x_s, w=["xbs"])
                    S.dma("pool", S_b[:], sg_d.rearrange("s h k v -> k (s h) v"), w=["S_b"])
                    S.dma("pool", cK[:], ck_d.rearrange("s r c -> r s c"), w=["cK"])
                    S.op("pool", lambda E: E.memset(cVa[:], 1.0), w=["cVa"])
                    for jk in range(2):
                        S.dma("pool", cVa[:, :, jk, 0:64], cv_d[:, :, jk * 64:(jk + 1) * 64].rearrange("s r d -> r s d"),
                              w=["cVa"], slot=f"cVa{jk}")
                    S.dma("sp", scs[:], sc_d, w=["scs"])
                    S.op("pool", lambda E: E.memset(vna[:], 1.0), w=["vna"])
                    S.dma("sp", nk_s[:, 0:124, :], ck_d[:, 4:128, :], slot="nk_s0")
                    S.dma("sp", nv_s[:, 0:124, :], cv_d[:, 4:128, :], slot="nv_s0")
                    S.op("pool", lambda E: E.iota(ii[:], pattern=[[1, 64]], base=0, channel_multiplier=0), w=["ii"])
                    S.op("pool", lambda E: E.iota(ip[:], pattern=[[0, 1]], base=0, channel_multiplier=1), w=["ip"])
                    S.op("dve", lambda E: E.tensor_single_scalar(out=ii[:], in_=ii[:], scalar=2, op=ALU.arith_shift_right),
                         r=["ii"], w=["ii"])
                    S.op("dve", lambda E: E.tensor_single_scalar(out=ip[:], in_=ip[:], scalar=2, op=ALU.arith_shift_right),
                         r=["ip"], w=["ip"])
                    S.cp("dve", colid[:], ii[:], r=["ii"], w=["colid"])
                    S.cp("dve", rowid[:], ip[:], r=["ip"], w=["rowid"])
                    S.ts("dve", blk[:], colid[:], rowid[:, 0:1], None, ALU.is_equal, None, r=["colid", "rowid"], w=["blk"])
                    S.op("pool", lambda E: E.iota(ii[:, 0:NS], pattern=[[1, NS]], base=0, channel_multiplier=0), r=["colid"], w=["ii"])
                    S.cp("dve", sidx[:], ii[:, 0:NS], r=["ii"], w=["sidx"])
                    S.ts("dve", rowm[:], sidx[:], rowid[:, 0:1], None, ALU.is_equal, None, r=["sidx", "rowid"], w=["rowm"])
                    S.tt("dve", U_s[:], m_up[0:n, 0:n], blk[:], ALU.mult, r=["blk"], w=["U_s"])
                    S.tt("dve", L_s[:], m_strict[0:n, 0:n], blk[:], ALU.mult, r=["blk"], w=["L_s"])
                    S.cp("dve", U_sb[:], U_s[:], r=["U_s"], w=["U_sb"])
                    S.op("pool", lambda E: E.memset(smf[:], 1.0), w=["smf"])
                    S.op("pool", lambda E: E.affine_select(out=smf[:], in_=smf[:], pattern=[[-4, NS], [1, n]],
                                                           compare_op=ALU.is_ge, fill=0.0, base=0, channel_multiplier=0),
                         r=["smf"], w=["smf"])
                    S.op("pool", lambda E: E.affine_select(out=smf[:], in_=smf[:], pattern=[[4, NS], [-1, n]],
                                                           compare_op=ALU.is_ge, fill=0.0, base=3, channel_multiplier=0),
                         r=["smf"], w=["smf"])
                    S.cp("dve", smb[:], smf[:], r=["smf"], w=["smb"])
                    S.ts("dve", nsmb[:], smf[:], -1.0, None, ALU.mult, None, r=["smf"], w=["nsmb"])
                    S.op("pool", lambda E: E.affine_select(out=mc4[:], in_=ones_bf[:, 0:4], pattern=[[-1, 4]],
                                                           compare_op=ALU.is_gt, fill=0.0, base=0, channel_multiplier=1),
                         w=["mc4"])
                    stop("sA")
                    pb, pbk = S.psb()
                    for kc in range(8):
                        S.tr(pb[:, kc * 128:kc * 128 + n], xbs[:, kc * 128:(kc + 1) * 128], idb[0:n, 0:n], r=["xbs"], w=[pbk],
                             inc=(kc == 7))
                    S.cp("act", xTs[:], pb[:].rearrange("p (k t) -> p k t", k=8)[:, :, 0:n], r=[pbk], w=["xTs"])
                    for grp in range(2):
                        pf, pfk = S.psf()
                        ncg = 8 if grp == 0 else 4
                        for cc in range(ncg):
                            c = grp * 8 + cc
                            S.tr(pf[:, cc * 48:(cc + 1) * 48], scs[:, c * 128:(c + 1) * 128], idf[0:48, 0:48], r=["scs"],
                                 w=[pfk], inc=(cc == ncg - 1))
                        S.cp("act", ext[:, grp * 8:grp * 8 + ncg, :, 0:3],
                             pf[:, 0:ncg * 48].rearrange("p (c s r) -> p c s r", c=ncg, s=NS), r=[pfk], w=["ext"])
                    for c in range(12):
                        pf, pfk = S.psf()
                        for kc in range(8):
                            S.mm(pf[:, 0:n], w_in_bf[:, kc, 768 + c * 128:768 + (c + 1) * 128], xTs[:, kc, :], kc == 0, kc == 7,
                                 r=[WIN[kc], "xTs"], w=[pfk])
                        S.cp("act" if c % 2 else "dve", ext[:, c, :, 3:7], pf[:, 0:n].rearrange("p (s t) -> p s t", s=NS),
                             r=[pfk], w=["ext"])
                    S.cp("pool", ncsf[:].rearrange("p c (s r) -> p c s r", s=NS), ext[:, :, :, 4:7], r=["ext"], w=["ncsf"])
                    for grp in range(3):
                        pf, pfk = S.psf()
                        for cc in range(4):
                            c = grp * 4 + cc
                            S.tr(pf[0:48, cc * 128:(cc + 1) * 128], ncsf[:, c, :], idf[:], r=["ncsf"], w=[pfk], inc=(cc == 3))
                        S.cp("act", scs[:, grp * 512:(grp + 1) * 512], pf[0:48, :], r=[pfk], w=["scs"])
                    S.dma("sp", nc_s, scs[:], r=["scs"], slot="nc_s")
                    stop("sB")
                    for k in range(4):
                        cwb = cw[:, k, :].unsqueeze(2).unsqueeze(3).to_broadcast([128, 12, NS, 4])
                        if k == 0:
                            S.tt("dve", cacs[:], ext[:, :, :, 0:4], cwb, ALU.mult, r=["ext", "cw"], w=["cacs"])
                        else:
                            S.tt("pool", cprod[:], ext[:, :, :, k:k + 4], cwb, ALU.mult, r=["ext", "cw"], w=["cprod"])
                            S.tt("dve", cacs[:], cacs[:], cprod[:], ALU.add, r=["cacs", "cprod"], w=["cacs"])
                    S.act(sTs[:].rearrange("p c (s t) -> p c s t", s=NS), cacs[:, 0:8], AF.Silu, r=["cacs"], w=["sTs"])
                    S.act(vTs[:].rearrange("p c (s t) -> p c s t", s=NS), cacs[:, 8:12], AF.Silu, r=["cacs"], w=["vTs"])
                    S.act(sq8[:], sTs[:], AF.Square, r=["sTs"], w=["sq8"])
                    pf, pfk = S.psf()
                    S.mm(pf[:, 0:8 * n], ones_bf[:], sq8[:].rearrange("p c t -> p (c t)"), True, True, r=["sq8"], w=[pfk])
                    S.act(ln8[:].rearrange("p c t -> p (c t)"), pf[:, 0:8 * n], AF.Ln, r=[pfk], w=["ln8"], bias=L2_EPS)
                    S.act(ln8[:, 0:4, :], ln8[:, 0:4, :], AF.Exp, r=["ln8"], w=["ln8"], bias=-0.5 * math.log(128.0), scale=-0.5)
                    S.act(ln8[:, 4:8, :], ln8[:, 4:8, :], AF.Exp, r=["ln8"], w=["ln8"], scale=-0.5)
                    S.tt("dve", qkTs[:], sTs[:], ln8[:], ALU.mult, r=["sTs", "ln8"], w=["qkTs"])
                    pb, pbk = S.psb()
                    for h in range(4):
                        S.tr(pb[0:n, h * 128:(h + 1) * 128], qkTs[:, 4 + h, :], idb[:], r=["qkTs"], w=[pbk], inc=False)
                    for h in range(4):
                        S.tr(pb[0:n, (4 + h) * 128:(5 + h) * 128], vTs[:, h, :], idb[:], r=["vTs"], w=[pbk], inc=(h == 3))
                    S.cp("act", knvs[:], pb[0:n, :].rearrange("p (k t) -> p k t", k=8), r=[pbk], w=["knvs"])
                    for (c0, c1, dst, dk_) in ((0, 512, tqkv[:, 0:512], "tq_s"), (512, 768, tqkv[:, 512:768], "tkv_s"),
                                               (2304, 2816, tz[:], "tz_s"), (2816, 2824, tab[:], "tab_s")):
                        pf, pfk = S.psf()
                        for kc in range(8):
                            S.mm(pf[0:n, 0:c1 - c0], xTs[:, kc, :], w_in_bf[:, kc, c0:c1], kc == 0, kc == 7,
                                 r=[WIN[kc], "xTs"], w=[pfk])
                        S.cp("act" if dk_ in ("tq_s", "tz_s") else "dve", dst, pf[0:n, 0:c1 - c0], r=[pfk], w=[dk_])
                    qk3 = tqkv[:, 0:640].rearrange("p (h d) -> p h d", d=64)
                    cb = cosT[0:n, NT, :].unsqueeze(1).to_broadcast([n, 10, 8])
                    sbb = sinT[0:n, NT, :].unsqueeze(1).to_broadcast([n, 10, 8])
                    RK = ["tq_s", "tkv_s"]
                    S.tt("dve", rtm[:, 0], qk3[:, :, 0:8], cb, ALU.mult, r=RK, w=["rtm0"])
                    S.tt("dve", rtm[:, 1], qk3[:, :, 8:16], sbb, ALU.mult, r=RK, w=["rtm1"])
                    S.tt("dve", rtm[:, 2], qk3[:, :, 8:16], cb, ALU.mult, r=RK, w=["rtm2"])
                    S.tt("dve", rtm[:, 3], qk3[:, :, 0:8], sbb, ALU.mult, r=RK, w=["rtm3"])
                    S.tt("dve", rots[:, :, 0:8], rtm[:, 0], rtm[:, 1], ALU.subtract, r=["rtm0", "rtm1"], w=["rots"])
                    S.tt("dve", rots[:, :, 8:16], rtm[:, 2], rtm[:, 3], ALU.add, r=["rtm2", "rtm3"], w=["rots"])
                    q4 = tqkv[:, 0:512].rearrange("p (j c d) -> p c j d", j=2, c=4)
                    S.cp("dve", qbs[:, :, :, 0:16], rots[:, 0:8, :].rearrange("p (j c) d -> p c j d", j=2), r=["rots"], w=["qbs"])
                    S.cp("dve", qbs[:, :, :, 16:64], q4[:, :, :, 16:64], r=["tq_s"], w=["qbs"])
                    k3 = tqkv[:, 512:640].rearrange("p (j d) -> p j d", j=2)
                    S.cp("dve", kbs[:, :, 0:16], rots[:, 8:10, :], r=["rots"], w=["kbs"])
                    S.cp("dve", kbs[:, :, 16:64], k3[:, :, 16:64], r=["tkv_s"], w=["kbs"])
                    S.cp("dve", krs[:, :, 0:16], rots[:, 8:10, :], r=["rots"], w=["krs"])
                    S.cp("dve", krs[:, :, 16:64], k3[:, :, 16:64], r=["tkv_s"], w=["krs"])
                    S.cp("dve", vna[:, :, 0:64], tqkv[:, 640:768].rearrange("p (j d) -> p j d", j=2), r=["tkv_s", "vna"], w=["vna"])
                    S.dma("sp", nk_s[:, 124:128, :].rearrange("s t c -> (s t) c") if False else nk_s[:, 124:128, :],
                          krs[:].rearrange("(s t) j d -> s t (j d)", t=TS) if False else krs[:].rearrange("p j d -> p (j d)"),
                          r=["krs"], slot="nk_s1")
                    S.dma("sp", nv_s[:, 124:128, :], tqkv[:, 640:768], r=["tkv_s"], slot="nv_s1")
                    stop("sC")
                    pb, pbk = S.psb()
                    for c in range(4):
                        S.tr(pb[:, c * 128:c * 128 + n], qbs[:, c].rearrange("p j d -> p (j d)"), idb[0:n, 0:n], r=["qbs"],
                             w=[pbk], inc=False)
                    S.tr(pb[:, 512:512 + n], kbs[:].rearrange("p j d -> p (j d)"), idb[0:n, 0:n], r=["kbs"], w=[pbk])
                    S.cp("act", qTs[:], pb[:, 0:512].rearrange("p (c t) -> p c t", c=4)[:, :, 0:n], r=[pbk], w=["qTs"])
                    S.cp("act", kTn[:], pb[:, 512:512 + n], r=[pbk], w=["kTn"])
                    S.cp("dve", qTsr[:].rearrange("p s (c t) -> p s c t", c=4),
                         qTs[:].rearrange("p c (s t) -> p s c t", s=NS), r=["qTs"], w=["qTsr"])
                    for grp in range(2):
                        pb, pbk = S.psb()
                        for ss in range(8):
                            s_ = grp * 8 + ss
                            S.tr(pb[:, ss * 128:(ss + 1) * 128], cK[:, s_, :], idb[:], r=["cK"], w=[pbk], inc=(ss == 7))
                        S.cp("act" if grp else "dve", cKT[:, grp * 8:(grp + 1) * 8, :], pb[:].rearrange("p (s t) -> p s t", s=8),
                             r=[pbk], w=["cKT"])
                    stop("sC2")
                    S.tt("dve", mk16[:].rearrange("p s (a t) -> p (s a) t", t=4), smb[:].rearrange("p s (a t) -> p (s a) t", t=4),
                         mc4[:].unsqueeze(1).to_broadcast([128, NS * NS, 4]), ALU.mult, r=["smb", "mc4"], w=["mk16"])
                    stop("sD0")
                    base = S.psf_i % 6
                    S.psf_i += 6
                    bk = [(base + d) % 6 for d in range(6)]
                    po_ = [(S.psf_list[bk[j]], f"psf{bk[j]}") for j in range(2)]
                    ps_ = [(S.psf_list[bk[2 + j]], f"psf{bk[2 + j]}") for j in range(4)]
                    for s_ in range(NS):
                        pt_, ptk_ = PTs[s_ % 2], f"PTs{s_ % 2}"
                        for jk in range(2):
                            psc, psck = ps_[(s_ % 2) * 2 + jk]
                            S.mm(psc[:, 0:256], cKT[jk * 64:(jk + 1) * 64, s_, :],
                                 qTs[jk * 64:(jk + 1) * 64, :, :].rearrange("p c t -> p (c t)"), True, True,
                                 r=["cKT", "qTs"], w=[psck], inc=True)
                            S.act(pt_[:, jk * 256:(jk + 1) * 256], psc[:, 0:256], AF.Exp, r=[psck], w=[ptk_], scale=0.125)
                        S.tt("dve", pt_[:].rearrange("p (a t) -> p a t", a=8), pt_[:].rearrange("p (a t) -> p a t", a=8),
                             mk16[:, s_, :].unsqueeze(1).to_broadcast([128, 8, n]), ALU.mult, r=[ptk_, "mk16"], w=[ptk_])
                        for jk in range(2):
                            S.mm(po_[jk][0][0:65, 0:256], cVa[:, s_, jk, :], pt_[:, jk * 256:(jk + 1) * 256], s_ == 0, False,
                                 r=["cVa", ptk_], w=[po_[jk][1]], inc=(jk == 1))
                    stop("sD1")
                    for jk in range(2):
                        psn, psnk = ps_[jk]
                        S.mm(psn[0:n, 0:256], kTn[jk * 64:(jk + 1) * 64, :],
                             qTs[jk * 64:(jk + 1) * 64, :, :].rearrange("p c t -> p (c t)"), True, True, r=["kTn", "qTs"],
                             w=[psnk], inc=True)
                        S.act(PTn[:, jk * 256:(jk + 1) * 256], psn[0:n, 0:256], AF.Exp, r=[psnk], w=["PTn"], scale=0.125)
                    S.tt("pool", PTn[:].rearrange("p (a t) -> p a t", a=8), PTn[:].rearrange("p (a t) -> p a t", a=8),
                         U_sb[:].unsqueeze(1).to_broadcast([n, 8, n]), ALU.mult, r=["PTn", "U_sb"], w=["PTn"])
                    for jk in range(2):
                        S.mm(po_[jk][0][0:65, 0:256], vna[:, jk, :], PTn[:, jk * 256:(jk + 1) * 256], False, True,
                             r=["vna", "PTn"], w=[po_[jk][1]], inc=True)
                    stop("sD3")
                    for jk in range(2):
                        S.cp("act" if jk else "dve", ot[:, jk * 256:(jk + 1) * 256], po_[jk][0][0:65, 0:256], r=[po_[jk][1]],
                             w=["ot"])
                    S.tt("dve", dn[64:65, :].rearrange("p (h t) -> p h t", h=8), ot[64:65, :].rearrange("p (h t) -> p h t", h=8),
                         esink[64:65, :].unsqueeze(2).to_broadcast([1, 8, n]), ALU.add, r=["ot", "esink"], w=["dn"])
                    S.op("dve", lambda E: E.reciprocal(out=dn[64:65, :], in_=dn[64:65, :]), r=["dn"], w=["dn"])
                    stop("sD4")
                    pbc, pbck = S.psf()
                    S.mm(pbc[0:64, :], onesf[64:65, 0:64], dn[64:65, :], True, True, r=["dn"], w=[pbck])
                    S.tt("dve", oTb[:].rearrange("p h t -> p (h t)"), pbc[0:64, :], ot[0:64, :], ALU.mult, r=[pbck, "ot"],
                         w=["oTb"])
                    stop("sD5")
                    pb, pbk = S.psb()
                    for h in range(8):
                        S.tr(pb[0:n, h * 64:(h + 1) * 64], oTb[:, h, :], idb[0:64, 0:64], r=["oTb"], w=[pbk], inc=(h == 7))
                    S.cp("act", mix_all[0:n, NT, 0:512], pb[0:n, 0:512], r=[pbk], w=["mix16"])
                    S.barrier()
                    esA.close()
                    gabs = sb(es, "gabs", [n, 8, 4])
                    gmk = sb(es, "gmk", [n, NS, 4])
                    abc = sb(es, "abc", [128, NS * 4])
                    gUs = sb(es, "gUs", [n, 4, n])
                    eDs = sb(es, "eDs", [n, 4, n])
                    eDTs = sb(es, "eDTs", [n, 4, n])
                    ATs = sb(es, "ATs", [n, 4, n], BF16)
                    P0s = sb(es, "P0s", [n, 4, n])
                    P0Ts = sb(es, "P0Ts", [n, 4, n])
                    P1s = sb(es, "P1s", [n, 4, n])
                    Q0s = sb(es, "Q0s", [n, 4, n])
                    Qbs = sb(es, "Qbs", [n, 4, n], BF16)
                    kbes = sb(es, "kbes", [n, 4, 128], BF16)
                    kdcs = sb(es, "kdcs", [n, 4, 128], BF16)
                    vbs = sb(es, "vbs", [n, 4, 128], BF16)
                    vns = sb(es, "vns", [n, 4, 128], BF16)
                    o1ss = sb(es, "o1ss", [n, 4, 128])
                    ogs = sb(es, "ogs", [n, 4, 128])
                    osqs = sb(es, "osqs", [n, 4, 128])
                    ssqs = sb(es, "ssqs", [n, 4])
                    zss = sb(es, "zss", [n, 512])
                    Stm = [sb(es, f"Stm{i}", [128, 4, 128]) for i in range(2)]
                    nwTh = [sb(es, f"nwTh{i}", [128, NS, n], BF16) for i in range(2)]
                    qTh = [sb(es, f"qTh{i}", [128, NS, n], BF16) for i in range(2)]
                    vnh = [sb(es, f"vnh{i}", [n, NS, 128], BF16) for i in range(2)]

                    stop("sD")
                    g_ = gabs[:, 0, :]
                    beta_ = gabs[:, 1, :]
                    gc_ = gabs[:, 2, :]
                    egc_ = gabs[:, 3, :]
                    kds_ = gabs[:, 4, :]
                    tmp_ = gabs[:, 6, :]
                    nbeta_ = gabs[:, 7, :]
                    S.tt("dve", tmp_, tab[:, 0:4], dtb[0:n, :], ALU.add, r=["tab_s"], w=["s_tmp"])
                    S.act(tmp_, tmp_, AF.Exp, r=["s_tmp"], w=["s_tmp"])
                    S.act(tmp_, tmp_, AF.Ln, r=["s_tmp"], w=["s_tmp"], bias=1.0)
                    S.tt("dve", g_, tmp_, nexpA[0:n, :], ALU.mult, r=["s_tmp"], w=["s_g"])
                    S.act(beta_, tab[:, 4:8], AF.Exp, r=["tab_s"], w=["s_beta"], scale=-1.0)
                    S.ts("dve", beta_, beta_, 1.0, None, ALU.add, None, r=["s_beta"], w=["s_beta"])
                    S.op("dve", lambda E: E.reciprocal(out=beta_, in_=beta_), r=["s_beta"], w=["s_beta"])
                    S.ts("dve", nbeta_, beta_, -1.0, None, ALU.mult, None, r=["s_beta"], w=["s_nbeta"])
                    S.tt("dve", gmk[:], g_.unsqueeze(1).to_broadcast([n, NS, 4]), rowm[:].unsqueeze(2).to_broadcast([n, NS, 4]),
                         ALU.mult, r=["s_g", "rowm"], w=["gmk"])
                    pf, pfk = S.psf()
                    S.mm(pf[0:n, 0:4], U_s[:], g_, True, True, r=["U_s", "s_g"], w=[pfk])
                    S.mm(pf[0:n, 4:8], blk[:], g_, True, True, r=["blk", "s_g"], w=[pfk])
                    S.cp("dve", gc_, pf[0:n, 0:4], r=[pfk], w=["s_gc"])
                    S.act(egc_, pf[0:n, 0:4], AF.Exp, r=[pfk], w=["s_egc"])
                    S.tt("dve", kds_, pf[0:n, 4:8], gc_, ALU.subtract, r=[pfk, "s_gc"], w=["s_kds"])
                    S.act(kds_, kds_, AF.Exp, r=["s_kds"], w=["s_kds"])
                    pa, pak = S.psf()
                    S.mm(pa[:, 0:NS * 4], onesf[0:n, :], gmk[:].rearrange("p s h -> p (s h)"), True, True, r=["gmk"], w=[pak])
                    S.act(abc[:], pa[:, 0:NS * 4], AF.Exp, r=[pak], w=["abc"])
                    pD, pDk = S.psf()
                    pDT, pDTk = S.psf()
                    pG, pGk = S.psf()
                    pA, pAk = S.psf()
                    v3 = lambda p: p[0:n, 0:4 * n].rearrange("p (h t) -> p h t", h=4)
                    pD3, pDT3, pG3, pA3 = v3(pD), v3(pDT), v3(pG), v3(pA)
                    for h in range(4):
                        S.ts("dve", gUs[:, h, :], U_s[:], gabs[:, 0, h:h + 1], None, ALU.mult, None, r=["U_s", "s_g"], w=["gUs"])
                    for h in range(4):
                        S.mm(pD3[:, h, :], gUs[:, h, :], L_s[:], True, True, r=["gUs", "L_s"], w=[pDk], inc=(h == 3))
                    for h in range(4):
                        S.mm(pDT3[:, h, :], L_s[:], gUs[:, h, :], True, True, r=["gUs", "L_s"], w=[pDTk], inc=(h == 3))
                    for h in range(4):
                        S.mm(pG3[:, h, :], qkTs[:, 4 + h, :], qkTs[:, 4 + h, :], True, True, r=["qkTs"], w=[pGk], inc=(h == 3))
                    for h in range(4):
                        S.mm(pA3[:, h, :], qkTs[:, 4 + h, :], qkTs[:, h, :], True, True, r=["qkTs"], w=[pAk], inc=(h == 3))
                    S.act(eDs[:], pD3, AF.Exp, r=[pDk], w=["eDs"])
                    S.tt("pool", eDs[:], eDs[:], L_s[:].unsqueeze(1).to_broadcast([n, 4, n]), ALU.mult, r=["eDs", "L_s"], w=["eDs"])
                    S.act(eDTs[:], pDT3, AF.Exp, r=[pDTk], w=["eDTs"])
                    S.tt("pool", eDTs[:], eDTs[:], U_s[:].unsqueeze(1).to_broadcast([n, 4, n]), ALU.mult, r=["eDTs", "U_s"],
                         w=["eDTs"])
                    for h in range(4):
                        S.stt("dve", P0s[:, h, :], pG3[:, h, :], gabs[:, 7, h:h + 1], eDs[:, h, :], ALU.mult, ALU.mult,
                              r=[pGk, "s_nbeta", "eDs"], w=["P0s"])
                    S.tt("dve", ATs[:], pA3, eDTs[:], ALU.mult, r=[pAk, "eDTs"], w=["ATs"])
                    pt, ptk = S.psf()
                    pt3 = v3(pt)
                    for h in range(4):
                        S.tr(pt3[:, h, :], P0s[:, h, :], idf[0:n, 0:n], r=["P0s"], w=[ptk], inc=(h == 3))
                    S.cp("act", P0Ts[:], pt3, r=[ptk], w=["P0Ts"])
                    S.tt("dve", Q0s[:], pt3, idf[0:n, 0:n].unsqueeze(1).to_broadcast([n, 4, n]), ALU.add, r=[ptk], w=["Q0s"])
                    pP, pPk = S.psf()
                    pP3 = v3(pP)
                    for h in range(4):
                        S.mm(pP3[:, h, :], P0Ts[:, h, :], P0s[:, h, :], True, True, r=["P0Ts", "P0s"], w=[pPk], inc=(h == 3))
                    S.cp("act", P1s[:], pP3, r=[pPk], w=["P1s"])
                    pQ, pQk = S.psf()
                    pQ3 = v3(pQ)
                    for h in range(4):
                        S.mm(pQ3[:, h, :], P1s[:, h, :], Q0s[:, h, :], True, True, r=["P1s", "Q0s"], w=[pQk], inc=(h == 3))
                    S.tt("dve", Qbs[:], pQ3, Q0s[:], ALU.add, r=[pQk, "Q0s"], w=["Qbs"])
                    stop("sE")
                    kn3 = knvs[:, 0:4, :]
                    vv3 = knvs[:, 4:8, :]
                    S.tt("dve", tmp_, beta_, egc_, ALU.mult, r=["s_beta", "s_egc", "s_tmp"], w=["s_tmp"])
                    S.tt("pool", kbes[:], kn3, tmp_.unsqueeze(2).to_broadcast([n, 4, 128]), ALU.mult, r=["knvs", "s_tmp"], w=["kbes"])
                    S.tt("pool", kdcs[:], kn3, kds_.unsqueeze(2).to_broadcast([n, 4, 128]), ALU.mult, r=["knvs", "s_kds"], w=["kdcs"])
                    S.tt("dve", vbs[:], vv3, beta_.unsqueeze(2).to_broadcast([n, 4, 128]), ALU.mult, r=["knvs", "s_beta"], w=["vbs"])
                    pw, pwk = S.psf()
                    pw3 = pw[:, 0:4 * n].rearrange("p (h t) -> p h t", h=4)
                    for h in range(4):
                        S.mm(pw3[:, h, :], kbes[:, h, :], Qbs[:, h, :], True, True, r=["kbes", "Qbs"], w=[pwk], inc=(h == 3))
                    pv, pvk = S.psf()
                    pv3 = pv[0:n, :].rearrange("p (h t) -> p h t", h=4)
                    for h in range(4):
                        S.tt("dve", nwTh[h % 2][:], pw3[:, h, :].unsqueeze(1).to_broadcast([128, NS, n]), nsmb[:], ALU.mult,
                             r=[pwk, "nsmb"], w=[f"nwTh{h % 2}"])
                        S.mm(pv3[:, h, :], Qbs[:, h, :], vbs[:, h, :], True, False, r=["Qbs", "vbs"], w=[pvk], inc=False)
                        for s_ in range(NS):
                            S.mm(pv3[:, h, :], nwTh[h % 2][:, s_, :], S_b[:, s_ * 4 + h, :], False, s_ == NS - 1,
                                 r=[f"nwTh{h % 2}", "S_b"], w=[pvk], inc=(s_ == NS - 1))
                    S.cp("act", vns[:], pv3, r=[pvk], w=["vns"])
                    po1, po1k = S.psf()
                    po13 = po1[0:n, :].rearrange("p (h t) -> p h t", h=4)
                    for h in range(4):
                        S.tt("pool", qTh[h % 2][:], qkTs[:, h, :].unsqueeze(1).to_broadcast([128, NS, n]), smb[:], ALU.mult,
                             r=["qkTs", "smb"], w=[f"qTh{h % 2}"])
                        for s_ in range(NS):
                            S.mm(po13[:, h, :], qTh[h % 2][:, s_, :], S_b[:, s_ * 4 + h, :], s_ == 0, s_ == NS - 1,
                                 r=[f"qTh{h % 2}", "S_b"], w=[po1k], inc=(s_ == NS - 1))
                    po2, po2k = S.psf()
                    po23 = po2[0:n, :].rearrange("p (h t) -> p h t", h=4)
                    for h in range(4):
                        S.mm(po23[:, h, :], ATs[:, h, :], vns[:, h, :], True, True, r=["ATs", "vns"], w=[po2k], inc=(h == 3))
                    S.tt("dve", o1ss[:], po13, egc_.unsqueeze(2).to_broadcast([n, 4, 128]), ALU.mult, r=[po1k, "s_egc"], w=["o1ss"])
                    S.tt("dve", ogs[:], po23, o1ss[:], ALU.add, r=[po2k, "o1ss"], w=["ogs"])
                    stop("sF")
                    S_f4 = S_f[:].rearrange("p (s h) v -> p s h v", h=4)
                    abc3 = abc[:].rearrange("p (s h) -> p s h", h=4)
                    for half in range(2):
                        S.dma("sp", S_f[:], sg_d[half * 8:(half + 1) * 8].rearrange("s h k v -> k (s h) v"), w=["S_f"])
                        for h in range(4):
                            vh, vhk = vnh[h % 2], f"vnh{h % 2}"
                            S.tt("dve", vh[:], vns[:, h, :].unsqueeze(1).to_broadcast([n, NS, 128]),
                                 rowm[:].unsqueeze(2).to_broadcast([n, NS, 128]), ALU.mult, r=["vns", "rowm"], w=[vhk])
                            for sg_ in range(2):
                                pS, pSk = S.psf()
                                pS3 = pS[:].rearrange("p (s t) -> p s t", s=4)
                                for sl in range(4):
                                    s_ = half * 8 + sg_ * 4 + sl
                                    S.mm(pS3[:, sl, :], kdcs[:, h, :], vh[:, s_, :], True, True, r=["kdcs", vhk], w=[pSk],
                                         inc=(sl == 3))
                                stt_, sttk = Stm[sg_ % 2], f"Stm{sg_ % 2}"
                                sl0 = sg_ * 4
                                s0 = half * 8 + sg_ * 4
                                S.tt("pool", stt_[:], S_f4[:, sl0:sl0 + 4, h, :],
                                     abc3[:, s0:s0 + 4, h].unsqueeze(2).to_broadcast([128, 4, 128]), ALU.mult,
                                     r=["S_f", "abc"], w=[sttk])
                                S.tt("dve", S_f4[:, sl0:sl0 + 4, h, :], pS3, stt_[:], ALU.add, r=[pSk, sttk], w=["S_f"])
                        S.dma("sp", ng_s[half * 8:(half + 1) * 8].rearrange("s h k v -> k (s h) v"), S_f[:], r=["S_f"], slot="ng_s")
                    S.tt("pool", osqs[:], ogs[:], ogs[:], ALU.mult, r=["ogs"], w=["osqs"])
                    S.op("dve", lambda E: E.tensor_reduce(out=ssqs[:], in_=osqs[:], axis=AX.X, op=ALU.add), r=["osqs"], w=["ssqs"])
                    S.act(ssqs[:], ssqs[:], AF.Ln, r=["ssqs"], w=["ssqs"], bias=NORM_EPS, scale=1.0 / 128.0)
                    S.act(ssqs[:], ssqs[:], AF.Exp, r=["ssqs"], w=["ssqs"], scale=-0.5)
                    S.act(zss[:], tz[:], AF.Silu, r=["tz_s"], w=["zss"])
                    S.tt("pool", zss[:].rearrange("p (h d) -> p h d", h=4), zss[:].rearrange("p (h d) -> p h d", h=4),
                         gnw[0:n, :].unsqueeze(1).to_broadcast([n, 4, 128]), ALU.mult, r=["zss"], w=["zss"])
                    S.tt("dve", ogs[:], ogs[:], ssqs[:].unsqueeze(2).to_broadcast([n, 4, 128]), ALU.mult, r=["ogs", "ssqs"], w=["ogs"])
                    S.tt("dve", mix_all[0:n, NT, 512:1024].rearrange("p (h d) -> p h d", h=4), ogs[:],
                         zss[:].rearrange("p (h d) -> p h d", h=4), ALU.mult, r=["ogs", "zss"], w=["mix16"])
                    S.barrier()

                with ExitStack() as es:
                    ST = 256
                    xb = [sb(es, f"xb{i}", [128, D], BF16) for i in range(2)]
                    xT = [sb(es, f"xT{i}", [128, 8, ST], BF16) for i in range(2)]
                    gT = sb(es, "gT", [128, 12, ST + 3])
                    cacc = [sb(es, f"cacc{i}", [128, ST]) for i in range(2)]
                    sT = sb(es, "sT", [128, 8, ST])
                    qkT2 = [sb(es, f"qkT_{i}", [128, 8, ST], BF16) for i in range(2)]
                    vT2 = [sb(es, f"vT_{i}", [128, 4, ST], BF16) for i in range(2)]
                    sq4 = sb(es, "sq4", [128, 4, ST], BF16)
                    ln4 = sb(es, "ln4", [128, 4, ST])
                    knv = sb(es, "knv", [128, 8, 128], BF16)
                    tm_qkv = sb(es, "tm_qkv", [128, 768])
                    zs4 = [sb(es, f"zs2_{i}", [128, 512]) for i in range(4)]
                    tm_ab = sb(es, "tm_ab", [128, 8])
                    rot = sb(es, "rot", [128, 10, 16])
                    rtmp = sb(es, "rtmp", [128, 4, 10, 8])
                    q_bf = sb(es, "q_bf", [128, 4, 2, 64], BF16)
                    k_bf = sb(es, "k_bf", [128, 2, 64], BF16)
                    k_rot = sb(es, "k_rot", [128, 2, 64])
                    v_aug = [sb(es, f"v_aug{i}", [128, 2, 65], BF16) for i in range(2)]
                    kTb = [sb(es, f"kTb{i}", [128, 128], BF16) for i in range(2)]
                    qT = sb(es, "qT", [128, 4, 128], BF16)
                    PTe = [sb(es, f"PTe{i}", [128, 4, 128], BF16) for i in range(2)]
                    den = sb(es, "den", [128, 2, 4])
                    gab = sb(es, "gab", [128, 8, 4])
                    gU = sb(es, "gU", [128, 4, 128])
                    eD = sb(es, "eD", [128, 4, 128])
                    eDT = sb(es, "eDT", [128, 4, 128])
                    ATb = sb(es, "ATb", [128, 4, 128], BF16)
                    Pm = [sb(es, f"Pm{i}", [128, 4, 128]) for i in range(2)]
                    PmT = [sb(es, f"PmT{i}", [128, 4, 128]) for i in range(2)]
                    Qm = [sb(es, f"Qm{i}", [128, 4, 128]) for i in range(2)]
                    Qb = sb(es, "Qb", [128, 4, 128], BF16)
                    kbe = sb(es, "kbe", [128, 4, 128], BF16)
                    kdc = sb(es, "kdc", [128, 4, 128], BF16)
                    vb = sb(es, "vb", [128, 4, 128], BF16)
                    nwT = sb(es, "nwT", [128, 4, 128], BF16)
                    vnew = sb(es, "vnew", [128, 4, 128], BF16)
                    Sf = sb(es, "Sf", [128, 4, 128])
                    Sb = sb(es, "Sb", [128, 4, 128], BF16)
                    Stmp = sb(es, "Stmp", [128, 4, 128])
                    o1s = sb(es, "o1s", [128, 4, 128])
                    og = sb(es, "og", [128, 4, 128])
                    osq = sb(es, "osq", [128, 4, 128])
                    ssq = sb(es, "ssq", [128, 4])

                    S.op("pool", lambda E: E.memset(gT[:, :, 0:3], 0.0), w=[f"gT{c}" for c in range(12)])
                    S.op("pool", lambda E: E.memset(Sf[:], 0.0), w=["Sf"])
                    S.op("pool", lambda E: E.memset(Sb[:], 0.0), w=["Sb"])
                    for i in range(2):
                        S.op("pool", lambda E, i=i: E.memset(v_aug[i][:], 1.0), w=[f"v_aug{i}"])

                    GT = [f"gT{c}" for c in range(12)]
                    def fm(st):
                        qkT, vT, zs2 = qkT2[st % 2], vT2[st % 2], zs4[(st % 2) * 2:(st % 2) * 2 + 2]
                        PAR = st % 2
                        xTs = xT[st % 2]
                        xTk = f"xT{st % 2}"
                        for j in range(ST // 128):
                            i = st * (ST // 128) + j
                            xbt = xb[i % 2]
                            xbk = f"xb{i % 2}"
                            S.dma("pool", xbt[:], x_p[i * 128:(i + 1) * 128, :], w=[xbk])
                            pb, pbk = S.psb()
                            for kc in range(8):
                                S.tr(pb[:, kc * 128:(kc + 1) * 128], xbt[:, kc * 128:(kc + 1) * 128], idb[:],
                                     r=[xbk, "idb"], w=[pbk], inc=(kc == 7))
                            S.cp("act", xTs[:, :, j * 128:(j + 1) * 128], pb[:].rearrange("p (k t) -> p k t", k=8),
                                 r=[pbk], w=[xTk])
                        for c in range(12):
                            pf, pfk = S.psf()
                            for kc in range(8):
                                S.mm(pf[:, 0:ST], w_in_bf[:, kc, 768 + c * 128:768 + (c + 1) * 128], xTs[:, kc, :],
                                     kc == 0, kc == 7, r=[WIN[kc], xTk], w=[pfk])
                            S.cp("act" if c % 2 else "dve", gT[:, c, 3:3 + ST], pf[:, 0:ST], r=[pfk], w=[GT[c]])
                            acc = cacc[c % 2]
                            ak = f"cacc{c % 2}"
                            ce = "dve"
                            S.ts(ce, acc[:], gT[:, c, 0:ST], cw[:, 0, c:c + 1], None, ALU.mult, None, r=[GT[c], "cw"], w=[ak])
                            for k in range(1, 4):
                                S.stt(ce, acc[:], gT[:, c, k:k + ST], cw[:, k, c:c + 1], acc[:], ALU.mult, ALU.add,
                                      r=[GT[c], "cw", ak], w=[ak])
                            if c < 8:
                                S.act(sT[:, c, :], acc[:], AF.Silu, r=[ak], w=[f"sT{c}"])
                            else:
                                S.act(vT[:, c - 8, :], acc[:], AF.Silu, r=[ak], w=[f"vT_{PAR}"])
                        for j in range(ST // 128):
                            pf, pfk = S.psf()
                            for kc in range(8):
                                S.mm(pf[:, :], xTs[:, kc, j * 128:(j + 1) * 128], w_in_bf[:, kc, 2304:2816], kc == 0, kc == 7,
                                     r=[WIN[kc], xTk], w=[pfk])
                            S.act(zs2[j][:], pf[:, :], AF.Silu, r=[pfk], w=[f"zs2_{PAR}_{j}"])
                            S.tt("pool", zs2[j][:].rearrange("p (h d) -> p h d", h=4), zs2[j][:].rearrange("p (h d) -> p h d", h=4),
                                 gnw[:].unsqueeze(1).to_broadcast([128, 4, 128]), ALU.mult, r=[f"zs2_{PAR}_{j}", "gnw"], w=[f"zs2_{PAR}_{j}"])
                        if st == SEQ // ST - 1:
                            for t in range(3):
                                S.dma("sp", nc_p[t].rearrange("(c p) -> p c", p=128), gT[:, :, ST + t], r=GT, w=[],
                                      slot="nc_p")
                        else:
                            S.cp("pool", gT[:, :, 0:3], gT[:, :, ST:ST + 3], r=GT, w=GT)
                        for half in range(2):
                            for hh in range(4):
                                c = half * 4 + hh
                                S.act(sq4[:, hh, :], sT[:, c, :], AF.Square, r=[f"sT{c}"], w=[f"sq{hh}"])
                            pfs = []
                            for hh in range(4):
                                pf, pfk = S.psf()
                                S.mm(pf[:, 0:ST], ones_bf[:], sq4[:, hh, :], True, True, r=["ones_bf", f"sq{hh}"], w=[pfk])
                                pfs.append((pf, pfk))
                            for hh in range(4):
                                pf, pfk = pfs[hh]
                                S.act(ln4[:, hh, :], pf[:, 0:ST], AF.Ln, r=[pfk], w=[f"ln{hh}"], bias=L2_EPS)
                            for hh in range(4):
                                bq = -0.5 * math.log(128.0) if half == 0 else 0.0
                                S.act(ln4[:, hh, :], ln4[:, hh, :], AF.Exp, r=[f"ln{hh}"], w=[f"ln{hh}"], bias=bq, scale=-0.5)
                            for hh in range(4):
                                c = half * 4 + hh
                                S.tt("pool" if hh % 2 else "dve", qkT[:, c, :], sT[:, c, :], ln4[:, hh, :], ALU.mult,
                                     r=[f"sT{c}", f"ln{hh}"], w=[f"qkT{c}_{PAR}"])
                        if st == 0:
                            stop("st0a")

                    def tile(st, j):
                        qkT, vT, zs2 = qkT2[st % 2], vT2[st % 2], zs4[(st % 2) * 2:(st % 2) * 2 + 2]
                        PAR = st % 2
                        xTs = xT[st % 2]
                        xTk = f"xT{st % 2}"
                        i = st * (ST // 128) + j
                        tok = slice(j * 128, (j + 1) * 128)
                        pb, pbk = S.psb()
                        for h in range(4):
                            S.tr(pb[:, h * 128:(h + 1) * 128], qkT[:, 4 + h, tok], idb[:], r=[f"qkT{4 + h}_{PAR}", "idb"],
                                 w=[pbk], inc=False)
                        for h in range(4):
                            S.tr(pb[:, (4 + h) * 128:(5 + h) * 128], vT[:, h, tok], idb[:], r=[f"vT_{PAR}", "idb"], w=[pbk],
                                 inc=(h == 3))
                        S.cp("act", knv[:], pb[:].rearrange("p (k t) -> p k t", k=8), r=[pbk], w=["knv"])
                        if i == 0:
                            stop("t0a")
                        for (c0, c1, dst, dk_) in ((0, 512, tm_qkv[:, 0:512], "tm_q"), (512, 768, tm_qkv[:, 512:768], "tm_kv"),
                                                   (2816, 2824, tm_ab[:], "tm_ab")):
                            pf, pfk = S.psf()
                            for kc in range(8):
                                S.mm(pf[:, 0:c1 - c0], xTs[:, kc, tok], w_in_bf[:, kc, c0:c1], kc == 0, kc == 7,
                                     r=[WIN[kc], xTk], w=[pfk])
                            S.cp("act" if dk_ in ("tm_q", "tm_z") else "dve", dst, pf[:, 0:c1 - c0], r=[pfk], w=[dk_])
                        if i == 0:
                            stop("t0b")
                        qk3 = tm_qkv[:, 0:640].rearrange("p (h d) -> p h d", d=64)
                        cb = cosT[:, i, :].unsqueeze(1).to_broadcast([128, 10, 8])
                        sbb = sinT[:, i, :].unsqueeze(1).to_broadcast([128, 10, 8])
                        RK = ["tm_q", "tm_kv", "cosT", "sinT"]
                        S.tt("dve", rtmp[:, 0], qk3[:, :, 0:8], cb, ALU.mult, r=RK, w=["rt0"])
                        S.tt("dve", rtmp[:, 1], qk3[:, :, 8:16], sbb, ALU.mult, r=RK, w=["rt1"])
                        S.tt("dve", rtmp[:, 2], qk3[:, :, 8:16], cb, ALU.mult, r=RK, w=["rt2"])
                        S.tt("dve", rtmp[:, 3], qk3[:, :, 0:8], sbb, ALU.mult, r=RK, w=["rt3"])
                        S.tt("dve", rot[:, :, 0:8], rtmp[:, 0], rtmp[:, 1], ALU.subtract, r=["rt0", "rt1"], w=["rot"])
                        S.tt("dve", rot[:, :, 8:16], rtmp[:, 2], rtmp[:, 3], ALU.add, r=["rt2", "rt3"], w=["rot"])
                        q4 = tm_qkv[:, 0:512].rearrange("p (j c d) -> p c j d", j=2, c=4)
                        S.cp("dve", q_bf[:, :, :, 0:16], rot[:, 0:8, :].rearrange("p (j c) d -> p c j d", j=2),
                             r=["rot"], w=["q_bf"])
                        S.cp("act", q_bf[:, :, :, 16:64], q4[:, :, :, 16:64], r=["tm_q"], w=["q_bf"])
                        k3 = tm_qkv[:, 512:640].rearrange("p (j d) -> p j d", j=2)
                        S.cp("dve", k_bf[:, :, 0:16], rot[:, 8:10, :], r=["rot"], w=["k_bf"])
                        S.cp("dve", k_bf[:, :, 16:64], k3[:, :, 16:64], r=["tm_kv"], w=["k_bf"])
                        va = v_aug[i % 2]
                        vak = f"v_aug{i % 2}"
                        S.cp("pool", va[:, :, 0:64], tm_qkv[:, 640:768].rearrange("p (j d) -> p j d", j=2), r=["tm_kv"],
                             w=[vak])
                        if i == 0:
                            stop("t0c")
                        if i == NT - 1:
                            S.cp("dve", k_rot[:, :, 0:16], rot[:, 8:10, :], r=["rot"], w=["k_rot"])
                            S.cp("dve", k_rot[:, :, 16:64], k3[:, :, 16:64], r=["tm_kv"], w=["k_rot"])
                            S.dma("sp", nk_p, k_rot[:].rearrange("p j d -> p (j d)"), r=["k_rot"], slot="nk_p")
                            S.dma("sp", nv_p, tm_qkv[:, 640:768], r=["tm_kv"], slot="nv_p")
                        pb, pbk = S.psb()
                        for c in range(4):
                            S.tr(pb[:, c * 128:(c + 1) * 128], q_bf[:, c].rearrange("p j d -> p (j d)"), idb[:],
                                 r=["q_bf", "idb"], w=[pbk], inc=False)
                        S.tr(pb[:, 512:640], k_bf[:].rearrange("p j d -> p (j d)"), idb[:], r=["k_bf", "idb"], w=[pbk])
                        kTc = kTb[i % 2]
                        kTk = f"kTb{i % 2}"
                        S.cp("act", qT[:], pb[:, 0:512].rearrange("p (c t) -> p c t", c=4), r=[pbk], w=["qT"])
                        S.cp("dve", kTc[:], pb[:, 512:640], r=[pbk], w=[kTk])
                        if i == 0:
                            stop("t0d")
                        for jk in range(2):
                            blocks = ([] if i == 0 else [(kTb[(i - 1) % 2], f"kTb{(i - 1) % 2}", mb_prev, "mb_prev",
                                                          v_aug[(i - 1) % 2], f"v_aug{(i - 1) % 2}")])
                            blocks.append((kTc, kTk, mb_cur, "mb_cur", va, vak))
                            for bi, (kt, ktk, mk, mkk, _, _) in enumerate(blocks):
                                pf, pfk = S.psf()
                                S.mm(pf[:], kt[jk * 64:(jk + 1) * 64, :],
                                     qT[jk * 64:(jk + 1) * 64, :, :].rearrange("p c t -> p (c t)"), True, True,
                                     r=[ktk, "qT"], w=[pfk])
                                S.act(PTe[bi][:].rearrange("p c t -> p (c t)"), pf[:], AF.Exp, r=[pfk], w=[f"PTe{bi}"],
                                      scale=0.125)
                                S.tt("dve", PTe[bi][:], PTe[bi][:], mk[:].unsqueeze(1).to_broadcast([128, 4, 128]),
                                     ALU.mult, r=[f"PTe{bi}", mkk], w=[f"PTe{bi}"])
                                if i == 0:
                                    stop("t0e")
                            po, pok = S.psf()
                            po3 = po[:, 0:260].rearrange("p (c d) -> p c d", c=4)
                            for c in range(4):
                                for bi, (_, _, _, _, vv, vvk) in enumerate(blocks):
                                    S.mm(po3[:, c, :], PTe[bi][:, c, :], vv[:, jk, :], bi == 0, bi == len(blocks) - 1,
                                         r=[f"PTe{bi}", vvk], w=[pok], inc=(c == 3 and bi == len(blocks) - 1))
                            S.tt("dve", den[:, jk, :], po3[:, :, 64], esink[:, jk * 4:(jk + 1) * 4], ALU.add,
                                 r=[pok, "esink"], w=["den"])
                            S.op("dve", lambda E, jk=jk: E.reciprocal(out=den[:, jk, :], in_=den[:, jk, :]), r=["den"],
                                 w=["den"])
                            S.tt("dve", mix_all[:, i, jk * 256:(jk + 1) * 256].rearrange("p (c d) -> p c d", c=4),
                                 po3[:, :, 0:64], den[:, jk, :].unsqueeze(2).to_broadcast([128, 4, 64]), ALU.mult,
                                 r=[pok, "den"], w=[f"mix{i}"])
                        if i == 0:
                            stop("t0attn")
                        if i == 1:
                            stop("t1attn")
                        g_ = gab[:, 0, :]
                        beta_ = gab[:, 1, :]
                        gc_ = gab[:, 2, :]
                        egc_ = gab[:, 3, :]
                        kds_ = gab[:, 4, :]
                        atot_ = gab[:, 5, :]
                        tmp_ = gab[:, 6, :]
                        nbeta_ = gab[:, 7, :]
                        S.tt("dve", tmp_, tm_ab[:, 0:4], dtb[:], ALU.add, r=["tm_ab", "dtb"], w=["g_tmp"])
                        S.act(tmp_, tmp_, AF.Exp, r=["g_tmp"], w=["g_tmp"])
                        S.act(tmp_, tmp_, AF.Ln, r=["g_tmp"], w=["g_tmp"], bias=1.0)
                        S.tt("dve", g_, tmp_, nexpA[:], ALU.mult, r=["g_tmp", "nexpA"], w=["g_g"])
                        S.act(beta_, tm_ab[:, 4:8], AF.Exp, r=["tm_ab"], w=["g_beta"], scale=-1.0)
                        S.ts("dve", beta_, beta_, 1.0, None, ALU.add, None, r=["g_beta"], w=["g_beta"])
                        S.op("dve", lambda E: E.reciprocal(out=beta_, in_=beta_), r=["g_beta"], w=["g_beta"])
                        S.ts("dve", nbeta_, beta_, -1.0, None, ALU.mult, None, r=["g_beta"], w=["g_nbeta"])
                        pf, pfk = S.psf()
                        S.mm(pf[:, 0:4], m_up[:], g_, True, True, r=["m_up", "g_g"], w=[pfk])
                        S.mm(pf[:, 4:8], onesf[:], g_, True, True, r=["onesf", "g_g"], w=[pfk])
                        S.cp("dve", gc_, pf[:, 0:4], r=[pfk], w=["g_gc"])
                        S.act(egc_, pf[:, 0:4], AF.Exp, r=[pfk], w=["g_egc"])
                        S.act(atot_, pf[:, 4:8], AF.Exp, r=[pfk], w=["g_atot"])
                        S.tt("dve", kds_, pf[:, 4:8], gc_, ALU.subtract, r=[pfk, "g_gc"], w=["g_kds"])
                        S.act(kds_, kds_, AF.Exp, r=["g_kds"], w=["g_kds"])
                        pD, pDk = S.psf()
                        pDT, pDTk = S.psf()
                        pG, pGk = S.psf()
                        pA, pAk = S.psf()
                        pD3 = pD[:].rearrange("p (h t) -> p h t", h=4)
                        pDT3 = pDT[:].rearrange("p (h t) -> p h t", h=4)
                        pG3 = pG[:].rearrange("p (h t) -> p h t", h=4)
                        pA3 = pA[:].rearrange("p (h t) -> p h t", h=4)
                        for h in range(4):
                            if h % 2:
                                S.act(gU[:, h, :], m_up[:], AF.Copy, r=["m_up", "g_g"], w=[f"gU{h}"], scale=gab[:, 0, h:h + 1])
                            else:
                                S.ts("dve", gU[:, h, :], m_up[:], gab[:, 0, h:h + 1], None, ALU.mult, None,
                                     r=["m_up", "g_g"], w=[f"gU{h}"])
                        for h in range(4):
                            S.mm(pD3[:, h, :], gU[:, h, :], m_strict[:], True, True, r=[f"gU{h}", "m_strict"], w=[pDk],
                                 inc=(h == 3))
                        for h in range(4):
                            S.mm(pDT3[:, h, :], m_strict[:], gU[:, h, :], True, True, r=[f"gU{h}", "m_strict"], w=[pDTk],
                                 inc=(h == 3))
                        for h in range(4):
                            S.mm(pG3[:, h, :], qkT[:, 4 + h, tok], qkT[:, 4 + h, tok], True, True, r=[f"qkT{4 + h}_{PAR}"],
                                 w=[pGk], inc=(h == 3))
                        for h in range(4):
                            S.mm(pA3[:, h, :], qkT[:, 4 + h, tok], qkT[:, h, tok], True, True,
                                 r=[f"qkT{4 + h}_{PAR}", f"qkT{h}_{PAR}"], w=[pAk], inc=(h == 3))
                        S.act(eD[:], pD3, AF.Exp, r=[pDk], w=["eD"])
                        S.tt("dve", eD[:], eD[:], m_strict[:].unsqueeze(1).to_broadcast([128, 4, 128]), ALU.mult,
                             r=["eD", "m_strict"], w=["eD"])
                        S.act(eDT[:], pDT3, AF.Exp, r=[pDTk], w=["eDT"])
                        S.tt("pool", eDT[:], eDT[:], m_up[:].unsqueeze(1).to_broadcast([128, 4, 128]), ALU.mult,
                             r=["eDT", "m_up"], w=["eDT"])
                        P0 = Pm[0]
                        for h in range(4):
                            S.stt("dve", P0[:, h, :], pG3[:, h, :], gab[:, 7, h:h + 1], eD[:, h, :], ALU.mult, ALU.mult,
                                  r=[pGk, "g_nbeta", "eD"], w=[f"Pm0g{h // 2}"])
                        S.tt("dve", ATb[:], pA3, eDT[:], ALU.mult, r=[pAk, "eDT"], w=["ATb"])
                        pt, ptk = S.psf()
                        pt3 = pt[:].rearrange("p (h t) -> p h t", h=4)
                        for h in range(4):
                            S.tr(pt3[:, h, :], P0[:, h, :], idf[:], r=[f"Pm0g{h // 2}", "idf"], w=[ptk], inc=(h == 3))
                        S.cp("act", PmT[0][:], pt3, r=[ptk], w=["PmT0g0", "PmT0g1"])
                        S.tt("dve", Qm[0][:], pt3, idf[:].unsqueeze(1).to_broadcast([128, 4, 128]), ALU.add,
                             r=[ptk, "idf"], w=["Qm0g0", "Qm0g1"])
                        NIT = 6
                        GR = ((0, 2), (2, 4))
                        for k in range(NIT):
                            a, b = k % 2, (k + 1) % 2
                            last = (k == NIT - 1)
                            pPs, pPTs = [], []
                            for gi, (h0, h1) in enumerate(GR):
                                pP, pPk = S.psf()
                                pP3 = pP[:, 0:256].rearrange("p (h t) -> p h t", h=2)
                                for h in range(h0, h1):
                                    S.mm(pP3[:, h - h0, :], PmT[a][:, h, :], Pm[a][:, h, :], True, True,
                                         r=[f"PmT{a}g{gi}", f"Pm{a}g{gi}"], w=[pPk], inc=(h == h1 - 1))
                                pPs.append((pP3, pPk))
                                if not last:
                                    pPT, pPTk = S.psf()
                                    pPT3 = pPT[:, 0:256].rearrange("p (h t) -> p h t", h=2)
                                    for h in range(h0, h1):
                                        S.mm(pPT3[:, h - h0, :], Pm[a][:, h, :], PmT[a][:, h, :], True, True,
                                             r=[f"PmT{a}g{gi}", f"Pm{a}g{gi}"], w=[pPTk], inc=(h == h1 - 1))
                                    pPTs.append((pPT3, pPTk))
                            for gi, (h0, h1) in enumerate(GR):
                                S.cp("act", Pm[b][:, h0:h1, :], pPs[gi][0], r=[pPs[gi][1]], w=[f"Pm{b}g{gi}"])
                                if not last:
                                    S.cp("dve", PmT[b][:, h0:h1, :], pPTs[gi][0], r=[pPTs[gi][1]], w=[f"PmT{b}g{gi}"])
                            pQs = []
                            for gi, (h0, h1) in enumerate(GR):
                                pQ, pQk = S.psf()
                                pQ3 = pQ[:, 0:256].rearrange("p (h t) -> p h t", h=2)
                                for h in range(h0, h1):
                                    S.mm(pQ3[:, h - h0, :], Pm[b][:, h, :], Qm[a][:, h, :], True, True,
                                         r=[f"Pm{b}g{gi}", f"Qm{a}g{gi}"], w=[pQk], inc=(h == h1 - 1))
                                pQs.append((pQ3, pQk))
                            for gi, (h0, h1) in enumerate(GR):
                                if not last:
                                    S.tt("dve", Qm[b][:, h0:h1, :], pQs[gi][0], Qm[a][:, h0:h1, :], ALU.add,
                                         r=[pQs[gi][1], f"Qm{a}g{gi}"], w=[f"Qm{b}g{gi}"])
                                else:
                                    S.tt("dve", Qb[:, h0:h1, :], pQs[gi][0], Qm[a][:, h0:h1, :], ALU.add,
                                         r=[pQs[gi][1], f"Qm{a}g{gi}"], w=["Qb"])
                        kn3 = knv[:, 0:4, :]
                        v3 = knv[:, 4:8, :]
                        S.tt("dve", tmp_, beta_, egc_, ALU.mult, r=["g_beta", "g_egc", "g_tmp"], w=["g_tmp"])
                        S.tt("pool", kbe[:], kn3, tmp_.unsqueeze(2).to_broadcast([128, 4, 128]), ALU.mult,
                             r=["knv", "g_tmp"], w=["kbe"])
                        S.tt("pool", kdc[:], kn3, kds_.unsqueeze(2).to_broadcast([128, 4, 128]), ALU.mult,
                             r=["knv", "g_kds"], w=["kdc"])
                        S.tt("dve", vb[:], v3, beta_.unsqueeze(2).to_broadcast([128, 4, 128]), ALU.mult,
                             r=["knv", "g_beta"], w=["vb"])
                        pw, pwk = S.psf()
                        pw3 = pw[:].rearrange("p (h t) -> p h t", h=4)
                        for h in range(4):
                            S.mm(pw3[:, h, :], kbe[:, h, :], Qb[:, h, :], True, True, r=["kbe", "Qb"], w=[pwk],
                                 inc=(h == 3))
                        S.op("act", lambda E: E.mul(out=nwT[:], in_=pw3, mul=-1.0), r=[pwk], w=["nwT"])
                        pv, pvk = S.psf()
                        pv3 = pv[:].rearrange("p (h t) -> p h t", h=4)
                        for h in range(4):
                            S.mm(pv3[:, h, :], Qb[:, h, :], vb[:, h, :], True, False, r=["Qb", "vb"], w=[pvk], inc=False)
                            S.mm(pv3[:, h, :], nwT[:, h, :], Sb[:, h, :], False, True, r=["nwT", "Sb"], w=[pvk],
                                 inc=(h == 3))
                        S.cp("act", vnew[:], pv3, r=[pvk], w=["vnew"])
                        po1, po1k = S.psf()
                        po13 = po1[:].rearrange("p (h t) -> p h t", h=4)
                        for h in range(4):
                            S.mm(po13[:, h, :], qkT[:, h, tok], Sb[:, h, :], True, True, r=[f"qkT{h}_{PAR}", "Sb"], w=[po1k],
                                 inc=(h == 3))
                        po2, po2k = S.psf()
                        po23 = po2[:].rearrange("p (h t) -> p h t", h=4)
                        for h in range(4):
                            S.mm(po23[:, h, :], ATb[:, h, :], vnew[:, h, :], True, True, r=["ATb", "vnew"], w=[po2k],
                                 inc=(h == 3))
                        pS, pSk = S.psf()
                        pS3 = pS[:].rearrange("p (h t) -> p h t", h=4)
                        for h in range(4):
                            S.mm(pS3[:, h, :], kdc[:, h, :], vnew[:, h, :], True, True, r=["kdc", "vnew"], w=[pSk],
                                 inc=(h == 3))
                        S.tt("dve", o1s[:], po13, egc_.unsqueeze(2).to_broadcast([128, 4, 128]), ALU.mult,
                             r=[po1k, "g_egc"], w=["o1s"])
                        S.tt("dve", og[:], po23, o1s[:], ALU.add, r=[po2k, "o1s"], w=["og"])
                        S.tt("pool", Stmp[:], Sf[:], atot_.unsqueeze(2).to_broadcast([128, 4, 128]), ALU.mult,
                             r=["Sf", "g_atot"], w=["Stmp"])
                        S.tt("dve", Sf[:], pS3, Stmp[:], ALU.add, r=[pSk, "Stmp"], w=["Sf"])
                        S.cp("act", Sb[:], Sf[:], r=["Sf"], w=["Sb"])
                        S.tt("dve", osq[:], og[:], og[:], ALU.mult, r=["og"], w=["osq"])
                        S.op("dve", lambda E: E.tensor_reduce(out=ssq[:], in_=osq[:], axis=AX.X, op=ALU.add), r=["osq"],
                             w=["ssq"])
                        S.act(ssq[:], ssq[:], AF.Ln, r=["ssq"], w=["ssq"], bias=NORM_EPS, scale=1.0 / 128.0)
                        S.act(ssq[:], ssq[:], AF.Exp, r=["ssq"], w=["ssq"], scale=-0.5)
                        S.tt("dve", og[:], og[:], ssq[:].unsqueeze(2).to_broadcast([128, 4, 128]), ALU.mult,
                             r=["og", "ssq"], w=["og"])
                        S.tt("dve", mix_all[:, i, 512:1024].rearrange("p (h d) -> p h d", h=4), og[:],
                             zs2[j][:].rearrange("p (h d) -> p h d", h=4), ALU.mult, r=["og", f"zs2_{PAR}_{j}"], w=[f"mix{i}"])
                        if i == 0:
                            stop("t0")
                        if i == 1:
                            stop("t1")
                        if i == 3:
                            stop("t3")

                    NST = SEQ // ST
                    fm(0)
                    for st in range(NST):
                        tile(st, 0)
                        if st + 1 < NST:
                            fm(st + 1)
                        tile(st, 1)
                    S.dma("sp", ng_p.rearrange("h k v -> k h v"), Sf[:], r=["Sf"], slot="ng_p")
                    if dbg:
                        dump("mix", mix_all[:, 0:NT, :], [128, NT, D], [f"mix{i}" for i in range(NT)])
                    S.barrier()


            y_acc = sb(es0, "y_acc", [128, NT + 1, D])
            with ExitStack() as es:
                w_out_bf = sb(es, "w_out_bf", [128, 8, D], BF16)
                g1 = sb(es, "g1", [128, D])
                b1 = sb(es, "b1", [128, D])
                mixT = [sb(es, f"mixT{i}", [128, 8, 128], BF16) for i in range(2)]
                xf = [sb(es, f"xf{i}", [128, D]) for i in range(2)]
                tb = [sb(es, f"tb{i}", [128, D]) for i in range(3)]
                stats = [sb(es, f"stats{i}", [128, 2, 6]) for i in range(3)]
                mv = [sb(es, f"mv{i}", [128, 2]) for i in range(3)]
                S.dma("pool", w_out_bf[:], w_out.rearrange("(c p) n -> p c n", p=128), w=["w_out"])
                S.dma("sp", g1[:], ln1g_d.partition_broadcast(128), w=["g1"])
                S.dma("sp", b1[:], ln1b_d.partition_broadcast(128), w=["b1"])
                def ln1_A(i):
                    n = 128 if i < NT else NS * TS
                    xsrc = x_p[i * 128:(i + 1) * 128, :] if i < NT else x_s
                    xft, xfk = xf[i % 2], f"xf{i % 2}"
                    S.dma("sp", xft[:n, :], xsrc, w=[xfk])
                    mt, mtk = mixT[i % 2], f"mixT{i % 2}"
                    pb, pbk = S.psb()
                    for c in range(8):
                        S.tr(pb[:, c * 128:c * 128 + n], mix_all[:n, i, c * 128:(c + 1) * 128], idb[:n, :n],
                             r=["idb"], w=[pbk], inc=(c == 7))
                    S.cp("act", mt[:, :, 0:n], pb[:].rearrange("p (c t) -> p c t", c=8)[:, :, 0:n], r=[pbk], w=[mtk])
                    tbt, tbk = tb[i % 3], f"tb{i % 3}"
                    st_, mv_, mvk = stats[i % 3], mv[i % 3], f"mv{i % 3}"
                    for half in range(2):
                        pf, pfk = S.psf()
                        for c in range(8):
                            S.mm(pf[:n, :], mt[:, c, 0:n], w_out_bf[:, c, half * 512:(half + 1) * 512], c == 0, c == 7,
                                 r=[mtk, "w_out"], w=[pfk])
                        S.stt("dve", tbt[:n, half * 512:(half + 1) * 512], xft[:n, half * 512:(half + 1) * 512], ALPHA,
                              pf[:n, :], ALU.mult, ALU.add, r=[xfk, pfk], w=[tbk])
                        S.op("dve", lambda E, half=half: E.bn_stats(out=st_[:n, half, :],
                                                                    in_=tbt[:n, half * 512:(half + 1) * 512]),
                             r=[tbk], w=[mvk])
                    S.op("dve", lambda E: E.bn_aggr(out=mv_[:n, :], in_=st_[:n].rearrange("p a b -> p (a b)")),
                         r=[mvk], w=[mvk])
                    S.act(mv_[:n, 1:2], mv_[:n, 1:2], AF.Ln, r=[mvk], w=[mvk], bias=NORM_EPS)
                    S.act(mv_[:n, 1:2], mv_[:n, 1:2], AF.Exp, r=[mvk], w=[mvk], scale=-0.5)

                def ln1_B(i):
                    n = 128 if i < NT else NS * TS
                    tbt, tbk = tb[i % 3], f"tb{i % 3}"
                    mv_, mvk = mv[i % 3], f"mv{i % 3}"
                    S.stt("dve", mv_[:n, 0:1], mv_[:n, 0:1], -1.0, mv_[:n, 1:2], ALU.mult, ALU.mult, r=[mvk], w=[mvk])
                    S.act(tbt[:n, :], tbt[:n, :], AF.Identity, r=[tbk, mvk], w=[tbk], bias=mv_[:n, 0:1], scale=mv_[:n, 1:2])
                    S.tt("dve", tbt[:n, :], tbt[:n, :], g1[:n, :], ALU.mult, r=[tbk, "g1"], w=[tbk])
                    S.tt("pool", y_acc[:n, i, :], tbt[:n, :], b1[:n, :], ALU.add, r=[tbk, "b1"], w=[f"y{i}"])

                for i in range(NT + 1):
                    ln1_A(i)
                    if i >= 1:
                        ln1_B(i - 1)
                ln1_B(NT)
                if dbg:
                    dump("x1", y_acc[:], [128, NT + 1, D], [f"y{i}" for i in range(NT + 1)])
                S.barrier()

            with ExitStack() as es:
                x1T = mix_all[:].rearrange("p a b -> p (a b)")[:, 0:8 * NTOK].rearrange("p (k t) -> p k t", k=8)
                comb = sb(es, "comb", [128, NT + 1, NE])
                wr_sb = sb(es, "wr_sb", [128, 8, 36])
                x1Tf = sb(es, "x1Tf", [128, 8, 128])
                T_ = NT + 1
                rl_all = sb(es, "rl_all", [128, T_, 36])
                r_oh = sb(es, "r_oh", [128, T_, 4])
                r_t4 = sb(es, "r_t4", [128, T_, 4])
                r_pr = sb(es, "r_pr", [128, T_, 4, 8])
                r_es = sb(es, "r_es", [128, T_, 8])
                r_m1 = sb(es, "r_m1", [128, T_, 8])
                r_e2 = sb(es, "r_e2", [128, T_, 8])
                r_m2 = sb(es, "r_m2", [128, T_, 8])
                r_ew = sb(es, "r_ew", [128, T_, 8])
                r_s = sb(es, "r_s", [128, 8, T_])
                S.op("pool", lambda E: E.memset(rl_all[:], 0.0), w=["rl_all"])
                S.dma("sp", wr_sb[:], wr_d.rearrange("(c p) n -> p c n", p=128), w=["wr"])
                YK = [f"y{i}" for i in range(NT + 1)]
                for i in range(NT + 1):
                    n = 128 if i < NT else NS * TS
                    t0 = i * 128
                    pfa, pfak = S.psf()
                    pfb, pfbk = S.psf()
                    for kc in range(8):
                        pf, pfk = (pfa, pfak) if kc < 4 else (pfb, pfbk)
                        S.tr(pf[:, (kc % 4) * 128:(kc % 4) * 128 + n], y_acc[:n, i, kc * 128:(kc + 1) * 128], idf[:n, :n],
                             r=[YK[i], "idf"], w=[pfk], inc=(kc % 4 == 3))
                    for hf, (pf, pfk) in enumerate(((pfa, pfak), (pfb, pfbk))):
                        src = pf[:].rearrange("p (k t) -> p k t", k=4)[:, :, 0:n]
                        S.cp("act", x1Tf[:, hf * 4:(hf + 1) * 4, 0:n], src, r=[pfk], w=["x1Tf"])
                        S.cp("dve", x1T[:, hf * 4:(hf + 1) * 4, t0:t0 + n], src, r=[pfk], w=["x1T"])
                    pr, prk = S.psf()
                    for kc in range(8):
                        S.mm(pr[:n, 0:36], x1Tf[:, kc, 0:n], wr_sb[:, kc, :], kc == 0, kc == 7, r=["x1Tf", "wr"], w=[prk])
                    S.cp("dve", rl_all[:n, i, :], pr[:n, 0:36], r=[prk], w=["rl_all"])
                    S.op("act", lambda E, n=n, i=i: E.mul(out=y_acc[:n, i, :], in_=y_acc[:n, i, :], mul=ALPHA),
                         r=[YK[i], "x1T", "x1Tf"], w=[YK[i]])
                R = ["rl_all", "rr"]
                gl = rl_all[:, :, 0:4]
                el = rl_all[:, :, 4:36].rearrange("p t (g e) -> p t g e", g=4)
                bc3 = lambda a, k: a.unsqueeze(2).to_broadcast([128, T_, k])
                gmax, gtp, m1, m2, ex, w1, w2 = (r_s[:, j, :] for j in range(7))
                S.op("dve", lambda E: E.tensor_reduce(out=gmax, in_=gl, axis=AX.X, op=ALU.max), r=R, w=R)
                S.tt("dve", r_oh[:], gl, bc3(gmax, 4), ALU.is_equal, r=R, w=R)
                S.tt("dve", r_t4[:], gl, bc3(gmax, 4), ALU.subtract, r=R, w=R)
                S.act(r_t4[:], r_t4[:], AF.Exp, r=R, w=R)
                S.op("dve", lambda E: E.tensor_reduce(out=gtp, in_=r_t4[:], axis=AX.X, op=ALU.add), r=R, w=R)
                S.op("dve", lambda E: E.reciprocal(out=gtp, in_=gtp), r=R, w=R)
                S.tt("dve", r_pr[:], el, r_oh[:].unsqueeze(3).to_broadcast([128, T_, 4, 8]), ALU.mult, r=R, w=R)
                S.op("dve", lambda E: E.tensor_reduce(out=r_es[:], in_=r_pr[:].rearrange("p t g e -> p t e g"), axis=AX.X,
                                                      op=ALU.add), r=R, w=R)
                S.op("dve", lambda E: E.tensor_reduce(out=m1, in_=r_es[:], axis=AX.X, op=ALU.max), r=R, w=R)
                S.tt("dve", r_m1[:], r_es[:], bc3(m1, 8), ALU.is_equal, r=R, w=R)
                S.stt("dve", r_e2[:], r_m1[:], -1e30, r_es[:], ALU.mult, ALU.add, r=R, w=R)
                S.op("dve", lambda E: E.tensor_reduce(out=m2, in_=r_e2[:], axis=AX.X, op=ALU.max), r=R, w=R)
                S.tt("dve", r_m2[:], r_e2[:], bc3(m2, 8), ALU.is_equal, r=R, w=R)
                S.tt("dve", ex, m2, m1, ALU.subtract, r=R, w=R)
                S.act(ex, ex, AF.Exp, r=R, w=R)
                S.ts("dve", w1, ex, 1.0, None, ALU.add, None, r=R, w=R)
                S.op("dve", lambda E: E.reciprocal(out=w1, in_=w1), r=R, w=R)
                S.tt("dve", w2, ex, w1, ALU.mult, r=R, w=R)
                S.tt("dve", w1, w1, gtp, ALU.mult, r=R, w=R)
                S.tt("dve", w2, w2, gtp, ALU.mult, r=R, w=R)
                S.tt("dve", r_ew[:], r_m1[:], bc3(w1, 8), ALU.mult, r=R, w=R)
                S.tt("dve", r_m2[:], r_m2[:], bc3(w2, 8), ALU.mult, r=R, w=R)
                S.tt("dve", r_ew[:], r_ew[:], r_m2[:], ALU.add, r=R, w=R)
                S.tt("dve", comb[:].rearrange("p t (g e) -> p t g e", g=4), r_oh[:].unsqueeze(3).to_broadcast([128, T_, 4, 8]),
                     r_ew[:].unsqueeze(2).to_broadcast([128, T_, 4, 8]), ALU.mult, r=R, w=["comb"])
                if dbg:
                    dump("comb", comb[:], [128, NT + 1, NE], ["comb"])

                wg = [sb(es, f"wg{i}", [128, 8, 256], BF16) for i in range(2)]
                wu = [sb(es, f"wu{i}", [128, 8, 256], BF16) for i in range(2)]
                wd = [sb(es, f"wd{i}", [128, 2, D], BF16) for i in range(2)]
                hT = [sb(es, f"hT{i}", [128, 2, NTOK], BF16) for i in range(2)]
                sgt = [sb(es, f"sgt{i}", [128, 512]) for i in range(2)]
                spans = [(t, min(512, NTOK - t)) for t in range(0, NTOK, 512)]
                for pbt in S.psb_list:
                    S.psf_list.append(pbt[:].bitcast(F32))
                acct = [sb(es, f"acct{i}", [128, 512]) for i in range(6)]
                g2 = sb(es, "g2", [128, D])
                b2 = sb(es, "b2", [128, D])
                ob = [sb(es, f"ob{i}", [128, D]) for i in range(3)]
                stats2 = [sb(es, f"stats2_{i}", [128, 2, 6]) for i in range(3)]
                mv2 = [sb(es, f"mv2_{i}", [128, 2]) for i in range(3)]
                S.dma("sp", g2[:], ln2g_d.partition_broadcast(128), w=["g2"])
                S.dma("sp", b2[:], ln2b_d.partition_broadcast(128), w=["b2"])
                acc_i = 0

                def ln2_A(i):
                    n = 128 if i < NT else NS * TS
                    st2, mvt, mk_ = stats2[i % 3], mv2[i % 3], f"mv2_{i % 3}"
                    for half in range(2):
                        S.op("dve", lambda E, half=half: E.bn_stats(out=st2[:n, half, :],
                                                                    in_=y_acc[:n, i, half * 512:(half + 1) * 512]),
                             r=[YK[i]], w=[mk_])
                    S.op("dve", lambda E: E.bn_aggr(out=mvt[:n, :], in_=st2[:n].rearrange("p a b -> p (a b)")),
                         r=[mk_], w=[mk_])
                    S.act(mvt[:n, 1:2], mvt[:n, 1:2], AF.Ln, r=[mk_], w=[mk_], bias=NORM_EPS)
                    S.act(mvt[:n, 1:2], mvt[:n, 1:2], AF.Exp, r=[mk_], w=[mk_], scale=-0.5)

                def ln2_B(i):
                    n = 128 if i < NT else NS * TS
                    mvt, mk_ = mv2[i % 3], f"mv2_{i % 3}"
                    obt, obk = ob[i % 3], f"ob{i % 3}"
                    S.stt("dve", mvt[:n, 0:1], mvt[:n, 0:1], -1.0, mvt[:n, 1:2], ALU.mult, ALU.mult, r=[mk_], w=[mk_])
                    S.act(obt[:n, :], y_acc[:n, i, :], AF.Identity, r=[YK[i], mk_], w=[obk], bias=mvt[:n, 0:1],
                          scale=mvt[:n, 1:2])
                    S.tt("dve", obt[:n, :], obt[:n, :], g2[:n, :], ALU.mult, r=[obk, "g2"], w=[obk])
                    S.tt("pool", obt[:n, :], obt[:n, :], b2[:n, :], ALU.add, r=[obk, "b2"], w=[obk])
                    dst = y_p[i * 128:(i + 1) * 128, :] if i < NT else y_s
                    S.dma("sp", dst, obt[:n, :], r=[obk], slot=obk + "o")

                def load_expert(e):
                    b = e % 2
                    S.dma("pool", wg[b][:], wg_d[e].rearrange("(c p) f -> p c f", p=128), w=[f"wg{b}"])
                    S.dma("pool", wu[b][:], wu_d[e].rearrange("(c p) f -> p c f", p=128), w=[f"wu{b}"])
                    S.dma("pool", wd[b][:], wd_d[e].rearrange("(c p) n -> p c n", p=128), w=[f"wd{b}"])

                load_expert(0)
                for e in range(NE):
                    b = e % 2
                    if e + 1 < NE:
                        load_expert(e + 1)
                    hk = f"hT{b}"
                    si = 0
                    for fc in range(2):
                        for (t0, tn) in spans:
                            pg, pgk = S.psf()
                            pu, puk = S.psf()
                            for kc in range(8):
                                S.mm(pg[:, 0:tn], wg[b][:, kc, fc * 128:(fc + 1) * 128], x1T[:, kc, t0:t0 + tn], kc == 0,
                                     kc == 7, r=[f"wg{b}", "x1T"], w=[pgk])
                            for kc in range(8):
                                S.mm(pu[:, 0:tn], wu[b][:, kc, fc * 128:(fc + 1) * 128], x1T[:, kc, t0:t0 + tn], kc == 0,
                                     kc == 7, r=[f"wu{b}", "x1T"], w=[puk])
                            sg_, sgk = sgt[si % 2], f"sgt{si % 2}"
                            si += 1
                            S.act(sg_[:, 0:tn], pg[:, 0:tn], AF.Silu, r=[pgk], w=[sgk])
                            S.tt("dve", hT[b][:, fc, t0:t0 + tn], pu[:, 0:tn], sg_[:, 0:tn], ALU.mult, r=[puk, sgk], w=[hk])
                    for i in range(NT + 1):
                        n = 128 if i < NT else NS * TS
                        t0 = i * 128
                        for half in range(2):
                            py, pyk = S.psf()
                            for fc in range(2):
                                S.mm(py[:n, :], hT[b][:, fc, t0:t0 + n], wd[b][:, fc, half * 512:(half + 1) * 512], fc == 0,
                                     fc == 1, r=[hk, f"wd{b}"], w=[pyk])
                            ysl = y_acc[:n, i, half * 512:(half + 1) * 512]
                            if (i * 2 + half) % 2 == 1:
                                at, atk = acct[acc_i % 6], f"acct{acc_i % 6}"
                                acc_i += 1
                                S.act(at[:n, :], py[:n, :], AF.Copy, r=[pyk, "comb"], w=[atk], scale=comb[:n, i, e:e + 1])
                                S.tt("pool", ysl, ysl, at[:n, :], ALU.add, r=[atk, YK[i]], w=[YK[i]])
                            else:
                                S.stt("dve", ysl, py[:n, :], comb[:n, i, e:e + 1], ysl, ALU.mult, ALU.add,
                                      r=[pyk, "comb", YK[i]], w=[YK[i]])
                        if e == NE - 1:
                            if i >= 1:
                                ln2_A(i - 1)
                            if i >= 2:
                                ln2_B(i - 2)
                ln2_A(NT)
                ln2_B(NT - 1)
                ln2_B(NT)
        except _Stop:
            pass
        S.final_wait()
    return nc, dbg_outs


_CACHE = {}


def _get_nc(dbg=False):
    if dbg not in _CACHE:
        _CACHE[dbg] = build(dbg)
    return _CACHE[dbg]


def make_in_maps(inputs):
    f = lambda a: np.ascontiguousarray(np.asarray(a, dtype=np.float32))
    g = {k: f(v) for k, v in inputs.items()}
    wr = np.ascontiguousarray(np.concatenate([g["w_router_group"][0], g["w_router_expert"][0]], axis=1))
    maps = []
    for c in range(NCORES):
        s0, s1 = c * NS, (c + 1) * NS
        maps.append({
            "x_p": g["x_prompt"][c],
            "x_s": np.ascontiguousarray(g["x_sample"][s0:s1].reshape(NS * TS, D)),
            "ck": np.ascontiguousarray(g["cache_attn_k"][0, s0:s1].reshape(NS, 128, 128)),
            "cv": np.ascontiguousarray(g["cache_attn_v"][0, s0:s1].reshape(NS, 128, 128)),
            "sg": np.ascontiguousarray(g["state_gdn"][0, s0:s1]),
            "sc": np.ascontiguousarray(g["state_conv"][0, s0:s1].reshape(NS * 3, 1536)),
            "w_in": g["w_in"][0], "w_out": g["w_out"][0], "sinks": g["attn_sinks"][0], "conv_w": g["conv_w"][0],
            "a_log": g["a_log"][0], "dt_bias": g["dt_bias"][0], "gnw": g["gdn_norm_w"][0],
            "ln1_g": g["ln1_g"][0], "ln1_b": g["ln1_b"][0], "w_r": wr,
            "w_gate": g["w_gate"][0], "w_up": g["w_up"][0], "w_down": g["w_down"][0],
            "ln2_g": g["ln2_g"][0], "ln2_b": g["ln2_b"][0],
        })
    return maps


def assemble(results):
    cat = lambda k: np.stack([np.asarray(r[k]) for r in results])
    y_p = cat("y_p")
    y_s = cat("y_s").reshape(128, TS, D)
    nk_p = cat("nk_p").reshape(1, 8, 128, 2, 64)
    nv_p = cat("nv_p").reshape(1, 8, 128, 2, 64)
    ng_p = cat("ng_p").reshape(1, 8, 4, 128, 128)
    nc_p = cat("nc_p").reshape(1, 8, 3, 1536)
    nk_s = cat("nk_s").reshape(1, 128, 128, 2, 64)
    nv_s = cat("nv_s").reshape(1, 128, 128, 2, 64)
    ng_s = cat("ng_s").reshape(1, 128, 4, 128, 128)
    nc_s = cat("nc_s").reshape(1, 128, 3, 1536)
    return tuple(np.ascontiguousarray(a.astype(np.float32)) for a in
                 (y_p, y_s, nk_p, nv_p, ng_p, nc_p, nk_s, nv_s, ng_s, nc_s))


def kernel(**inputs):
    nc, _ = _get_nc(False)
    maps = make_in_maps(inputs)
    res = run_bass_kernel_spmd(nc, maps, core_ids=list(range(NCORES)))
    return assemble(res.results)
```

```python
import math
from contextlib import ExitStack

import numpy as np
import concourse.bass as bass
import concourse.mybir as mybir
from concourse.bass_utils import run_bass_kernel_spmd

F32 = mybir.dt.float32
BF16 = mybir.dt.bfloat16
I32 = mybir.dt.int32
AF = mybir.ActivationFunctionType
ALU = mybir.AluOpType
AX = mybir.AxisListType

NCORES = 8
D = 1024
SEQ = 2048
NT = 16
NS = 16
TS = 4
NTOK = SEQ + NS * TS
PAST = 8192
INC = 2824
ALPHA = 2.0 ** 0.25
NORM_EPS = 1e-5
L2_EPS = 1e-6
THETA = 500000.0
NE = 32
MAGIC = 12582912.0


class _Stop(Exception):
    pass


import os
STOP = os.environ.get("K_STOP", "")


_SCHED = []


def stop(tag):
    if STOP == tag:
        _SCHED[-1].dead = True


class Sched:
    def __init__(self, nc, es):
        self.nc = nc
        self.es = es
        self.E = {"pe": nc.tensor, "act": nc.scalar, "dve": nc.vector, "pool": nc.gpsimd, "sp": nc.sync}
        self.semh = {}
        for k in self.E:
            self.semh["e_" + k] = es.enter_context(nc.semaphore("sem_" + k))
        self.cnt = {k: 0 for k in self.E}
        self.seen = {k: {} for k in self.E}
        self.lastw = {}
        self.readers = {}
        self.pend = {k: [] for k in self.E}
        self.pend_r = {k: set() for k in self.E}
        self.pend_w = {k: set() for k in self.E}
        self.slots = {}
        self.psf_list = []
        self.psb_list = []
        self.psf_i = 0
        self.psb_i = 0
        self.dead = False
        _SCHED.append(self)

    def _waits(self, e, r, w, is_dma):
        need = {}

        def add(ev):
            semk, val, eng = ev
            if need.get(semk, 0) < val:
                need[semk] = val

        for k in list(r) + list(w):
            for e2 in self.E:
                if e2 != e or is_dma:
                    assert k not in self.pend_w[e2], f"key {k} pending write on {e2}"
        for k in w:
            for e2 in self.E:
                if e2 != e or is_dma:
                    assert k not in self.pend_r[e2], f"key {k} pending read on {e2}"
        for k in r:
            ev = self.lastw.get(k)
            if ev is not None:
                if ev[2] == e and e == "pe" and not is_dma:
                    continue
                add(ev)
            if k.startswith("ps"):
                for ev in self.readers.get(k, ()):
                    if ev[2] != e:
                        add(ev)
        for k in w:
            ev = self.lastw.get(k)
            if ev is not None and (is_dma or ev[2] != e or e != "pe"):
                add(ev)
            for ev in self.readers.get(k, ()):
                if is_dma or ev[2] != e or e != "pe":
                    add(ev)
        for semk, val in need.items():
            if self.seen[e].get(semk, 0) < val:
                self.E[e].wait_ge(self.semh[semk], val)
                self.seen[e][semk] = val

    def _register(self, r, w, ev):
        for k in r:
            self.readers.setdefault(k, []).append(ev)
        for k in w:
            self.lastw[k] = ev
            self.readers[k] = []

    def op(self, e, fn, r=(), w=(), inc=True):
        if self.dead:
            return None
        self._waits(e, r, w, False)
        ins = fn(self.E[e])
        if not inc:
            self.pend[e].append((tuple(r), tuple(w)))
            self.pend_r[e].update(r)
            self.pend_w[e].update(w)
            return ins
        self.cnt[e] += 1
        ins.then_inc(self.semh["e_" + e], 1)
        ev = ("e_" + e, self.cnt[e], e)
        for (pr, pw) in self.pend[e]:
            self._register(pr, pw, ev)
        self.pend[e] = []
        self.pend_r[e] = set()
        self.pend_w[e] = set()
        self._register(r, w, ev)
        return ins

    def dma(self, q, out, in_, r=(), w=(), slot=None):
        if slot is None:
            slot = w[0] if w else r[0]
        if self.dead:
            return None
        sk = "d_" + slot
        if sk not in self.slots:
            self.semh[sk] = self.es.enter_context(self.nc.semaphore(sk))
            self.slots[sk] = 0
        self._waits(q, r, w, True)
        ins = self.E[q].dma_start(out=out, in_=in_)
        self.slots[sk] += 16
        ins.then_inc(self.semh[sk], 16)
        ev = (sk, self.slots[sk], "dma")
        self._register(r, w, ev)
        return ins

    def barrier(self):
        if self.dead:
            return
        for e in self.E:
            assert not self.pend[e]
        for e in self.E:
            for e2 in self.E:
                if self.cnt[e2] > self.seen[e].get("e_" + e2, 0):
                    self.E[e].wait_ge(self.semh["e_" + e2], self.cnt[e2])
                    self.seen[e]["e_" + e2] = self.cnt[e2]
            for sk, v in self.slots.items():
                if v > self.seen[e].get(sk, 0):
                    self.E[e].wait_ge(self.semh[sk], v)
                    self.seen[e][sk] = v
        self.lastw = {}
        self.readers = {}

    def final_wait(self):
        self.dead = False
        for sk, v in self.slots.items():
            if v > self.seen["sp"].get(sk, 0):
                self.E["sp"].wait_ge(self.semh[sk], v)
                self.seen["sp"][sk] = v
        for e2 in self.E:
            if e2 != "sp" and self.cnt[e2] > self.seen["sp"].get("e_" + e2, 0):
                self.E["sp"].wait_ge(self.semh["e_" + e2], self.cnt[e2])

    def psf(self):
        i = self.psf_i % len(self.psf_list)
        self.psf_i += 1
        return self.psf_list[i], f"psf{i}"

    def psb(self):
        i = self.psb_i % len(self.psb_list)
        self.psb_i += 1
        return self.psb_list[i], f"psb{i}"

    def mm(self, out, lhsT, rhs, start, stop, r, w, inc=None):
        if inc is None:
            inc = stop
        return self.op("pe", lambda E: E.matmul(out, lhsT=lhsT, rhs=rhs, start=start, stop=stop), r, w, inc)

    def tr(self, out, in_, ident, r, w, inc=True):
        return self.op("pe", lambda E: E.transpose(out=out, in_=in_, identity=ident), r, w, inc)

    def act(self, out, in_, func, r, w, bias=0.0, scale=1.0, accum_out=None):
        if accum_out is not None:
            return self.op("act", lambda E: E.activation(out=out, in_=in_, func=func, bias=bias, scale=scale,
                                                         accum_out=accum_out), r, w)
        return self.op("act", lambda E: E.activation(out=out, in_=in_, func=func, bias=bias, scale=scale), r, w)

    def tt(self, e, out, in0, in1, op, r, w):
        return self.op(e, lambda E: E.tensor_tensor(out=out, in0=in0, in1=in1, op=op), r, w)

    def ts(self, e, out, in0, s1, s2, op0, op1, r, w):
        if s2 is None:
            return self.op(e, lambda E: E.tensor_scalar(out=out, in0=in0, scalar1=s1, scalar2=None, op0=op0), r, w)
        return self.op(e, lambda E: E.tensor_scalar(out=out, in0=in0, scalar1=s1, scalar2=s2, op0=op0, op1=op1), r, w)

    def stt(self, e, out, in0, scalar, in1, op0, op1, r, w):
        return self.op(e, lambda E: E.scalar_tensor_tensor(out=out, in0=in0, scalar=scalar, in1=in1, op0=op0, op1=op1),
                       r, w)

    def cp(self, e, out, in_, r, w):
        if e == "act":
            return self.op("act", lambda E: E.copy(out=out, in_=in_), r, w)
        return self.op(e, lambda E: E.tensor_copy(out=out, in_=in_), r, w)


def build(dbg=False):
    nc = bass.Bass("TRN2", target_bir_lowering=False)

    def din(name, shape, dt=F32):
        return nc.dram_tensor(name, shape, dt, kind="ExternalInput").ap()

    def dout(name, shape):
        return nc.dram_tensor(name, shape, F32, kind="ExternalOutput").ap()

    x_p = din("x_p", [SEQ, D])
    x_s = din("x_s", [NS * TS, D])
    ck_d = din("ck", [NS, 128, 128])
    cv_d = din("cv", [NS, 128, 128])
    sg_d = din("sg", [NS, 4, 128, 128])
    sc_d = din("sc", [NS * 3, 1536])
    w_in = din("w_in", [D, INC])
    w_out = din("w_out", [D, D])
    sinks_d = din("sinks", [8])
    convw_d = din("conv_w", [4, 1536])
    alog_d = din("a_log", [4])
    dtb_d = din("dt_bias", [4])
    gnw_d = din("gnw", [128])
    ln1g_d = din("ln1_g", [D])
    ln1b_d = din("ln1_b", [D])
    wr_d = din("w_r", [D, 36])
    wg_d = din("w_gate", [NE, D, 256])
    wu_d = din("w_up", [NE, D, 256])
    wd_d = din("w_down", [NE, 256, D])
    ln2g_d = din("ln2_g", [D])
    ln2b_d = din("ln2_b", [D])

    y_p = dout("y_p", [SEQ, D])
    y_s = dout("y_s", [NS * TS, D])
    nk_p = dout("nk_p", [128, 128])
    nv_p = dout("nv_p", [128, 128])
    ng_p = dout("ng_p", [4, 128, 128])
    nc_p = dout("nc_p", [3, 1536])
    nk_s = dout("nk_s", [NS, 128, 128])
    nv_s = dout("nv_s", [NS, 128, 128])
    ng_s = dout("ng_s", [NS, 4, 128, 128])
    nc_s = dout("nc_s", [NS * 3, 1536])
    dbg_outs = {}

    with ExitStack() as es0, nc.allow_non_contiguous_dma(reason="small transposed param loads"):
        S = Sched(nc, es0)
        try:

            def sb(es, name, shape, dt=F32):
                return es.enter_context(nc.sbuf_tensor(name, shape, dt))

            for i in range(6):
                S.psf_list.append(es0.enter_context(nc.psum_tensor(f"psf{i}", [128, 512], F32)))
            for i in range(2):
                S.psb_list.append(es0.enter_context(nc.psum_tensor(f"psb{i}", [128, 1024], BF16)))

            def dump(name, ap_src, shape, keys):
                if not dbg:
                    return
                o = dout("dbg_" + name, shape)
                dbg_outs[name] = o
                S.dma("pool" if ap_src.dtype != F32 else "sp", o, ap_src, r=keys, w=[], slot="dbg_" + name)

            onesf = sb(es0, "onesf", [128, 128])
            ones_bf = sb(es0, "ones_bf", [128, 128], BF16)
            idf = sb(es0, "idf", [128, 128])
            idb = sb(es0, "idb", [128, 128], BF16)
            m_strict = sb(es0, "m_strict", [128, 128])
            m_up = sb(es0, "m_up", [128, 128])
            mb_prev = sb(es0, "mb_prev", [128, 128], BF16)
            mb_cur = sb(es0, "mb_cur", [128, 128], BF16)
            S.op("pool", lambda E: E.memset(onesf[:], 1.0), w=["onesf"])
            S.op("pool", lambda E: E.memset(ones_bf[:], 1.0), w=["ones_bf"])
            S.op("pool", lambda E: E.affine_select(out=idf[:], in_=onesf[:], pattern=[[-1, 128]], compare_op=ALU.is_equal,
                                                   fill=0.0, base=0, channel_multiplier=1), r=["onesf"], w=["idf"])
            S.op("pool", lambda E: E.affine_select(out=m_strict[:], in_=onesf[:], pattern=[[-1, 128]], compare_op=ALU.is_gt,
                                                   fill=0.0, base=0, channel_multiplier=1), r=["onesf"], w=["m_strict"])
            S.op("pool", lambda E: E.affine_select(out=m_up[:], in_=onesf[:], pattern=[[1, 128]], compare_op=ALU.is_ge,
                                                   fill=0.0, base=0, channel_multiplier=-1), r=["onesf"], w=["m_up"])
            S.cp("dve", idb[:], idf[:], r=["idf"], w=["idb"])
            S.cp("dve", mb_prev[:], m_strict[:], r=["m_strict"], w=["mb_prev"])
            S.cp("dve", mb_cur[:], m_up[:], r=["m_up"], w=["mb_cur"])

            esink = sb(es0, "esink", [128, 8])
            nexpA = sb(es0, "nexpA", [128, 4])
            dtb = sb(es0, "dtb", [128, 4])
            gnw = sb(es0, "gnw_bc", [128, 128])
            cw = sb(es0, "cw", [128, 4, 12])
            S.dma("sp", esink[:], sinks_d.partition_broadcast(128), w=["esink"])
            S.dma("sp", nexpA[:], alog_d.partition_broadcast(128), w=["nexpA"])
            S.dma("sp", dtb[:], dtb_d.partition_broadcast(128), w=["dtb"])
            S.dma("sp", gnw[:], gnw_d.partition_broadcast(128), w=["gnw"])
            for k in range(4):
                S.dma("sp", cw[:, k, :], convw_d[k].rearrange("(c p) -> p c", p=128), w=["cw"])
            S.act(esink[:], esink[:], AF.Exp, r=["esink"], w=["esink"])
            S.act(nexpA[:], nexpA[:], AF.Exp, r=["nexpA"], w=["nexpA"])
            S.op("act", lambda E: E.mul(out=nexpA[:], in_=nexpA[:], mul=-1.0), r=["nexpA"], w=["nexpA"])

            cosT = sb(es0, "cosT", [128, NT + 1, 8])
            sinT = sb(es0, "sinT", [128, NT + 1, 8])
            with ExitStack() as esr:
                posi = sb(esr, "posi", [128, NT + 1], I32)
                pos1 = sb(esr, "pos1", [128, 1], I32)
                posf = sb(esr, "posf", [128, NT + 1])
                ang = sb(esr, "ang", [128, NT + 1, 8])
                kk = sb(esr, "kk", [128, NT + 1, 8])
                anl = sb(esr, "anl", [128, NT + 1, 8])
                S.op("pool", lambda E: E.iota(posi[:], pattern=[[128, NT + 1]], base=0, channel_multiplier=1), w=["posi"])
                S.op("pool", lambda E: E.iota(pos1[:], pattern=[[0, 1]], base=0, channel_multiplier=1), w=["pos1"])
                S.op("dve", lambda E: E.tensor_single_scalar(out=pos1[:], in_=pos1[:], scalar=3, op=ALU.bitwise_and),
                     r=["pos1"], w=["pos1"])
                S.cp("dve", posf[:], posi[:], r=["posi"], w=["posf"])
                S.cp("dve", posf[:, NT:NT + 1], pos1[:], r=["pos1", "posf"], w=["posf"])
                S.ts("dve", posf[:, NT:NT + 1], posf[:, NT:NT + 1], float(PAST), None, ALU.add, None, r=["posf"], w=["posf"])
                for j in range(8):
                    f = THETA ** (-(2.0 * j) / 16.0)
                    m_, e_ = math.frexp(f)
                    f_hi = math.ldexp(round(m_ * 1024.0) / 1024.0, e_)
                    f_lo = f - f_hi
                    S.ts("dve", ang[:, :, j], posf[:], float(f_hi), None, ALU.mult, None, r=["posf"], w=["ang"])
                    S.ts("dve", anl[:, :, j], posf[:], float(f_lo), None, ALU.mult, None, r=["posf"], w=["anl"])
                S.tt("dve", kk[:], ang[:], anl[:], ALU.add, r=["ang", "anl"], w=["kk"])
                S.ts("dve", kk[:], kk[:], float(1.0 / (2 * math.pi)), MAGIC, ALU.mult, ALU.add, r=["kk"], w=["kk"])
                S.ts("dve", kk[:], kk[:], MAGIC, None, ALU.subtract, None, r=["kk"], w=["kk"])
                C1 = 6.28125
                C2 = 0.00193548202514648
                C3 = 2 * math.pi - C1 - C2
                for cc in (C1, C2, C3):
                    S.stt("dve", ang[:], kk[:], float(-cc), ang[:], ALU.mult, ALU.add, r=["kk", "ang"], w=["ang"])
                S.tt("dve", ang[:], ang[:], anl[:], ALU.add, r=["ang", "anl"], w=["ang"])
                PI_S = 3.1415925
                S.ts("dve", ang[:], ang[:], -PI_S, PI_S, ALU.max, ALU.min, r=["ang"], w=["ang"])
                S.act(sinT[:], ang[:], AF.Sin, r=["ang"], w=["sinT"])
                S.stt("dve", ang[:], ang[:], -1.0, ang[:], ALU.mult, ALU.max, r=["ang"], w=["ang"])
                S.ts("dve", ang[:], ang[:], -1.0, float(math.pi / 2), ALU.mult, ALU.add, r=["ang"], w=["ang"])
                S.act(cosT[:], ang[:], AF.Sin, r=["ang"], w=["cosT"])
                S.barrier()
            stop("p0")

            mix_all = sb(es0, "mix_all", [128, NT + 1, D], BF16)

            with ExitStack() as es1:
                w_in_bf = sb(es1, "w_in_bf", [128, 8, INC], BF16)
                for kc in range(8):
                    S.dma("pool", w_in_bf[:, kc, :], w_in[kc * 128:(kc + 1) * 128, :], w=[f"w_in{kc}"])
                WIN = [f"w_in{kc}" for kc in range(8)]
                S.op("pool", lambda E: E.memset(mix_all[:, NT, :], 0.0), w=["mix16"])

                with ExitStack() as es:
                    n = NS * TS
                    xTs = sb(es, "xTs", [128, 8, n], BF16)
                    S_b = sb(es, "S_b", [128, NS * 4, 128], BF16)
                    qkTs = sb(es, "qkTs", [128, 8, n], BF16)
                    vTs = sb(es, "vTs", [128, 4, n], BF16)
                    knvs = sb(es, "knvs", [n, 8, 128], BF16)
                    tqkv = sb(es, "tqkv", [n, 768])
                    tz = sb(es, "tz", [n, 512])
                    tab = sb(es, "tab", [n, 8])
                    blk = sb(es, "blk", [n, n])
                    rowm = sb(es, "rowm", [n, NS])
                    U_s = sb(es, "U_s", [n, n])
                    L_s = sb(es, "L_s", [n, n])
                    U_sb = sb(es, "U_sb", [n, n], BF16)
                    smb = sb(es, "smb", [128, NS, n], BF16)
                    nsmb = sb(es, "nsmb", [128, NS, n], BF16)
                    S_f = sb(es, "S_f", [128, 32, 128])
                    esA = ExitStack()
                    xbs = sb(esA, "xbs", [n, D], BF16)
                    cK = sb(esA, "cK", [128, NS, 128], BF16)
                    cVa = sb(esA, "cVa", [128, NS, 2, 65], BF16)
                    cKT = sb(esA, "cKT", [128, NS, 128], BF16)
                    scs = sb(esA, "scs", [NS * 3, 1536])
                    ext = sb(esA, "ext", [128, 12, NS, 7])
                    cprod = sb(esA, "cprod", [128, 12, NS, 4])
                    cacs = sb(esA, "cacs", [128, 12, NS, 4])
                    ncsf = sb(esA, "ncsf", [128, 12, NS * 3])
                    sTs = sb(esA, "sTs", [128, 8, n])
                    sq8 = sb(esA, "sq8", [128, 8, n], BF16)
                    ln8 = sb(esA, "ln8", [128, 8, n])
                    rots = sb(esA, "rots", [n, 10, 16])
                    rtm = sb(esA, "rtm", [n, 4, 10, 8])
                    qbs = sb(esA, "qbs", [n, 4, 2, 64], BF16)
                    kbs = sb(esA, "kbs", [n, 2, 64], BF16)
                    krs = sb(esA, "krs", [n, 2, 64])
                    vna = sb(esA, "vna", [n, 2, 65], BF16)
                    qTs = sb(esA, "qTs", [128, 4, n], BF16)
                    qTsr = sb(esA, "qTsr", [128, NS, 16], BF16)
                    kTn = sb(esA, "kTn", [128, n], BF16)
                    PTs = [sb(esA, f"PTs{i}", [128, 512], BF16) for i in range(2)]
                    mk16 = sb(esA, "mk16", [128, NS, n], BF16)
                    PTn = sb(esA, "PTn", [n, 512], BF16)
                    mc4 = sb(esA, "mc4", [128, 4], BF16)
                    oc = sb(esA, "oc", [65, 512])
                    ot = sb(esA, "ot", [65, 512])
                    dn = sb(esA, "dn", [65, 512])
                    oTb = sb(esA, "oTb", [64, 8, n], BF16)
                    ii = sb(esA, "ii", [n, 64], I32)
                    ip = sb(esA, "ip", [n, 1], I32)
                    colid = sb(esA, "colid", [n, 64])
                    rowid = sb(esA, "rowid", [n, 1])
                    sidx = sb(esA, "sidx", [n, NS])
                    smf = sb(esA, "smf", [128, NS, n])
                    S.dma("pool", xbs[:], x_s, w=["xbs"])
                    S.dma("pool", S_b[:], sg_d.rearrange("s h k v -> k (s h) v"), w=["S_b"])
                    S.dma("pool", cK[:], ck_d.rearrange("s r c -> r s c"), w=["cK"])
                    S.op("pool", lambda E: E.memset(cVa[:], 1.0), w=["cVa"])
                    for jk in range(2):
                        S.dma("pool", cVa[:, :, jk, 0:64], cv_d[:, :, jk * 64:(jk + 1) * 64].rearrange("s r d -> r s d"),
                              w=["cVa"], slot=f"cVa{jk}")
                    S.dma("sp", scs[:], sc_d, w=["scs"])
                    S.op("pool", lambda E: E.memset(vna[:], 1.0), w=["vna"])
                    S.dma("sp", nk_s[:, 0:124, :], ck_d[:, 4:128, :], slot="nk_s0")
                    S.dma("sp", nv_s[:, 0:124, :], cv_d[:, 4:128, :], slot="nv_s0")
                    S.op("pool", lambda E: E.iota(ii[:], pattern=[[1, 64]], base=0, channel_multiplier=0), w=["ii"])
                    S.op("pool", lambda E: E.iota(ip[:], pattern=[[0, 1]], base=0, channel_multiplier=1), w=["ip"])
                    S.op("dve", lambda E: E.tensor_single_scalar(out=ii[:], in_=ii[:], scalar=2, op=ALU.arith_shift_right),
                         r=["ii"], w=["ii"])
                    S.op("dve", lambda E: E.tensor_single_scalar(out=ip[:], in_=ip[:], scalar=2, op=ALU.arith_shift_right),
                         r=["ip"], w=["ip"])
                    S.cp("dve", colid[:], ii[:], r=["ii"], w=["colid"])
                    S.cp("dve", rowid[:], ip[:], r=["ip"], w=["rowid"])
                    S.ts("dve", blk[:], colid[:], rowid[:, 0:1], None, ALU.is_equal, None, r=["colid", "rowid"], w=["blk"])
                    S.op("pool", lambda E: E.iota(ii[:, 0:NS], pattern=[[1, NS]], base=0, channel_multiplier=0), r=["colid"], w=["ii"])
                    S.cp("dve", sidx[:], ii[:, 0:NS], r=["ii"], w=["sidx"])
                    S.ts("dve", rowm[:], sidx[:], rowid[:, 0:1], None, ALU.is_equal, None, r=["sidx", "rowid"], w=["rowm"])
                    S.tt("dve", U_s[:], m_up[0:n, 0:n], blk[:], ALU.mult, r=["blk"], w=["U_s"])
                    S.tt("dve", L_s[:], m_strict[0:n, 0:n], blk[:], ALU.mult, r=["blk"], w=["L_s"])
                    S.cp("dve", U_sb[:], U_s[:], r=["U_s"], w=["U_sb"])
                    S.op("pool", lambda E: E.memset(smf[:], 1.0), w=["smf"])
                    S.op("pool", lambda E: E.affine_select(out=smf[:], in_=smf[:], pattern=[[-4, NS], [1, n]],
                                                           compare_op=ALU.is_ge, fill=0.0, base=0, channel_multiplier=0),
                         r=["smf"], w=["smf"])
                    S.op("pool", lambda E: E.affine_select(out=smf[:], in_=smf[:], pattern=[[4, NS], [-1, n]],
                                                           compare_op=ALU.is_ge, fill=0.0, base=3, channel_multiplier=0),
                         r=["smf"], w=["smf"])
                    S.cp("dve", smb[:], smf[:], r=["smf"], w=["smb"])
                    S.ts("dve", nsmb[:], smf[:], -1.0, None, ALU.mult, None, r=["smf"], w=["nsmb"])
                    S.op("pool", lambda E: E.affine_select(out=mc4[:], in_=ones_bf[:, 0:4], pattern=[[-1, 4]],
                                                           compare_op=ALU.is_gt, fill=0.0, base=0, channel_multiplier=1),
                         w=["mc4"])
                    stop("sA")
                    pb, pbk = S.psb()
                    for kc in range(8):
                        S.tr(pb[:, kc * 128:kc * 128 + n], xbs[:, kc * 128:(kc + 1) * 128], idb[0:n, 0:n], r=["xbs"], w=[pbk],
                             inc=(kc == 7))
                    S.cp("act", xTs[:], pb[:].rearrange("p (k t) -> p k t", k=8)[:, :, 0:n], r=[pbk], w=["xTs"])
                    for grp in range(2):
                        pf, pfk = S.psf()
                        ncg = 8 if grp == 0 else 4
                        for cc in range(ncg):
                            c = grp * 8 + cc
                            S.tr(pf[:, cc * 48:(cc + 1) * 48], scs[:, c * 128:(c + 1) * 128], idf[0:48, 0:48], r=["scs"],
                                 w=[pfk], inc=(cc == ncg - 1))
                        S.cp("act", ext[:, grp * 8:grp * 8 + ncg, :, 0:3],
                             pf[:, 0:ncg * 48].rearrange("p (c s r) -> p c s r", c=ncg, s=NS), r=[pfk], w=["ext"])
                    for c in range(12):
                        pf, pfk = S.psf()
                        for kc in range(8):
                            S.mm(pf[:, 0:n], w_in_bf[:, kc, 768 + c * 128:768 + (c + 1) * 128], xTs[:, kc, :], kc == 0, kc == 7,
                                 r=[WIN[kc], "xTs"], w=[pfk])
                        S.cp("act" if c % 2 else "dve", ext[:, c, :, 3:7], pf[:, 0:n].rearrange("p (s t) -> p s t", s=NS),
                             r=[pfk], w=["ext"])
                    S.cp("pool", ncsf[:].rearrange("p c (s r) -> p c s r", s=NS), ext[:, :, :, 4:7], r=["ext"], w=["ncsf"])
                    for grp in range(3):
                        pf, pfk = S.psf()
                        for cc in range(4):
                            c = grp * 4 + cc
                            S.tr(pf[0:48, cc * 128:(cc + 1) * 128], ncsf[:, c, :], idf[:], r=["ncsf"], w=[pfk], inc=(cc == 3))
                        S.cp("act", scs[:, grp * 512:(grp + 1) * 512], pf[0:48, :], r=[pfk], w=["scs"])
                    S.dma("sp", nc_s, scs[:], r=["scs"], slot="nc_s")
                    stop("sB")
                    for k in range(4):
                        cwb = cw[:, k, :].unsqueeze(2).unsqueeze(3).to_broadcast([128, 12, NS, 4])
                        if k == 0:
                            S.tt("dve", cacs[:], ext[:, :, :, 0:4], cwb, ALU.mult, r=["ext", "cw"], w=["cacs"])
                        else:
                            S.tt("pool", cprod[:], ext[:, :, :, k:k + 4], cwb, ALU.mult, r=["ext", "cw"], w=["cprod"])
                            S.tt("dve", cacs[:], cacs[:], cprod[:], ALU.add, r=["cacs", "cprod"], w=["cacs"])
                    S.act(sTs[:].rearrange("p c (s t) -> p c s t", s=NS), cacs[:, 0:8], AF.Silu, r=["cacs"], w=["sTs"])
                    S.act(vTs[:].rearrange("p c (s t) -> p c s t", s=NS), cacs[:, 8:12], AF.Silu, r=["cacs"], w=["vTs"])
                    S.act(sq8[:], sTs[:], AF.Square, r=["sTs"], w=["sq8"])
                    pf, pfk = S.psf()
                    S.mm(pf[:, 0:8 * n], ones_bf[:], sq8[:].rearrange("p c t -> p (c t)"), True, True, r=["sq8"], w=[pfk])
                    S.act(ln8[:].rearrange("p c t -> p (c t)"), pf[:, 0:8 * n], AF.Ln, r=[pfk], w=["ln8"], bias=L2_EPS)
                    S.act(ln8[:, 0:4, :], ln8[:, 0:4, :], AF.Exp, r=["ln8"], w=["ln8"], bias=-0.5 * math.log(128.0), scale=-0.5)
                    S.act(ln8[:, 4:8, :], ln8[:, 4:8, :], AF.Exp, r=["ln8"], w=["ln8"], scale=-0.5)
                    S.tt("dve", qkTs[:], sTs[:], ln8[:], ALU.mult, r=["sTs", "ln8"], w=["qkTs"])
                    pb, pbk = S.psb()
                    for h in range(4):
                        S.tr(pb[0:n, h * 128:(h + 1) * 128], qkTs[:, 4 + h, :], idb[:], r=["qkTs"], w=[pbk], inc=False)
                    for h in range(4):
                        S.tr(pb[0:n, (4 + h) * 128:(5 + h) * 128], vTs[:, h, :], idb[:], r=["vTs"], w=[pbk], inc=(h == 3))
                    S.cp("act", knvs[:], pb[0:n, :].rearrange("p (k t) -> p k t", k=8), r=[pbk], w=["knvs"])
                    for (c0, c1, dst, dk_) in ((0, 512, tqkv[:, 0:512], "tq_s"), (512, 768, tqkv[:, 512:768], "tkv_s"),
                                               (2304, 2816, tz[:], "tz_s"), (2816, 2824, tab[:], "tab_s")):
                        pf, pfk = S.psf()
                        for kc in range(8):
                            S.mm(pf[0:n, 0:c1 - c0], xTs[:, kc, :], w_in_bf[:, kc, c0:c1], kc == 0, kc == 7,
                                 r=[WIN[kc], "xTs"], w=[pfk])
                        S.cp("act" if dk_ in ("tq_s", "tz_s") else "dve", dst, pf[0:n, 0:c1 - c0], r=[pfk], w=[dk_])
                    qk3 = tqkv[:, 0:640].rearrange("p (h d) -> p h d", d=64)
                    cb = cosT[0:n, NT, :].unsqueeze(1).to_broadcast([n, 10, 8])
                    sbb = sinT[0:n, NT, :].unsqueeze(1).to_broadcast([n, 10, 8])
                    RK = ["tq_s", "tkv_s"]
                    S.tt("dve", rtm[:, 0], qk3[:, :, 0:8], cb, ALU.mult, r=RK, w=["rtm0"])
                    S.tt("dve", rtm[:, 1], qk3[:, :, 8:16], sbb, ALU.mult, r=RK, w=["rtm1"])
                    S.tt("dve", rtm[:, 2], qk3[:, :, 8:16], cb, ALU.mult, r=RK, w=["rtm2"])
                    S.tt("dve", rtm[:, 3], qk3[:, :, 0:8], sbb, ALU.mult, r=RK, w=["rtm3"])
                    S.tt("dve", rots[:, :, 0:8], rtm[:, 0], rtm[:, 1], ALU.subtract, r=["rtm0", "rtm1"], w=["rots"])
                    S.tt("dve", rots[:, :, 8:16], rtm[:, 2], rtm[:, 3], ALU.add, r=["rtm2", "rtm3"], w=["rots"])
                    q4 = tqkv[:, 0:512].rearrange("p (j c d) -> p c j d", j=2, c=4)
                    S.cp("dve", qbs[:, :, :, 0:16], rots[:, 0:8, :].rearrange("p (j c) d -> p c j d", j=2), r=["rots"], w=["qbs"])
                    S.cp("dve", qbs[:, :, :, 16:64], q4[:, :, :, 16:64], r=["tq_s"], w=["qbs"])
                    k3 = tqkv[:, 512:640].rearrange("p (j d) -> p j d", j=2)
                    S.cp("dve", kbs[:, :, 0:16], rots[:, 8:10, :], r=["rots"], w=["kbs"])
                    S.cp("dve", kbs[:, :, 16:64], k3[:, :, 16:64], r=["tkv_s"], w=["kbs"])
                    S.cp("dve", krs[:, :, 0:16], rots[:, 8:10, :], r=["rots"], w=["krs"])
                    S.cp("dve", krs[:, :, 16:64], k3[:, :, 16:64], r=["tkv_s"], w=["krs"])
                    S.cp("dve", vna[:, :, 0:64], tqkv[:, 640:768].rearrange("p (j d) -> p j d", j=2), r=["tkv_s", "vna"], w=["vna"])
                    S.dma("sp", nk_s[:, 124:128, :].rearrange("s t c -> (s t) c") if False else nk_s[:, 124:128, :],
                          krs[:].rearrange("(s t) j d -> s t (j d)", t=TS) if False else krs[:].rearrange("p j d -> p (j d)"),
                          r=["krs"], slot="nk_s1")
                    S.dma("sp", nv_s[:, 124:128, :], tqkv[:, 640:768], r=["tkv_s"], slot="nv_s1")
                    stop("sC")
                    pb, pbk = S.psb()
                    for c in range(4):
                        S.tr(pb[:, c * 128:c * 128 + n], qbs[:, c].rearrange("p j d -> p (j d)"), idb[0:n, 0:n], r=["qbs"],
                             w=[pbk], inc=False)
                    S.tr(pb[:, 512:512 + n], kbs[:].rearrange("p j d -> p (j d)"), idb[0:n, 0:n], r=["kbs"], w=[pbk])
                    S.cp("act", qTs[:], pb[:, 0:512].rearrange("p (c t) -> p c t", c=4)[:, :, 0:n], r=[pbk], w=["qTs"])
                    S.cp("act", kTn[:], pb[:, 512:512 + n], r=[pbk], w=["kTn"])
                    S.cp("dve", qTsr[:].rearrange("p s (c t) -> p s c t", c=4),
                         qTs[:].rearrange("p c (s t) -> p s c t", s=NS), r=["qTs"], w=["qTsr"])
                    for grp in range(2):
                        pb, pbk = S.psb()
                        for ss in range(8):
                            s_ = grp * 8 + ss
                            S.tr(pb[:, ss * 128:(ss + 1) * 128], cK[:, s_, :], idb[:], r=["cK"], w=[pbk], inc=(ss == 7))
                        S.cp("act" if grp else "dve", cKT[:, grp * 8:(grp + 1) * 8, :], pb[:].rearrange("p (s t) -> p s t", s=8),
                             r=[pbk], w=["cKT"])
                    stop("sC2")
                    S.tt("dve", mk16[:].rearrange("p s (a t) -> p (s a) t", t=4), smb[:].rearrange("p s (a t) -> p (s a) t", t=4),
                         mc4[:].unsqueeze(1).to_broadcast([128, NS * NS, 4]), ALU.mult, r=["smb", "mc4"], w=["mk16"])
                    stop("sD0")
                    base = S.psf_i % 6
                    S.psf_i += 6
                    bk = [(base + d) % 6 for d in range(6)]
                    po_ = [(S.psf_list[bk[j]], f"psf{bk[j]}") for j in range(2)]
                    ps_ = [(S.psf_list[bk[2 + j]], f"psf{bk[2 + j]}") for j in range(4)]
                    for s_ in range(NS):
                        pt_, ptk_ = PTs[s_ % 2], f"PTs{s_ % 2}"
                        for jk in range(2):
                            psc, psck = ps_[(s_ % 2) * 2 + jk]
                            S.mm(psc[:, 0:256], cKT[jk * 64:(jk + 1) * 64, s_, :],
                                 qTs[jk * 64:(jk + 1) * 64, :, :].rearrange("p c t -> p (c t)"), True, True,
                                 r=["cKT", "qTs"], w=[psck], inc=True)
                            S.act(pt_[:, jk * 256:(jk + 1) * 256], psc[:, 0:256], AF.Exp, r=[psck], w=[ptk_], scale=0.125)
                        S.tt("dve", pt_[:].rearrange("p (a t) -> p a t", a=8), pt_[:].rearrange("p (a t) -> p a t", a=8),
                             mk16[:, s_, :].unsqueeze(1).to_broadcast([128, 8, n]), ALU.mult, r=[ptk_, "mk16"], w=[ptk_])
                        for jk in range(2):
                            S.mm(po_[jk][0][0:65, 0:256], cVa[:, s_, jk, :], pt_[:, jk * 256:(jk + 1) * 256], s_ == 0, False,
                                 r=["cVa", ptk_], w=[po_[jk][1]], inc=(jk == 1))
                    stop("sD1")
                    for jk in range(2):
                        psn, psnk = ps_[jk]
                        S.mm(psn[0:n, 0:256], kTn[jk * 64:(jk + 1) * 64, :],
                             qTs[jk * 64:(jk + 1) * 64, :, :].rearrange("p c t -> p (c t)"), True, True, r=["kTn", "qTs"],
                             w=[psnk], inc=True)
                        S.act(PTn[:, jk * 256:(jk + 1) * 256], psn[0:n, 0:256], AF.Exp, r=[psnk], w=["PTn"], scale=0.125)
                    S.tt("pool", PTn[:].rearrange("p (a t) -> p a t", a=8), PTn[:].rearrange("p (a t) -> p a t", a=8),
                         U_sb[:].unsqueeze(1).to_broadcast([n, 8, n]), ALU.mult, r=["PTn", "U_sb"], w=["PTn"])
                    for jk in range(2):
                        S.mm(po_[jk][0][0:65, 0:256], vna[:, jk, :], PTn[:, jk * 256:(jk + 1) * 256], False, True,
                             r=["vna", "PTn"], w=[po_[jk][1]], inc=True)
                    stop("sD3")
                    for jk in range(2):
                        S.cp("act" if jk else "dve", ot[:, jk * 256:(jk + 1) * 256], po_[jk][0][0:65, 0:256], r=[po_[jk][1]],
                             w=["ot"])
                    S.tt("dve", dn[64:65, :].rearrange("p (h t) -> p h t", h=8), ot[64:65, :].rearrange("p (h t) -> p h t", h=8),
                         esink[64:65, :].unsqueeze(2).to_broadcast([1, 8, n]), ALU.add, r=["ot", "esink"], w=["dn"])
                    S.op("dve", lambda E: E.reciprocal(out=dn[64:65, :], in_=dn[64:65, :]), r=["dn"], w=["dn"])
                    stop("sD4")
                    pbc, pbck = S.psf()
                    S.mm(pbc[0:64, :], onesf[64:65, 0:64], dn[64:65, :], True, True, r=["dn"], w=[pbck])
                    S.tt("dve", oTb[:].rearrange("p h t -> p (h t)"), pbc[0:64, :], ot[0:64, :], ALU.mult, r=[pbck, "ot"],
                         w=["oTb"])
                    stop("sD5")
                    pb, pbk = S.psb()
                    for h in range(8):
                        S.tr(pb[0:n, h * 64:(h + 1) * 64], oTb[:, h, :], idb[0:64, 0:64], r=["oTb"], w=[pbk], inc=(h == 7))
                    S.cp("act", mix_all[0:n, NT, 0:512], pb[0:n, 0:512], r=[pbk], w=["mix16"])
                    S.barrier()
                    esA.close()
                    gabs = sb(es, "gabs", [n, 8, 4])
                    gmk = sb(es, "gmk", [n, NS, 4])
                    abc = sb(es, "abc", [128, NS * 4])
                    gUs = sb(es, "gUs", [n, 4, n])
                    eDs = sb(es, "eDs", [n, 4, n])
                    eDTs = sb(es, "eDTs", [n, 4, n])
                    ATs = sb(es, "ATs", [n, 4, n], BF16)
                    P0s = sb(es, "P0s", [n, 4, n])
                    P0Ts = sb(es, "P0Ts", [n, 4, n])
                    P1s = sb(es, "P1s", [n, 4, n])
                    Q0s = sb(es, "Q0s", [n, 4, n])
                    Qbs = sb(es, "Qbs", [n, 4, n], BF16)
                    kbes = sb(es, "kbes", [n, 4, 128], BF16)
                    kdcs = sb(es, "kdcs", [n, 4, 128], BF16)
                    vbs = sb(es, "vbs", [n, 4, 128], BF16)
                    vns = sb(es, "vns", [n, 4, 128], BF16)
                    o1ss = sb(es, "o1ss", [n, 4, 128])
                    ogs = sb(es, "ogs", [n, 4, 128])
                    osqs = sb(es, "osqs", [n, 4, 128])
                    ssqs = sb(es, "ssqs", [n, 4])
                    zss = sb(es, "zss", [n, 512])
                    Stm = [sb(es, f"Stm{i}", [128, 4, 128]) for i in range(2)]
                    nwTh = [sb(es, f"nwTh{i}", [128, NS, n], BF16) for i in range(2)]
                    qTh = [sb(es, f"qTh{i}", [128, NS, n], BF16) for i in range(2)]
                    vnh = [sb(es, f"vnh{i}", [n, NS, 128], BF16) for i in range(2)]

                    stop("sD")
                    g_ = gabs[:, 0, :]
                    beta_ = gabs[:, 1, :]
                    gc_ = gabs[:, 2, :]
                    egc_ = gabs[:, 3, :]
                    kds_ = gabs[:, 4, :]
                    tmp_ = gabs[:, 6, :]
                    nbeta_ = gabs[:, 7, :]
                    S.tt("dve", tmp_, tab[:, 0:4], dtb[0:n, :], ALU.add, r=["tab_s"], w=["s_tmp"])
                    S.act(tmp_, tmp_, AF.Exp, r=["s_tmp"], w=["s_tmp"])
                    S.act(tmp_, tmp_, AF.Ln, r=["s_tmp"], w=["s_tmp"], bias=1.0)
                    S.tt("dve", g_, tmp_, nexpA[0:n, :], ALU.mult, r=["s_tmp"], w=["s_g"])
                    S.act(beta_, tab[:, 4:8], AF.Exp, r=["tab_s"], w=["s_beta"], scale=-1.0)
                    S.ts("dve", beta_, beta_, 1.0, None, ALU.add, None, r=["s_beta"], w=["s_beta"])
                    S.op("dve", lambda E: E.reciprocal(out=beta_, in_=beta_), r=["s_beta"], w=["s_beta"])
                    S.ts("dve", nbeta_, beta_, -1.0, None, ALU.mult, None, r=["s_beta"], w=["s_nbeta"])
                    S.tt("dve", gmk[:], g_.unsqueeze(1).to_broadcast([n, NS, 4]), rowm[:].unsqueeze(2).to_broadcast([n, NS, 4]),
                         ALU.mult, r=["s_g", "rowm"], w=["gmk"])
                    pf, pfk = S.psf()
                    S.mm(pf[0:n, 0:4], U_s[:], g_, True, True, r=["U_s", "s_g"], w=[pfk])
                    S.mm(pf[0:n, 4:8], blk[:], g_, True, True, r=["blk", "s_g"], w=[pfk])
                    S.cp("dve", gc_, pf[0:n, 0:4], r=[pfk], w=["s_gc"])
                    S.act(egc_, pf[0:n, 0:4], AF.Exp, r=[pfk], w=["s_egc"])
                    S.tt("dve", kds_, pf[0:n, 4:8], gc_, ALU.subtract, r=[pfk, "s_gc"], w=["s_kds"])
                    S.act(kds_, kds_, AF.Exp, r=["s_kds"], w=["s_kds"])
                    pa, pak = S.psf()
                    S.mm(pa[:, 0:NS * 4], onesf[0:n, :], gmk[:].rearrange("p s h -> p (s h)"), True, True, r=["gmk"], w=[pak])
                    S.act(abc[:], pa[:, 0:NS * 4], AF.Exp, r=[pak], w=["abc"])
                    pD, pDk = S.psf()
                    pDT, pDTk = S.psf()
                    pG, pGk = S.psf()
                    pA, pAk = S.psf()
                    v3 = lambda p: p[0:n, 0:4 * n].rearrange("p (h t) -> p h t", h=4)
                    pD3, pDT3, pG3, pA3 = v3(pD), v3(pDT), v3(pG), v3(pA)
                    for h in range(4):
                        S.ts("dve", gUs[:, h, :], U_s[:], gabs[:, 0, h:h + 1], None, ALU.mult, None, r=["U_s", "s_g"], w=["gUs"])
                    for h in range(4):
                        S.mm(pD3[:, h, :], gUs[:, h, :], L_s[:], True, True, r=["gUs", "L_s"], w=[pDk], inc=(h == 3))
                    for h in range(4):
                        S.mm(pDT3[:, h, :], L_s[:], gUs[:, h, :], True, True, r=["gUs", "L_s"], w=[pDTk], inc=(h == 3))
                    for h in range(4):
                        S.mm(pG3[:, h, :], qkTs[:, 4 + h, :], qkTs[:, 4 + h, :], True, True, r=["qkTs"], w=[pGk], inc=(h == 3))
                    for h in range(4):
                        S.mm(pA3[:, h, :], qkTs[:, 4 + h, :], qkTs[:, h, :], True, True, r=["qkTs"], w=[pAk], inc=(h == 3))
                    S.act(eDs[:], pD3, AF.Exp, r=[pDk], w=["eDs"])
                    S.tt("pool", eDs[:], eDs[:], L_s[:].unsqueeze(1).to_broadcast([n, 4, n]), ALU.mult, r=["eDs", "L_s"], w=["eDs"])
                    S.act(eDTs[:], pDT3, AF.Exp, r=[pDTk], w=["eDTs"])
                    S.tt("pool", eDTs[:], eDTs[:], U_s[:].unsqueeze(1).to_broadcast([n, 4, n]), ALU.mult, r=["eDTs", "U_s"],
                         w=["eDTs"])
                    for h in range(4):
                        S.stt("dve", P0s[:, h, :], pG3[:, h, :], gabs[:, 7, h:h + 1], eDs[:, h, :], ALU.mult, ALU.mult,
                              r=[pGk, "s_nbeta", "eDs"], w=["P0s"])
                    S.tt("dve", ATs[:], pA3, eDTs[:], ALU.mult, r=[pAk, "eDTs"], w=["ATs"])
                    pt, ptk = S.psf()
                    pt3 = v3(pt)
                    for h in range(4):
                        S.tr(pt3[:, h, :], P0s[:, h, :], idf[0:n, 0:n], r=["P0s"], w=[ptk], inc=(h == 3))
                    S.cp("act", P0Ts[:], pt3, r=[ptk], w=["P0Ts"])
                    S.tt("dve", Q0s[:], pt3, idf[0:n, 0:n].unsqueeze(1).to_broadcast([n, 4, n]), ALU.add, r=[ptk], w=["Q0s"])
                    pP, pPk = S.psf()
                    pP3 = v3(pP)
                    for h in range(4):
                        S.mm(pP3[:, h, :], P0Ts[:, h, :], P0s[:, h, :], True, True, r=["P0Ts", "P0s"], w=[pPk], inc=(h == 3))
                    S.cp("act", P1s[:], pP3, r=[pPk], w=["P1s"])
                    pQ, pQk = S.psf()
                    pQ3 = v3(pQ)
                    for h in range(4):
                        S.mm(pQ3[:, h, :], P1s[:, h, :], Q0s[:, h, :], True, True, r=["P1s", "Q0s"], w=[pQk], inc=(h == 3))
                    S.tt("dve", Qbs[:], pQ3, Q0s[:], ALU.add, r=[pQk, "Q0s"], w=["Qbs"])
                    stop("sE")
                    kn3 = knvs[:, 0:4, :]
                    vv3 = knvs[:, 4:8, :]
                    S.tt("dve", tmp_, beta_, egc_, ALU.mult, r=["s_beta", "s_egc", "s_tmp"], w=["s_tmp"])
                    S.tt("pool", kbes[:], kn3, tmp_.unsqueeze(2).to_broadcast([n, 4, 128]), ALU.mult, r=["knvs", "s_tmp"], w=["kbes"])
                    S.tt("pool", kdcs[:], kn3, kds_.unsqueeze(2).to_broadcast([n, 4, 128]), ALU.mult, r=["knvs", "s_kds"], w=["kdcs"])
                    S.tt("dve", vbs[:], vv3, beta_.unsqueeze(2).to_broadcast([n, 4, 128]), ALU.mult, r=["knvs", "s_beta"], w=["vbs"])
                    pw, pwk = S.psf()
                    pw3 = pw[:, 0:4 * n].rearrange("p (h t) -> p h t", h=4)
                    for h in range(4):
                        S.mm(pw3[:, h, :], kbes[:, h, :], Qbs[:, h, :], True, True, r=["kbes", "Qbs"], w=[pwk], inc=(h == 3))
                    pv, pvk = S.psf()
                    pv3 = pv[0:n, :].rearrange("p (h t) -> p h t", h=4)
                    for h in range(4):
                        S.tt("dve", nwTh[h % 2][:], pw3[:, h, :].unsqueeze(1).to_broadcast([128, NS, n]), nsmb[:], ALU.mult,
                             r=[pwk, "nsmb"], w=[f"nwTh{h % 2}"])
                        S.mm(pv3[:, h, :], Qbs[:, h, :], vbs[:, h, :], True, False, r=["Qbs", "vbs"], w=[pvk], inc=False)
                        for s_ in range(NS):
                            S.mm(pv3[:, h, :], nwTh[h % 2][:, s_, :], S_b[:, s_ * 4 + h, :], False, s_ == NS - 1,
                                 r=[f"nwTh{h % 2}", "S_b"], w=[pvk], inc=(s_ == NS - 1))
                    S.cp("act", vns[:], pv3, r=[pvk], w=["vns"])
                    po1, po1k = S.psf()
                    po13 = po1[0:n, :].rearrange("p (h t) -> p h t", h=4)
                    for h in range(4):
                        S.tt("pool", qTh[h % 2][:], qkTs[:, h, :].unsqueeze(1).to_broadcast([128, NS, n]), smb[:], ALU.mult,
                             r=["qkTs", "smb"], w=[f"qTh{h % 2}"])
                        for s_ in range(NS):
                            S.mm(po13[:, h, :], qTh[h % 2][:, s_, :], S_b[:, s_ * 4 + h, :], s_ == 0, s_ == NS - 1,
                                 r=[f"qTh{h % 2}", "S_b"], w=[po1k], inc=(s_ == NS - 1))
                    po2, po2k = S.psf()
                    po23 = po2[0:n, :].rearrange("p (h t) -> p h t", h=4)
                    for h in range(4):
                        S.mm(po23[:, h, :], ATs[:, h, :], vns[:, h, :], True, True, r=["ATs", "vns"], w=[po2k], inc=(h == 3))
                    S.tt("dve", o1ss[:], po13, egc_.unsqueeze(2).to_broadcast([n, 4, 128]), ALU.mult, r=[po1k, "s_egc"], w=["o1ss"])
                    S.tt("dve", ogs[:], po23, o1ss[:], ALU.add, r=[po2k, "o1ss"], w=["ogs"])
                    stop("sF")
                    S_f4 = S_f[:].rearrange("p (s h) v -> p s h v", h=4)
                    abc3 = abc[:].rearrange("p (s h) -> p s h", h=4)
                    for half in range(2):
                        S.dma("sp", S_f[:], sg_d[half * 8:(half + 1) * 8].rearrange("s h k v -> k (s h) v"), w=["S_f"])
                        for h in range(4):
                            vh, vhk = vnh[h % 2], f"vnh{h % 2}"
                            S.tt("dve", vh[:], vns[:, h, :].unsqueeze(1).to_broadcast([n, NS, 128]),
                                 rowm[:].unsqueeze(2).to_broadcast([n, NS, 128]), ALU.mult, r=["vns", "rowm"], w=[vhk])
                            for sg_ in range(2):
                                pS, pSk = S.psf()
                                pS3 = pS[:].rearrange("p (s t) -> p s t", s=4)
                                for sl in range(4):
                                    s_ = half * 8 + sg_ * 4 + sl
                                    S.mm(pS3[:, sl, :], kdcs[:, h, :], vh[:, s_, :], True, True, r=["kdcs", vhk], w=[pSk],
                                         inc=(sl == 3))
                                stt_, sttk = Stm[sg_ % 2], f"Stm{sg_ % 2}"
                                sl0 = sg_ * 4
                                s0 = half * 8 + sg_ * 4
                                S.tt("pool", stt_[:], S_f4[:, sl0:sl0 + 4, h, :],
                                     abc3[:, s0:s0 + 4, h].unsqueeze(2).to_broadcast([128, 4, 128]), ALU.mult,
                                     r=["S_f", "abc"], w=[sttk])
                                S.tt("dve", S_f4[:, sl0:sl0 + 4, h, :], pS3, stt_[:], ALU.add, r=[pSk, sttk], w=["S_f"])
                        S.dma("sp", ng_s[half * 8:(half + 1) * 8].rearrange("s h k v -> k (s h) v"), S_f[:], r=["S_f"], slot="ng_s")
                    S.tt("pool", osqs[:], ogs[:], ogs[:], ALU.mult, r=["ogs"], w=["osqs"])
                    S.op("dve", lambda E: E.tensor_reduce(out=ssqs[:], in_=osqs[:], axis=AX.X, op=ALU.add), r=["osqs"], w=["ssqs"])
                    S.act(ssqs[:], ssqs[:], AF.Ln, r=["ssqs"], w=["ssqs"], bias=NORM_EPS, scale=1.0 / 128.0)
                    S.act(ssqs[:], ssqs[:], AF.Exp, r=["ssqs"], w=["ssqs"], scale=-0.5)
                    S.act(zss[:], tz[:], AF.Silu, r=["tz_s"], w=["zss"])
                    S.tt("pool", zss[:].rearrange("p (h d) -> p h d", h=4), zss[:].rearrange("p (h d) -> p h d", h=4),
                         gnw[0:n, :].unsqueeze(1).to_broadcast([n, 4, 128]), ALU.mult, r=["zss"], w=["zss"])
                    S.tt("dve", ogs[:], ogs[:], ssqs[:].unsqueeze(2).to_broadcast([n, 4, 128]), ALU.mult, r=["ogs", "ssqs"], w=["ogs"])
                    S.tt("dve", mix_all[0:n, NT, 512:1024].rearrange("p (h d) -> p h d", h=4), ogs[:],
                         zss[:].rearrange("p (h d) -> p h d", h=4), ALU.mult, r=["ogs", "zss"], w=["mix16"])
                    S.barrier()

                with ExitStack() as es:
                    ST = 256
                    xb = [sb(es, f"xb{i}", [128, D], BF16) for i in range(2)]
                    xT = [sb(es, f"xT{i}", [128, 8, ST], BF16) for i in range(2)]
                    gT = sb(es, "gT", [128, 12, ST + 3])
                    cacc = [sb(es, f"cacc{i}", [128, ST]) for i in range(2)]
                    sT = sb(es, "sT", [128, 8, ST])
                    qkT2 = [sb(es, f"qkT_{i}", [128, 8, ST], BF16) for i in range(2)]
                    vT2 = [sb(es, f"vT_{i}", [128, 4, ST], BF16) for i in range(2)]
                    sq4 = sb(es, "sq4", [128, 4, ST], BF16)
                    ln4 = sb(es, "ln4", [128, 4, ST])
                    knv = sb(es, "knv", [128, 8, 128], BF16)
                    tm_qkv = sb(es, "tm_qkv", [128, 768])
                    zs4 = [sb(es, f"zs2_{i}", [128, 512]) for i in range(4)]
                    tm_ab = sb(es, "tm_ab", [128, 8])
                    rot = sb(es, "rot", [128, 10, 16])
                    rtmp = sb(es, "rtmp", [128, 4, 10, 8])
                    q_bf = sb(es, "q_bf", [128, 4, 2, 64], BF16)
                    k_bf = sb(es, "k_bf", [128, 2, 64], BF16)
                    k_rot = sb(es, "k_rot", [128, 2, 64])
                    v_aug = [sb(es, f"v_aug{i}", [128, 2, 65], BF16) for i in range(2)]
                    kTb = [sb(es, f"kTb{i}", [128, 128], BF16) for i in range(2)]
                    qT = sb(es, "qT", [128, 4, 128], BF16)
                    PTe = [sb(es, f"PTe{i}", [128, 4, 128], BF16) for i in range(2)]
                    den = sb(es, "den", [128, 2, 4])
                    gab = sb(es, "gab", [128, 8, 4])
                    gU = sb(es, "gU", [128, 4, 128])
                    eD = sb(es, "eD", [128, 4, 128])
                    eDT = sb(es, "eDT", [128, 4, 128])
                    ATb = sb(es, "ATb", [128, 4, 128], BF16)
                    Pm = [sb(es, f"Pm{i}", [128, 4, 128]) for i in range(2)]
                    PmT = [sb(es, f"PmT{i}", [128, 4, 128]) for i in range(2)]
                    Qm = [sb(es, f"Qm{i}", [128, 4, 128]) for i in range(2)]
                    Qb = sb(es, "Qb", [128, 4, 128], BF16)
                    kbe = sb(es, "kbe", [128, 4, 128], BF16)
                    kdc = sb(es, "kdc", [128, 4, 128], BF16)
                    vb = sb(es, "vb", [128, 4, 128], BF16)
                    nwT = sb(es, "nwT", [128, 4, 128], BF16)
                    vnew = sb(es, "vnew", [128, 4, 128], BF16)
                    Sf = sb(es, "Sf", [128, 4, 128])
                    Sb = sb(es, "Sb", [128, 4, 128], BF16)
                    Stmp = sb(es, "Stmp", [128, 4, 128])
                    o1s = sb(es, "o1s", [128, 4, 128])
                    og = sb(es, "og", [128, 4, 128])
                    osq = sb(es, "osq", [128, 4, 128])
                    ssq = sb(es, "ssq", [128, 4])

                    S.op("pool", lambda E: E.memset(gT[:, :, 0:3], 0.0), w=[f"gT{c}" for c in range(12)])
                    S.op("pool", lambda E: E.memset(Sf[:], 0.0), w=["Sf"])
                    S.op("pool", lambda E: E.memset(Sb[:], 0.0), w=["Sb"])
                    for i in range(2):
                        S.op("pool", lambda E, i=i: E.memset(v_aug[i][:], 1.0), w=[f"v_aug{i}"])

                    GT = [f"gT{c}" for c in range(12)]
                    def fm(st):
                        qkT, vT, zs2 = qkT2[st % 2], vT2[st % 2], zs4[(st % 2) * 2:(st % 2) * 2 + 2]
                        PAR = st % 2
                        xTs = xT[st % 2]
                        xTk = f"xT{st % 2}"
                        for j in range(ST // 128):
                            i = st * (ST // 128) + j
                            xbt = xb[i % 2]
                            xbk = f"xb{i % 2}"
                            S.dma("pool", xbt[:], x_p[i * 128:(i + 1) * 128, :], w=[xbk])
                            pb, pbk = S.psb()
                            for kc in range(8):
                                S.tr(pb[:, kc * 128:(kc + 1) * 128], xbt[:, kc * 128:(kc + 1) * 128], idb[:],
                                     r=[xbk, "idb"], w=[pbk], inc=(kc == 7))
                            S.cp("act", xTs[:, :, j * 128:(j + 1) * 128], pb[:].rearrange("p (k t) -> p k t", k=8),
                                 r=[pbk], w=[xTk])
                        for c in range(12):
                            pf, pfk = S.psf()
                            for kc in range(8):
                                S.mm(pf[:, 0:ST], w_in_bf[:, kc, 768 + c * 128:768 + (c + 1) * 128], xTs[:, kc, :],
                                     kc == 0, kc == 7, r=[WIN[kc], xTk], w=[pfk])
                            S.cp("act" if c % 2 else "dve", gT[:, c, 3:3 + ST], pf[:, 0:ST], r=[pfk], w=[GT[c]])
                            acc = cacc[c % 2]
                            ak = f"cacc{c % 2}"
                            ce = "dve"
                            S.ts(ce, acc[:], gT[:, c, 0:ST], cw[:, 0, c:c + 1], None, ALU.mult, None, r=[GT[c], "cw"], w=[ak])
                            for k in range(1, 4):
                                S.stt(ce, acc[:], gT[:, c, k:k + ST], cw[:, k, c:c + 1], acc[:], ALU.mult, ALU.add,
                                      r=[GT[c], "cw", ak], w=[ak])
                            if c < 8:
                                S.act(sT[:, c, :], acc[:], AF.Silu, r=[ak], w=[f"sT{c}"])
                            else:
                                S.act(vT[:, c - 8, :], acc[:], AF.Silu, r=[ak], w=[f"vT_{PAR}"])
                        for j in range(ST // 128):
                            pf, pfk = S.psf()
                            for kc in range(8):
                                S.mm(pf[:, :], xTs[:, kc, j * 128:(j + 1) * 128], w_in_bf[:, kc, 2304:2816], kc == 0, kc == 7,
                                     r=[WIN[kc], xTk], w=[pfk])
                            S.act(zs2[j][:], pf[:, :], AF.Silu, r=[pfk], w=[f"zs2_{PAR}_{j}"])
                            S.tt("pool", zs2[j][:].rearrange("p (h d) -> p h d", h=4), zs2[j][:].rearrange("p (h d) -> p h d", h=4),
                                 gnw[:].unsqueeze(1).to_broadcast([128, 4, 128]), ALU.mult, r=[f"zs2_{PAR}_{j}", "gnw"], w=[f"zs2_{PAR}_{j}"])
                        if st == SEQ // ST - 1:
                            for t in range(3):
                                S.dma("sp", nc_p[t].rearrange("(c p) -> p c", p=128), gT[:, :, ST + t], r=GT, w=[],
                                      slot="nc_p")
                        else:
                            S.cp("pool", gT[:, :, 0:3], gT[:, :, ST:ST + 3], r=GT, w=GT)
                        for half in range(2):
                            for hh in range(4):
                                c = half * 4 + hh
                                S.act(sq4[:, hh, :], sT[:, c, :], AF.Square, r=[f"sT{c}"], w=[f"sq{hh}"])
                            pfs = []
                            for hh in range(4):
                                pf, pfk = S.psf()
                                S.mm(pf[:, 0:ST], ones_bf[:], sq4[:, hh, :], True, True, r=["ones_bf", f"sq{hh}"], w=[pfk])
                                pfs.append((pf, pfk))
                            for hh in range(4):
                                pf, pfk = pfs[hh]
                                S.act(ln4[:, hh, :], pf[:, 0:ST], AF.Ln, r=[pfk], w=[f"ln{hh}"], bias=L2_EPS)
                            for hh in range(4):
                                bq = -0.5 * math.log(128.0) if half == 0 else 0.0
                                S.act(ln4[:, hh, :], ln4[:, hh, :], AF.Exp, r=[f"ln{hh}"], w=[f"ln{hh}"], bias=bq, scale=-0.5)
                            for hh in range(4):
                                c = half * 4 + hh
                                S.tt("pool" if hh % 2 else "dve", qkT[:, c, :], sT[:, c, :], ln4[:, hh, :], ALU.mult,
                                     r=[f"sT{c}", f"ln{hh}"], w=[f"qkT{c}_{PAR}"])
                        if st == 0:
                            stop("st0a")

                    def tile(st, j):
                        qkT, vT, zs2 = qkT2[st % 2], vT2[st % 2], zs4[(st % 2) * 2:(st % 2) * 2 + 2]
                        PAR = st % 2
                        xTs = xT[st % 2]
                        xTk = f"xT{st % 2}"
                        i = st * (ST // 128) + j
                        tok = slice(j * 128, (j + 1) * 128)
                        pb, pbk = S.psb()
                        for h in range(4):
                            S.tr(pb[:, h * 128:(h + 1) * 128], qkT[:, 4 + h, tok], idb[:], r=[f"qkT{4 + h}_{PAR}", "idb"],
                                 w=[pbk], inc=False)
                        for h in range(4):
                            S.tr(pb[:, (4 + h) * 128:(5 + h) * 128], vT[:, h, tok], idb[:], r=[f"vT_{PAR}", "idb"], w=[pbk],
                                 inc=(h == 3))
                        S.cp("act", knv[:], pb[:].rearrange("p (k t) -> p k t", k=8), r=[pbk], w=["knv"])
                        if i == 0:
                            stop("t0a")
                        for (c0, c1, dst, dk_) in ((0, 512, tm_qkv[:, 0:512], "tm_q"), (512, 768, tm_qkv[:, 512:768], "tm_kv"),
                                                   (2816, 2824, tm_ab[:], "tm_ab")):
                            pf, pfk = S.psf()
                            for kc in range(8):
                                S.mm(pf[:, 0:c1 - c0], xTs[:, kc, tok], w_in_bf[:, kc, c0:c1], kc == 0, kc == 7,
                                     r=[WIN[kc], xTk], w=[pfk])
                            S.cp("act" if dk_ in ("tm_q", "tm_z") else "dve", dst, pf[:, 0:c1 - c0], r=[pfk], w=[dk_])
                        if i == 0:
                            stop("t0b")
                        qk3 = tm_qkv[:, 0:640].rearrange("p (h d) -> p h d", d=64)
                        cb = cosT[:, i, :].unsqueeze(1).to_broadcast([128, 10, 8])
                        sbb = sinT[:, i, :].unsqueeze(1).to_broadcast([128, 10, 8])
                        RK = ["tm_q", "tm_kv", "cosT", "sinT"]
                        S.tt("dve", rtmp[:, 0], qk3[:, :, 0:8], cb, ALU.mult, r=RK, w=["rt0"])
                        S.tt("dve", rtmp[:, 1], qk3[:, :, 8:16], sbb, ALU.mult, r=RK, w=["rt1"])
                        S.tt("dve", rtmp[:, 2], qk3[:, :, 8:16], cb, ALU.mult, r=RK, w=["rt2"])
                        S.tt("dve", rtmp[:, 3], qk3[:, :, 0:8], sbb, ALU.mult, r=RK, w=["rt3"])
                        S.tt("dve", rot[:, :, 0:8], rtmp[:, 0], rtmp[:, 1], ALU.subtract, r=["rt0", "rt1"], w=["rot"])
                        S.tt("dve", rot[:, :, 8:16], rtmp[:, 2], rtmp[:, 3], ALU.add, r=["rt2", "rt3"], w=["rot"])
                        q4 = tm_qkv[:, 0:512].rearrange("p (j c d) -> p c j d", j=2, c=4)
                        S.cp("dve", q_bf[:, :, :, 0:16], rot[:, 0:8, :].rearrange("p (j c) d -> p c j d", j=2),
                             r=["rot"], w=["q_bf"])
                        S.cp("act", q_bf[:, :, :, 16:64], q4[:, :, :, 16:64], r=["tm_q"], w=["q_bf"])
                        k3 = tm_qkv[:, 512:640].rearrange("p (j d) -> p j d", j=2)
                        S.cp("dve", k_bf[:, :, 0:16], rot[:, 8:10, :], r=["rot"], w=["k_bf"])
                        S.cp("dve", k_bf[:, :, 16:64], k3[:, :, 16:64], r=["tm_kv"], w=["k_bf"])
                        va = v_aug[i % 2]
                        vak = f"v_aug{i % 2}"
                        S.cp("pool", va[:, :, 0:64], tm_qkv[:, 640:768].rearrange("p (j d) -> p j d", j=2), r=["tm_kv"],
                             w=[vak])
                        if i == 0:
                            stop("t0c")
                        if i == NT - 1:
                            S.cp("dve", k_rot[:, :, 0:16], rot[:, 8:10, :], r=["rot"], w=["k_rot"])
                            S.cp("dve", k_rot[:, :, 16:64], k3[:, :, 16:64], r=["tm_kv"], w=["k_rot"])
                            S.dma("sp", nk_p, k_rot[:].rearrange("p j d -> p (j d)"), r=["k_rot"], slot="nk_p")
                            S.dma("sp", nv_p, tm_qkv[:, 640:768], r=["tm_kv"], slot="nv_p")
                        pb, pbk = S.psb()
                        for c in range(4):
                            S.tr(pb[:, c * 128:(c + 1) * 128], q_bf[:, c].rearrange("p j d -> p (j d)"), idb[:],
                                 r=["q_bf", "idb"], w=[pbk], inc=False)
                        S.tr(pb[:, 512:640], k_bf[:].rearrange("p j d -> p (j d)"), idb[:], r=["k_bf", "idb"], w=[pbk])
                        kTc = kTb[i % 2]
                        kTk = f"kTb{i % 2}"
                        S.cp("act", qT[:], pb[:, 0:512].rearrange("p (c t) -> p c t", c=4), r=[pbk], w=["qT"])
                        S.cp("dve", kTc[:], pb[:, 512:640], r=[pbk], w=[kTk])
                        if i == 0:
                            stop("t0d")
                        for jk in range(2):
                            blocks = ([] if i == 0 else [(kTb[(i - 1) % 2], f"kTb{(i - 1) % 2}", mb_prev, "mb_prev",
                                                          v_aug[(i - 1) % 2], f"v_aug{(i - 1) % 2}")])
                            blocks.append((kTc, kTk, mb_cur, "mb_cur", va, vak))
                            for bi, (kt, ktk, mk, mkk, _, _) in enumerate(blocks):
                                pf, pfk = S.psf()
                                S.mm(pf[:], kt[jk * 64:(jk + 1) * 64, :],
                                     qT[jk * 64:(jk + 1) * 64, :, :].rearrange("p c t -> p (c t)"), True, True,
                                     r=[ktk, "qT"], w=[pfk])
                                S.act(PTe[bi][:].rearrange("p c t -> p (c t)"), pf[:], AF.Exp, r=[pfk], w=[f"PTe{bi}"],
                                      scale=0.125)
                                S.tt("dve", PTe[bi][:], PTe[bi][:], mk[:].unsqueeze(1).to_broadcast([128, 4, 128]),
                                     ALU.mult, r=[f"PTe{bi}", mkk], w=[f"PTe{bi}"])
                                if i == 0:
                                    stop("t0e")
                            po, pok = S.psf()
                            po3 = po[:, 0:260].rearrange("p (c d) -> p c d", c=4)
                            for c in range(4):
                                for bi, (_, _, _, _, vv, vvk) in enumerate(blocks):
                                    S.mm(po3[:, c, :], PTe[bi][:, c, :], vv[:, jk, :], bi == 0, bi == len(blocks) - 1,
                                         r=[f"PTe{bi}", vvk], w=[pok], inc=(c == 3 and bi == len(blocks) - 1))
                            S.tt("dve", den[:, jk, :], po3[:, :, 64], esink[:, jk * 4:(jk + 1) * 4], ALU.add,
                                 r=[pok, "esink"], w=["den"])
                            S.op("dve", lambda E, jk=jk: E.reciprocal(out=den[:, jk, :], in_=den[:, jk, :]), r=["den"],
                                 w=["den"])
                            S.tt("dve", mix_all[:, i, jk * 256:(jk + 1) * 256].rearrange("p (c d) -> p c d", c=4),
                                 po3[:, :, 0:64], den[:, jk, :].unsqueeze(2).to_broadcast([128, 4, 64]), ALU.mult,
                                 r=[pok, "den"], w=[f"mix{i}"])
                        if i == 0:
                            stop("t0attn")
                        if i == 1:
                            stop("t1attn")
                        g_ = gab[:, 0, :]
                        beta_ = gab[:, 1, :]
                        gc_ = gab[:, 2, :]
                        egc_ = gab[:, 3, :]
                        kds_ = gab[:, 4, :]
                        atot_ = gab[:, 5, :]
                        tmp_ = gab[:, 6, :]
                        nbeta_ = gab[:, 7, :]
                        S.tt("dve", tmp_, tm_ab[:, 0:4], dtb[:], ALU.add, r=["tm_ab", "dtb"], w=["g_tmp"])
                        S.act(tmp_, tmp_, AF.Exp, r=["g_tmp"], w=["g_tmp"])
                        S.act(tmp_, tmp_, AF.Ln, r=["g_tmp"], w=["g_tmp"], bias=1.0)
                        S.tt("dve", g_, tmp_, nexpA[:], ALU.mult, r=["g_tmp", "nexpA"], w=["g_g"])
                        S.act(beta_, tm_ab[:, 4:8], AF.Exp, r=["tm_ab"], w=["g_beta"], scale=-1.0)
                        S.ts("dve", beta_, beta_, 1.0, None, ALU.add, None, r=["g_beta"], w=["g_beta"])
                        S.op("dve", lambda E: E.reciprocal(out=beta_, in_=beta_), r=["g_beta"], w=["g_beta"])
                        S.ts("dve", nbeta_, beta_, -1.0, None, ALU.mult, None, r=["g_beta"], w=["g_nbeta"])
                        pf, pfk = S.psf()
                        S.mm(pf[:, 0:4], m_up[:], g_, True, True, r=["m_up", "g_g"], w=[pfk])
                        S.mm(pf[:, 4:8], onesf[:], g_, True, True, r=["onesf", "g_g"], w=[pfk])
                        S.cp("dve", gc_, pf[:, 0:4], r=[pfk], w=["g_gc"])
                        S.act(egc_, pf[:, 0:4], AF.Exp, r=[pfk], w=["g_egc"])
                        S.act(atot_, pf[:, 4:8], AF.Exp, r=[pfk], w=["g_atot"])
                        S.tt("dve", kds_, pf[:, 4:8], gc_, ALU.subtract, r=[pfk, "g_gc"], w=["g_kds"])
                        S.act(kds_, kds_, AF.Exp, r=["g_kds"], w=["g_kds"])
                        pD, pDk = S.psf()
                        pDT, pDTk = S.psf()
                        pG, pGk = S.psf()
                        pA, pAk = S.psf()
                        pD3 = pD[:].rearrange("p (h t) -> p h t", h=4)
                        pDT3 = pDT[:].rearrange("p (h t) -> p h t", h=4)
                        pG3 = pG[:].rearrange("p (h t) -> p h t", h=4)
                        pA3 = pA[:].rearrange("p (h t) -> p h t", h=4)
                        for h in range(4):
                            if h % 2:
                                S.act(gU[:, h, :], m_up[:], AF.Copy, r=["m_up", "g_g"], w=[f"gU{h}"], scale=gab[:, 0, h:h + 1])
                            else:
                                S.ts("dve", gU[:, h, :], m_up[:], gab[:, 0, h:h + 1], None, ALU.mult, None,
                                     r=["m_up", "g_g"], w=[f"gU{h}"])
                        for h in range(4):
                            S.mm(pD3[:, h, :], gU[:, h, :], m_strict[:], True, True, r=[f"gU{h}", "m_strict"], w=[pDk],
                                 inc=(h == 3))
                        for h in range(4):
                            S.mm(pDT3[:, h, :], m_strict[:], gU[:, h, :], True, True, r=[f"gU{h}", "m_strict"], w=[pDTk],
                                 inc=(h == 3))
                        for h in range(4):
                            S.mm(pG3[:, h, :], qkT[:, 4 + h, tok], qkT[:, 4 + h, tok], True, True, r=[f"qkT{4 + h}_{PAR}"],
                                 w=[pGk], inc=(h == 3))
                        for h in range(4):
                            S.mm(pA3[:, h, :], qkT[:, 4 + h, tok], qkT[:, h, tok], True, True,
                                 r=[f"qkT{4 + h}_{PAR}", f"qkT{h}_{PAR}"], w=[pAk], inc=(h == 3))
                        S.act(eD[:], pD3, AF.Exp, r=[pDk], w=["eD"])
                        S.tt("dve", eD[:], eD[:], m_strict[:].unsqueeze(1).to_broadcast([128, 4, 128]), ALU.mult,
                             r=["eD", "m_strict"], w=["eD"])
                        S.act(eDT[:], pDT3, AF.Exp, r=[pDTk], w=["eDT"])
                        S.tt("pool", eDT[:], eDT[:], m_up[:].unsqueeze(1).to_broadcast([128, 4, 128]), ALU.mult,
                             r=["eDT", "m_up"], w=["eDT"])
                        P0 = Pm[0]
                        for h in range(4):
                            S.stt("dve", P0[:, h, :], pG3[:, h, :], gab[:, 7, h:h + 1], eD[:, h, :], ALU.mult, ALU.mult,
                                  r=[pGk, "g_nbeta", "eD"], w=[f"Pm0g{h // 2}"])
                        S.tt("dve", ATb[:], pA3, eDT[:], ALU.mult, r=[pAk, "eDT"], w=["ATb"])
                        pt, ptk = S.psf()
                        pt3 = pt[:].rearrange("p (h t) -> p h t", h=4)
                        for h in range(4):
                            S.tr(pt3[:, h, :], P0[:, h, :], idf[:], r=[f"Pm0g{h // 2}", "idf"], w=[ptk], inc=(h == 3))
                        S.cp("act", PmT[0][:], pt3, r=[ptk], w=["PmT0g0", "PmT0g1"])
                        S.tt("dve", Qm[0][:], pt3, idf[:].unsqueeze(1).to_broadcast([128, 4, 128]), ALU.add,
                             r=[ptk, "idf"], w=["Qm0g0", "Qm0g1"])
                        NIT = 6
                        GR = ((0, 2), (2, 4))
                        for k in range(NIT):
                            a, b = k % 2, (k + 1) % 2
                            last = (k == NIT - 1)
                            pPs, pPTs = [], []
                            for gi, (h0, h1) in enumerate(GR):
                                pP, pPk = S.psf()
                                pP3 = pP[:, 0:256].rearrange("p (h t) -> p h t", h=2)
                                for h in range(h0, h1):
                                    S.mm(pP3[:, h - h0, :], PmT[a][:, h, :], Pm[a][:, h, :], True, True,
                                         r=[f"PmT{a}g{gi}", f"Pm{a}g{gi}"], w=[pPk], inc=(h == h1 - 1))
                                pPs.append((pP3, pPk))
                                if not last:
                                    pPT, pPTk = S.psf()
                                    pPT3 = pPT[:, 0:256].rearrange("p (h t) -> p h t", h=2)
                                    for h in range(h0, h1):
                                        S.mm(pPT3[:, h - h0, :], Pm[a][:, h, :], PmT[a][:, h, :], True, True,
                                             r=[f"PmT{a}g{gi}", f"Pm{a}g{gi}"], w=[pPTk], inc=(h == h1 - 1))
                                    pPTs.append((pPT3, pPTk))
                            for gi, (h0, h1) in enumerate(GR):
                                S.cp("act", Pm[b][:, h0:h1, :], pPs[gi][0], r=[pPs[gi][1]], w=[f"Pm{b}g{gi}"])
                                if not last:
                                    S.cp("dve", PmT[b][:, h0:h1, :], pPTs[gi][0], r=[pPTs[gi][1]], w=[f"PmT{b}g{gi}"])
                            pQs = []
                            for gi, (h0, h1) in enumerate(GR):
                                pQ, pQk = S.psf()
                                pQ3 = pQ[:, 0:256].rearrange("p (h t) -> p h t", h=2)
                                for h in range(h0, h1):
                                    S.mm(pQ3[:, h - h0, :], Pm[b][:, h, :], Qm[a][:, h, :], True, True,
                                         r=[f"Pm{b}g{gi}", f"Qm{a}g{gi}"], w=[pQk], inc=(h == h1 - 1))
                                pQs.append((pQ3, pQk))
                            for gi, (h0, h1) in enumerate(GR):
                                if not last:
                                    S.tt("dve", Qm[b][:, h0:h1, :], pQs[gi][0], Qm[a][:, h0:h1, :], ALU.add,
                                         r=[pQs[gi][1], f"Qm{a}g{gi}"], w=[f"Qm{b}g{gi}"])
                                else:
                                    S.tt("dve", Qb[:, h0:h1, :], pQs[gi][0], Qm[a][:, h0:h1, :], ALU.add,
                                         r=[pQs[gi][1], f"Qm{a}g{gi}"], w=["Qb"])
                        kn3 = knv[:, 0:4, :]
                        v3 = knv[:, 4:8, :]
                        S.tt("dve", tmp_, beta_, egc_, ALU.mult, r=["g_beta", "g_egc", "g_tmp"], w=["g_tmp"])
                        S.tt("pool", kbe[:], kn3, tmp_.unsqueeze(2).to_broadcast([128, 4, 128]), ALU.mult,
                             r=["knv", "g_tmp"], w=["kbe"])
                        S.tt("pool", kdc[:], kn3, kds_.unsqueeze(2).to_broadcast([128, 4, 128]), ALU.mult,
                             r=["knv", "g_kds"], w=["kdc"])
                        S.tt("dve", vb[:], v3, beta_.unsqueeze(2).to_broadcast([128, 4, 128]), ALU.mult,
                             r=["knv", "g_beta"], w=["vb"])
                        pw, pwk = S.psf()
                        pw3 = pw[:].rearrange("p (h t) -> p h t", h=4)
                        for h in range(4):
                            S.mm(pw3[:, h, :], kbe[:, h, :], Qb[:, h, :], True, True, r=["kbe", "Qb"], w=[pwk],
                                 inc=(h == 3))
                        S.op("act", lambda E: E.mul(out=nwT[:], in_=pw3, mul=-1.0), r=[pwk], w=["nwT"])
                        pv, pvk = S.psf()
                        pv3 = pv[:].rearrange("p (h t) -> p h t", h=4)
                        for h in range(4):
                            S.mm(pv3[:, h, :], Qb[:, h, :], vb[:, h, :], True, False, r=["Qb", "vb"], w=[pvk], inc=False)
                            S.mm(pv3[:, h, :], nwT[:, h, :], Sb[:, h, :], False, True, r=["nwT", "Sb"], w=[pvk],
                                 inc=(h == 3))
                        S.cp("act", vnew[:], pv3, r=[pvk], w=["vnew"])
                        po1, po1k = S.psf()
                        po13 = po1[:].rearrange("p (h t) -> p h t", h=4)
                        for h in range(4):
                            S.mm(po13[:, h, :], qkT[:, h, tok], Sb[:, h, :], True, True, r=[f"qkT{h}_{PAR}", "Sb"], w=[po1k],
                                 inc=(h == 3))
                        po2, po2k = S.psf()
                        po23 = po2[:].rearrange("p (h t) -> p h t", h=4)
                        for h in range(4):
                            S.mm(po23[:, h, :], ATb[:, h, :], vnew[:, h, :], True, True, r=["ATb", "vnew"], w=[po2k],
                                 inc=(h == 3))
                        pS, pSk = S.psf()
                        pS3 = pS[:].rearrange("p (h t) -> p h t", h=4)
                        for h in range(4):
                            S.mm(pS3[:, h, :], kdc[:, h, :], vnew[:, h, :], True, True, r=["kdc", "vnew"], w=[pSk],
                                 inc=(h == 3))
                        S.tt("dve", o1s[:], po13, egc_.unsqueeze(2).to_broadcast([128, 4, 128]), ALU.mult,
                             r=[po1k, "g_egc"], w=["o1s"])
                        S.tt("dve", og[:], po23, o1s[:], ALU.add, r=[po2k, "o1s"], w=["og"])
                        S.tt("pool", Stmp[:], Sf[:], atot_.unsqueeze(2).to_broadcast([128, 4, 128]), ALU.mult,
                             r=["Sf", "g_atot"], w=["Stmp"])
                        S.tt("dve", Sf[:], pS3, Stmp[:], ALU.add, r=[pSk, "Stmp"], w=["Sf"])
                        S.cp("act", Sb[:], Sf[:], r=["Sf"], w=["Sb"])
                        S.tt("dve", osq[:], og[:], og[:], ALU.mult, r=["og"], w=["osq"])
                        S.op("dve", lambda E: E.tensor_reduce(out=ssq[:], in_=osq[:], axis=AX.X, op=ALU.add), r=["osq"],
                             w=["ssq"])
                        S.act(ssq[:], ssq[:], AF.Ln, r=["ssq"], w=["ssq"], bias=NORM_EPS, scale=1.0 / 128.0)
                        S.act(ssq[:], ssq[:], AF.Exp, r=["ssq"], w=["ssq"], scale=-0.5)
                        S.tt("dve", og[:], og[:], ssq[:].unsqueeze(2).to_broadcast([128, 4, 128]), ALU.mult,
                             r=["og", "ssq"], w=["og"])
                        S.tt("dve", mix_all[:, i, 512:1024].rearrange("p (h d) -> p h d", h=4), og[:],
                             zs2[j][:].rearrange("p (h d) -> p h d", h=4), ALU.mult, r=["og", f"zs2_{PAR}_{j}"], w=[f"mix{i}"])
                        if i == 0:
                            stop("t0")
                        if i == 1:
                            stop("t1")
                        if i == 3:
                            stop("t3")

                    NST = SEQ // ST
                    fm(0)
                    for st in range(NST):
                        tile(st, 0)
                        if st + 1 < NST:
                            fm(st + 1)
                        tile(st, 1)
                    S.dma("sp", ng_p.rearrange("h k v -> k h v"), Sf[:], r=["Sf"], slot="ng_p")
                    if dbg:
                        dump("mix", mix_all[:, 0:NT, :], [128, NT, D], [f"mix{i}" for i in range(NT)])
                    S.barrier()


            y_acc = sb(es0, "y_acc", [128, NT + 1, D])
            with ExitStack() as es:
                w_out_bf = sb(es, "w_out_bf", [128, 8, D], BF16)
                g1 = sb(es, "g1", [128, D])
                b1 = sb(es, "b1", [128, D])
                mixT = [sb(es, f"mixT{i}", [128, 8, 128], BF16) for i in range(2)]
                xf = [sb(es, f"xf{i}", [128, D]) for i in range(2)]
                tb = [sb(es, f"tb{i}", [128, D]) for i in range(3)]
                stats = [sb(es, f"stats{i}", [128, 2, 6]) for i in range(3)]
                mv = [sb(es, f"mv{i}", [128, 2]) for i in range(3)]
                S.dma("pool", w_out_bf[:], w_out.rearrange("(c p) n -> p c n", p=128), w=["w_out"])
                S.dma("sp", g1[:], ln1g_d.partition_broadcast(128), w=["g1"])
                S.dma("sp", b1[:], ln1b_d.partition_broadcast(128), w=["b1"])
                def ln1_A(i):
                    n = 128 if i < NT else NS * TS
                    xsrc = x_p[i * 128:(i + 1) * 128, :] if i < NT else x_s
                    xft, xfk = xf[i % 2], f"xf{i % 2}"
                    S.dma("sp", xft[:n, :], xsrc, w=[xfk])
                    mt, mtk = mixT[i % 2], f"mixT{i % 2}"
                    pb, pbk = S.psb()
                    for c in range(8):
                        S.tr(pb[:, c * 128:c * 128 + n], mix_all[:n, i, c * 128:(c + 1) * 128], idb[:n, :n],
                             r=["idb"], w=[pbk], inc=(c == 7))
                    S.cp("act", mt[:, :, 0:n], pb[:].rearrange("p (c t) -> p c t", c=8)[:, :, 0:n], r=[pbk], w=[mtk])
                    tbt, tbk = tb[i % 3], f"tb{i % 3}"
                    st_, mv_, mvk = stats[i % 3], mv[i % 3], f"mv{i % 3}"
                    for half in range(2):
                        pf, pfk = S.psf()
                        for c in range(8):
                            S.mm(pf[:n, :], mt[:, c, 0:n], w_out_bf[:, c, half * 512:(half + 1) * 512], c == 0, c == 7,
                                 r=[mtk, "w_out"], w=[pfk])
                        S.stt("dve", tbt[:n, half * 512:(half + 1) * 512], xft[:n, half * 512:(half + 1) * 512], ALPHA,
                              pf[:n, :], ALU.mult, ALU.add, r=[xfk, pfk], w=[tbk])
                        S.op("dve", lambda E, half=half: E.bn_stats(out=st_[:n, half, :],
                                                                    in_=tbt[:n, half * 512:(half + 1) * 512]),
                             r=[tbk], w=[mvk])
                    S.op("dve", lambda E: E.bn_aggr(out=mv_[:n, :], in_=st_[:n].rearrange("p a b -> p (a b)")),
                         r=[mvk], w=[mvk])
                    S.act(mv_[:n, 1:2], mv_[:n, 1:2], AF.Ln, r=[mvk], w=[mvk], bias=NORM_EPS)
                    S.act(mv_[:n, 1:2], mv_[:n, 1:2], AF.Exp, r=[mvk], w=[mvk], scale=-0.5)

                def ln1_B(i):
                    n = 128 if i < NT else NS * TS
                    tbt, tbk = tb[i % 3], f"tb{i % 3}"
                    mv_, mvk = mv[i % 3], f"mv{i % 3}"
                    S.stt("dve", mv_[:n, 0:1], mv_[:n, 0:1], -1.0, mv_[:n, 1:2], ALU.mult, ALU.mult, r=[mvk], w=[mvk])
                    S.act(tbt[:n, :], tbt[:n, :], AF.Identity, r=[tbk, mvk], w=[tbk], bias=mv_[:n, 0:1], scale=mv_[:n, 1:2])
                    S.tt("dve", tbt[:n, :], tbt[:n, :], g1[:n, :], ALU.mult, r=[tbk, "g1"], w=[tbk])
                    S.tt("pool", y_acc[:n, i, :], tbt[:n, :], b1[:n, :], ALU.add, r=[tbk, "b1"], w=[f"y{i}"])

                for i in range(NT + 1):
                    ln1_A(i)
                    if i >= 1:
                        ln1_B(i - 1)
                ln1_B(NT)
                if dbg:
                    dump("x1", y_acc[:], [128, NT + 1, D], [f"y{i}" for i in range(NT + 1)])
                S.barrier()

            with ExitStack() as es:
                x1T = mix_all[:].rearrange("p a b -> p (a b)")[:, 0:8 * NTOK].rearrange("p (k t) -> p k t", k=8)
                comb = sb(es, "comb", [128, NT + 1, NE])
                wr_sb = sb(es, "wr_sb", [128, 8, 36])
                x1Tf = sb(es, "x1Tf", [128, 8, 128])
                T_ = NT + 1
                rl_all = sb(es, "rl_all", [128, T_, 36])
                r_oh = sb(es, "r_oh", [128, T_, 4])
                r_t4 = sb(es, "r_t4", [128, T_, 4])
                r_pr = sb(es, "r_pr", [128, T_, 4, 8])
                r_es = sb(es, "r_es", [128, T_, 8])
                r_m1 = sb(es, "r_m1", [128, T_, 8])
                r_e2 = sb(es, "r_e2", [128, T_, 8])
                r_m2 = sb(es, "r_m2", [128, T_, 8])
                r_ew = sb(es, "r_ew", [128, T_, 8])
                r_s = sb(es, "r_s", [128, 8, T_])
                S.op("pool", lambda E: E.memset(rl_all[:], 0.0), w=["rl_all"])
                S.dma("sp", wr_sb[:], wr_d.rearrange("(c p) n -> p c n", p=128), w=["wr"])
                YK = [f"y{i}" for i in range(NT + 1)]
                for i in range(NT + 1):
                    n = 128 if i < NT else NS * TS
                    t0 = i * 128
                    pfa, pfak = S.psf()
                    pfb, pfbk = S.psf()
                    for kc in range(8):
                        pf, pfk = (pfa, pfak) if kc < 4 else (pfb, pfbk)
                        S.tr(pf[:, (kc % 4) * 128:(kc % 4) * 128 + n], y_acc[:n, i, kc * 128:(kc + 1) * 128], idf[:n, :n],
                             r=[YK[i], "idf"], w=[pfk], inc=(kc % 4 == 3))
                    for hf, (pf, pfk) in enumerate(((pfa, pfak), (pfb, pfbk))):
                        src = pf[:].rearrange("p (k t) -> p k t", k=4)[:, :, 0:n]
                        S.cp("act", x1Tf[:, hf * 4:(hf + 1) * 4, 0:n], src, r=[pfk], w=["x1Tf"])
                        S.cp("dve", x1T[:, hf * 4:(hf + 1) * 4, t0:t0 + n], src, r=[pfk], w=["x1T"])
                    pr, prk = S.psf()
                    for kc in range(8):
                        S.mm(pr[:n, 0:36], x1Tf[:, kc, 0:n], wr_sb[:, kc, :], kc == 0, kc == 7, r=["x1Tf", "wr"], w=[prk])
                    S.cp("dve", rl_all[:n, i, :], pr[:n, 0:36], r=[prk], w=["rl_all"])
                    S.op("act", lambda E, n=n, i=i: E.mul(out=y_acc[:n, i, :], in_=y_acc[:n, i, :], mul=ALPHA),
                         r=[YK[i], "x1T", "x1Tf"], w=[YK[i]])
                R = ["rl_all", "rr"]
                gl = rl_all[:, :, 0:4]
                el = rl_all[:, :, 4:36].rearrange("p t (g e) -> p t g e", g=4)
                bc3 = lambda a, k: a.unsqueeze(2).to_broadcast([128, T_, k])
                gmax, gtp, m1, m2, ex, w1, w2 = (r_s[:, j, :] for j in range(7))
                S.op("dve", lambda E: E.tensor_reduce(out=gmax, in_=gl, axis=AX.X, op=ALU.max), r=R, w=R)
                S.tt("dve", r_oh[:], gl, bc3(gmax, 4), ALU.is_equal, r=R, w=R)
                S.tt("dve", r_t4[:], gl, bc3(gmax, 4), ALU.subtract, r=R, w=R)
                S.act(r_t4[:], r_t4[:], AF.Exp, r=R, w=R)
                S.op("dve", lambda E: E.tensor_reduce(out=gtp, in_=r_t4[:], axis=AX.X, op=ALU.add), r=R, w=R)
                S.op("dve", lambda E: E.reciprocal(out=gtp, in_=gtp), r=R, w=R)
                S.tt("dve", r_pr[:], el, r_oh[:].unsqueeze(3).to_broadcast([128, T_, 4, 8]), ALU.mult, r=R, w=R)
                S.op("dve", lambda E: E.tensor_reduce(out=r_es[:], in_=r_pr[:].rearrange("p t g e -> p t e g"), axis=AX.X,
                                                      op=ALU.add), r=R, w=R)
                S.op("dve", lambda E: E.tensor_reduce(out=m1, in_=r_es[:], axis=AX.X, op=ALU.max), r=R, w=R)
                S.tt("dve", r_m1[:], r_es[:], bc3(m1, 8), ALU.is_equal, r=R, w=R)
                S.stt("dve", r_e2[:], r_m1[:], -1e30, r_es[:], ALU.mult, ALU.add, r=R, w=R)
                S.op("dve", lambda E: E.tensor_reduce(out=m2, in_=r_e2[:], axis=AX.X, op=ALU.max), r=R, w=R)
                S.tt("dve", r_m2[:], r_e2[:], bc3(m2, 8), ALU.is_equal, r=R, w=R)
                S.tt("dve", ex, m2, m1, ALU.subtract, r=R, w=R)
                S.act(ex, ex, AF.Exp, r=R, w=R)
                S.ts("dve", w1, ex, 1.0, None, ALU.add, None, r=R, w=R)
                S.op("dve", lambda E: E.reciprocal(out=w1, in_=w1), r=R, w=R)
                S.tt("dve", w2, ex, w1, ALU.mult, r=R, w=R)
                S.tt("dve", w1, w1, gtp, ALU.mult, r=R, w=R)
                S.tt("dve", w2, w2, gtp, ALU.mult, r=R, w=R)
                S.tt("dve", r_ew[:], r_m1[:], bc3(w1, 8), ALU.mult, r=R, w=R)
                S.tt("dve", r_m2[:], r_m2[:], bc3(w2, 8), ALU.mult, r=R, w=R)
                S.tt("dve", r_ew[:], r_ew[:], r_m2[:], ALU.add, r=R, w=R)
                S.tt("dve", comb[:].rearrange("p t (g e) -> p t g e", g=4), r_oh[:].unsqueeze(3).to_broadcast([128, T_, 4, 8]),
                     r_ew[:].unsqueeze(2).to_broadcast([128, T_, 4, 8]), ALU.mult, r=R, w=["comb"])
                if dbg:
                    dump("comb", comb[:], [128, NT + 1, NE], ["comb"])

                wg = [sb(es, f"wg{i}", [128, 8, 256], BF16) for i in range(2)]
                wu = [sb(es, f"wu{i}", [128, 8, 256], BF16) for i in range(2)]
                wd = [sb(es, f"wd{i}", [128, 2, D], BF16) for i in range(2)]
                hT = [sb(es, f"hT{i}", [128, 2, NTOK], BF16) for i in range(2)]
                sgt = [sb(es, f"sgt{i}", [128, 512]) for i in range(2)]
                spans = [(t, min(512, NTOK - t)) for t in range(0, NTOK, 512)]
                for pbt in S.psb_list:
                    S.psf_list.append(pbt[:].bitcast(F32))
                acct = [sb(es, f"acct{i}", [128, 512]) for i in range(8)]
                g2 = sb(es, "g2", [128, D])
                b2 = sb(es, "b2", [128, D])
                ob = [sb(es, f"ob{i}", [128, D]) for i in range(3)]
                stats2 = [sb(es, f"stats2_{i}", [128, 2, 6]) for i in range(3)]
                mv2 = [sb(es, f"mv2_{i}", [128, 2]) for i in range(3)]
                S.dma("sp", g2[:], ln2g_d.partition_broadcast(128), w=["g2"])
                S.dma("sp", b2[:], ln2b_d.partition_broadcast(128), w=["b2"])
                acc_i = 0

                def ln2_A(i):
                    n = 128 if i < NT else NS * TS
                    st2, mvt, mk_ = stats2[i % 3], mv2[i % 3], f"mv2_{i % 3}"
                    for half in range(2):
                        S.op("dve", lambda E, half=half: E.bn_stats(out=st2[:n, half, :],
                                                                    in_=y_acc[:n, i, half * 512:(half + 1) * 512]),
                             r=[YK[i]], w=[mk_])
                    S.op("dve", lambda E: E.bn_aggr(out=mvt[:n, :], in_=st2[:n].rearrange("p a b -> p (a b)")),
                         r=[mk_], w=[mk_])
                    S.act(mvt[:n, 1:2], mvt[:n, 1:2], AF.Ln, r=[mk_], w=[mk_], bias=NORM_EPS)
                    S.act(mvt[:n, 1:2], mvt[:n, 1:2], AF.Exp, r=[mk_], w=[mk_], scale=-0.5)

                def ln2_B(i):
                    n = 128 if i < NT else NS * TS
                    mvt, mk_ = mv2[i % 3], f"mv2_{i % 3}"
                    obt, obk = ob[i % 3], f"ob{i % 3}"
                    S.stt("dve", mvt[:n, 0:1], mvt[:n, 0:1], -1.0, mvt[:n, 1:2], ALU.mult, ALU.mult, r=[mk_], w=[mk_])
                    S.act(obt[:n, :], y_acc[:n, i, :], AF.Identity, r=[YK[i], mk_], w=[obk], bias=mvt[:n, 0:1],
                          scale=mvt[:n, 1:2])
                    S.tt("dve", obt[:n, :], obt[:n, :], g2[:n, :], ALU.mult, r=[obk, "g2"], w=[obk])
                    S.tt("pool", obt[:n, :], obt[:n, :], b2[:n, :], ALU.add, r=[obk, "b2"], w=[obk])
                    dst = y_p[i * 128:(i + 1) * 128, :] if i < NT else y_s
                    S.dma("sp", dst, obt[:n, :], r=[obk], slot=obk + "o")

                def load_expert(e):
                    b = e % 2
                    S.dma("pool", wg[b][:], wg_d[e].rearrange("(c p) f -> p c f", p=128), w=[f"wg{b}"])
                    S.dma("pool", wu[b][:], wu_d[e].rearrange("(c p) f -> p c f", p=128), w=[f"wu{b}"])
                    S.dma("pool", wd[b][:], wd_d[e].rearrange("(c p) n -> p c n", p=128), w=[f"wd{b}"])

                load_expert(0)
                for e in range(NE):
                    b = e % 2
                    if e + 1 < NE:
                        load_expert(e + 1)
                    hk = f"hT{b}"
                    si = 0
                    for fc in range(2):
                        for (t0, tn) in spans:
                            pg, pgk = S.psf()
                            pu, puk = S.psf()
                            for kc in range(8):
                                S.mm(pg[:, 0:tn], wg[b][:, kc, fc * 128:(fc + 1) * 128], x1T[:, kc, t0:t0 + tn], kc == 0,
                                     kc == 7, r=[f"wg{b}", "x1T"], w=[pgk])
                            for kc in range(8):
                                S.mm(pu[:, 0:tn], wu[b][:, kc, fc * 128:(fc + 1) * 128], x1T[:, kc, t0:t0 + tn], kc == 0,
                                     kc == 7, r=[f"wu{b}", "x1T"], w=[puk])
                            sg_, sgk = sgt[si % 2], f"sgt{si % 2}"
                            si += 1
                            S.act(sg_[:, 0:tn], pg[:, 0:tn], AF.Silu, r=[pgk], w=[sgk])
                            S.tt("dve", hT[b][:, fc, t0:t0 + tn], pu[:, 0:tn], sg_[:, 0:tn], ALU.mult, r=[puk, sgk], w=[hk])
                    for i in range(NT + 1):
                        n = 128 if i < NT else NS * TS
                        t0 = i * 128
                        for half in range(2):
                            py, pyk = S.psf()
                            for fc in range(2):
                                S.mm(py[:n, :], hT[b][:, fc, t0:t0 + n], wd[b][:, fc, half * 512:(half + 1) * 512], fc == 0,
                                     fc == 1, r=[hk, f"wd{b}"], w=[pyk])
                            ysl = y_acc[:n, i, half * 512:(half + 1) * 512]
                            if (i * 2 + half) % 2 == 1:
                                at, atk = acct[acc_i % 8], f"acct{acc_i % 8}"
                                acc_i += 1
                                S.act(at[:n, :], py[:n, :], AF.Copy, r=[pyk, "comb"], w=[atk], scale=comb[:n, i, e:e + 1])
                                S.tt("pool", ysl, ysl, at[:n, :], ALU.add, r=[atk, YK[i]], w=[YK[i]])
                            else:
                                S.stt("dve", ysl, py[:n, :], comb[:n, i, e:e + 1], ysl, ALU.mult, ALU.add,
                                      r=[pyk, "comb", YK[i]], w=[YK[i]])
                        if e == NE - 1:
                            if i >= 1:
                                ln2_A(i - 1)
                            if i >= 2:
                                ln2_B(i - 2)
                ln2_A(NT)
                ln2_B(NT - 1)
                ln2_B(NT)
        except _Stop:
            pass
        S.final_wait()
    return nc, dbg_outs


_CACHE = {}


def _get_nc(dbg=False):
    if dbg not in _CACHE:
        _CACHE[dbg] = build(dbg)
    return _CACHE[dbg]


def make_in_maps(inputs):
    f = lambda a: np.ascontiguousarray(np.asarray(a, dtype=np.float32))
    g = {k: f(v) for k, v in inputs.items()}
    wr = np.ascontiguousarray(np.concatenate([g["w_router_group"][0], g["w_router_expert"][0]], axis=1))
    maps = []
    for c in range(NCORES):
        s0, s1 = c * NS, (c + 1) * NS
        maps.append({
            "x_p": g["x_prompt"][c],
            "x_s": np.ascontiguousarray(g["x_sample"][s0:s1].reshape(NS * TS, D)),
            "ck": np.ascontiguousarray(g["cache_attn_k"][0, s0:s1].reshape(NS, 128, 128)),
            "cv": np.ascontiguousarray(g["cache_attn_v"][0, s0:s1].reshape(NS, 128, 128)),
            "sg": np.ascontiguousarray(g["state_gdn"][0, s0:s1]),
            "sc": np.ascontiguousarray(g["state_conv"][0, s0:s1].reshape(NS * 3, 1536)),
            "w_in": g["w_in"][0], "w_out": g["w_out"][0], "sinks": g["attn_sinks"][0], "conv_w": g["conv_w"][0],
            "a_log": g["a_log"][0], "dt_bias": g["dt_bias"][0], "gnw": g["gdn_norm_w"][0],
            "ln1_g": g["ln1_g"][0], "ln1_b": g["ln1_b"][0], "w_r": wr,
            "w_gate": g["w_gate"][0], "w_up": g["w_up"][0], "w_down": g["w_down"][0],
            "ln2_g": g["ln2_g"][0], "ln2_b": g["ln2_b"][0],
        })
    return maps


def assemble(results):
    cat = lambda k: np.stack([np.asarray(r[k]) for r in results])
    y_p = cat("y_p")
    y_s = cat("y_s").reshape(128, TS, D)
    nk_p = cat("nk_p").reshape(1, 8, 128, 2, 64)
    nv_p = cat("nv_p").reshape(1, 8, 128, 2, 64)
    ng_p = cat("ng_p").reshape(1, 8, 4, 128, 128)
    nc_p = cat("nc_p").reshape(1, 8, 3, 1536)
    nk_s = cat("nk_s").reshape(1, 128, 128, 2, 64)
    nv_s = cat("nv_s").reshape(1, 128, 128, 2, 64)
    ng_s = cat("ng_s").reshape(1, 128, 4, 128, 128)
    nc_s = cat("nc_s").reshape(1, 128, 3, 1536)
    return tuple(np.ascontiguousarray(a.astype(np.float32)) for a in
                 (y_p, y_s, nk_p, nv_p, ng_p, nc_p, nk_s, nv_s, ng_s, nc_s))


def kernel(**inputs):
    nc, _ = _get_nc(False)
    maps = make_in_maps(inputs)
    res = run_bass_kernel_spmd(nc, maps, core_ids=list(range(NCORES)))
    return assemble(res.results)
```

```python
import math
from contextlib import ExitStack

import numpy as np
import concourse.bass as bass
import concourse.mybir as mybir
from concourse.bass_utils import run_bass_kernel_spmd

F32 = mybir.dt.float32
BF16 = mybir.dt.bfloat16
I32 = mybir.dt.int32
AF = mybir.ActivationFunctionType
ALU = mybir.AluOpType
AX = mybir.AxisListType

NCORES = 8
D = 1024
SEQ = 2048
NT = 16
NS = 16
TS = 4
NTOK = SEQ + NS * TS
PAST = 8192
INC = 2824
ALPHA = 2.0 ** 0.25
NORM_EPS = 1e-5
L2_EPS = 1e-6
THETA = 500000.0
NE = 32
MAGIC = 12582912.0


class _Stop(Exception):
    pass


import os
STOP = os.environ.get("K_STOP", "")


_SCHED = []


def stop(tag):
    if STOP == tag:
        _SCHED[-1].dead = True


class Sched:
    def __init__(self, nc, es):
        self.nc = nc
        self.es = es
        self.E = {"pe": nc.tensor, "act": nc.scalar, "dve": nc.vector, "pool": nc.gpsimd, "sp": nc.sync}
        self.semh = {}
        for k in self.E:
            self.semh["e_" + k] = es.enter_context(nc.semaphore("sem_" + k))
        self.cnt = {k: 0 for k in self.E}
        self.seen = {k: {} for k in self.E}
        self.lastw = {}
        self.readers = {}
        self.pend = {k: [] for k in self.E}
        self.pend_r = {k: set() for k in self.E}
        self.pend_w = {k: set() for k in self.E}
        self.slots = {}
        self.psf_list = []
        self.psb_list = []
        self.psf_i = 0
        self.psb_i = 0
        self.dead = False
        _SCHED.append(self)

    def _waits(self, e, r, w, is_dma):
        need = {}

        def add(ev):
            semk, val, eng = ev
            if need.get(semk, 0) < val:
                need[semk] = val

        for k in list(r) + list(w):
            for e2 in self.E:
                if e2 != e or is_dma:
                    assert k not in self.pend_w[e2], f"key {k} pending write on {e2}"
        for k in w:
            for e2 in self.E:
                if e2 != e or is_dma:
                    assert k not in self.pend_r[e2], f"key {k} pending read on {e2}"
        for k in r:
            ev = self.lastw.get(k)
            if ev is not None:
                if ev[2] == e and e == "pe" and not is_dma:
                    continue
                add(ev)
            if k.startswith("ps"):
                for ev in self.readers.get(k, ()):
                    if ev[2] != e:
                        add(ev)
        for k in w:
            ev = self.lastw.get(k)
            if ev is not None and (is_dma or ev[2] != e or e != "pe"):
                add(ev)
            for ev in self.readers.get(k, ()):
                if is_dma or ev[2] != e or e != "pe":
                    add(ev)
        for semk, val in need.items():
            if self.seen[e].get(semk, 0) < val:
                self.E[e].wait_ge(self.semh[semk], val)
                self.seen[e][semk] = val

    def _register(self, r, w, ev):
        for k in r:
            self.readers.setdefault(k, []).append(ev)
        for k in w:
            self.lastw[k] = ev
            self.readers[k] = []

    def op(self, e, fn, r=(), w=(), inc=True):
        if self.dead:
            return None
        self._waits(e, r, w, False)
        ins = fn(self.E[e])
        if not inc:
            self.pend[e].append((tuple(r), tuple(w)))
            self.pend_r[e].update(r)
            self.pend_w[e].update(w)
            return ins
        self.cnt[e] += 1
        ins.then_inc(self.semh["e_" + e], 1)
        ev = ("e_" + e, self.cnt[e], e)
        for (pr, pw) in self.pend[e]:
            self._register(pr, pw, ev)
        self.pend[e] = []
        self.pend_r[e] = set()
        self.pend_w[e] = set()
        self._register(r, w, ev)
        return ins

    def dma(self, q, out, in_, r=(), w=(), slot=None):
        if slot is None:
            slot = w[0] if w else r[0]
        if self.dead:
            return None
        sk = "d_" + slot
        if sk not in self.slots:
            self.semh[sk] = self.es.enter_context(self.nc.semaphore(sk))
            self.slots[sk] = 0
        self._waits(q, r, w, True)
        ins = self.E[q].dma_start(out=out, in_=in_)
        self.slots[sk] += 16
        ins.then_inc(self.semh[sk], 16)
        ev = (sk, self.slots[sk], "dma")
        self._register(r, w, ev)
        return ins

    def barrier(self):
        if self.dead:
            return
        for e in self.E:
            assert not self.pend[e]
        for e in self.E:
            for e2 in self.E:
                if self.cnt[e2] > self.seen[e].get("e_" + e2, 0):
                    self.E[e].wait_ge(self.semh["e_" + e2], self.cnt[e2])
                    self.seen[e]["e_" + e2] = self.cnt[e2]
            for sk, v in self.slots.items():
                if v > self.seen[e].get(sk, 0):
                    self.E[e].wait_ge(self.semh[sk], v)
                    self.seen[e][sk] = v
        self.lastw = {}
        self.readers = {}

    def final_wait(self):
        self.dead = False
        for sk, v in self.slots.items():
            if v > self.seen["sp"].get(sk, 0):
                self.E["sp"].wait_ge(self.semh[sk], v)
                self.seen["sp"][sk] = v
        for e2 in self.E:
            if e2 != "sp" and self.cnt[e2] > self.seen["sp"].get("e_" + e2, 0):
                self.E["sp"].wait_ge(self.semh["e_" + e2], self.cnt[e2])

    def psf(self):
        i = self.psf_i % len(self.psf_list)
        self.psf_i += 1
        return self.psf_list[i], f"psf{i}"

    def psb(self):
        i = self.psb_i % len(self.psb_list)
        self.psb_i += 1
        return self.psb_list[i], f"psb{i}"

    def mm(self, out, lhsT, rhs, start, stop, r, w, inc=None):
        if inc is None:
            inc = stop
        return self.op("pe", lambda E: E.matmul(out, lhsT=lhsT, rhs=rhs, start=start, stop=stop), r, w, inc)

    def tr(self, out, in_, ident, r, w, inc=True):
        return self.op("pe", lambda E: E.transpose(out=out, in_=in_, identity=ident), r, w, inc)

    def act(self, out, in_, func, r, w, bias=0.0, scale=1.0, accum_out=None):
        if accum_out is not None:
            return self.op("act", lambda E: E.activation(out=out, in_=in_, func=func, bias=bias, scale=scale,
                                                         accum_out=accum_out), r, w)
        return self.op("act", lambda E: E.activation(out=out, in_=in_, func=func, bias=bias, scale=scale), r, w)

    def tt(self, e, out, in0, in1, op, r, w):
        return self.op(e, lambda E: E.tensor_tensor(out=out, in0=in0, in1=in1, op=op), r, w)

    def ts(self, e, out, in0, s1, s2, op0, op1, r, w):
        if s2 is None:
            return self.op(e, lambda E: E.tensor_scalar(out=out, in0=in0, scalar1=s1, scalar2=None, op0=op0), r, w)
        return self.op(e, lambda E: E.tensor_scalar(out=out, in0=in0, scalar1=s1, scalar2=s2, op0=op0, op1=op1), r, w)

    def stt(self, e, out, in0, scalar, in1, op0, op1, r, w):
        return self.op(e, lambda E: E.scalar_tensor_tensor(out=out, in0=in0, scalar=scalar, in1=in1, op0=op0, op1=op1),
                       r, w)

    def cp(self, e, out, in_, r, w):
        if e == "act":
            return self.op("act", lambda E: E.copy(out=out, in_=in_), r, w)
        return self.op(e, lambda E: E.tensor_copy(out=out, in_=in_), r, w)


def build(dbg=False):
    nc = bass.Bass("TRN2", target_bir_lowering=False)

    def din(name, shape, dt=F32):
        return nc.dram_tensor(name, shape, dt, kind="ExternalInput").ap()

    def dout(name, shape):
        return nc.dram_tensor(name, shape, F32, kind="ExternalOutput").ap()

    x_p = din("x_p", [SEQ, D])
    x_s = din("x_s", [NS * TS, D])
    ck_d = din("ck", [NS, 128, 128])
    cv_d = din("cv", [NS, 128, 128])
    sg_d = din("sg", [NS, 4, 128, 128])
    sc_d = din("sc", [NS * 3, 1536])
    w_in = din("w_in", [D, INC])
    w_out = din("w_out", [D, D])
    sinks_d = din("sinks", [8])
    convw_d = din("conv_w", [4, 1536])
    alog_d = din("a_log", [4])
    dtb_d = din("dt_bias", [4])
    gnw_d = din("gnw", [128])
    ln1g_d = din("ln1_g", [D])
    ln1b_d = din("ln1_b", [D])
    wr_d = din("w_r", [D, 36])
    wg_d = din("w_gate", [NE, D, 256])
    wu_d = din("w_up", [NE, D, 256])
    wd_d = din("w_down", [NE, 256, D])
    ln2g_d = din("ln2_g", [D])
    ln2b_d = din("ln2_b", [D])

    y_p = dout("y_p", [SEQ, D])
    y_s = dout("y_s", [NS * TS, D])
    nk_p = dout("nk_p", [128, 128])
    nv_p = dout("nv_p", [128, 128])
    ng_p = dout("ng_p", [4, 128, 128])
    nc_p = dout("nc_p", [3, 1536])
    nk_s = dout("nk_s", [NS, 128, 128])
    nv_s = dout("nv_s", [NS, 128, 128])
    ng_s = dout("ng_s", [NS, 4, 128, 128])
    nc_s = dout("nc_s", [NS * 3, 1536])
    dbg_outs = {}

    with ExitStack() as es0, nc.allow_non_contiguous_dma(reason="small transposed param loads"):
        S = Sched(nc, es0)
        try:

            def sb(es, name, shape, dt=F32):
                return es.enter_context(nc.sbuf_tensor(name, shape, dt))

            for i in range(6):
                S.psf_list.append(es0.enter_context(nc.psum_tensor(f"psf{i}", [128, 512], F32)))
            for i in range(2):
                S.psb_list.append(es0.enter_context(nc.psum_tensor(f"psb{i}", [128, 1024], BF16)))

            def dump(name, ap_src, shape, keys):
                if not dbg:
                    return
                o = dout("dbg_" + name, shape)
                dbg_outs[name] = o
                S.dma("pool" if ap_src.dtype != F32 else "sp", o, ap_src, r=keys, w=[], slot="dbg_" + name)

            onesf = sb(es0, "onesf", [128, 128])
            ones_bf = sb(es0, "ones_bf", [128, 128], BF16)
            idf = sb(es0, "idf", [128, 128])
            idb = sb(es0, "idb", [128, 128], BF16)
            m_strict = sb(es0, "m_strict", [128, 128])
            m_up = sb(es0, "m_up", [128, 128])
            mb_prev = sb(es0, "mb_prev", [128, 128], BF16)
            mb_cur = sb(es0, "mb_cur", [128, 128], BF16)
            S.op("pool", lambda E: E.memset(onesf[:], 1.0), w=["onesf"])
            S.op("pool", lambda E: E.memset(ones_bf[:], 1.0), w=["ones_bf"])
            S.op("pool", lambda E: E.affine_select(out=idf[:], in_=onesf[:], pattern=[[-1, 128]], compare_op=ALU.is_equal,
                                                   fill=0.0, base=0, channel_multiplier=1), r=["onesf"], w=["idf"])
            S.op("pool", lambda E: E.affine_select(out=m_strict[:], in_=onesf[:], pattern=[[-1, 128]], compare_op=ALU.is_gt,
                                                   fill=0.0, base=0, channel_multiplier=1), r=["onesf"], w=["m_strict"])
            S.op("pool", lambda E: E.affine_select(out=m_up[:], in_=onesf[:], pattern=[[1, 128]], compare_op=ALU.is_ge,
                                                   fill=0.0, base=0, channel_multiplier=-1), r=["onesf"], w=["m_up"])
            S.cp("dve", idb[:], idf[:], r=["idf"], w=["idb"])
            S.cp("dve", mb_prev[:], m_strict[:], r=["m_strict"], w=["mb_prev"])
            S.cp("dve", mb_cur[:], m_up[:], r=["m_up"], w=["mb_cur"])

            esink = sb(es0, "esink", [128, 8])
            nexpA = sb(es0, "nexpA", [128, 4])
            dtb = sb(es0, "dtb", [128, 4])
            gnw = sb(es0, "gnw_bc", [128, 128])
            cw = sb(es0, "cw", [128, 4, 12])
            S.dma("sp", esink[:], sinks_d.partition_broadcast(128), w=["esink"])
            S.dma("sp", nexpA[:], alog_d.partition_broadcast(128), w=["nexpA"])
            S.dma("sp", dtb[:], dtb_d.partition_broadcast(128), w=["dtb"])
            S.dma("sp", gnw[:], gnw_d.partition_broadcast(128), w=["gnw"])
            for k in range(4):
                S.dma("sp", cw[:, k, :], convw_d[k].rearrange("(c p) -> p c", p=128), w=["cw"])
            S.act(esink[:], esink[:], AF.Exp, r=["esink"], w=["esink"])
            S.act(nexpA[:], nexpA[:], AF.Exp, r=["nexpA"], w=["nexpA"])
            S.op("act", lambda E: E.mul(out=nexpA[:], in_=nexpA[:], mul=-1.0), r=["nexpA"], w=["nexpA"])

            cosT = sb(es0, "cosT", [128, NT + 1, 8])
            sinT = sb(es0, "sinT", [128, NT + 1, 8])
            with ExitStack() as esr:
                posi = sb(esr, "posi", [128, NT + 1], I32)
                pos1 = sb(esr, "pos1", [128, 1], I32)
                posf = sb(esr, "posf", [128, NT + 1])
                ang = sb(esr, "ang", [128, NT + 1, 8])
                kk = sb(esr, "kk", [128, NT + 1, 8])
                anl = sb(esr, "anl", [128, NT + 1, 8])
                S.op("pool", lambda E: E.iota(posi[:], pattern=[[128, NT + 1]], base=0, channel_multiplier=1), w=["posi"])
                S.op("pool", lambda E: E.iota(pos1[:], pattern=[[0, 1]], base=0, channel_multiplier=1), w=["pos1"])
                S.op("dve", lambda E: E.tensor_single_scalar(out=pos1[:], in_=pos1[:], scalar=3, op=ALU.bitwise_and),
                     r=["pos1"], w=["pos1"])
                S.cp("dve", posf[:], posi[:], r=["posi"], w=["posf"])
                S.cp("dve", posf[:, NT:NT + 1], pos1[:], r=["pos1", "posf"], w=["posf"])
                S.ts("dve", posf[:, NT:NT + 1], posf[:, NT:NT + 1], float(PAST), None, ALU.add, None, r=["posf"], w=["posf"])
                for j in range(8):
                    f = THETA ** (-(2.0 * j) / 16.0)
                    m_, e_ = math.frexp(f)
                    f_hi = math.ldexp(round(m_ * 1024.0) / 1024.0, e_)
                    f_lo = f - f_hi
                    S.ts("dve", ang[:, :, j], posf[:], float(f_hi), None, ALU.mult, None, r=["posf"], w=["ang"])
                    S.ts("dve", anl[:, :, j], posf[:], float(f_lo), None, ALU.mult, None, r=["posf"], w=["anl"])
                S.tt("dve", kk[:], ang[:], anl[:], ALU.add, r=["ang", "anl"], w=["kk"])
                S.ts("dve", kk[:], kk[:], float(1.0 / (2 * math.pi)), MAGIC, ALU.mult, ALU.add, r=["kk"], w=["kk"])
                S.ts("dve", kk[:], kk[:], MAGIC, None, ALU.subtract, None, r=["kk"], w=["kk"])
                C1 = 6.28125
                C2 = 0.00193548202514648
                C3 = 2 * math.pi - C1 - C2
                for cc in (C1, C2, C3):
                    S.stt("dve", ang[:], kk[:], float(-cc), ang[:], ALU.mult, ALU.add, r=["kk", "ang"], w=["ang"])
                S.tt("dve", ang[:], ang[:], anl[:], ALU.add, r=["ang", "anl"], w=["ang"])
                PI_S = 3.1415925
                S.ts("dve", ang[:], ang[:], -PI_S, PI_S, ALU.max, ALU.min, r=["ang"], w=["ang"])
                S.act(sinT[:], ang[:], AF.Sin, r=["ang"], w=["sinT"])
                S.stt("dve", ang[:], ang[:], -1.0, ang[:], ALU.mult, ALU.max, r=["ang"], w=["ang"])
                S.ts("dve", ang[:], ang[:], -1.0, float(math.pi / 2), ALU.mult, ALU.add, r=["ang"], w=["ang"])
                S.act(cosT[:], ang[:], AF.Sin, r=["ang"], w=["cosT"])
                S.barrier()
            stop("p0")

            mix_all = sb(es0, "mix_all", [128, NT + 1, D], BF16)

            with ExitStack() as es1:
                w_in_bf = sb(es1, "w_in_bf", [128, 8, INC], BF16)
                for kc in range(8):
                    S.dma("pool", w_in_bf[:, kc, :], w_in[kc * 128:(kc + 1) * 128, :], w=[f"w_in{kc}"])
                WIN = [f"w_in{kc}" for kc in range(8)]
                S.op("pool", lambda E: E.memset(mix_all[:, NT, :], 0.0), w=["mix16"])

                with ExitStack() as es:
                    n = NS * TS
                    xTs = sb(es, "xTs", [128, 8, n], BF16)
                    S_b = sb(es, "S_b", [128, NS * 4, 128], BF16)
                    qkTs = sb(es, "qkTs", [128, 8, n], BF16)
                    vTs = sb(es, "vTs", [128, 4, n], BF16)
                    knvs = sb(es, "knvs", [n, 8, 128], BF16)
                    tqkv = sb(es, "tqkv", [n, 768])
                    tz = sb(es, "tz", [n, 512])
                    tab = sb(es, "tab", [n, 8])
                    blk = sb(es, "blk", [n, n])
                    rowm = sb(es, "rowm", [n, NS])
                    U_s = sb(es, "U_s", [n, n])
                    L_s = sb(es, "L_s", [n, n])
                    U_sb = sb(es, "U_sb", [n, n], BF16)
                    smb = sb(es, "smb", [128, NS, n], BF16)
                    nsmb = sb(es, "nsmb", [128, NS, n], BF16)
                    S_f = sb(es, "S_f", [128, 32, 128])
                    esA = ExitStack()
                    xbs = sb(esA, "xbs", [n, D], BF16)
                    cK = sb(esA, "cK", [128, NS, 128], BF16)
                    cVa = sb(esA, "cVa", [128, NS, 2, 65], BF16)
                    cKT = sb(esA, "cKT", [128, NS, 128], BF16)
                    scs = sb(esA, "scs", [NS * 3, 1536])
                    ext = sb(esA, "ext", [128, 12, NS, 7])
                    cprod = sb(esA, "cprod", [128, 12, NS, 4])
                    cacs = sb(esA, "cacs", [128, 12, NS, 4])
                    ncsf = sb(esA, "ncsf", [128, 12, NS * 3])
                    sTs = sb(esA, "sTs", [128, 8, n])
                    sq8 = sb(esA, "sq8", [128, 8, n], BF16)
                    ln8 = sb(esA, "ln8", [128, 8, n])
                    rots = sb(esA, "rots", [n, 10, 16])
                    rtm = sb(esA, "rtm", [n, 4, 10, 8])
                    qbs = sb(esA, "qbs", [n, 4, 2, 64], BF16)
                    kbs = sb(esA, "kbs", [n, 2, 64], BF16)
                    krs = sb(esA, "krs", [n, 2, 64])
                    vna = sb(esA, "vna", [n, 2, 65], BF16)
                    qTs = sb(esA, "qTs", [128, 4, n], BF16)
                    qTsr = sb(esA, "qTsr", [128, NS, 16], BF16)
                    kTn = sb(esA, "kTn", [128, n], BF16)
                    PTs = [sb(esA, f"PTs{i}", [128, 512], BF16) for i in range(2)]
                    mk16 = sb(esA, "mk16", [128, NS, n], BF16)
                    PTn = sb(esA, "PTn", [n, 512], BF16)
                    mc4 = sb(esA, "mc4", [128, 4], BF16)
                    oc = sb(esA, "oc", [65, 512])
                    ot = sb(esA, "ot", [65, 512])
                    dn = sb(esA, "dn", [65, 512])
                    oTb = sb(esA, "oTb", [64, 8, n], BF16)
                    ii = sb(esA, "ii", [n, 64], I32)
                    ip = sb(esA, "ip", [n, 1], I32)
                    colid = sb(esA, "colid", [n, 64])
                    rowid = sb(esA, "rowid", [n, 1])
                    sidx = sb(esA, "sidx", [n, NS])
                    smf = sb(esA, "smf", [128, NS, n])
                    S.dma("pool", xbs[:], x_s, w=["xbs"])
                    S.dma("pool", S_b[:], sg_d.rearrange("s h k v -> k (s h) v"), w=["S_b"])
                    S.dma("pool", cK[:], ck_d.rearrange("s r c -> r s c"), w=["cK"])
                    S.op("pool", lambda E: E.memset(cVa[:], 1.0), w=["cVa"])
                    for jk in range(2):
                        S.dma("pool", cVa[:, :, jk, 0:64], cv_d[:, :, jk * 64:(jk + 1) * 64].rearrange("s r d -> r s d"),
                              w=["cVa"], slot=f"cVa{jk}")
                    S.dma("sp", scs[:], sc_d, w=["scs"])
                    S.op("pool", lambda E: E.memset(vna[:], 1.0), w=["vna"])
                    S.dma("sp", nk_s[:, 0:124, :], ck_d[:, 4:128, :], slot="nk_s0")
                    S.dma("sp", nv_s[:, 0:124, :], cv_d[:, 4:128, :], slot="nv_s0")
                    S.op("pool", lambda E: E.iota(ii[:], pattern=[[1, 64]], base=0, channel_multiplier=0), w=["ii"])
                    S.op("pool", lambda E: E.iota(ip[:], pattern=[[0, 1]], base=0, channel_multiplier=1), w=["ip"])
                    S.op("dve", lambda E: E.tensor_single_scalar(out=ii[:], in_=ii[:], scalar=2, op=ALU.arith_shift_right),
                         r=["ii"], w=["ii"])
                    S.op("dve", lambda E: E.tensor_single_scalar(out=ip[:], in_=ip[:], scalar=2, op=ALU.arith_shift_right),
                         r=["ip"], w=["ip"])
                    S.cp("dve", colid[:], ii[:], r=["ii"], w=["colid"])
                    S.cp("dve", rowid[:], ip[:], r=["ip"], w=["rowid"])
                    S.ts("dve", blk[:], colid[:], rowid[:, 0:1], None, ALU.is_equal, None, r=["colid", "rowid"], w=["blk"])
                    S.op("pool", lambda E: E.iota(ii[:, 0:NS], pattern=[[1, NS]], base=0, channel_multiplier=0), r=["colid"], w=["ii"])
                    S.cp("dve", sidx[:], ii[:, 0:NS], r=["ii"], w=["sidx"])
                    S.ts("dve", rowm[:], sidx[:], rowid[:, 0:1], None, ALU.is_equal, None, r=["sidx", "rowid"], w=["rowm"])
                    S.tt("dve", U_s[:], m_up[0:n, 0:n], blk[:], ALU.mult, r=["blk"], w=["U_s"])
                    S.tt("dve", L_s[:], m_strict[0:n, 0:n], blk[:], ALU.mult, r=["blk"], w=["L_s"])
                    S.cp("dve", U_sb[:], U_s[:], r=["U_s"], w=["U_sb"])
                    S.op("pool", lambda E: E.memset(smf[:], 1.0), w=["smf"])
                    S.op("pool", lambda E: E.affine_select(out=smf[:], in_=smf[:], pattern=[[-4, NS], [1, n]],
                                                           compare_op=ALU.is_ge, fill=0.0, base=0, channel_multiplier=0),
                         r=["smf"], w=["smf"])
                    S.op("pool", lambda E: E.affine_select(out=smf[:], in_=smf[:], pattern=[[4, NS], [-1, n]],
                                                           compare_op=ALU.is_ge, fill=0.0, base=3, channel_multiplier=0),
                         r=["smf"], w=["smf"])
                    S.cp("dve", smb[:], smf[:], r=["smf"], w=["smb"])
                    S.ts("dve", nsmb[:], smf[:], -1.0, None, ALU.mult, None, r=["smf"], w=["nsmb"])
                    S.op("pool", lambda E: E.affine_select(out=mc4[:], in_=ones_bf[:, 0:4], pattern=[[-1, 4]],
                                                           compare_op=ALU.is_gt, fill=0.0, base=0, channel_multiplier=1),
                         w=["mc4"])
                    stop("sA")
                    pb, pbk = S.psb()
                    for kc in range(8):
                        S.tr(pb[:, kc * 128:kc * 128 + n], xbs[:, kc * 128:(kc + 1) * 128], idb[0:n, 0:n], r=["xbs"], w=[pbk],
                             inc=(kc == 7))
                    S.cp("act", xTs[:], pb[:].rearrange("p (k t) -> p k t", k=8)[:, :, 0:n], r=[pbk], w=["xTs"])
                    for grp in range(2):
                        pf, pfk = S.psf()
                        ncg = 8 if grp == 0 else 4
                        for cc in range(ncg):
                            c = grp * 8 + cc
                            S.tr(pf[:, cc * 48:(cc + 1) * 48], scs[:, c * 128:(c + 1) * 128], idf[0:48, 0:48], r=["scs"],
                                 w=[pfk], inc=(cc == ncg - 1))
                        S.cp("act", ext[:, grp * 8:grp * 8 + ncg, :, 0:3],
                             pf[:, 0:ncg * 48].rearrange("p (c s r) -> p c s r", c=ncg, s=NS), r=[pfk], w=["ext"])
                    for c in range(12):
                        pf, pfk = S.psf()
                        for kc in range(8):
                            S.mm(pf[:, 0:n], w_in_bf[:, kc, 768 + c * 128:768 + (c + 1) * 128], xTs[:, kc, :], kc == 0, kc == 7,
                                 r=[WIN[kc], "xTs"], w=[pfk])
                        S.cp("act" if c % 2 else "dve", ext[:, c, :, 3:7], pf[:, 0:n].rearrange("p (s t) -> p s t", s=NS),
                             r=[pfk], w=["ext"])
                    S.cp("pool", ncsf[:].rearrange("p c (s r) -> p c s r", s=NS), ext[:, :, :, 4:7], r=["ext"], w=["ncsf"])
                    for grp in range(3):
                        pf, pfk = S.psf()
                        for cc in range(4):
                            c = grp * 4 + cc
                            S.tr(pf[0:48, cc * 128:(cc + 1) * 128], ncsf[:, c, :], idf[:], r=["ncsf"], w=[pfk], inc=(cc == 3))
                        S.cp("act", scs[:, grp * 512:(grp + 1) * 512], pf[0:48, :], r=[pfk], w=["scs"])
                    S.dma("sp", nc_s, scs[:], r=["scs"], slot="nc_s")
                    stop("sB")
                    for k in range(4):
                        cwb = cw[:, k, :].unsqueeze(2).unsqueeze(3).to_broadcast([128, 12, NS, 4])
                        if k == 0:
                            S.tt("dve", cacs[:], ext[:, :, :, 0:4], cwb, ALU.mult, r=["ext", "cw"], w=["cacs"])
                        else:
                            S.tt("pool", cprod[:], ext[:, :, :, k:k + 4], cwb, ALU.mult, r=["ext", "cw"], w=["cprod"])
                            S.tt("dve", cacs[:], cacs[:], cprod[:], ALU.add, r=["cacs", "cprod"], w=["cacs"])
                    S.act(sTs[:].rearrange("p c (s t) -> p c s t", s=NS), cacs[:, 0:8], AF.Silu, r=["cacs"], w=["sTs"])
                    S.act(vTs[:].rearrange("p c (s t) -> p c s t", s=NS), cacs[:, 8:12], AF.Silu, r=["cacs"], w=["vTs"])
                    S.act(sq8[:], sTs[:], AF.Square, r=["sTs"], w=["sq8"])
                    pf, pfk = S.psf()
                    S.mm(pf[:, 0:8 * n], ones_bf[:], sq8[:].rearrange("p c t -> p (c t)"), True, True, r=["sq8"], w=[pfk])
                    S.act(ln8[:].rearrange("p c t -> p (c t)"), pf[:, 0:8 * n], AF.Ln, r=[pfk], w=["ln8"], bias=L2_EPS)
                    S.act(ln8[:, 0:4, :], ln8[:, 0:4, :], AF.Exp, r=["ln8"], w=["ln8"], bias=-0.5 * math.log(128.0), scale=-0.5)
                    S.act(ln8[:, 4:8, :], ln8[:, 4:8, :], AF.Exp, r=["ln8"], w=["ln8"], scale=-0.5)
                    S.tt("dve", qkTs[:], sTs[:], ln8[:], ALU.mult, r=["sTs", "ln8"], w=["qkTs"])
                    pb, pbk = S.psb()
                    for h in range(4):
                        S.tr(pb[0:n, h * 128:(h + 1) * 128], qkTs[:, 4 + h, :], idb[:], r=["qkTs"], w=[pbk], inc=False)
                    for h in range(4):
                        S.tr(pb[0:n, (4 + h) * 128:(5 + h) * 128], vTs[:, h, :], idb[:], r=["vTs"], w=[pbk], inc=(h == 3))
                    S.cp("act", knvs[:], pb[0:n, :].rearrange("p (k t) -> p k t", k=8), r=[pbk], w=["knvs"])
                    for (c0, c1, dst, dk_) in ((0, 512, tqkv[:, 0:512], "tq_s"), (512, 768, tqkv[:, 512:768], "tkv_s"),
                                               (2304, 2816, tz[:], "tz_s"), (2816, 2824, tab[:], "tab_s")):
                        pf, pfk = S.psf()
                        for kc in range(8):
                            S.mm(pf[0:n, 0:c1 - c0], xTs[:, kc, :], w_in_bf[:, kc, c0:c1], kc == 0, kc == 7,
                                 r=[WIN[kc], "xTs"], w=[pfk])
                        S.cp("act" if dk_ in ("tq_s", "tz_s") else "dve", dst, pf[0:n, 0:c1 - c0], r=[pfk], w=[dk_])
                    qk3 = tqkv[:, 0:640].rearrange("p (h d) -> p h d", d=64)
                    cb = cosT[0:n, NT, :].unsqueeze(1).to_broadcast([n, 10, 8])
                    sbb = sinT[0:n, NT, :].unsqueeze(1).to_broadcast([n, 10, 8])
                    RK = ["tq_s", "tkv_s"]
                    S.tt("dve", rtm[:, 0], qk3[:, :, 0:8], cb, ALU.mult, r=RK, w=["rtm0"])
                    S.tt("dve", rtm[:, 1], qk3[:, :, 8:16], sbb, ALU.mult, r=RK, w=["rtm1"])
                    S.tt("dve", rtm[:, 2], qk3[:, :, 8:16], cb, ALU.mult, r=RK, w=["rtm2"])
                    S.tt("dve", rtm[:, 3], qk3[:, :, 0:8], sbb, ALU.mult, r=RK, w=["rtm3"])
                    S.tt("dve", rots[:, :, 0:8], rtm[:, 0], rtm[:, 1], ALU.subtract, r=["rtm0", "rtm1"], w=["rots"])
                    S.tt("dve", rots[:, :, 8:16], rtm[:, 2], rtm[:, 3], ALU.add, r=["rtm2", "rtm3"], w=["rots"])
                    q4 = tqkv[:, 0:512].rearrange("p (j c d) -> p c j d", j=2, c=4)
                    S.cp("dve", qbs[:, :, :, 0:16], rots[:, 0:8, :].rearrange("p (j c) d -> p c j d", j=2), r=["rots"], w=["qbs"])
                    S.cp("dve", qbs[:, :, :, 16:64], q4[:, :, :, 16:64], r=["tq_s"], w=["qbs"])
                    k3 = tqkv[:, 512:640].rearrange("p (j d) -> p j d", j=2)
                    S.cp("dve", kbs[:, :, 0:16], rots[:, 8:10, :], r=["rots"], w=["kbs"])
                    S.cp("dve", kbs[:, :, 16:64], k3[:, :, 16:64], r=["tkv_s"], w=["kbs"])
                    S.cp("dve", krs[:, :, 0:16], rots[:, 8:10, :], r=["rots"], w=["krs"])
                    S.cp("dve", krs[:, :, 16:64], k3[:, :, 16:64], r=["tkv_s"], w=["krs"])
                    S.cp("dve", vna[:, :, 0:64], tqkv[:, 640:768].rearrange("p (j d) -> p j d", j=2), r=["tkv_s", "vna"], w=["vna"])
                    S.dma("sp", nk_s[:, 124:128, :].rearrange("s t c -> (s t) c") if False else nk_s[:, 124:128, :],
                          krs[:].rearrange("(s t) j d -> s t (j d)", t=TS) if False else krs[:].rearrange("p j d -> p (j d)"),
                          r=["krs"], slot="nk_s1")
                    S.dma("sp", nv_s[:, 124:128, :], tqkv[:, 640:768], r=["tkv_s"], slot="nv_s1")
                    stop("sC")
                    pb, pbk = S.psb()
                    for c in range(4):
                        S.tr(pb[:, c * 128:c * 128 + n], qbs[:, c].rearrange("p j d -> p (j d)"), idb[0:n, 0:n], r=["qbs"],
                             w=[pbk], inc=False)
                    S.tr(pb[:, 512:512 + n], kbs[:].rearrange("p j d -> p (j d)"), idb[0:n, 0:n], r=["kbs"], w=[pbk])
                    S.cp("act", qTs[:], pb[:, 0:512].rearrange("p (c t) -> p c t", c=4)[:, :, 0:n], r=[pbk], w=["qTs"])
                    S.cp("act", kTn[:], pb[:, 512:512 + n], r=[pbk], w=["kTn"])
                    S.cp("dve", qTsr[:].rearrange("p s (c t) -> p s c t", c=4),
                         qTs[:].rearrange("p c (s t) -> p s c t", s=NS), r=["qTs"], w=["qTsr"])
                    for grp in range(2):
                        pb, pbk = S.psb()
                        for ss in range(8):
                            s_ = grp * 8 + ss
                            S.tr(pb[:, ss * 128:(ss + 1) * 128], cK[:, s_, :], idb[:], r=["cK"], w=[pbk], inc=(ss == 7))
                        S.cp("act" if grp else "dve", cKT[:, grp * 8:(grp + 1) * 8, :], pb[:].rearrange("p (s t) -> p s t", s=8),
                             r=[pbk], w=["cKT"])
                    stop("sC2")
                    S.tt("dve", mk16[:].rearrange("p s (a t) -> p (s a) t", t=4), smb[:].rearrange("p s (a t) -> p (s a) t", t=4),
                         mc4[:].unsqueeze(1).to_broadcast([128, NS * NS, 4]), ALU.mult, r=["smb", "mc4"], w=["mk16"])
                    stop("sD0")
                    base = S.psf_i % 6
                    S.psf_i += 6
                    bk = [(base + d) % 6 for d in range(6)]
                    po_ = [(S.psf_list[bk[j]], f"psf{bk[j]}") for j in range(2)]
                    ps_ = [(S.psf_list[bk[2 + j]], f"psf{bk[2 + j]}") for j in range(4)]
                    for s_ in range(NS):
                        pt_, ptk_ = PTs[s_ % 2], f"PTs{s_ % 2}"
                        for jk in range(2):
                            psc, psck = ps_[(s_ % 2) * 2 + jk]
                            S.mm(psc[:, 0:256], cKT[jk * 64:(jk + 1) * 64, s_, :],
                                 qTs[jk * 64:(jk + 1) * 64, :, :].rearrange("p c t -> p (c t)"), True, True,
                                 r=["cKT", "qTs"], w=[psck], inc=True)
                            S.act(pt_[:, jk * 256:(jk + 1) * 256], psc[:, 0:256], AF.Exp, r=[psck], w=[ptk_], scale=0.125)
                        S.tt("dve", pt_[:].rearrange("p (a t) -> p a t", a=8), pt_[:].rearrange("p (a t) -> p a t", a=8),
                             mk16[:, s_, :].unsqueeze(1).to_broadcast([128, 8, n]), ALU.mult, r=[ptk_, "mk16"], w=[ptk_])
                        for jk in range(2):
                            S.mm(po_[jk][0][0:65, 0:256], cVa[:, s_, jk, :], pt_[:, jk * 256:(jk + 1) * 256], s_ == 0, False,
                                 r=["cVa", ptk_], w=[po_[jk][1]], inc=(jk == 1))
                    stop("sD1")
                    for jk in range(2):
                        psn, psnk = ps_[jk]
                        S.mm(psn[0:n, 0:256], kTn[jk * 64:(jk + 1) * 64, :],
                             qTs[jk * 64:(jk + 1) * 64, :, :].rearrange("p c t -> p (c t)"), True, True, r=["kTn", "qTs"],
                             w=[psnk], inc=True)
                        S.act(PTn[:, jk * 256:(jk + 1) * 256], psn[0:n, 0:256], AF.Exp, r=[psnk], w=["PTn"], scale=0.125)
                    S.tt("pool", PTn[:].rearrange("p (a t) -> p a t", a=8), PTn[:].rearrange("p (a t) -> p a t", a=8),
                         U_sb[:].unsqueeze(1).to_broadcast([n, 8, n]), ALU.mult, r=["PTn", "U_sb"], w=["PTn"])
                    for jk in range(2):
                        S.mm(po_[jk][0][0:65, 0:256], vna[:, jk, :], PTn[:, jk * 256:(jk + 1) * 256], False, True,
                             r=["vna", "PTn"], w=[po_[jk][1]], inc=True)
                    stop("sD3")
                    for jk in range(2):
                        S.cp("act" if jk else "dve", ot[:, jk * 256:(jk + 1) * 256], po_[jk][0][0:65, 0:256], r=[po_[jk][1]],
                             w=["ot"])
                    S.tt("dve", dn[64:65, :].rearrange("p (h t) -> p h t", h=8), ot[64:65, :].rearrange("p (h t) -> p h t", h=8),
                         esink[64:65, :].unsqueeze(2).to_broadcast([1, 8, n]), ALU.add, r=["ot", "esink"], w=["dn"])
                    S.op("dve", lambda E: E.reciprocal(out=dn[64:65, :], in_=dn[64:65, :]), r=["dn"], w=["dn"])
                    stop("sD4")
                    pbc, pbck = S.psf()
                    S.mm(pbc[0:64, :], onesf[64:65, 0:64], dn[64:65, :], True, True, r=["dn"], w=[pbck])
                    S.tt("dve", oTb[:].rearrange("p h t -> p (h t)"), pbc[0:64, :], ot[0:64, :], ALU.mult, r=[pbck, "ot"],
                         w=["oTb"])
                    stop("sD5")
                    pb, pbk = S.psb()
                    for h in range(8):
                        S.tr(pb[0:n, h * 64:(h + 1) * 64], oTb[:, h, :], idb[0:64, 0:64], r=["oTb"], w=[pbk], inc=(h == 7))
                    S.cp("act", mix_all[0:n, NT, 0:512], pb[0:n, 0:512], r=[pbk], w=["mix16"])
                    S.barrier()
                    esA.close()
                    gabs = sb(es, "gabs", [n, 8, 4])
                    gmk = sb(es, "gmk", [n, NS, 4])
                    abc = sb(es, "abc", [128, NS * 4])
                    gUs = sb(es, "gUs", [n, 4, n])
                    eDs = sb(es, "eDs", [n, 4, n])
                    eDTs = sb(es, "eDTs", [n, 4, n])
                    ATs = sb(es, "ATs", [n, 4, n], BF16)
                    P0s = sb(es, "P0s", [n, 4, n])
                    P0Ts = sb(es, "P0Ts", [n, 4, n])
                    P1s = sb(es, "P1s", [n, 4, n])
                    Q0s = sb(es, "Q0s", [n, 4, n])
                    Qbs = sb(es, "Qbs", [n, 4, n], BF16)
                    kbes = sb(es, "kbes", [n, 4, 128], BF16)
                    kdcs = sb(es, "kdcs", [n, 4, 128], BF16)
                    vbs = sb(es, "vbs", [n, 4, 128], BF16)
                    vns = sb(es, "vns", [n, 4, 128], BF16)
                    o1ss = sb(es, "o1ss", [n, 4, 128])
                    ogs = sb(es, "ogs", [n, 4, 128])
                    osqs = sb(es, "osqs", [n, 4, 128])
                    ssqs = sb(es, "ssqs", [n, 4])
                    zss = sb(es, "zss", [n, 512])
                    Stm = [sb(es, f"Stm{i}", [128, 4, 128]) for i in range(2)]
                    nwTh = [sb(es, f"nwTh{i}", [128, NS, n], BF16) for i in range(2)]
                    qTh = [sb(es, f"qTh{i}", [128, NS, n], BF16) for i in range(2)]
                    vnh = [sb(es, f"vnh{i}", [n, NS, 128], BF16) for i in range(2)]

                    stop("sD")
                    g_ = gabs[:, 0, :]
                    beta_ = gabs[:, 1, :]
                    gc_ = gabs[:, 2, :]
                    egc_ = gabs[:, 3, :]
                    kds_ = gabs[:, 4, :]
                    tmp_ = gabs[:, 6, :]
                    nbeta_ = gabs[:, 7, :]
                    S.tt("dve", tmp_, tab[:, 0:4], dtb[0:n, :], ALU.add, r=["tab_s"], w=["s_tmp"])
                    S.act(tmp_, tmp_, AF.Exp, r=["s_tmp"], w=["s_tmp"])
                    S.act(tmp_, tmp_, AF.Ln, r=["s_tmp"], w=["s_tmp"], bias=1.0)
                    S.tt("dve", g_, tmp_, nexpA[0:n, :], ALU.mult, r=["s_tmp"], w=["s_g"])
                    S.act(beta_, tab[:, 4:8], AF.Exp, r=["tab_s"], w=["s_beta"], scale=-1.0)
                    S.ts("dve", beta_, beta_, 1.0, None, ALU.add, None, r=["s_beta"], w=["s_beta"])
                    S.op("dve", lambda E: E.reciprocal(out=beta_, in_=beta_), r=["s_beta"], w=["s_beta"])
                    S.ts("dve", nbeta_, beta_, -1.0, None, ALU.mult, None, r=["s_beta"], w=["s_nbeta"])
                    S.tt("dve", gmk[:], g_.unsqueeze(1).to_broadcast([n, NS, 4]), rowm[:].unsqueeze(2).to_broadcast([n, NS, 4]),
                         ALU.mult, r=["s_g", "rowm"], w=["gmk"])
                    pf, pfk = S.psf()
                    S.mm(pf[0:n, 0:4], U_s[:], g_, True, True, r=["U_s", "s_g"], w=[pfk])
                    S.mm(pf[0:n, 4:8], blk[:], g_, True, True, r=["blk", "s_g"], w=[pfk])
                    S.cp("dve", gc_, pf[0:n, 0:4], r=[pfk], w=["s_gc"])
                    S.act(egc_, pf[0:n, 0:4], AF.Exp, r=[pfk], w=["s_egc"])
                    S.tt("dve", kds_, pf[0:n, 4:8], gc_, ALU.subtract, r=[pfk, "s_gc"], w=["s_kds"])
                    S.act(kds_, kds_, AF.Exp, r=["s_kds"], w=["s_kds"])
                    pa, pak = S.psf()
                    S.mm(pa[:, 0:NS * 4], onesf[0:n, :], gmk[:].rearrange("p s h -> p (s h)"), True, True, r=["gmk"], w=[pak])
                    S.act(abc[:], pa[:, 0:NS * 4], AF.Exp, r=[pak], w=["abc"])
                    pD, pDk = S.psf()
                    pDT, pDTk = S.psf()
                    pG, pGk = S.psf()
                    pA, pAk = S.psf()
                    v3 = lambda p: p[0:n, 0:4 * n].rearrange("p (h t) -> p h t", h=4)
                    pD3, pDT3, pG3, pA3 = v3(pD), v3(pDT), v3(pG), v3(pA)
                    for h in range(4):
                        S.ts("dve", gUs[:, h, :], U_s[:], gabs[:, 0, h:h + 1], None, ALU.mult, None, r=["U_s", "s_g"], w=["gUs"])
                    for h in range(4):
                        S.mm(pD3[:, h, :], gUs[:, h, :], L_s[:], True, True, r=["gUs", "L_s"], w=[pDk], inc=(h == 3))
                    for h in range(4):
                        S.mm(pDT3[:, h, :], L_s[:], gUs[:, h, :], True, True, r=["gUs", "L_s"], w=[pDTk], inc=(h == 3))
                    for h in range(4):
                        S.mm(pG3[:, h, :], qkTs[:, 4 + h, :], qkTs[:, 4 + h, :], True, True, r=["qkTs"], w=[pGk], inc=(h == 3))
                    for h in range(4):
                        S.mm(pA3[:, h, :], qkTs[:, 4 + h, :], qkTs[:, h, :], True, True, r=["qkTs"], w=[pAk], inc=(h == 3))
                    S.act(eDs[:], pD3, AF.Exp, r=[pDk], w=["eDs"])
                    S.tt("pool", eDs[:], eDs[:], L_s[:].unsqueeze(1).to_broadcast([n, 4, n]), ALU.mult, r=["eDs", "L_s"], w=["eDs"])
                    S.act(eDTs[:], pDT3, AF.Exp, r=[pDTk], w=["eDTs"])
                    S.tt("pool", eDTs[:], eDTs[:], U_s[:].unsqueeze(1).to_broadcast([n, 4, n]), ALU.mult, r=["eDTs", "U_s"],
                         w=["eDTs"])
                    for h in range(4):
                        S.stt("dve", P0s[:, h, :], pG3[:, h, :], gabs[:, 7, h:h + 1], eDs[:, h, :], ALU.mult, ALU.mult,
                              r=[pGk, "s_nbeta", "eDs"], w=["P0s"])
                    S.tt("dve", ATs[:], pA3, eDTs[:], ALU.mult, r=[pAk, "eDTs"], w=["ATs"])
                    pt, ptk = S.psf()
                    pt3 = v3(pt)
                    for h in range(4):
                        S.tr(pt3[:, h, :], P0s[:, h, :], idf[0:n, 0:n], r=["P0s"], w=[ptk], inc=(h == 3))
                    S.cp("act", P0Ts[:], pt3, r=[ptk], w=["P0Ts"])
                    S.tt("dve", Q0s[:], pt3, idf[0:n, 0:n].unsqueeze(1).to_broadcast([n, 4, n]), ALU.add, r=[ptk], w=["Q0s"])
                    pP, pPk = S.psf()
                    pP3 = v3(pP)
                    for h in range(4):
                        S.mm(pP3[:, h, :], P0Ts[:, h, :], P0s[:, h, :], True, True, r=["P0Ts", "P0s"], w=[pPk], inc=(h == 3))
                    S.cp("act", P1s[:], pP3, r=[pPk], w=["P1s"])
                    pQ, pQk = S.psf()
                    pQ3 = v3(pQ)
                    for h in range(4):
                        S.mm(pQ3[:, h, :], P1s[:, h, :], Q0s[:, h, :], True, True, r=["P1s", "Q0s"], w=[pQk], inc=(h == 3))
                    S.tt("dve", Qbs[:], pQ3, Q0s[:], ALU.add, r=[pQk, "Q0s"], w=["Qbs"])
                    stop("sE")
                    kn3 = knvs[:, 0:4, :]
                    vv3 = knvs[:, 4:8, :]
                    S.tt("dve", tmp_, beta_, egc_, ALU.mult, r=["s_beta", "s_egc", "s_tmp"], w=["s_tmp"])
                    S.tt("pool", kbes[:], kn3, tmp_.unsqueeze(2).to_broadcast([n, 4, 128]), ALU.mult, r=["knvs", "s_tmp"], w=["kbes"])
                    S.tt("pool", kdcs[:], kn3, kds_.unsqueeze(2).to_broadcast([n, 4, 128]), ALU.mult, r=["knvs", "s_kds"], w=["kdcs"])
                    S.tt("dve", vbs[:], vv3, beta_.unsqueeze(2).to_broadcast([n, 4, 128]), ALU.mult, r=["knvs", "s_beta"], w=["vbs"])
                    pw, pwk = S.psf()
                    pw3 = pw[:, 0:4 * n].rearrange("p (h t) -> p h t", h=4)
                    for h in range(4):
                        S.mm(pw3[:, h, :], kbes[:, h, :], Qbs[:, h, :], True, True, r=["kbes", "Qbs"], w=[pwk], inc=(h == 3))
                    pv, pvk = S.psf()
                    pv3 = pv[0:n, :].rearrange("p (h t) -> p h t", h=4)
                    for h in range(4):
                        S.tt("dve", nwTh[h % 2][:], pw3[:, h, :].unsqueeze(1).to_broadcast([128, NS, n]), nsmb[:], ALU.mult,
                             r=[pwk, "nsmb"], w=[f"nwTh{h % 2}"])
                        S.mm(pv3[:, h, :], Qbs[:, h, :], vbs[:, h, :], True, False, r=["Qbs", "vbs"], w=[pvk], inc=False)
                        for s_ in range(NS):
                            S.mm(pv3[:, h, :], nwTh[h % 2][:, s_, :], S_b[:, s_ * 4 + h, :], False, s_ == NS - 1,
                                 r=[f"nwTh{h % 2}", "S_b"], w=[pvk], inc=(s_ == NS - 1))
                    S.cp("act", vns[:], pv3, r=[pvk], w=["vns"])
                    po1, po1k = S.psf()
                    po13 = po1[0:n, :].rearrange("p (h t) -> p h t", h=4)
                    for h in range(4):
                        S.tt("pool", qTh[h % 2][:], qkTs[:, h, :].unsqueeze(1).to_broadcast([128, NS, n]), smb[:], ALU.mult,
                             r=["qkTs", "smb"], w=[f"qTh{h % 2}"])
                        for s_ in range(NS):
                            S.mm(po13[:, h, :], qTh[h % 2][:, s_, :], S_b[:, s_ * 4 + h, :], s_ == 0, s_ == NS - 1,
                                 r=[f"qTh{h % 2}", "S_b"], w=[po1k], inc=(s_ == NS - 1))
                    po2, po2k = S.psf()
                    po23 = po2[0:n, :].rearrange("p (h t) -> p h t", h=4)
                    for h in range(4):
                        S.mm(po23[:, h, :], ATs[:, h, :], vns[:, h, :], True, True, r=["ATs", "vns"], w=[po2k], inc=(h == 3))
                    S.tt("dve", o1ss[:], po13, egc_.unsqueeze(2).to_broadcast([n, 4, 128]), ALU.mult, r=[po1k, "s_egc"], w=["o1ss"])
                    S.tt("dve", ogs[:], po23, o1ss[:], ALU.add, r=[po2k, "o1ss"], w=["ogs"])
                    stop("sF")
                    S_f4 = S_f[:].rearrange("p (s h) v -> p s h v", h=4)
                    abc3 = abc[:].rearrange("p (s h) -> p s h", h=4)
                    for half in range(2):
                        S.dma("sp", S_f[:], sg_d[half * 8:(half + 1) * 8].rearrange("s h k v -> k (s h) v"), w=["S_f"])
                        for h in range(4):
                            vh, vhk = vnh[h % 2], f"vnh{h % 2}"
                            S.tt("dve", vh[:], vns[:, h, :].unsqueeze(1).to_broadcast([n, NS, 128]),
                                 rowm[:].unsqueeze(2).to_broadcast([n, NS, 128]), ALU.mult, r=["vns", "rowm"], w=[vhk])
                            for sg_ in range(2):
                                pS, pSk = S.psf()
                                pS3 = pS[:].rearrange("p (s t) -> p s t", s=4)
                                for sl in range(4):
                                    s_ = half * 8 + sg_ * 4 + sl
                                    S.mm(pS3[:, sl, :], kdcs[:, h, :], vh[:, s_, :], True, True, r=["kdcs", vhk], w=[pSk],
                                         inc=(sl == 3))
                                stt_, sttk = Stm[sg_ % 2], f"Stm{sg_ % 2}"
                                sl0 = sg_ * 4
                                s0 = half * 8 + sg_ * 4
                                S.tt("pool", stt_[:], S_f4[:, sl0:sl0 + 4, h, :],
                                     abc3[:, s0:s0 + 4, h].unsqueeze(2).to_broadcast([128, 4, 128]), ALU.mult,
                                     r=["S_f", "abc"], w=[sttk])
                                S.tt("dve", S_f4[:, sl0:sl0 + 4, h, :], pS3, stt_[:], ALU.add, r=[pSk, sttk], w=["S_f"])
                        S.dma("sp", ng_s[half * 8:(half + 1) * 8].rearrange("s h k v -> k (s h) v"), S_f[:], r=["S_f"], slot="ng_s")
                    S.tt("pool", osqs[:], ogs[:], ogs[:], ALU.mult, r=["ogs"], w=["osqs"])
                    S.op("dve", lambda E: E.tensor_reduce(out=ssqs[:], in_=osqs[:], axis=AX.X, op=ALU.add), r=["osqs"], w=["ssqs"])
                    S.act(ssqs[:], ssqs[:], AF.Ln, r=["ssqs"], w=["ssqs"], bias=NORM_EPS, scale=1.0 / 128.0)
                    S.act(ssqs[:], ssqs[:], AF.Exp, r=["ssqs"], w=["ssqs"], scale=-0.5)
                    S.act(zss[:], tz[:], AF.Silu, r=["tz_s"], w=["zss"])
                    S.tt("pool", zss[:].rearrange("p (h d) -> p h d", h=4), zss[:].rearrange("p (h d) -> p h d", h=4),
                         gnw[0:n, :].unsqueeze(1).to_broadcast([n, 4, 128]), ALU.mult, r=["zss"], w=["zss"])
                    S.tt("dve", ogs[:], ogs[:], ssqs[:].unsqueeze(2).to_broadcast([n, 4, 128]), ALU.mult, r=["ogs", "ssqs"], w=["ogs"])
                    S.tt("dve", mix_all[0:n, NT, 512:1024].rearrange("p (h d) -> p h d", h=4), ogs[:],
                         zss[:].rearrange("p (h d) -> p h d", h=4), ALU.mult, r=["ogs", "zss"], w=["mix16"])
                    S.barrier()

                with ExitStack() as es:
                    ST = 256
                    xb = [sb(es, f"xb{i}", [128, D], BF16) for i in range(2)]
                    xT = [sb(es, f"xT{i}", [128, 8, ST], BF16) for i in range(2)]
                    gT = sb(es, "gT", [128, 12, ST + 3])
                    cacc = [sb(es, f"cacc{i}", [128, ST]) for i in range(2)]
                    sT = sb(es, "sT", [128, 8, ST])
                    qkT2 = [sb(es, f"qkT_{i}", [128, 8, ST], BF16) for i in range(2)]
                    vT2 = [sb(es, f"vT_{i}", [128, 4, ST], BF16) for i in range(2)]
                    sq4 = sb(es, "sq4", [128, 4, ST], BF16)
                    ln4 = sb(es, "ln4", [128, 4, ST])
                    knv = sb(es, "knv", [128, 8, 128], BF16)
                    tm_qkv = sb(es, "tm_qkv", [128, 768])
                    zs4 = [sb(es, f"zs2_{i}", [128, 512]) for i in range(4)]
                    tm_ab = sb(es, "tm_ab", [128, 8])
                    rot = sb(es, "rot", [128, 10, 16])
                    rtmp = sb(es, "rtmp", [128, 4, 10, 8])
                    q_bf = sb(es, "q_bf", [128, 4, 2, 64], BF16)
                    k_bf = sb(es, "k_bf", [128, 2, 64], BF16)
                    k_rot = sb(es, "k_rot", [128, 2, 64])
                    v_aug = [sb(es, f"v_aug{i}", [128, 2, 65], BF16) for i in range(2)]
                    kTb = [sb(es, f"kTb{i}", [128, 128], BF16) for i in range(2)]
                    qT = sb(es, "qT", [128, 4, 128], BF16)
                    PTe = [sb(es, f"PTe{i}", [128, 4, 128], BF16) for i in range(2)]
                    den = sb(es, "den", [128, 2, 4])
                    gab = sb(es, "gab", [128, 8, 4])
                    gU = sb(es, "gU", [128, 4, 128])
                    eD = sb(es, "eD", [128, 4, 128])
                    eDT = sb(es, "eDT", [128, 4, 128])
                    ATb = sb(es, "ATb", [128, 4, 128], BF16)
                    Pm = [sb(es, f"Pm{i}", [128, 4, 128]) for i in range(2)]
                    PmT = [sb(es, f"PmT{i}", [128, 4, 128]) for i in range(2)]
                    Qm = [sb(es, f"Qm{i}", [128, 4, 128]) for i in range(2)]
                    Qb = sb(es, "Qb", [128, 4, 128], BF16)
                    kbe = sb(es, "kbe", [128, 4, 128], BF16)
                    kdc = sb(es, "kdc", [128, 4, 128], BF16)
                    vb = sb(es, "vb", [128, 4, 128], BF16)
                    nwT = sb(es, "nwT", [128, 4, 128], BF16)
                    vnew = sb(es, "vnew", [128, 4, 128], BF16)
                    Sf = sb(es, "Sf", [128, 4, 128])
                    Sb = sb(es, "Sb", [128, 4, 128], BF16)
                    Stmp = sb(es, "Stmp", [128, 4, 128])
                    o1s = sb(es, "o1s", [128, 4, 128])
                    og = sb(es, "og", [128, 4, 128])
                    osq = sb(es, "osq", [128, 4, 128])
                    ssq = sb(es, "ssq", [128, 4])

                    S.op("pool", lambda E: E.memset(gT[:, :, 0:3], 0.0), w=[f"gT{c}" for c in range(12)])
                    S.op("pool", lambda E: E.memset(Sf[:], 0.0), w=["Sf"])
                    S.op("pool", lambda E: E.memset(Sb[:], 0.0), w=["Sb"])
                    for i in range(2):
                        S.op("pool", lambda E, i=i: E.memset(v_aug[i][:], 1.0), w=[f"v_aug{i}"])

                    GT = [f"gT{c}" for c in range(12)]
                    def fm(st):
                        qkT, vT, zs2 = qkT2[st % 2], vT2[st % 2], zs4[(st % 2) * 2:(st % 2) * 2 + 2]
                        PAR = st % 2
                        xTs = xT[st % 2]
                        xTk = f"xT{st % 2}"
                        for j in range(ST // 128):
                            i = st * (ST // 128) + j
                            xbt = xb[i % 2]
                            xbk = f"xb{i % 2}"
                            S.dma("pool", xbt[:], x_p[i * 128:(i + 1) * 128, :], w=[xbk])
                            pb, pbk = S.psb()
                            for kc in range(8):
                                S.tr(pb[:, kc * 128:(kc + 1) * 128], xbt[:, kc * 128:(kc + 1) * 128], idb[:],
                                     r=[xbk, "idb"], w=[pbk], inc=(kc == 7))
                            S.cp("act", xTs[:, :, j * 128:(j + 1) * 128], pb[:].rearrange("p (k t) -> p k t", k=8),
                                 r=[pbk], w=[xTk])
                        for c in range(12):
                            pf, pfk = S.psf()
                            for kc in range(8):
                                S.mm(pf[:, 0:ST], w_in_bf[:, kc, 768 + c * 128:768 + (c + 1) * 128], xTs[:, kc, :],
                                     kc == 0, kc == 7, r=[WIN[kc], xTk], w=[pfk])
                            S.cp("act" if c % 2 else "dve", gT[:, c, 3:3 + ST], pf[:, 0:ST], r=[pfk], w=[GT[c]])
                            acc = cacc[c % 2]
                            ak = f"cacc{c % 2}"
                            ce = "dve"
                            S.ts(ce, acc[:], gT[:, c, 0:ST], cw[:, 0, c:c + 1], None, ALU.mult, None, r=[GT[c], "cw"], w=[ak])
                            for k in range(1, 4):
                                S.stt(ce, acc[:], gT[:, c, k:k + ST], cw[:, k, c:c + 1], acc[:], ALU.mult, ALU.add,
                                      r=[GT[c], "cw", ak], w=[ak])
                            if c < 8:
                                S.act(sT[:, c, :], acc[:], AF.Silu, r=[ak], w=[f"sT{c}"])
                            else:
                                S.act(vT[:, c - 8, :], acc[:], AF.Silu, r=[ak], w=[f"vT_{PAR}"])
                        for j in range(ST // 128):
                            pf, pfk = S.psf()
                            for kc in range(8):
                                S.mm(pf[:, :], xTs[:, kc, j * 128:(j + 1) * 128], w_in_bf[:, kc, 2304:2816], kc == 0, kc == 7,
                                     r=[WIN[kc], xTk], w=[pfk])
                            S.act(zs2[j][:], pf[:, :], AF.Silu, r=[pfk], w=[f"zs2_{PAR}_{j}"])
                            S.tt("pool", zs2[j][:].rearrange("p (h d) -> p h d", h=4), zs2[j][:].rearrange("p (h d) -> p h d", h=4),
                                 gnw[:].unsqueeze(1).to_broadcast([128, 4, 128]), ALU.mult, r=[f"zs2_{PAR}_{j}", "gnw"], w=[f"zs2_{PAR}_{j}"])
                        if st == SEQ // ST - 1:
                            for t in range(3):
                                S.dma("sp", nc_p[t].rearrange("(c p) -> p c", p=128), gT[:, :, ST + t], r=GT, w=[],
                                      slot="nc_p")
                        else:
                            S.cp("pool", gT[:, :, 0:3], gT[:, :, ST:ST + 3], r=GT, w=GT)
                        for half in range(2):
                            for hh in range(4):
                                c = half * 4 + hh
                                S.act(sq4[:, hh, :], sT[:, c, :], AF.Square, r=[f"sT{c}"], w=[f"sq{hh}"])
                            pfs = []
                            for hh in range(4):
                                pf, pfk = S.psf()
                                S.mm(pf[:, 0:ST], ones_bf[:], sq4[:, hh, :], True, True, r=["ones_bf", f"sq{hh}"], w=[pfk])
                                pfs.append((pf, pfk))
                            for hh in range(4):
                                pf, pfk = pfs[hh]
                                S.act(ln4[:, hh, :], pf[:, 0:ST], AF.Ln, r=[pfk], w=[f"ln{hh}"], bias=L2_EPS)
                            for hh in range(4):
                                bq = -0.5 * math.log(128.0) if half == 0 else 0.0
                                S.act(ln4[:, hh, :], ln4[:, hh, :], AF.Exp, r=[f"ln{hh}"], w=[f"ln{hh}"], bias=bq, scale=-0.5)
                            for hh in range(4):
                                c = half * 4 + hh
                                S.tt("pool" if hh % 2 else "dve", qkT[:, c, :], sT[:, c, :], ln4[:, hh, :], ALU.mult,
                                     r=[f"sT{c}", f"ln{hh}"], w=[f"qkT{c}_{PAR}"])
                        if st == 0:
                            stop("st0a")

                    def tile(st, j):
                        qkT, vT, zs2 = qkT2[st % 2], vT2[st % 2], zs4[(st % 2) * 2:(st % 2) * 2 + 2]
                        PAR = st % 2
                        xTs = xT[st % 2]
                        xTk = f"xT{st % 2}"
                        i = st * (ST // 128) + j
                        tok = slice(j * 128, (j + 1) * 128)
                        pb, pbk = S.psb()
                        for h in range(4):
                            S.tr(pb[:, h * 128:(h + 1) * 128], qkT[:, 4 + h, tok], idb[:], r=[f"qkT{4 + h}_{PAR}", "idb"],
                                 w=[pbk], inc=False)
                        for h in range(4):
                            S.tr(pb[:, (4 + h) * 128:(5 + h) * 128], vT[:, h, tok], idb[:], r=[f"vT_{PAR}", "idb"], w=[pbk],
                                 inc=(h == 3))
                        S.cp("act", knv[:], pb[:].rearrange("p (k t) -> p k t", k=8), r=[pbk], w=["knv"])
                        if i == 0:
                            stop("t0a")
                        for (c0, c1, dst, dk_) in ((0, 512, tm_qkv[:, 0:512], "tm_q"), (512, 768, tm_qkv[:, 512:768], "tm_kv"),
                                                   (2816, 2824, tm_ab[:], "tm_ab")):
                            pf, pfk = S.psf()
                            for kc in range(8):
                                S.mm(pf[:, 0:c1 - c0], xTs[:, kc, tok], w_in_bf[:, kc, c0:c1], kc == 0, kc == 7,
                                     r=[WIN[kc], xTk], w=[pfk])
                            S.cp("act" if dk_ in ("tm_q", "tm_z") else "dve", dst, pf[:, 0:c1 - c0], r=[pfk], w=[dk_])
                        if i == 0:
                            stop("t0b")
                        qk3 = tm_qkv[:, 0:640].rearrange("p (h d) -> p h d", d=64)
                        cb = cosT[:, i, :].unsqueeze(1).to_broadcast([128, 10, 8])
                        sbb = sinT[:, i, :].unsqueeze(1).to_broadcast([128, 10, 8])
                        RK = ["tm_q", "tm_kv", "cosT", "sinT"]
                        S.tt("dve", rtmp[:, 0], qk3[:, :, 0:8], cb, ALU.mult, r=RK, w=["rt0"])
                        S.tt("dve", rtmp[:, 1], qk3[:, :, 8:16], sbb, ALU.mult, r=RK, w=["rt1"])
                        S.tt("dve", rtmp[:, 2], qk3[:, :, 8:16], cb, ALU.mult, r=RK, w=["rt2"])
                        S.tt("dve", rtmp[:, 3], qk3[:, :, 0:8], sbb, ALU.mult, r=RK, w=["rt3"])
                        S.tt("dve", rot[:, :, 0:8], rtmp[:, 0], rtmp[:, 1], ALU.subtract, r=["rt0", "rt1"], w=["rot"])
                        S.tt("dve", rot[:, :, 8:16], rtmp[:, 2], rtmp[:, 3], ALU.add, r=["rt2", "rt3"], w=["rot"])
                        q4 = tm_qkv[:, 0:512].rearrange("p (j c d) -> p c j d", j=2, c=4)
                        S.cp("dve", q_bf[:, :, :, 0:16], rot[:, 0:8, :].rearrange("p (j c) d -> p c j d", j=2),
                             r=["rot"], w=["q_bf"])
                        S.cp("act", q_bf[:, :, :, 16:64], q4[:, :, :, 16:64], r=["tm_q"], w=["q_bf"])
                        k3 = tm_qkv[:, 512:640].rearrange("p (j d) -> p j d", j=2)
                        S.cp("dve", k_bf[:, :, 0:16], rot[:, 8:10, :], r=["rot"], w=["k_bf"])
                        S.cp("dve", k_bf[:, :, 16:64], k3[:, :, 16:64], r=["tm_kv"], w=["k_bf"])
                        va = v_aug[i % 2]
                        vak = f"v_aug{i % 2}"
                        S.cp("pool", va[:, :, 0:64], tm_qkv[:, 640:768].rearrange("p (j d) -> p j d", j=2), r=["tm_kv"],
                             w=[vak])
                        if i == 0:
                            stop("t0c")
                        if i == NT - 1:
                            S.cp("dve", k_rot[:, :, 0:16], rot[:, 8:10, :], r=["rot"], w=["k_rot"])
                            S.cp("dve", k_rot[:, :, 16:64], k3[:, :, 16:64], r=["tm_kv"], w=["k_rot"])
                            S.dma("sp", nk_p, k_rot[:].rearrange("p j d -> p (j d)"), r=["k_rot"], slot="nk_p")
                            S.dma("sp", nv_p, tm_qkv[:, 640:768], r=["tm_kv"], slot="nv_p")
                        pb, pbk = S.psb()
                        for c in range(4):
                            S.tr(pb[:, c * 128:(c + 1) * 128], q_bf[:, c].rearrange("p j d -> p (j d)"), idb[:],
                                 r=["q_bf", "idb"], w=[pbk], inc=False)
                        S.tr(pb[:, 512:640], k_bf[:].rearrange("p j d -> p (j d)"), idb[:], r=["k_bf", "idb"], w=[pbk])
                        kTc = kTb[i % 2]
                        kTk = f"kTb{i % 2}"
                        S.cp("act", qT[:], pb[:, 0:512].rearrange("p (c t) -> p c t", c=4), r=[pbk], w=["qT"])
                        S.cp("dve", kTc[:], pb[:, 512:640], r=[pbk], w=[kTk])
                        if i == 0:
                            stop("t0d")
                        for jk in range(2):
                            blocks = ([] if i == 0 else [(kTb[(i - 1) % 2], f"kTb{(i - 1) % 2}", mb_prev, "mb_prev",
                                                          v_aug[(i - 1) % 2], f"v_aug{(i - 1) % 2}")])
                            blocks.append((kTc, kTk, mb_cur, "mb_cur", va, vak))
                            for bi, (kt, ktk, mk, mkk, _, _) in enumerate(blocks):
                                pf, pfk = S.psf()
                                S.mm(pf[:], kt[jk * 64:(jk + 1) * 64, :],
                                     qT[jk * 64:(jk + 1) * 64, :, :].rearrange("p c t -> p (c t)"), True, True,
                                     r=[ktk, "qT"], w=[pfk])
                                S.act(PTe[bi][:].rearrange("p c t -> p (c t)"), pf[:], AF.Exp, r=[pfk], w=[f"PTe{bi}"],
                                      scale=0.125)
                                S.tt("dve", PTe[bi][:], PTe[bi][:], mk[:].unsqueeze(1).to_broadcast([128, 4, 128]),
                                     ALU.mult, r=[f"PTe{bi}", mkk], w=[f"PTe{bi}"])
                                if i == 0:
                                    stop("t0e")
                            po, pok = S.psf()
                            po3 = po[:, 0:260].rearrange("p (c d) -> p c d", c=4)
                            for c in range(4):
                                for bi, (_, _, _, _, vv, vvk) in enumerate(blocks):
                                    S.mm(po3[:, c, :], PTe[bi][:, c, :], vv[:, jk, :], bi == 0, bi == len(blocks) - 1,
                                         r=[f"PTe{bi}", vvk], w=[pok], inc=(c == 3 and bi == len(blocks) - 1))
                            S.tt("dve", den[:, jk, :], po3[:, :, 64], esink[:, jk * 4:(jk + 1) * 4], ALU.add,
                                 r=[pok, "esink"], w=["den"])
                            S.op("dve", lambda E, jk=jk: E.reciprocal(out=den[:, jk, :], in_=den[:, jk, :]), r=["den"],
                                 w=["den"])
                            S.tt("dve", mix_all[:, i, jk * 256:(jk + 1) * 256].rearrange("p (c d) -> p c d", c=4),
                                 po3[:, :, 0:64], den[:, jk, :].unsqueeze(2).to_broadcast([128, 4, 64]), ALU.mult,
                                 r=[pok, "den"], w=[f"mix{i}"])
                        if i == 0:
                            stop("t0attn")
                        if i == 1:
                            stop("t1attn")
                        g_ = gab[:, 0, :]
                        beta_ = gab[:, 1, :]
                        gc_ = gab[:, 2, :]
                        egc_ = gab[:, 3, :]
                        kds_ = gab[:, 4, :]
                        atot_ = gab[:, 5, :]
                        tmp_ = gab[:, 6, :]
                        nbeta_ = gab[:, 7, :]
                        S.tt("dve", tmp_, tm_ab[:, 0:4], dtb[:], ALU.add, r=["tm_ab", "dtb"], w=["g_tmp"])
                        S.act(tmp_, tmp_, AF.Exp, r=["g_tmp"], w=["g_tmp"])
                        S.act(tmp_, tmp_, AF.Ln, r=["g_tmp"], w=["g_tmp"], bias=1.0)
                        S.tt("dve", g_, tmp_, nexpA[:], ALU.mult, r=["g_tmp", "nexpA"], w=["g_g"])
                        S.act(beta_, tm_ab[:, 4:8], AF.Exp, r=["tm_ab"], w=["g_beta"], scale=-1.0)
                        S.ts("dve", beta_, beta_, 1.0, None, ALU.add, None, r=["g_beta"], w=["g_beta"])
                        S.op("dve", lambda E: E.reciprocal(out=beta_, in_=beta_), r=["g_beta"], w=["g_beta"])
                        S.ts("dve", nbeta_, beta_, -1.0, None, ALU.mult, None, r=["g_beta"], w=["g_nbeta"])
                        pf, pfk = S.psf()
                        S.mm(pf[:, 0:4], m_up[:], g_, True, True, r=["m_up", "g_g"], w=[pfk])
                        S.mm(pf[:, 4:8], onesf[:], g_, True, True, r=["onesf", "g_g"], w=[pfk])
                        S.cp("dve", gc_, pf[:, 0:4], r=[pfk], w=["g_gc"])
                        S.act(egc_, pf[:, 0:4], AF.Exp, r=[pfk], w=["g_egc"])
                        S.act(atot_, pf[:, 4:8], AF.Exp, r=[pfk], w=["g_atot"])
                        S.tt("dve", kds_, pf[:, 4:8], gc_, ALU.subtract, r=[pfk, "g_gc"], w=["g_kds"])
                        S.act(kds_, kds_, AF.Exp, r=["g_kds"], w=["g_kds"])
                        pD, pDk = S.psf()
                        pDT, pDTk = S.psf()
                        pG, pGk = S.psf()
                        pA, pAk = S.psf()
                        pD3 = pD[:].rearrange("p (h t) -> p h t", h=4)
                        pDT3 = pDT[:].rearrange("p (h t) -> p h t", h=4)
                        pG3 = pG[:].rearrange("p (h t) -> p h t", h=4)
                        pA3 = pA[:].rearrange("p (h t) -> p h t", h=4)
                        for h in range(4):
                            if h % 2:
                                S.act(gU[:, h, :], m_up[:], AF.Copy, r=["m_up", "g_g"], w=[f"gU{h}"], scale=gab[:, 0, h:h + 1])
                            else:
                                S.ts("dve", gU[:, h, :], m_up[:], gab[:, 0, h:h + 1], None, ALU.mult, None,
                                     r=["m_up", "g_g"], w=[f"gU{h}"])
                        for h in range(4):
                            S.mm(pD3[:, h, :], gU[:, h, :], m_strict[:], True, True, r=[f"gU{h}", "m_strict"], w=[pDk],
                                 inc=(h == 3))
                        for h in range(4):
                            S.mm(pDT3[:, h, :], m_strict[:], gU[:, h, :], True, True, r=[f"gU{h}", "m_strict"], w=[pDTk],
                                 inc=(h == 3))
                        for h in range(4):
                            S.mm(pG3[:, h, :], qkT[:, 4 + h, tok], qkT[:, 4 + h, tok], True, True, r=[f"qkT{4 + h}_{PAR}"],
                                 w=[pGk], inc=(h == 3))
                        for h in range(4):
                            S.mm(pA3[:, h, :], qkT[:, 4 + h, tok], qkT[:, h, tok], True, True,
                                 r=[f"qkT{4 + h}_{PAR}", f"qkT{h}_{PAR}"], w=[pAk], inc=(h == 3))
                        S.act(eD[:], pD3, AF.Exp, r=[pDk], w=["eD"])
                        S.tt("dve", eD[:], eD[:], m_strict[:].unsqueeze(1).to_broadcast([128, 4, 128]), ALU.mult,
                             r=["eD", "m_strict"], w=["eD"])
                        S.act(eDT[:], pDT3, AF.Exp, r=[pDTk], w=["eDT"])
                        S.tt("pool", eDT[:], eDT[:], m_up[:].unsqueeze(1).to_broadcast([128, 4, 128]), ALU.mult,
                             r=["eDT", "m_up"], w=["eDT"])
                        P0 = Pm[0]
                        for h in range(4):
                            S.stt("dve", P0[:, h, :], pG3[:, h, :], gab[:, 7, h:h + 1], eD[:, h, :], ALU.mult, ALU.mult,
                                  r=[pGk, "g_nbeta", "eD"], w=[f"Pm0g{h // 2}"])
                        S.tt("dve", ATb[:], pA3, eDT[:], ALU.mult, r=[pAk, "eDT"], w=["ATb"])
                        pt, ptk = S.psf()
                        pt3 = pt[:].rearrange("p (h t) -> p h t", h=4)
                        for h in range(4):
                            S.tr(pt3[:, h, :], P0[:, h, :], idf[:], r=[f"Pm0g{h // 2}", "idf"], w=[ptk], inc=(h == 3))
                        S.cp("act", PmT[0][:], pt3, r=[ptk], w=["PmT0g0", "PmT0g1"])
                        S.tt("dve", Qm[0][:], pt3, idf[:].unsqueeze(1).to_broadcast([128, 4, 128]), ALU.add,
                             r=[ptk, "idf"], w=["Qm0g0", "Qm0g1"])
                        NIT = 6
                        GR = ((0, 2), (2, 4))
                        for k in range(NIT):
                            a, b = k % 2, (k + 1) % 2
                            last = (k == NIT - 1)
                            pPs, pPTs = [], []
                            for gi, (h0, h1) in enumerate(GR):
                                pP, pPk = S.psf()
                                pP3 = pP[:, 0:256].rearrange("p (h t) -> p h t", h=2)
                                for h in range(h0, h1):
                                    S.mm(pP3[:, h - h0, :], PmT[a][:, h, :], Pm[a][:, h, :], True, True,
                                         r=[f"PmT{a}g{gi}", f"Pm{a}g{gi}"], w=[pPk], inc=(h == h1 - 1))
                                pPs.append((pP3, pPk))
                                if not last:
                                    pPT, pPTk = S.psf()
                                    pPT3 = pPT[:, 0:256].rearrange("p (h t) -> p h t", h=2)
                                    for h in range(h0, h1):
                                        S.mm(pPT3[:, h - h0, :], Pm[a][:, h, :], PmT[a][:, h, :], True, True,
                                             r=[f"PmT{a}g{gi}", f"Pm{a}g{gi}"], w=[pPTk], inc=(h == h1 - 1))
                                    pPTs.append((pPT3, pPTk))
                            for gi, (h0, h1) in enumerate(GR):
                                S.cp("act", Pm[b][:, h0:h1, :], pPs[gi][0], r=[pPs[gi][1]], w=[f"Pm{b}g{gi}"])
                                if not last:
                                    S.cp("dve", PmT[b][:, h0:h1, :], pPTs[gi][0], r=[pPTs[gi][1]], w=[f"PmT{b}g{gi}"])
                            pQs = []
                            for gi, (h0, h1) in enumerate(GR):
                                pQ, pQk = S.psf()
                                pQ3 = pQ[:, 0:256].rearrange("p (h t) -> p h t", h=2)
                                for h in range(h0, h1):
                                    S.mm(pQ3[:, h - h0, :], Pm[b][:, h, :], Qm[a][:, h, :], True, True,
                                         r=[f"Pm{b}g{gi}", f"Qm{a}g{gi}"], w=[pQk], inc=(h == h1 - 1))
                                pQs.append((pQ3, pQk))
                            for gi, (h0, h1) in enumerate(GR):
                                if not last:
                                    S.tt("dve", Qm[b][:, h0:h1, :], pQs[gi][0], Qm[a][:, h0:h1, :], ALU.add,
                                         r=[pQs[gi][1], f"Qm{a}g{gi}"], w=[f"Qm{b}g{gi}"])
                                else:
                                    S.tt("dve", Qb[:, h0:h1, :], pQs[gi][0], Qm[a][:, h0:h1, :], ALU.add,
                                         r=[pQs[gi][1], f"Qm{a}g{gi}"], w=["Qb"])
                        kn3 = knv[:, 0:4, :]
                        v3 = knv[:, 4:8, :]
                        S.tt("dve", tmp_, beta_, egc_, ALU.mult, r=["g_beta", "g_egc", "g_tmp"], w=["g_tmp"])
                        S.tt("pool", kbe[:], kn3, tmp_.unsqueeze(2).to_broadcast([128, 4, 128]), ALU.mult,
                             r=["knv", "g_tmp"], w=["kbe"])
                        S.tt("pool", kdc[:], kn3, kds_.unsqueeze(2).to_broadcast([128, 4, 128]), ALU.mult,
                             r=["knv", "g_kds"], w=["kdc"])
                        S.tt("dve", vb[:], v3, beta_.unsqueeze(2).to_broadcast([128, 4, 128]), ALU.mult,
                             r=["knv", "g_beta"], w=["vb"])
                        pw, pwk = S.psf()
                        pw3 = pw[:].rearrange("p (h t) -> p h t", h=4)
                        for h in range(4):
                            S.mm(pw3[:, h, :], kbe[:, h, :], Qb[:, h, :], True, True, r=["kbe", "Qb"], w=[pwk],
                                 inc=(h == 3))
                        S.op("act", lambda E: E.mul(out=nwT[:], in_=pw3, mul=-1.0), r=[pwk], w=["nwT"])
                        pv, pvk = S.psf()
                        pv3 = pv[:].rearrange("p (h t) -> p h t", h=4)
                        for h in range(4):
                            S.mm(pv3[:, h, :], Qb[:, h, :], vb[:, h, :], True, False, r=["Qb", "vb"], w=[pvk], inc=False)
                            S.mm(pv3[:, h, :], nwT[:, h, :], Sb[:, h, :], False, True, r=["nwT", "Sb"], w=[pvk],
                                 inc=(h == 3))
                        S.cp("act", vnew[:], pv3, r=[pvk], w=["vnew"])
                        po1, po1k = S.psf()
                        po13 = po1[:].rearrange("p (h t) -> p h t", h=4)
                        for h in range(4):
                            S.mm(po13[:, h, :], qkT[:, h, tok], Sb[:, h, :], True, True, r=[f"qkT{h}_{PAR}", "Sb"], w=[po1k],
                                 inc=(h == 3))
                        po2, po2k = S.psf()
                        po23 = po2[:].rearrange("p (h t) -> p h t", h=4)
                        for h in range(4):
                            S.mm(po23[:, h, :], ATb[:, h, :], vnew[:, h, :], True, True, r=["ATb", "vnew"], w=[po2k],
                                 inc=(h == 3))
                        pS, pSk = S.psf()
                        pS3 = pS[:].rearrange("p (h t) -> p h t", h=4)
                        for h in range(4):
                            S.mm(pS3[:, h, :], kdc[:, h, :], vnew[:, h, :], True, True, r=["kdc", "vnew"], w=[pSk],
                                 inc=(h == 3))
                        S.tt("dve", o1s[:], po13, egc_.unsqueeze(2).to_broadcast([128, 4, 128]), ALU.mult,
                             r=[po1k, "g_egc"], w=["o1s"])
                        S.tt("dve", og[:], po23, o1s[:], ALU.add, r=[po2k, "o1s"], w=["og"])
                        S.tt("pool", Stmp[:], Sf[:], atot_.unsqueeze(2).to_broadcast([128, 4, 128]), ALU.mult,
                             r=["Sf", "g_atot"], w=["Stmp"])
                        S.tt("dve", Sf[:], pS3, Stmp[:], ALU.add, r=[pSk, "Stmp"], w=["Sf"])
                        S.cp("act", Sb[:], Sf[:], r=["Sf"], w=["Sb"])
                        S.tt("dve", osq[:], og[:], og[:], ALU.mult, r=["og"], w=["osq"])
                        S.op("dve", lambda E: E.tensor_reduce(out=ssq[:], in_=osq[:], axis=AX.X, op=ALU.add), r=["osq"],
                             w=["ssq"])
                        S.act(ssq[:], ssq[:], AF.Ln, r=["ssq"], w=["ssq"], bias=NORM_EPS, scale=1.0 / 128.0)
                        S.act(ssq[:], ssq[:], AF.Exp, r=["ssq"], w=["ssq"], scale=-0.5)
                        S.tt("dve", og[:], og[:], ssq[:].unsqueeze(2).to_broadcast([128, 4, 128]), ALU.mult,
                             r=["og", "ssq"], w=["og"])
                        S.tt("dve", mix_all[:, i, 512:1024].rearrange("p (h d) -> p h d", h=4), og[:],
                             zs2[j][:].rearrange("p (h d) -> p h d", h=4), ALU.mult, r=["og", f"zs2_{PAR}_{j}"], w=[f"mix{i}"])
                        if i == 0:
                            stop("t0")
                        if i == 1:
                            stop("t1")
                        if i == 3:
                            stop("t3")

                    NST = SEQ // ST
                    fm(0)
                    for st in range(NST):
                        tile(st, 0)
                        if st + 1 < NST:
                            fm(st + 1)
                        tile(st, 1)
                    S.dma("sp", ng_p.rearrange("h k v -> k h v"), Sf[:], r=["Sf"], slot="ng_p")
                    if dbg:
                        dump("mix", mix_all[:, 0:NT, :], [128, NT, D], [f"mix{i}" for i in range(NT)])
                    S.barrier()


            y_acc = sb(es0, "y_acc", [128, NT + 1, D])
            with ExitStack() as es:
                w_out_bf = sb(es, "w_out_bf", [128, 8, D], BF16)
                g1 = sb(es, "g1", [128, D])
                b1 = sb(es, "b1", [128, D])
                mixT = [sb(es, f"mixT{i}", [128, 8, 128], BF16) for i in range(2)]
                xf = [sb(es, f"xf{i}", [128, D]) for i in range(2)]
                tb = [sb(es, f"tb{i}", [128, D]) for i in range(3)]
                stats = [sb(es, f"stats{i}", [128, 2, 6]) for i in range(3)]
                mv = [sb(es, f"mv{i}", [128, 2]) for i in range(3)]
                S.dma("pool", w_out_bf[:], w_out.rearrange("(c p) n -> p c n", p=128), w=["w_out"])
                S.dma("sp", g1[:], ln1g_d.partition_broadcast(128), w=["g1"])
                S.dma("sp", b1[:], ln1b_d.partition_broadcast(128), w=["b1"])
                def ln1_A(i):
                    n = 128 if i < NT else NS * TS
                    xsrc = x_p[i * 128:(i + 1) * 128, :] if i < NT else x_s
                    xft, xfk = xf[i % 2], f"xf{i % 2}"
                    S.dma("sp", xft[:n, :], xsrc, w=[xfk])
                    mt, mtk = mixT[i % 2], f"mixT{i % 2}"
                    pb, pbk = S.psb()
                    for c in range(8):
                        S.tr(pb[:, c * 128:c * 128 + n], mix_all[:n, i, c * 128:(c + 1) * 128], idb[:n, :n],
                             r=["idb"], w=[pbk], inc=(c == 7))
                    S.cp("act", mt[:, :, 0:n], pb[:].rearrange("p (c t) -> p c t", c=8)[:, :, 0:n], r=[pbk], w=[mtk])
                    tbt, tbk = tb[i % 3], f"tb{i % 3}"
                    st_, mv_, mvk = stats[i % 3], mv[i % 3], f"mv{i % 3}"
                    for half in range(2):
                        pf, pfk = S.psf()
                        for c in range(8):
                            S.mm(pf[:n, :], mt[:, c, 0:n], w_out_bf[:, c, half * 512:(half + 1) * 512], c == 0, c == 7,
                                 r=[mtk, "w_out"], w=[pfk])
                        S.stt("dve", tbt[:n, half * 512:(half + 1) * 512], xft[:n, half * 512:(half + 1) * 512], ALPHA,
                              pf[:n, :], ALU.mult, ALU.add, r=[xfk, pfk], w=[tbk])
                        S.op("dve", lambda E, half=half: E.bn_stats(out=st_[:n, half, :],
                                                                    in_=tbt[:n, half * 512:(half + 1) * 512]),
                             r=[tbk], w=[mvk])
                    S.op("dve", lambda E: E.bn_aggr(out=mv_[:n, :], in_=st_[:n].rearrange("p a b -> p (a b)")),
                         r=[mvk], w=[mvk])
                    S.act(mv_[:n, 1:2], mv_[:n, 1:2], AF.Ln, r=[mvk], w=[mvk], bias=NORM_EPS)
                    S.act(mv_[:n, 1:2], mv_[:n, 1:2], AF.Exp, r=[mvk], w=[mvk], scale=-0.5)

                def ln1_B(i):
                    n = 128 if i < NT else NS * TS
                    tbt, tbk = tb[i % 3], f"tb{i % 3}"
                    mv_, mvk = mv[i % 3], f"mv{i % 3}"
                    S.stt("dve", mv_[:n, 0:1], mv_[:n, 0:1], -1.0, mv_[:n, 1:2], ALU.mult, ALU.mult, r=[mvk], w=[mvk])
                    S.act(tbt[:n, :], tbt[:n, :], AF.Identity, r=[tbk, mvk], w=[tbk], bias=mv_[:n, 0:1], scale=mv_[:n, 1:2])
                    S.tt("dve", tbt[:n, :], tbt[:n, :], g1[:n, :], ALU.mult, r=[tbk, "g1"], w=[tbk])
                    S.tt("pool", y_acc[:n, i, :], tbt[:n, :], b1[:n, :], ALU.add, r=[tbk, "b1"], w=[f"y{i}"])

                for i in range(NT + 1):
                    ln1_A(i)
                    if i >= 1:
                        ln1_B(i - 1)
                ln1_B(NT)
                if dbg:
                    dump("x1", y_acc[:], [128, NT + 1, D], [f"y{i}" for i in range(NT + 1)])
                S.barrier()

            with ExitStack() as es:
                x1T = mix_all[:].rearrange("p a b -> p (a b)")[:, 0:8 * NTOK].rearrange("p (k t) -> p k t", k=8)
                comb = sb(es, "comb", [128, NT + 1, NE])
                wr_sb = sb(es, "wr_sb", [128, 8, 36])
                x1Tf = sb(es, "x1Tf", [128, 8, 128])
                T_ = NT + 1
                rl_all = sb(es, "rl_all", [128, T_, 36])
                r_oh = sb(es, "r_oh", [128, T_, 4])
                r_t4 = sb(es, "r_t4", [128, T_, 4])
                r_pr = sb(es, "r_pr", [128, T_, 4, 8])
                r_es = sb(es, "r_es", [128, T_, 8])
                r_m1 = sb(es, "r_m1", [128, T_, 8])
                r_e2 = sb(es, "r_e2", [128, T_, 8])
                r_m2 = sb(es, "r_m2", [128, T_, 8])
                r_ew = sb(es, "r_ew", [128, T_, 8])
                r_s = sb(es, "r_s", [128, 8, T_])
                S.op("pool", lambda E: E.memset(rl_all[:], 0.0), w=["rl_all"])
                S.dma("sp", wr_sb[:], wr_d.rearrange("(c p) n -> p c n", p=128), w=["wr"])
                YK = [f"y{i}" for i in range(NT + 1)]
                for i in range(NT + 1):
                    n = 128 if i < NT else NS * TS
                    t0 = i * 128
                    pfa, pfak = S.psf()
                    pfb, pfbk = S.psf()
                    for kc in range(8):
                        pf, pfk = (pfa, pfak) if kc < 4 else (pfb, pfbk)
                        S.tr(pf[:, (kc % 4) * 128:(kc % 4) * 128 + n], y_acc[:n, i, kc * 128:(kc + 1) * 128], idf[:n, :n],
                             r=[YK[i], "idf"], w=[pfk], inc=(kc % 4 == 3))
                    for hf, (pf, pfk) in enumerate(((pfa, pfak), (pfb, pfbk))):
                        src = pf[:].rearrange("p (k t) -> p k t", k=4)[:, :, 0:n]
                        S.cp("act", x1Tf[:, hf * 4:(hf + 1) * 4, 0:n], src, r=[pfk], w=["x1Tf"])
                        S.cp("dve", x1T[:, hf * 4:(hf + 1) * 4, t0:t0 + n], src, r=[pfk], w=["x1T"])
                    pr, prk = S.psf()
                    for kc in range(8):
                        S.mm(pr[:n, 0:36], x1Tf[:, kc, 0:n], wr_sb[:, kc, :], kc == 0, kc == 7, r=["x1Tf", "wr"], w=[prk])
                    S.cp("dve", rl_all[:n, i, :], pr[:n, 0:36], r=[prk], w=["rl_all"])
                    S.op("act", lambda E, n=n, i=i: E.mul(out=y_acc[:n, i, :], in_=y_acc[:n, i, :], mul=ALPHA),
                         r=[YK[i], "x1T", "x1Tf"], w=[YK[i]])
                R = ["rl_all", "rr"]
                gl = rl_all[:, :, 0:4]
                el = rl_all[:, :, 4:36].rearrange("p t (g e) -> p t g e", g=4)
                bc3 = lambda a, k: a.unsqueeze(2).to_broadcast([128, T_, k])
                gmax, gtp, m1, m2, ex, w1, w2 = (r_s[:, j, :] for j in range(7))
                S.op("dve", lambda E: E.tensor_reduce(out=gmax, in_=gl, axis=AX.X, op=ALU.max), r=R, w=R)
                S.tt("dve", r_oh[:], gl, bc3(gmax, 4), ALU.is_equal, r=R, w=R)
                S.tt("dve", r_t4[:], gl, bc3(gmax, 4), ALU.subtract, r=R, w=R)
                S.act(r_t4[:], r_t4[:], AF.Exp, r=R, w=R)
                S.op("dve", lambda E: E.tensor_reduce(out=gtp, in_=r_t4[:], axis=AX.X, op=ALU.add), r=R, w=R)
                S.op("dve", lambda E: E.reciprocal(out=gtp, in_=gtp), r=R, w=R)
                S.tt("dve", r_pr[:], el, r_oh[:].unsqueeze(3).to_broadcast([128, T_, 4, 8]), ALU.mult, r=R, w=R)
                S.op("dve", lambda E: E.tensor_reduce(out=r_es[:], in_=r_pr[:].rearrange("p t g e -> p t e g"), axis=AX.X,
                                                      op=ALU.add), r=R, w=R)
                S.op("dve", lambda E: E.tensor_reduce(out=m1, in_=r_es[:], axis=AX.X, op=ALU.max), r=R, w=R)
                S.tt("dve", r_m1[:], r_es[:], bc3(m1, 8), ALU.is_equal, r=R, w=R)
                S.stt("dve", r_e2[:], r_m1[:], -1e30, r_es[:], ALU.mult, ALU.add, r=R, w=R)
                S.op("dve", lambda E: E.tensor_reduce(out=m2, in_=r_e2[:], axis=AX.X, op=ALU.max), r=R, w=R)
                S.tt("dve", r_m2[:], r_e2[:], bc3(m2, 8), ALU.is_equal, r=R, w=R)
                S.tt("dve", ex, m2, m1, ALU.subtract, r=R, w=R)
                S.act(ex, ex, AF.Exp, r=R, w=R)
                S.ts("dve", w1, ex, 1.0, None, ALU.add, None, r=R, w=R)
                S.op("dve", lambda E: E.reciprocal(out=w1, in_=w1), r=R, w=R)
                S.tt("dve", w2, ex, w1, ALU.mult, r=R, w=R)
                S.tt("dve", w1, w1, gtp, ALU.mult, r=R, w=R)
                S.tt("dve", w2, w2, gtp, ALU.mult, r=R, w=R)
                S.tt("dve", r_ew[:], r_m1[:], bc3(w1, 8), ALU.mult, r=R, w=R)
                S.tt("dve", r_m2[:], r_m2[:], bc3(w2, 8), ALU.mult, r=R, w=R)
                S.tt("dve", r_ew[:], r_ew[:], r_m2[:], ALU.add, r=R, w=R)
                S.tt("dve", comb[:].rearrange("p t (g e) -> p t g e", g=4), r_oh[:].unsqueeze(3).to_broadcast([128, T_, 4, 8]),
                     r_ew[:].unsqueeze(2).to_broadcast([128, T_, 4, 8]), ALU.mult, r=R, w=["comb"])
                if dbg:
                    dump("comb", comb[:], [128, NT + 1, NE], ["comb"])

                wg = [sb(es, f"wg{i}", [128, 8, 256], BF16) for i in range(2)]
                wu = [sb(es, f"wu{i}", [128, 8, 256], BF16) for i in range(2)]
                wd = [sb(es, f"wd{i}", [128, 2, D], BF16) for i in range(2)]
                hT = [sb(es, f"hT{i}", [128, 2, NTOK], BF16) for i in range(2)]
                sgt = [sb(es, f"sgt{i}", [128, 512]) for i in range(2)]
                spans = [(t, min(512, NTOK - t)) for t in range(0, NTOK, 512)]
                for pbt in S.psb_list:
                    S.psf_list.append(pbt[:].bitcast(F32))
                acct = [sb(es, f"acct{i}", [128, 512]) for i in range(6)]
                g2 = sb(es, "g2", [128, D])
                b2 = sb(es, "b2", [128, D])
                ob = [sb(es, f"ob{i}", [128, D]) for i in range(3)]
                stats2 = [sb(es, f"stats2_{i}", [128, 2, 6]) for i in range(3)]
                mv2 = [sb(es, f"mv2_{i}", [128, 2]) for i in range(3)]
                S.dma("sp", g2[:], ln2g_d.partition_broadcast(128), w=["g2"])
                S.dma("sp", b2[:], ln2b_d.partition_broadcast(128), w=["b2"])
                acc_i = 0

                def ln2_A(i):
                    n = 128 if i < NT else NS * TS
                    st2, mvt, mk_ = stats2[i % 3], mv2[i % 3], f"mv2_{i % 3}"
                    for half in range(2):
                        S.op("dve", lambda E, half=half: E.bn_stats(out=st2[:n, half, :],
                                                                    in_=y_acc[:n, i, half * 512:(half + 1) * 512]),
                             r=[YK[i], f"{YK[i]}_0", f"{YK[i]}_1"], w=[mk_])
                    S.op("dve", lambda E: E.bn_aggr(out=mvt[:n, :], in_=st2[:n].rearrange("p a b -> p (a b)")),
                         r=[mk_], w=[mk_])
                    S.act(mvt[:n, 1:2], mvt[:n, 1:2], AF.Ln, r=[mk_], w=[mk_], bias=NORM_EPS)
                    S.act(mvt[:n, 1:2], mvt[:n, 1:2], AF.Exp, r=[mk_], w=[mk_], scale=-0.5)

                def ln2_B(i):
                    n = 128 if i < NT else NS * TS
                    mvt, mk_ = mv2[i % 3], f"mv2_{i % 3}"
                    obt, obk = ob[i % 3], f"ob{i % 3}"
                    S.stt("dve", mvt[:n, 0:1], mvt[:n, 0:1], -1.0, mvt[:n, 1:2], ALU.mult, ALU.mult, r=[mk_], w=[mk_])
                    S.act(obt[:n, :], y_acc[:n, i, :], AF.Identity, r=[YK[i], f"{YK[i]}_0", f"{YK[i]}_1", mk_], w=[obk], bias=mvt[:n, 0:1],
                          scale=mvt[:n, 1:2])
                    S.tt("dve", obt[:n, :], obt[:n, :], g2[:n, :], ALU.mult, r=[obk, "g2"], w=[obk])
                    S.tt("pool", obt[:n, :], obt[:n, :], b2[:n, :], ALU.add, r=[obk, "b2"], w=[obk])
                    dst = y_p[i * 128:(i + 1) * 128, :] if i < NT else y_s
                    S.dma("sp", dst, obt[:n, :], r=[obk], slot=obk + "o")

                def load_expert(e):
                    b = e % 2
                    S.dma("pool", wg[b][:], wg_d[e].rearrange("(c p) f -> p c f", p=128), w=[f"wg{b}"])
                    S.dma("pool", wu[b][:], wu_d[e].rearrange("(c p) f -> p c f", p=128), w=[f"wu{b}"])
                    S.dma("pool", wd[b][:], wd_d[e].rearrange("(c p) n -> p c n", p=128), w=[f"wd{b}"])

                load_expert(0)
                for e in range(NE):
                    b = e % 2
                    if e + 1 < NE:
                        load_expert(e + 1)
                    hk = f"hT{b}"
                    si = 0
                    for fc in range(2):
                        for (t0, tn) in spans:
                            pg, pgk = S.psf()
                            pu, puk = S.psf()
                            for kc in range(8):
                                S.mm(pg[:, 0:tn], wg[b][:, kc, fc * 128:(fc + 1) * 128], x1T[:, kc, t0:t0 + tn], kc == 0,
                                     kc == 7, r=[f"wg{b}", "x1T"], w=[pgk])
                            for kc in range(8):
                                S.mm(pu[:, 0:tn], wu[b][:, kc, fc * 128:(fc + 1) * 128], x1T[:, kc, t0:t0 + tn], kc == 0,
                                     kc == 7, r=[f"wu{b}", "x1T"], w=[puk])
                            sg_, sgk = sgt[si % 2], f"sgt{si % 2}"
                            si += 1
                            S.act(sg_[:, 0:tn], pg[:, 0:tn], AF.Silu, r=[pgk], w=[sgk])
                            S.tt("dve", hT[b][:, fc, t0:t0 + tn], pu[:, 0:tn], sg_[:, 0:tn], ALU.mult, r=[puk, sgk], w=[hk])
                    for i in range(NT + 1):
                        n = 128 if i < NT else NS * TS
                        t0 = i * 128
                        for half in range(2):
                            py, pyk = S.psf()
                            for fc in range(2):
                                S.mm(py[:n, :], hT[b][:, fc, t0:t0 + n], wd[b][:, fc, half * 512:(half + 1) * 512], fc == 0,
                                     fc == 1, r=[hk, f"wd{b}"], w=[pyk])
                            ysl = y_acc[:n, i, half * 512:(half + 1) * 512]
                            if (i * 2 + half) % 2 == 1:
                                at, atk = acct[acc_i % 6], f"acct{acc_i % 6}"
                                acc_i += 1
                                S.act(at[:n, :], py[:n, :], AF.Copy, r=[pyk, "comb"], w=[atk], scale=comb[:n, i, e:e + 1])
                                S.tt("pool", ysl, ysl, at[:n, :], ALU.add, r=[atk, YK[i], f"{YK[i]}_{half}"], w=[f"{YK[i]}_{half}"])
                            else:
                                S.stt("dve", ysl, py[:n, :], comb[:n, i, e:e + 1], ysl, ALU.mult, ALU.add,
                                      r=[pyk, "comb", YK[i], f"{YK[i]}_{half}"], w=[f"{YK[i]}_{half}"])
                        if e == NE - 1:
                            if i >= 1:
                                ln2_A(i - 1)
                            if i >= 2:
                                ln2_B(i - 2)
                ln2_A(NT)
                ln2_B(NT - 1)
                ln2_B(NT)
        except _Stop:
            pass
        S.final_wait()
    return nc, dbg_outs


_CACHE = {}


def _get_nc(dbg=False):
    if dbg not in _CACHE:
        _CACHE[dbg] = build(dbg)
    return _CACHE[dbg]


def make_in_maps(inputs):
    f = lambda a: np.ascontiguousarray(np.asarray(a, dtype=np.float32))
    g = {k: f(v) for k, v in inputs.items()}
    wr = np.ascontiguousarray(np.concatenate([g["w_router_group"][0], g["w_router_expert"][0]], axis=1))
    maps = []
    for c in range(NCORES):
        s0, s1 = c * NS, (c + 1) * NS
        maps.append({
            "x_p": g["x_prompt"][c],
            "x_s": np.ascontiguousarray(g["x_sample"][s0:s1].reshape(NS * TS, D)),
            "ck": np.ascontiguousarray(g["cache_attn_k"][0, s0:s1].reshape(NS, 128, 128)),
            "cv": np.ascontiguousarray(g["cache_attn_v"][0, s0:s1].reshape(NS, 128, 128)),
            "sg": np.ascontiguousarray(g["state_gdn"][0, s0:s1]),
            "sc": np.ascontiguousarray(g["state_conv"][0, s0:s1].reshape(NS * 3, 1536)),
            "w_in": g["w_in"][0], "w_out": g["w_out"][0], "sinks": g["attn_sinks"][0], "conv_w": g["conv_w"][0],
            "a_log": g["a_log"][0], "dt_bias": g["dt_bias"][0], "gnw": g["gdn_norm_w"][0],
            "ln1_g": g["ln1_g"][0], "ln1_b": g["ln1_b"][0], "w_r": wr,
            "w_gate": g["w_gate"][0], "w_up": g["w_up"][0], "w_down": g["w_down"][0],
            "ln2_g": g["ln2_g"][0], "ln2_b": g["ln2_b"][0],
        })
    return maps


def assemble(results):
    cat = lambda k: np.stack([np.asarray(r[k]) for r in results])
    y_p = cat("y_p")
    y_s = cat("y_s").reshape(128, TS, D)
    nk_p = cat("nk_p").reshape(1, 8, 128, 2, 64)
    nv_p = cat("nv_p").reshape(1, 8, 128, 2, 64)
    ng_p = cat("ng_p").reshape(1, 8, 4, 128, 128)
    nc_p = cat("nc_p").reshape(1, 8, 3, 1536)
    nk_s = cat("nk_s").reshape(1, 128, 128, 2, 64)
    nv_s = cat("nv_s").reshape(1, 128, 128, 2, 64)
    ng_s = cat("ng_s").reshape(1, 128, 4, 128, 128)
    nc_s = cat("nc_s").reshape(1, 128, 3, 1536)
    return tuple(np.ascontiguousarray(a.astype(np.float32)) for a in
                 (y_p, y_s, nk_p, nv_p, ng_p, nc_p, nk_s, nv_s, ng_s, nc_s))


def kernel(**inputs):
    nc, _ = _get_nc(False)
    maps = make_in_maps(inputs)
    res = run_bass_kernel_spmd(nc, maps, core_ids=list(range(NCORES)))
    return assemble(res.results)
```
